# Optimizing a Trainium2 kernel written in Bass

```python
import jax, jax.numpy as jnp
from jax import lax
import numpy as np


D_MODEL = 1024
BATCH = 16
SEQ = 2048
DEPTH = 2

HEAD_DIM = 64
NSA_HEADS = 8
NSA_KV_HEADS = 2
NSA_GROUP = NSA_HEADS // NSA_KV_HEADS
NSA_WIDTH = NSA_HEADS * HEAD_DIM
NSA_KV_WIDTH = NSA_KV_HEADS * HEAD_DIM
CMP_BLOCK = 32
CMP_STRIDE = 16
CMP_HIDDEN = 256
SLC_BLOCK = 64
SLC_TOPK = 8
WINDOW = 512
NSA_Q_CHUNK = 128
MOBA_HEADS = 8
MOBA_WIDTH = MOBA_HEADS * HEAD_DIM
MOBA_BLOCK = 256
MOBA_TOPK = 3
MOBA_Q_CHUNK = 32
D_FF = 2816
TOTAL_HEADS = NSA_HEADS + MOBA_HEADS
NEG_INF = -1e30
FORCE = 1e9
IN_SPLITS = (NSA_WIDTH, NSA_KV_WIDTH, NSA_KV_WIDTH, NSA_KV_WIDTH, NSA_KV_WIDTH, NSA_KV_WIDTH, NSA_KV_WIDTH,
             3 * NSA_HEADS, MOBA_WIDTH, MOBA_WIDTH, MOBA_WIDTH, D_MODEL, D_MODEL)
IN_COLS = sum(IN_SPLITS)

kernel_name = "hybrid_nsa_moba_macaron_alibi"


def rms_norm(x, g, eps=1e-6):
    xf = x.astype(jnp.float32)
    y = xf * lax.rsqrt(jnp.mean(xf * xf, axis=-1, keepdims=True) + eps)
    return (y * g.astype(jnp.float32)).astype(x.dtype)


def swiglu(x, w1, w3, w2):
    return (jax.nn.silu(x @ w1) * (x @ w3)) @ w2


def masked_softmax(s, mask):
    p = jax.nn.softmax(jnp.where(mask, s, NEG_INF), axis=-1)
    return jnp.where(mask, p, 0.0)


def alibi_slopes():
    s = jnp.asarray((2.0 ** (-8.0 * np.arange(1, TOTAL_HEADS + 1) / TOTAL_HEADS)).astype(np.float32))
    return s[0::2], s[1::2]


def split_heads(t, n_heads):
    b, s, _ = t.shape
    return t.reshape(b, s, n_heads, HEAD_DIM).transpose(0, 2, 1, 3)


def gather_blocks(blocks, idx):
    return jax.vmap(jax.vmap(lambda kb, ii: kb[ii]))(blocks, idx)


def compress_kv(t, pos, w1, w2):
    b, g, s, dh = t.shape
    r = CMP_BLOCK // CMP_STRIDE
    n_chunks = s // CMP_STRIDE
    n_cmp = n_chunks - r + 1
    ch = t.reshape(b, g, n_chunks, CMP_STRIDE, dh)
    blocks = jnp.concatenate([ch[:, :, j:j + n_cmp] for j in range(r)], axis=3)
    flat = (blocks + pos).reshape(b, g, n_cmp, CMP_BLOCK * dh)
    return jax.nn.gelu(flat @ w1) @ w2


def nsa_mixer(q, k_cmp, v_cmp, k_slc, v_slc, k_win, v_win, gates, g_qk, cmp_pos, cmp_w1, cmp_w2, slopes):
    b, s, _ = q.shape
    f32 = jnp.float32
    scale = HEAD_DIM ** -0.5
    qh = q.reshape(b, s, NSA_KV_HEADS, NSA_GROUP, HEAD_DIM).transpose(0, 2, 3, 1, 4)
    qh = rms_norm(qh, g_qk[0])
    slope = slopes.reshape(NSA_KV_HEADS, NSA_GROUP)[None, :, :, None, None]
    pos_t = jnp.arange(s, dtype=jnp.int32)

    kc = rms_norm(compress_kv(split_heads(k_cmp, NSA_KV_HEADS), cmp_pos[0], cmp_w1[0], cmp_w2[0]), g_qk[1])
    vc = compress_kv(split_heads(v_cmp, NSA_KV_HEADS), cmp_pos[1], cmp_w1[1], cmp_w2[1])
    n_cmp = kc.shape[2]
    c_start = jnp.arange(n_cmp, dtype=jnp.int32) * CMP_STRIDE
    c_valid = (c_start[None, :] + CMP_BLOCK - 1) <= pos_t[:, None]
    c_dist = pos_t[:, None].astype(f32) - (c_start.astype(f32) + (CMP_BLOCK - 1) / 2)[None, :]
    s_cmp = jnp.einsum('bgrsd,bgcd->bgrsc', qh, kc).astype(f32) * scale - slope * c_dist
    p_cmp = masked_softmax(s_cmp, c_valid)
    o_cmp = jnp.einsum('bgrsc,bgcd->bgrsd', p_cmp.astype(vc.dtype), vc)

    n_slc = s // SLC_BLOCK
    ci = np.arange(n_cmp)[:, None] * CMP_STRIDE
    sj = np.arange(n_slc)[None, :] * SLC_BLOCK
    overlap = np.clip(np.minimum(ci + CMP_BLOCK, sj + SLC_BLOCK) - np.maximum(ci, sj), 0, None)
    cmp_to_slc = jnp.asarray((overlap / CMP_BLOCK).astype(np.float32))
    imp = jnp.einsum('bgrsc,cj->bgsj', p_cmp, cmp_to_slc)
    blk = jnp.arange(n_slc, dtype=jnp.int32)[None, :]
    cur = (pos_t // SLC_BLOCK)[:, None]
    forced = (blk == 0) | (blk == cur) | (blk == cur - 1)
    sel_score = jnp.where(forced, FORCE, jnp.where(blk <= cur, imp, NEG_INF))
    n_sel = min(SLC_TOPK, n_slc)
    _, sel_idx = lax.top_k(sel_score, n_sel)

    ks_blocks = rms_norm(split_heads(k_slc, NSA_KV_HEADS), g_qk[2]).reshape(b, NSA_KV_HEADS, n_slc, SLC_BLOCK, HEAD_DIM)
    vs_blocks = split_heads(v_slc, NSA_KV_HEADS).reshape(b, NSA_KV_HEADS, n_slc, SLC_BLOCK, HEAD_DIM)
    pad = ((0, 0), (0, 0), (WINDOW, 0), (0, 0))
    kw_pad = jnp.pad(rms_norm(split_heads(k_win, NSA_KV_HEADS), g_qk[3]), pad)
    vw_pad = jnp.pad(split_heads(v_win, NSA_KV_HEADS), pad)
    n_tok = n_sel * SLC_BLOCK

    def chunk(c):
        start = c * NSA_Q_CHUNK
        t = start + jnp.arange(NSA_Q_CHUNK, dtype=jnp.int32)
        qc = lax.dynamic_slice_in_dim(qh, start, NSA_Q_CHUNK, axis=3)
        idx = lax.dynamic_slice_in_dim(sel_idx, start, NSA_Q_CHUNK, axis=2)
        ks = gather_blocks(ks_blocks, idx).reshape(b, NSA_KV_HEADS, NSA_Q_CHUNK, n_tok, HEAD_DIM)
        vs = gather_blocks(vs_blocks, idx).reshape(b, NSA_KV_HEADS, NSA_Q_CHUNK, n_tok, HEAD_DIM)
        s_pos = (idx[..., None] * SLC_BLOCK + jnp.arange(SLC_BLOCK, dtype=jnp.int32)).reshape(b, NSA_KV_HEADS, NSA_Q_CHUNK, n_tok)
        s_dist = (t[:, None] - s_pos)[:, :, None]
        sc = jnp.einsum('bgrqd,bgqkd->bgrqk', qc, ks).astype(f32) * scale - slope * s_dist.astype(f32)
        p = masked_softmax(sc, s_dist >= 0)
        o_s = jnp.einsum('bgrqk,bgqkd->bgrqd', p.astype(vs.dtype), vs)
        kwc = lax.dynamic_slice_in_dim(kw_pad, start, NSA_Q_CHUNK + WINDOW, axis=2)
        vwc = lax.dynamic_slice_in_dim(vw_pad, start, NSA_Q_CHUNK + WINDOW, axis=2)
        w_pos = start - WINDOW + jnp.arange(NSA_Q_CHUNK + WINDOW, dtype=jnp.int32)
        w_dist = t[:, None] - w_pos[None, :]
        w_mask = (w_dist >= 0) & (w_dist < WINDOW) & (w_pos[None, :] >= 0)
        sw = jnp.einsum('bgrqd,bgkd->bgrqk', qc, kwc).astype(f32) * scale - slope * w_dist.astype(f32)
        pw = masked_softmax(sw, w_mask)
        o_w = jnp.einsum('bgrqk,bgkd->bgrqd', pw.astype(vwc.dtype), vwc)
        return o_s, o_w

    o_slc, o_win = lax.map(chunk, jnp.arange(s // NSA_Q_CHUNK, dtype=jnp.int32))

    def unchunk(o):
        return o.transpose(1, 2, 3, 0, 4, 5).reshape(b, NSA_KV_HEADS, NSA_GROUP, s, HEAD_DIM)

    g = jax.nn.sigmoid(gates.astype(f32)).reshape(b, s, NSA_KV_HEADS, NSA_GROUP, 3).transpose(0, 2, 3, 1, 4).astype(q.dtype)
    o = g[..., 0:1] * o_cmp + g[..., 1:2] * unchunk(o_slc) + g[..., 2:3] * unchunk(o_win)
    return o.transpose(0, 3, 1, 2, 4).reshape(b, s, NSA_WIDTH)


def moba_mixer(q, k, v, g_qk, slopes):
    b, s, _ = q.shape
    f32 = jnp.float32
    scale = HEAD_DIM ** -0.5
    qh = rms_norm(split_heads(q, MOBA_HEADS), g_qk[0])
    kh = rms_norm(split_heads(k, MOBA_HEADS), g_qk[1])
    vh = split_heads(v, MOBA_HEADS)
    n_blk = -(-s // MOBA_BLOCK)
    pad = ((0, 0), (0, 0), (0, n_blk * MOBA_BLOCK - s), (0, 0))
    k_pad = jnp.pad(kh, pad)
    v_pad = jnp.pad(vh, pad)
    kb = k_pad.reshape(b, MOBA_HEADS, n_blk, MOBA_BLOCK, HEAD_DIM)
    vb = v_pad.reshape(b, MOBA_HEADS, n_blk, MOBA_BLOCK, HEAD_DIM)
    k_mean = jnp.mean(kb.astype(f32), axis=3).astype(kh.dtype)
    pos_t = jnp.arange(s, dtype=jnp.int32)
    cur = (pos_t // MOBA_BLOCK)[:, None]
    past = jnp.arange(n_blk, dtype=jnp.int32)[None, :] < cur
    gate = jnp.einsum('bhsd,bhnd->bhsn', qh, k_mean).astype(f32)
    n_top = min(MOBA_TOPK, n_blk)
    _, top_idx = lax.top_k(jnp.where(past, gate, NEG_INF), n_top)
    top_valid = top_idx < cur
    slope = slopes[None, :, None, None]
    n_tok = n_top * MOBA_BLOCK

    def chunk(c):
        start = c * MOBA_Q_CHUNK
        t = start + jnp.arange(MOBA_Q_CHUNK, dtype=jnp.int32)
        qc = lax.dynamic_slice_in_dim(qh, start, MOBA_Q_CHUNK, axis=2)
        idx = lax.dynamic_slice_in_dim(top_idx, start, MOBA_Q_CHUNK, axis=2)
        valid = lax.dynamic_slice_in_dim(top_valid, start, MOBA_Q_CHUNK, axis=2)
        kt = gather_blocks(kb, idx).reshape(b, MOBA_HEADS, MOBA_Q_CHUNK, n_tok, HEAD_DIM)
        vt = gather_blocks(vb, idx).reshape(b, MOBA_HEADS, MOBA_Q_CHUNK, n_tok, HEAD_DIM)
        t_pos = (idx[..., None] * MOBA_BLOCK + jnp.arange(MOBA_BLOCK, dtype=jnp.int32)).reshape(b, MOBA_HEADS, MOBA_Q_CHUNK, n_tok)
        t_dist = (t[:, None] - t_pos).astype(f32)
        t_mask = jnp.repeat(valid, MOBA_BLOCK, axis=-1)
        s_top = jnp.einsum('bhqd,bhqkd->bhqk', qc, kt).astype(f32) * scale - slope * t_dist
        own_start = (start // MOBA_BLOCK) * MOBA_BLOCK
        ko = lax.dynamic_slice_in_dim(k_pad, own_start, MOBA_BLOCK, axis=2)
        vo = lax.dynamic_slice_in_dim(v_pad, own_start, MOBA_BLOCK, axis=2)
        o_dist = t[:, None] - (own_start + jnp.arange(MOBA_BLOCK, dtype=jnp.int32))[None, :]
        s_own = jnp.einsum('bhqd,bhkd->bhqk', qc, ko).astype(f32) * scale - slope * o_dist.astype(f32)
        sc = jnp.concatenate([s_top, s_own], axis=-1)
        mask = jnp.concatenate([t_mask, jnp.broadcast_to(o_dist >= 0, (b, MOBA_HEADS, MOBA_Q_CHUNK, MOBA_BLOCK))], axis=-1)
        p = masked_softmax(sc, mask).astype(vh.dtype)
        return (jnp.einsum('bhqk,bhqkd->bhqd', p[..., :n_tok], vt)
                + jnp.einsum('bhqk,bhkd->bhqd', p[..., n_tok:], vo))

    o = lax.map(chunk, jnp.arange(s // MOBA_Q_CHUNK, dtype=jnp.int32))
    return o.transpose(1, 0, 3, 2, 4).reshape(b, s, MOBA_WIDTH)


def token_mixing(h, w_in, g_qk_nsa, g_qk_moba, cmp_pos, cmp_w1, cmp_w2, w_up_nsa, w_up_moba, w_out, slopes_nsa, slopes_moba):
    proj = h @ w_in
    cuts = np.cumsum(IN_SPLITS)[:-1].tolist()
    (q_n, kc, vc, ks, vs, kw, vw, g_n, q_m, k_m, v_m, gate_n, gate_m) = jnp.split(proj, cuts, axis=-1)
    o_n = nsa_mixer(q_n, kc, vc, ks, vs, kw, vw, g_n, g_qk_nsa, cmp_pos, cmp_w1, cmp_w2, slopes_nsa)
    o_m = moba_mixer(q_m, k_m, v_m, g_qk_moba, slopes_moba)
    y = jax.nn.sigmoid(gate_n) * (o_n @ w_up_nsa) + jax.nn.sigmoid(gate_m) * (o_m @ w_up_moba)
    return y @ w_out


def setup_inputs(seed: int = 0) -> dict:
    key = jax.random.key(seed)
    ks = jax.random.split(key, 15)
    f32 = jnp.float32

    def w(k, shape, fan_in):
        return jax.random.normal(k, shape, f32) * fan_in ** -0.5

    return {
        "x": jax.random.normal(ks[0], (BATCH, SEQ, D_MODEL), f32),
        "norm_g": 1.0 + 0.02 * jax.random.normal(ks[1], (DEPTH, 3, D_MODEL), f32),
        "ffn_w1": w(ks[2], (DEPTH, 2, D_MODEL, D_FF), D_MODEL),
        "ffn_w3": w(ks[3], (DEPTH, 2, D_MODEL, D_FF), D_MODEL),
        "ffn_w2": w(ks[4], (DEPTH, 2, D_FF, D_MODEL), D_FF),
        "w_in": w(ks[5], (DEPTH, D_MODEL, IN_COLS), D_MODEL),
        "g_qk_nsa": 1.0 + 0.02 * jax.random.normal(ks[6], (DEPTH, 4, HEAD_DIM), f32),
        "g_qk_moba": 1.0 + 0.02 * jax.random.normal(ks[7], (DEPTH, 2, HEAD_DIM), f32),
        "cmp_pos": 0.02 * jax.random.normal(ks[8], (DEPTH, 2, CMP_BLOCK, HEAD_DIM), f32),
        "cmp_w1": w(ks[9], (DEPTH, 2, CMP_BLOCK * HEAD_DIM, CMP_HIDDEN), CMP_BLOCK * HEAD_DIM),
        "cmp_w2": w(ks[10], (DEPTH, 2, CMP_HIDDEN, HEAD_DIM), CMP_HIDDEN),
        "w_up_nsa": w(ks[11], (DEPTH, NSA_WIDTH, D_MODEL), NSA_WIDTH),
        "w_up_moba": w(ks[12], (DEPTH, MOBA_WIDTH, D_MODEL), MOBA_WIDTH),
        "w_out": w(ks[13], (DEPTH, D_MODEL, D_MODEL), D_MODEL),
    }


def reference(x, norm_g, ffn_w1, ffn_w3, ffn_w2, w_in, g_qk_nsa, g_qk_moba, cmp_pos, cmp_w1, cmp_w2, w_up_nsa, w_up_moba, w_out):
    slopes_nsa, slopes_moba = alibi_slopes()
    for l in range(DEPTH):
        x = x + 0.5 * swiglu(rms_norm(x, norm_g[l, 0]), ffn_w1[l, 0], ffn_w3[l, 0], ffn_w2[l, 0])
        x = x + token_mixing(rms_norm(x, norm_g[l, 1]), w_in[l], g_qk_nsa[l], g_qk_moba[l], cmp_pos[l],
                             cmp_w1[l], cmp_w2[l], w_up_nsa[l], w_up_moba[l], w_out[l], slopes_nsa, slopes_moba)
        x = x + 0.5 * swiglu(rms_norm(x, norm_g[l, 2]), ffn_w1[l, 1], ffn_w3[l, 1], ffn_w2[l, 1])
    return x
```

```python
import numpy as np
from contextlib import ExitStack
import concourse.bass as bass
import concourse.mybir as mybir
from concourse.bass_utils import run_bass_kernel_spmd

F32 = mybir.dt.float32
BF16 = mybir.dt.bfloat16
AF = mybir.ActivationFunctionType
ALU = mybir.AluOpType
AX = mybir.AxisListType

NCORES = 8
D = 1024
T = 2048
DFF = 2816
NF = DFF // 128
DEPTH = 2
EPS = 1e-6


class Res:
    __slots__ = ("w", "r", "name")

    def __init__(self, name=""):
        self.w = None
        self.r = {}
        self.name = name


class _Eng:
    def __init__(self, name, sem):
        self.name = name
        self.sem = sem
        self.count = 0
        self.known = {}
        self.ops = []
        self.dma_sems = []
        self.dma_count = 0


class Sched:
    NDMA = 8

    def __init__(self):
        self.nsem = 0
        self.eng = {}
        for n in ("pe", "act", "dve", "pool", "sp"):
            self.eng[n] = _Eng(n, self._newsem())
        for n in ("sp", "pool", "act"):
            self.eng[n].dma_sems = [self._newsem() for _ in range(self.NDMA)]

    def _newsem(self):
        s = self.nsem
        self.nsem += 1
        return s

    def op(self, eng, fns, reads=(), writes=(), dma=False):
        E = self.eng[eng]
        if not isinstance(fns, (list, tuple)):
            fns = [fns]
        need = {}

        def req(tok):
            if tok is None:
                return
            s, v, clk = tok
            o = need.get(s)
            if o is None or o[0] < v:
                need[s] = (v, clk)

        for r in reads:
            req(r.w)
        for w in writes:
            req(w.w)
            for s, (v, clk) in w.r.items():
                req((s, v, clk))
        implied = {}
        for s, (v, clk) in need.items():
            for cs, cv in clk.items():
                if implied.get(cs, 0) < cv:
                    implied[cs] = cv
        waits = []
        known = E.known
        for s, (v, clk) in need.items():
            if known.get(s, 0) >= v or implied.get(s, 0) >= v:
                continue
            if eng == "pe" and s == E.sem:
                continue
            waits.append((s, v))
        for s, v in implied.items():
            if known.get(s, 0) < v:
                known[s] = v
        for s, (v, clk) in need.items():
            if known.get(s, 0) < v:
                known[s] = v
        if dma:
            j = E.dma_count
            E.dma_count += 1
            s = E.dma_sems[j % self.NDMA]
            prev = 16 * (j // self.NDMA)
            if prev > 0 and known.get(s, 0) < prev:
                waits.append((s, prev))
                known[s] = prev
            val = prev + 16
            inc = 16
        else:
            E.count += 1
            s = E.sem
            val = E.count
            inc = 1
        clk = dict(known)
        tok = (s, val, clk)
        E.ops.append((waits, list(fns), s, inc))
        for r in reads:
            o = r.r.get(s)
            if o is None or o[0] < val:
                r.r[s] = (val, clk)
        for w in writes:
            w.w = tok
            w.r = {}
        return tok

    def finish(self, eng, resources):
        E = self.eng[eng]
        need = {}
        for r in resources:
            toks = []
            if r.w is not None:
                toks.append(r.w)
            for s, (v, clk) in r.r.items():
                toks.append((s, v, clk))
            for s, v, clk in toks:
                if need.get(s, 0) < v:
                    need[s] = v
        waits = [(s, v) for s, v in need.items() if E.known.get(s, 0) < v]
        E.ops.append((waits, [], None, 0))

    def emit(self, nc, sems):
        def replay(E):
            def body(e):
                for waits, fns, s, inc in E.ops:
                    for ws, wv in waits:
                        e.wait_ge(sems[ws], wv)
                    if not fns:
                        continue
                    for fn in fns[:-1]:
                        fn(e)
                    fns[-1](e).then_inc(sems[s], inc)
            return body

        with nc.Block() as block:
            block.sync(replay(self.eng["sp"]))
            block.scalar(replay(self.eng["act"]))
            block.vector(replay(self.eng["dve"]))
            block.gpsimd(replay(self.eng["pool"]))
            block.tensor(replay(self.eng["pe"]))


class Ring:
    def __init__(self, items):
        self.items = items
        self.i = 0

    def next(self):
        it = self.items[self.i % len(self.items)]
        self.i += 1
        return it


def _barrier(self):
    toks = {}
    for E in self.eng.values():
        if E.count > 0:
            toks[E.sem] = E.count
        for i, s in enumerate(E.dma_sems):
            if E.dma_count > i:
                toks[s] = 16 * ((E.dma_count - i + self.NDMA - 1) // self.NDMA)
    for E in self.eng.values():
        waits = []
        for s, v in toks.items():
            if E.known.get(s, 0) >= v:
                continue
            if E.name == "pe" and s == E.sem:
                continue
            waits.append((s, v))
            E.known[s] = v
        if waits:
            E.ops.append((waits, [], None, 0))


Sched.barrier = _barrier


class Arena:
    def __init__(self, t, nel):
        self.t = t
        self.nel = nel
        self.off = 0

    def alloc(self, shape, dt):
        n = 1
        for d in shape[1:]:
            n *= d
        sz = n * (2 if dt == F32 else 1)
        self.off = (self.off + 1) // 2 * 2
        o = self.off
        self.off += sz
        assert self.off <= self.nel, ("arena overflow", self.off, self.nel)
        ap = self.t[0:shape[0], o:o + sz]
        if dt == F32:
            ap = ap.bitcast(F32)
        if len(shape) == 3:
            ap = ap.rearrange("p (a b) -> p a b", a=shape[1])
        elif len(shape) == 4:
            ap = ap.rearrange("p (a b c) -> p a b c", a=shape[1], b=shape[2])
        return ap


BIG = 30000.0
NSA_COLS = dict(q=0, kc=512, vc=640, ks=768, vs=896, kw=1024, vw=1152, g=1280)
MOBA_COLS = dict(q=1304, k=1816, v=2328)
GATE_N, GATE_M = 2840, 3864
INC = 4888


def slopes_all():
    s = (2.0 ** (-8.0 * np.arange(1, 17) / 16)).astype(np.float32)
    return s[0::2].copy(), s[1::2].copy()


def _bf16_round(a):
    a = np.asarray(a, dtype=np.float32)
    u = a.view(np.uint32).astype(np.uint64)
    r = ((u + 0x7FFF + ((u >> 16) & 1)) >> 16) << 16
    return r.astype(np.uint32).view(np.float32)


def make_consts():
    c = {}
    c["ident"] = np.eye(128, dtype=np.float32)
    j = np.arange(128)[:, None]
    i = np.arange(128)[None, :]
    c["tri_c"] = np.where(j > i, -BIG, 0.0).astype(np.float32)
    c["tri_a"] = np.where(j <= i, -BIG, 0.0).astype(np.float32)
    c["ones64"] = np.ones((64, 64), np.float32)
    sn, sm = slopes_all()
    sl = np.concatenate([sn, sm])
    ab = np.zeros((128, 16, 19), np.float32)
    for h in range(16):
        for d in range(-15, 4):
            ab[:, h, d + 15] = sl[h].astype(np.float64) * (128 * d + np.arange(128))
    c["abias"] = ab.reshape(128, 16 * 19)
    cb = np.zeros((128, 8, 4), np.float32)
    cc = np.arange(128)
    for h in range(8):
        for qb in range(4):
            cb[:, h, qb] = sn[h].astype(np.float64) * (16 * cc + 15.5 - 512 * qb)
    c["cbias"] = cb.reshape(128, 32)
    qa = np.zeros((16, 3, 512), np.float32)
    for h in range(16):
        v = (-(sl[h].astype(np.float64)) * np.arange(512)).astype(np.float32)
        v1 = _bf16_round(v)
        v2 = _bf16_round(v - v1)
        v3 = _bf16_round(v - v1 - v2)
        qa[h, 0], qa[h, 1], qa[h, 2] = v1, v2, v3
    c["qalibi"] = qa
    key = np.arange(T)
    c["e32"] = (key[None, :] // 64 == np.arange(32)[:, None]).astype(np.float32)
    c["e8"] = (key[None, :] // 256 == np.arange(8)[:, None]).astype(np.float32)
    c["ones3"] = np.ones((3, T), np.float32)
    c["zeros32"] = np.zeros((32, T), np.float32)
    cm = np.zeros((128, T), np.float32)
    cidx = np.arange(128)[:, None]
    cm[:] = np.where(16 * cidx + 31 <= key[None, :], 0.0, -BIG)
    c["cmask"] = cm
    ci = np.arange(127)[:, None] * 16
    sj = np.arange(32)[None, :] * 64
    ov = np.clip(np.minimum(ci + 32, sj + 64) - np.maximum(ci, sj), 0, None)
    M = np.zeros((128, 32), np.float32)
    M[:127] = ov / 32.0
    c["cmp2slc"] = M
    blk = np.arange(32)[None, None, :]
    t = (np.arange(16)[None, :, None] * 128 + np.arange(128)[:, None, None])
    cur = t // 64
    forced = (blk == 0) | (blk == cur) | (blk == cur - 1)
    valid = blk <= cur
    c["nsa_mult"] = np.where(forced | ~valid, 0.0, 1.0).astype(np.float32).reshape(128, 16 * 32)
    c["nsa_add"] = np.where(forced, 1e9, np.where(valid, 0.0, -1e30)).astype(np.float32).reshape(128, 16 * 32)
    n8 = np.arange(8)[None, None, :]
    curm = t // 256
    c["moba_add"] = np.broadcast_to(np.where(n8 < curm, 0.0, -1e30), (128, 16, 8)).astype(np.float32).reshape(128, 128).copy()
    return c


CONST_SHAPES = dict(ident=[128, 128], tri_c=[128, 128], tri_a=[128, 128], ones64=[64, 64], abias=[128, 304],
                    cbias=[128, 32], qalibi=[16, 3, 512], e32=[32, T], e8=[8, T], ones3=[3, T], zeros32=[32, T],
                    cmask=[128, T], cmp2slc=[128, 32], nsa_mult=[128, 512], nsa_add=[128, 512], moba_add=[128, 128])


def build_program(nseq=2, phases=("all",), depth=DEPTH, dbg=None):
    nc = bass.Bass("TRN2", target_bir_lowering=False)
    NT = nseq * T
    NTT = NT // 128
    S = Sched()
    es = ExitStack()

    def dram_in(name, shape, dt=F32):
        return nc.dram_tensor(name, list(shape), dt, kind="ExternalInput").ap()

    x_d = dram_in("x", [NT, D])
    normg_d = dram_in("norm_g", [DEPTH, 3, D])
    w1_d = dram_in("ffn_w1", [DEPTH, 2, D, DFF])
    w3_d = dram_in("ffn_w3", [DEPTH, 2, D, DFF])
    w2_d = dram_in("ffn_w2", [DEPTH, 2, DFF, D])
    win_d = dram_in("w_in", [DEPTH, D, INC])
    gqn_d = dram_in("g_qk_nsa", [DEPTH, 4, 64])
    gqnT_d = dram_in("g_qk_nsaT", [DEPTH, 64, 4])
    gqmT_d = dram_in("g_qk_mobaT", [DEPTH, 64, 2])
    posT_d = dram_in("cmp_posT", [DEPTH, 2, 64, 32])
    cw1_d = dram_in("cmp_w1", [DEPTH, 2, 2048, 256])
    cw2_d = dram_in("cmp_w2", [DEPTH, 2, 256, 64])
    wupn_d = dram_in("w_up_nsa", [DEPTH, 512, D])
    wupm_d = dram_in("w_up_moba", [DEPTH, 512, D])
    wout_d = dram_in("w_out", [DEPTH, D, D])
    C = {k: dram_in(k, v) for k, v in CONST_SHAPES.items()}
    y_d = nc.dram_tensor("y", [NT, D], F32, kind="ExternalOutput").ap()
    dbg_d = {}
    if dbg:
        for k, shp in dbg.items():
            dbg_d[k] = nc.dram_tensor("dbg_" + k, list(shp), F32, kind="ExternalOutput").ap()

    def sb(name, shape, dt):
        return es.enter_context(nc.sbuf_tensor(name, list(shape), dt))

    banks = []
    for i in range(8):
        t = es.enter_context(nc.psum_tensor(f"ps{i}", [128, 512], F32))
        banks.append((t, Res(f"ps{i}")))
    ps_ring = Ring(banks)
    acc_banks = banks[0:4]
    sc_ring = Ring(banks[4:7])
    misc_ring = Ring(banks[7:8])

    ident_b = sb("ident_b", [128, 128], BF16)
    r_ident = Res("ident")
    S.op("pool", lambda e: e.dma_start(out=ident_b[:], in_=C["ident"][:, :]), writes=[r_ident], dma=True)
    stat = [(sb(f"stat{i}", [128, 4], F32), Res(f"stat{i}")) for i in range(4)]
    stat_ring = Ring(stat)
    ARENA_EL = 99000
    arena_t = sb("arena", [128, ARENA_EL], BF16)
    A = Arena(arena_t, ARENA_EL)

    r_y = [Res(f"y{i}") for i in range(NTT)]
    r_x = [Res(f"x{i}") for i in range(NTT)]

    def rmsnorm_tile(x_ap, r_x_, g_ap, r_gres, out_ap, r_out, jk, r_jk):
        st, r_st = stat_ring.next()
        S.op("act", lambda e: e.activation(out=jk, in_=x_ap, func=AF.Square, accum_out=st[:, 0:1]),
             reads=[r_x_], writes=[r_jk, r_st])
        S.op("act", lambda e: e.activation(out=st[:, 1:2], in_=st[:, 0:1], func=AF.Sqrt, scale=1.0 / D, bias=EPS),
             reads=[r_st], writes=[r_st])
        S.op("dve", lambda e: e.reciprocal(out=st[:, 2:3], in_=st[:, 1:2]), reads=[r_st], writes=[r_st])
        S.op("dve", lambda e: e.scalar_tensor_tensor(out=out_ap, in0=x_ap, scalar=st[:, 2:3], in1=g_ap,
                                                     op0=ALU.mult, op1=ALU.mult),
             reads=[r_x_, r_st, r_gres], writes=[r_out])

    def transpose_tile(in_tile, r_in, out_ap3, r_out, nk=8, evac="act", ring=None):
        ps, r_ps = (ring or ps_ring).next()
        psb = ps[:].bitcast(BF16)
        fns = []
        for k in range(nk):
            fns.append(lambda e, k=k: e.transpose(out=psb[:, k * 128:(k + 1) * 128],
                                                  in_=in_tile[:, k * 128:(k + 1) * 128], identity=ident_b[:]))
        S.op("pe", fns, reads=[r_in, r_ident], writes=[r_ps])
        src = psb[:, 0:nk * 128].rearrange("p (k t) -> p k t", k=nk)
        if evac == "act":
            S.op("act", lambda e: e.copy(out=out_ap3, in_=src), reads=[r_ps], writes=[r_out])
        else:
            S.op("dve", lambda e: e.tensor_copy(out=out_ap3, in_=src), reads=[r_ps], writes=[r_out])

    def ffn_phase(l, j, src_d, r_src):
        S.barrier()
        A.off = 0
        w1_sb = A.alloc([128, 8, DFF], BF16)
        w3_sb = A.alloc([128, 8, DFF], BF16)
        w2_sb = A.alloc([128, NF, D], BF16)
        r_w1 = [Res() for f in range(NF)]
        r_w3 = [Res() for f in range(NF)]
        r_w2 = [Res() for f in range(NF)]
        g_rep = A.alloc([128, D], F32)
        r_g = Res()
        xt = [(A.alloc([128, D], F32), Res()) for i in range(4)]
        hb_ring = Ring([(A.alloc([128, D], BF16), Res()) for i in range(2)])
        hT = A.alloc([128, 8, 512], BF16)
        r_hT = [Res() for i in range(4)]
        gT = A.alloc([128, NF, 512], BF16)
        r_gT = [Res() for f in range(NF)]
        su_ring = Ring([(A.alloc([128, 512], F32), Res()) for i in range(2)])
        junk = (A.alloc([128, D], BF16), Res())

        S.op("pool", lambda e: e.dma_start(out=g_rep, in_=normg_d[l, 2 * j, :].partition_broadcast(128)),
             writes=[r_g], dma=True)
        for f in range(NF):
            S.op("pool", lambda e, f=f: e.dma_start(
                out=w1_sb[:, :, f * 128:(f + 1) * 128],
                in_=w1_d[l, j, :, f * 128:(f + 1) * 128].rearrange("(k p) c -> p k c", p=128)),
                writes=[r_w1[f]], dma=True)
            S.op("pool", lambda e, f=f: e.dma_start(
                out=w3_sb[:, :, f * 128:(f + 1) * 128],
                in_=w3_d[l, j, :, f * 128:(f + 1) * 128].rearrange("(k p) c -> p k c", p=128)),
                writes=[r_w3[f]], dma=True)
        for f in range(NF):
            S.op("pool", lambda e, f=f: e.dma_start(out=w2_sb[:, f, :], in_=w2_d[l, j, f * 128:(f + 1) * 128, :]),
                 writes=[r_w2[f]], dma=True)

        for blk in range(NT // 512):
            for i in range(4):
                tt = blk * 4 + i
                xa, r_xa = xt[i]
                S.op("sp", lambda e, xa=xa, tt=tt: e.dma_start(out=xa, in_=src_d[tt * 128:(tt + 1) * 128, :]),
                     reads=[r_src[tt]], writes=[r_xa], dma=True)
                hbt, r_hb = hb_ring.next()
                rmsnorm_tile(xa, r_xa, g_rep, r_g, hbt, r_hb, junk[0], junk[1])
                transpose_tile(hbt, r_hb, hT[:, :, i * 128:(i + 1) * 128], r_hT[i])
            for f in range(NF):
                pu, r_pu = ps_ring.next()
                pv, r_pv = ps_ring.next()
                fns = []
                for k in range(8):
                    fns.append(lambda e, k=k, f=f, pu=pu: e.matmul(pu[:], lhsT=w1_sb[:, k, f * 128:(f + 1) * 128],
                                                                    rhs=hT[:, k, :], start=(k == 0), stop=(k == 7)))
                S.op("pe", fns, reads=[r_w1[f]] + r_hT, writes=[r_pu])
                fns = []
                for k in range(8):
                    fns.append(lambda e, k=k, f=f, pv=pv: e.matmul(pv[:], lhsT=w3_sb[:, k, f * 128:(f + 1) * 128],
                                                                    rhs=hT[:, k, :], start=(k == 0), stop=(k == 7)))
                S.op("pe", fns, reads=[r_w3[f]] + r_hT, writes=[r_pv])
                s_t, r_s = su_ring.next()
                S.op("act", lambda e, s_t=s_t, pu=pu: e.activation(out=s_t, in_=pu[:], func=AF.Silu),
                     reads=[r_pu], writes=[r_s])
                S.op("dve", lambda e, s_t=s_t, pv=pv, f=f: e.tensor_tensor(out=gT[:, f, :], in0=s_t, in1=pv[:],
                                                                             op=ALU.mult),
                     reads=[r_s, r_pv], writes=[r_gT[f]])
            for i in range(4):
                tt = blk * 4 + i
                xa, r_xa = xt[i]
                for h in range(2):
                    po, r_po = ps_ring.next()
                    fns = []
                    for f in range(NF):
                        fns.append(lambda e, f=f, po=po, i=i, h=h: e.matmul(
                            po[:], lhsT=gT[:, f, i * 128:(i + 1) * 128], rhs=w2_sb[:, f, h * 512:(h + 1) * 512],
                            start=(f == 0), stop=(f == NF - 1)))
                    S.op("pe", fns, reads=r_gT + r_w2, writes=[r_po])
                    S.op("dve", lambda e, po=po, xa=xa, h=h: e.scalar_tensor_tensor(
                        out=xa[:, h * 512:(h + 1) * 512], in0=po[:], scalar=0.5, in1=xa[:, h * 512:(h + 1) * 512],
                        op0=ALU.mult, op1=ALU.add), reads=[r_po, r_xa], writes=[r_xa])
                S.op("sp", lambda e, xa=xa, tt=tt: e.dma_start(out=y_d[tt * 128:(tt + 1) * 128, :], in_=xa),
                     reads=[r_xa], writes=[r_y[tt]], dma=True)

    def mix_phase(l, src_d, r_src):
        S.barrier()
        A.off = 0
        r_c = Res()
        tri_c = A.alloc([128, 128], BF16)
        tri_a = A.alloc([128, 128], BF16)
        ones64 = A.alloc([64, 64], BF16)
        abias = A.alloc([128, 304], F32)
        cbias = A.alloc([128, 32], F32)
        cmask = A.alloc([128, T], BF16)
        nsa_mult = A.alloc([128, 16, 32], F32)
        nsa_add = A.alloc([128, 16, 32], F32)
        moba_add = A.alloc([128, 16, 8], F32)
        gq_n = A.alloc([64, 4], F32)
        gq_m = A.alloc([64, 2], F32)
        gkc_rep = A.alloc([128, 64], F32)
        g_rep = A.alloc([128, D], F32)
        for dst, src in ((tri_c, C["tri_c"][:, :]), (tri_a, C["tri_a"][:, :]), (ones64, C["ones64"][:, :]),
                         (abias, C["abias"][:, :]), (cbias, C["cbias"][:, :]), (cmask, C["cmask"][:, :]),
                         (nsa_mult, C["nsa_mult"][:, :].rearrange("p (a b) -> p a b", a=16)),
                         (nsa_add, C["nsa_add"][:, :].rearrange("p (a b) -> p a b", a=16)),
                         (moba_add, C["moba_add"][:, :].rearrange("p (a b) -> p a b", a=16)),
                         (gq_n, gqnT_d[l, :, :]), (gq_m, gqmT_d[l, :, :]),
                         (gkc_rep, gqn_d[l, 1, :].partition_broadcast(128)),
                         (g_rep, normg_d[l, 1, :].partition_broadcast(128))):
            S.op("pool", lambda e, dst=dst, src=src: e.dma_start(out=dst, in_=src), writes=[r_c], dma=True)
        S.op("dve", lambda e: e.tensor_scalar(out=gq_n[:, 0:1], in0=gq_n[:, 0:1], scalar1=0.125, scalar2=None,
                                              op0=ALU.mult), reads=[r_c], writes=[r_c])
        S.op("dve", lambda e: e.tensor_scalar(out=gq_m[:, 0:1], in0=gq_m[:, 0:1], scalar1=0.125, scalar2=None,
                                              op0=ALU.mult), reads=[r_c], writes=[r_c])

        hT = A.alloc([128, 8, T], BF16)
        r_hT = [Res() for _ in range(16)]
        onT = A.alloc([128, 4, T], BF16)
        omT = A.alloc([128, 4, T], BF16)
        r_onT = [Res() for _ in range(16)]
        r_omT = [Res() for _ in range(16)]
        gsig = A.alloc([128, 16, 24], F32)
        r_gsig = Res()
        wch_ring = Ring([(A.alloc([128, 8, 256], BF16), Res()) for _ in range(3)])
        pT_ring = Ring([(A.alloc([128, 512], BF16), Res()) for _ in range(3)])
        tmpf_ring = Ring([(A.alloc([128, 512], F32), Res()) for _ in range(3)])
        sqb_ring = Ring([(A.alloc([128, 512], BF16), Res()) for _ in range(2)])
        xt_ring = Ring([(A.alloc([128, D], F32), Res()) for _ in range(2)])
        hb_ring = Ring([(A.alloc([128, D], BF16), Res()) for _ in range(2)])
        junk = (A.alloc([128, D], BF16), Res())
        sm_ring = Ring([(A.alloc([128, 16], F32), Res()) for _ in range(8)])
        region0 = A.off

        def load_w(src_ap3, ncols):
            w, r_w = wch_ring.next()
            S.op("pool", lambda e: e.dma_start(out=w[:, :, 0:ncols], in_=src_ap3), writes=[r_w], dma=True)
            return w, r_w

        def win_cols(c0, n):
            return win_d[l, :, c0:c0 + n].rearrange("(k p) c -> p k c", p=128)

        def proj_fm_head(c0, dest_fn, r_dest_fn, gcol, want_norm):
            w, r_w = load_w(win_cols(c0, 64), 64)
            for b in range(4):
                ps, r_ps = ps_ring.next()
                fns = [lambda e, k=k, ps=ps, b=b: e.matmul(ps[0:64, :], lhsT=w[:, k, 0:64],
                                                           rhs=hT[:, k, b * 512:(b + 1) * 512],
                                                           start=(k == 0), stop=(k == 7)) for k in range(8)]
                S.op("pe", fns, reads=[r_w] + r_hT[4 * b:4 * b + 4], writes=[r_ps])
                dst, r_dst = dest_fn(b), r_dest_fn(b)
                if not want_norm:
                    S.op("act", lambda e, ps=ps, dst=dst: e.copy(out=dst, in_=ps[0:64, :]),
                         reads=[r_ps], writes=[r_dst])
                    continue
                sq, r_sq = sqb_ring.next()
                S.op("act", lambda e, ps=ps, sq=sq: e.activation(out=sq[0:64, :], in_=ps[0:64, :], func=AF.Square),
                     reads=[r_ps], writes=[r_sq])
                p2, r_p2 = ps_ring.next()
                S.op("pe", lambda e, p2=p2, sq=sq: e.matmul(p2[0:64, :], lhsT=ones64[:, :], rhs=sq[0:64, :],
                                                            start=True, stop=True),
                     reads=[r_sq, r_c], writes=[r_p2])
                tf, r_tf = tmpf_ring.next()
                st, r_st = sm_ring.next()
                S.op("act", lambda e, p2=p2, tf=tf: e.activation(out=tf[0:64, :], in_=p2[0:64, :], func=AF.Ln,
                                                                 scale=1.0 / 64, bias=EPS),
                     reads=[r_p2], writes=[r_tf])
                S.op("act", lambda e, tf=tf: e.activation(out=tf[0:64, :], in_=tf[0:64, :], func=AF.Exp, scale=-0.5),
                     reads=[r_tf], writes=[r_tf])
                S.op("dve", lambda e, ps=ps, tf=tf, dst=dst: e.scalar_tensor_tensor(
                    out=dst, in0=ps[0:64, :], scalar=gcol, in1=tf[0:64, :], op0=ALU.mult, op1=ALU.mult),
                    reads=[r_ps, r_tf, r_c], writes=[r_dst])

        def causal_tiles(qb):
            tl = []
            for kt in range(4 * qb + 4):
                c = kt - 4 * qb
                if c < 0:
                    tl.append((kt, 0, 512, None, None))
                else:
                    tl.append((kt, 128 * c, 512, "c", c))
            return tl

        def window_tiles(qb):
            tl = []
            for c in (0, 1, 2, 3, -1, -2, -3, -4):
                kt = 4 * qb + c
                if kt < 0:
                    continue
                if c >= 0:
                    tl.append((kt, 128 * c, 512, "c", c))
                else:
                    m = 4 + c
                    tl.append((kt, 0, 128 * (m + 1), "a", m))
            return tl

        def run_rounds(rounds):
            items = []
            for R in rounds:
                for ti, tile in enumerate(R["tiles"]):
                    items.append((R, ti, tile))

            def emit_qk(it):
                R, ti, (kt, c0, c1, tri, tu) = it
                ps, r_ps = sc_ring.next()
                kp = R.get("kpart", 128)
                K = R["krows"]
                qb = R["qb"]
                fns = [lambda e: e.matmul(ps[0:kp, c0:c1], lhsT=R["kT"][0:K, kt * 128:kt * 128 + kp],
                                          rhs=R["q"][0:K, qb * 512 + c0:qb * 512 + c1], start=True,
                                          stop=(tri is None and "mask" not in R))]
                reads = [R["r_k"]] + R["r_q"] + [r_c]
                if "mask" in R:
                    fns.append(lambda e: e.matmul(ps[0:kp, c0:c1], lhsT=ident_b[0:kp, 0:kp],
                                                  rhs=R["mask"][0:kp, qb * 512 + c0:qb * 512 + c1],
                                                  start=False, stop=True))
                if tri is not None:
                    tm = tri_c if tri == "c" else tri_a
                    fns.append(lambda e: e.matmul(ps[:, 128 * tu:128 * tu + 128], lhsT=ident_b[:, :], rhs=tm[:, :],
                                                  start=False, stop=True))
                S.op("pe", fns, reads=reads + [r_ident], writes=[r_ps])
                return ps, r_ps

            pend = emit_qk(items[0]) if items else None
            for idx, it in enumerate(items):
                R, ti, (kt, c0, c1, tri, tu) = it
                ps, r_ps = pend
                if idx + 1 < len(items):
                    pend = emit_qk(items[idx + 1])
                kp = R.get("kpart", 128)
                pT, r_pT = pT_ring.next()
                bias_ap = R["bias"](kt)
                S.op("act", lambda e, ps=ps, pT=pT, bias_ap=bias_ap, kp=kp, c0=c0, c1=c1: e.activation(
                    out=pT[0:kp, c0:c1], in_=ps[0:kp, c0:c1], func=AF.Exp, bias=bias_ap, scale=1.0),
                    reads=[r_ps, r_c], writes=[r_pT])
                us = [u for u in range(4) if c0 <= 128 * u < c1]
                nv = R["nv"]
                fns = []
                wr = []
                for u in us:
                    cov = [j for j, tl in enumerate(R["tiles"]) if tl[1] <= 128 * u < tl[2]]
                    acc, r_acc = acc_banks[u]
                    V = R["V"](kt)
                    fns.append(lambda e, u=u, acc=acc, V=V, first=(ti == cov[0]), last=(ti == cov[-1]), pT=pT, kp=kp:
                               e.matmul(acc[:, 0:nv], lhsT=pT[0:kp, 128 * u:128 * u + 128], rhs=V,
                                        start=first, stop=last))
                    wr.append(r_acc)
                S.op("pe", fns, reads=[r_pT, R["r_v"]], writes=wr)
                if ti == len(R["tiles"]) - 1:
                    for u in range(4):
                        R["evac"](u, acc_banks[u][0], acc_banks[u][1])

        sn_, sm_ = slopes_all()

        def dump(name, ap, reads):
            if name in dbg_d:
                dst = dbg_d[name][:, :]
                if len(ap.shape) == 3:
                    dst = dst.rearrange("p (a b) -> p a b", a=ap.shape[1])
                S.op("pool", lambda e: e.dma_start(out=dst[0:ap.shape[0]], in_=ap), reads=reads, writes=[Res()], dma=True)

        for s in range(nseq):
            tt0 = s * 16
            for i in range(16):
                xa, r_xa = xt_ring.next()
                S.op("sp", lambda e, xa=xa, i=i, tt0=tt0: e.dma_start(out=xa, in_=src_d[(tt0 + i) * 128:(tt0 + i + 1) * 128, :]),
                     reads=[r_src[tt0 + i]], writes=[r_xa], dma=True)
                hbt, r_hb = hb_ring.next()
                rmsnorm_tile(xa, r_xa, g_rep, r_c, hbt, r_hb, junk[0], junk[1])
                transpose_tile(hbt, r_hb, hT[:, :, i * 128:(i + 1) * 128], r_hT[i])

            w, r_w = load_w(win_cols(NSA_COLS["g"], 64), 64)
            for i in range(16):
                ps, r_ps = ps_ring.next()
                fns = [lambda e, k=k, ps=ps, i=i, w=w: e.matmul(ps[:, 0:24], lhsT=hT[:, k, i * 128:(i + 1) * 128],
                                                                rhs=w[:, k, 0:24], start=(k == 0), stop=(k == 7))
                       for k in range(8)]
                S.op("pe", fns, reads=[r_w, r_hT[i]], writes=[r_ps])
                S.op("act", lambda e, ps=ps, i=i: e.activation(out=gsig[:, i, :], in_=ps[:, 0:24], func=AF.Sigmoid),
                     reads=[r_ps], writes=[r_gsig])

            if dbg and s == 0 and l == 0:
                dump("gsig", gsig, [r_gsig])
            for g in range(2):
                S.barrier()
                A.off = region0
                q_aug = [A.alloc([128, T], BF16) for _ in range(4)]
                r_q = [[Res() for _ in range(4)] for _ in range(4)]
                r_qs = [[Res() for _ in range(4)] for _ in range(4)]
                r_qst = Res()
                ks_aug = A.alloc([128, T], BF16)
                kw_aug = A.alloc([128, T], BF16)
                r_ks = Res()
                r_kw = Res()
                kcraw = A.alloc([64, T], BF16)
                r_kcraw = Res()
                vsA = A.alloc([128, 16, 65], BF16)
                vwA = A.alloc([128, 16, 65], BF16)
                r_vs = Res()
                r_vw = Res()
                w1c = A.alloc([64, 32, 256], BF16)
                r_w1c = Res()
                w2c = A.alloc([128, 2, 64], BF16)
                posT = A.alloc([64, 32], BF16)
                r_w2c = Res()
                kc_aug = A.alloc([128, 128], BF16)
                r_kc = Res()
                kctm = A.alloc([128, 128], BF16)
                r_kctm = Res()
                vcA = A.alloc([128, 97], BF16)
                r_vc = Res()
                hid = A.alloc([128, 2, 128], BF16)
                r_hid = Res()
                pbias = A.alloc([128, 2], F32)
                r_pb = Res()
                oacc = A.alloc([128, 16, 256], F32)
                r_oacc = [Res() for _ in range(16)]
                impacc = A.alloc([128, 16, 32], F32)
                r_imp = [Res() for _ in range(16)]
                trin_ring = Ring([(A.alloc([128, 96], BF16), Res()) for _ in range(2)])
                ob_ring = Ring([(A.alloc([128, 256], BF16), Res()) for _ in range(2)])

                for hl in range(4):
                    h = 4 * g + hl
                    S.op("pool", lambda e, hl=hl, h=h: e.dma_start(
                        out=q_aug[hl][96:99, :].rearrange("p (b i) -> p b i", b=4),
                        in_=C["qalibi"][h, :, :].unsqueeze(1).to_broadcast([3, 4, 512])), writes=[r_qst], dma=True)
                    S.op("pool", lambda e, hl=hl: e.dma_start(out=q_aug[hl][64:96, :], in_=C["zeros32"][:, :]),
                         writes=r_qs[hl], dma=True)
                for dst, r_dst, mid in ((ks_aug, r_ks, C["e32"]), (kw_aug, r_kw, C["zeros32"])):
                    S.op("pool", lambda e, dst=dst, mid=mid: e.dma_start(out=dst[64:96, :], in_=mid[:, :]),
                         writes=[r_dst], dma=True)
                    S.op("pool", lambda e, dst=dst: e.dma_start(out=dst[96:99, :], in_=C["ones3"][:, :]),
                         writes=[r_dst], dma=True)
                S.op("pool", lambda e: e.memset(kctm[:, 64:96], 0.0), writes=[r_kctm])
                S.op("pool", lambda e: e.memset(kctm[:, 96:99], 1.0), writes=[r_kctm])
                S.op("pool", lambda e: e.memset(kctm[:, 0:64], 0.0), writes=[r_kctm])
                S.op("pool", lambda e: e.memset(vsA[:, :, 64:65], 1.0), writes=[r_vs])
                S.op("pool", lambda e: e.memset(vwA[:, :, 64:65], 1.0), writes=[r_vw])
                S.op("pool", lambda e: e.memset(vcA[:, 64:65], 1.0), writes=[r_vc])
                S.op("pool", lambda e: e.dma_start(out=vcA[:, 65:97], in_=C["cmp2slc"][:, :]), writes=[r_vc], dma=True)
                for tr, r_tr in trin_ring.items:
                    S.op("pool", lambda e, tr=tr: e.memset(tr[:, 0:64], 0.0), writes=[r_tr])

                for hl in range(4):
                    h = 4 * g + hl
                    proj_fm_head(NSA_COLS["q"] + 64 * h, lambda b, hl=hl: q_aug[hl][0:64, b * 512:(b + 1) * 512],
                                 lambda b, hl=hl: r_q[hl][b], gq_n[:, 0:1], True)
                proj_fm_head(NSA_COLS["ks"] + 64 * g, lambda b: ks_aug[0:64, b * 512:(b + 1) * 512],
                             lambda b: r_ks, gq_n[:, 2:3], True)
                proj_fm_head(NSA_COLS["kw"] + 64 * g, lambda b: kw_aug[0:64, b * 512:(b + 1) * 512],
                             lambda b: r_kw, gq_n[:, 3:4], True)
                for nm, dstA, r_dst in (("vs", vsA, r_vs), ("vw", vwA, r_vw)):
                    w, r_w = load_w(win_cols(NSA_COLS[nm] + 64 * g, 64), 64)
                    for i in range(16):
                        ps, r_ps = ps_ring.next()
                        fns = [lambda e, k=k, ps=ps, i=i, w=w: e.matmul(ps[:, 0:64], lhsT=hT[:, k, i * 128:(i + 1) * 128],
                                                                        rhs=w[:, k, 0:64], start=(k == 0), stop=(k == 7))
                               for k in range(8)]
                        S.op("pe", fns, reads=[r_w, r_hT[i]], writes=[r_ps])
                        S.op("act", lambda e, ps=ps, i=i, dstA=dstA: e.copy(out=dstA[:, i, 0:64], in_=ps[:, 0:64]),
                             reads=[r_ps], writes=[r_dst])
                for kv in range(2):
                    proj_fm_head(NSA_COLS["kc" if kv == 0 else "vc"] + 64 * g,
                                 lambda b: kcraw[0:64, b * 512:(b + 1) * 512], lambda b: r_kcraw, None, False)
                    S.op("pool", lambda e, kv=kv: e.dma_start(
                        out=w1c, in_=cw1_d[l, kv, :, :].rearrange("(l d) h -> d l h", d=64)), writes=[r_w1c], dma=True)
                    S.op("pool", lambda e, kv=kv: e.dma_start(
                        out=w2c, in_=cw2_d[l, kv, :, :].rearrange("(a p) d -> p a d", p=128)), writes=[r_w2c], dma=True)
                    S.op("pool", lambda e, kv=kv: e.dma_start(out=posT, in_=posT_d[l, kv, :, :]), writes=[r_w2c], dma=True)
                    for hh in range(2):
                        ps, r_ps = ps_ring.next()
                        fns = [lambda e, ll=ll, ps=ps, hh=hh: e.matmul(
                            ps[:, 0:127], lhsT=w1c[:, ll, hh * 128:(hh + 1) * 128],
                            rhs=kcraw[0:64, ll:ll + 16 * 126 + 1:16], start=(ll == 0), stop=(ll == 31)) for ll in range(32)]
                        fns += [lambda e, ll=ll, ps=ps, hh=hh: e.matmul(
                            ps[:, 128:129], lhsT=w1c[:, ll, hh * 128:(hh + 1) * 128],
                            rhs=posT[:, ll:ll + 1], start=(ll == 0), stop=(ll == 31)) for ll in range(32)]
                        S.op("pe", fns, reads=[r_w1c, r_w2c, r_kcraw], writes=[r_ps])
                        S.op("dve", lambda e, ps=ps, hh=hh: e.tensor_copy(out=pbias[:, hh:hh + 1], in_=ps[:, 128:129]),
                             reads=[r_ps], writes=[r_pb])
                        xh, r_xh = tmpf_ring.next()
                        x2, r_x2 = tmpf_ring.next()
                        S.op("dve", lambda e, ps=ps, hh=hh, xh=xh: e.tensor_scalar(
                            out=xh[:, 0:127], in0=ps[:, 0:127], scalar1=pbias[:, hh:hh + 1], scalar2=None, op0=ALU.add),
                            reads=[r_ps, r_pb], writes=[r_xh])
                        S.op("dve", lambda e, xh=xh, x2=x2: e.tensor_tensor(out=x2[:, 0:127], in0=xh[:, 0:127],
                                                                            in1=xh[:, 0:127], op=ALU.mult),
                             reads=[r_xh], writes=[r_x2])
                        S.op("dve", lambda e, x2=x2: e.tensor_scalar(out=x2[:, 0:127], in0=x2[:, 0:127], scalar1=0.044715,
                                                                     scalar2=1.0, op0=ALU.mult, op1=ALU.add),
                             reads=[r_x2], writes=[r_x2])
                        S.op("dve", lambda e, xh=xh, x2=x2: e.tensor_tensor(out=x2[:, 0:127], in0=x2[:, 0:127],
                                                                            in1=xh[:, 0:127], op=ALU.mult),
                             reads=[r_xh, r_x2], writes=[r_x2])
                        S.op("act", lambda e, x2=x2: e.activation(out=x2[:, 0:127], in_=x2[:, 0:127], func=AF.Tanh,
                                                                  scale=0.7978845608028654),
                             reads=[r_x2], writes=[r_x2])
                        S.op("dve", lambda e, xh=xh, x2=x2, hh=hh: e.scalar_tensor_tensor(
                            out=hid[:, hh, 0:127], in0=x2[:, 0:127], scalar=1.0, in1=xh[:, 0:127],
                            op0=ALU.add, op1=ALU.mult), reads=[r_xh, r_x2], writes=[r_hid])
                    ps, r_ps = ps_ring.next()
                    fns = [lambda e, hh=hh, ps=ps: e.matmul(ps[0:127, 0:64], lhsT=hid[:, hh, 0:127], rhs=w2c[:, hh, :],
                                                            start=(hh == 0), stop=(hh == 1)) for hh in range(2)]
                    S.op("pe", fns, reads=[r_hid, r_w2c], writes=[r_ps])
                    if kv == 1:
                        S.op("dve", lambda e, ps=ps: e.tensor_scalar(out=vcA[0:127, 0:64], in0=ps[0:127, 0:64],
                                                                     scalar1=0.5, scalar2=None, op0=ALU.mult),
                             reads=[r_ps], writes=[r_vc])
                    else:
                        tf, r_tf = tmpf_ring.next()
                        st, r_st = sm_ring.next()
                        S.op("dve", lambda e, ps=ps, tf=tf: e.tensor_scalar(out=tf[0:127, 0:64], in0=ps[0:127, 0:64],
                                                                            scalar1=0.5, scalar2=None, op0=ALU.mult),
                             reads=[r_ps], writes=[r_tf])
                        S.op("act", lambda e, tf=tf, st=st: e.activation(out=tf[0:127, 64:128], in_=tf[0:127, 0:64],
                                                                         func=AF.Square, accum_out=st[0:127, 0:1]),
                             reads=[r_tf], writes=[r_tf, r_st])
                        S.op("act", lambda e, st=st: e.activation(out=st[0:127, 1:2], in_=st[0:127, 0:1], func=AF.Sqrt,
                                                                  scale=1.0 / 64, bias=EPS), reads=[r_st], writes=[r_st])
                        S.op("dve", lambda e, st=st: e.reciprocal(out=st[0:127, 2:3], in_=st[0:127, 1:2]),
                             reads=[r_st], writes=[r_st])
                        S.op("dve", lambda e, tf=tf, st=st: e.scalar_tensor_tensor(
                            out=kctm[0:127, 0:64], in0=tf[0:127, 0:64], scalar=st[0:127, 2:3], in1=gkc_rep[0:127, :],
                            op0=ALU.mult, op1=ALU.mult), reads=[r_tf, r_st, r_c], writes=[r_kctm])
                        ps2, r_ps2 = ps_ring.next()
                        psb2 = ps2[:].bitcast(BF16)
                        S.op("pe", lambda e, psb2=psb2: e.transpose(out=psb2[0:99, 0:128], in_=kctm[:, 0:99],
                                                                    identity=ident_b[:]),
                             reads=[r_kctm, r_ident], writes=[r_ps2])
                        S.op("dve", lambda e, psb2=psb2: e.tensor_copy(out=kc_aug[0:99, 0:128], in_=psb2[0:99, 0:128]),
                             reads=[r_ps2], writes=[r_kc])

                def mk_cmp_evac(hl, qb):
                    h = 4 * g + hl

                    def ev(u, acc, r_acc):
                        tt = 4 * qb + u
                        st, r_st = sm_ring.next()
                        S.op("dve", lambda e: e.tensor_scalar(out=st[:, 0:1], in0=acc[:, 64:65], scalar1=1e-30,
                                                              scalar2=None, op0=ALU.add), reads=[r_acc], writes=[r_st])
                        S.op("dve", lambda e: e.reciprocal(out=st[:, 1:2], in_=st[:, 0:1]), reads=[r_st], writes=[r_st])
                        S.op("dve", lambda e: e.tensor_tensor(out=st[:, 2:3], in0=st[:, 1:2],
                                                              in1=gsig[:, tt, 3 * h:3 * h + 1], op=ALU.mult),
                             reads=[r_st, r_gsig], writes=[r_st])
                        S.op("dve", lambda e: e.tensor_scalar(out=oacc[:, tt, hl * 64:(hl + 1) * 64], in0=acc[:, 0:64],
                                                              scalar1=st[:, 2:3], scalar2=None, op0=ALU.mult),
                             reads=[r_acc, r_st], writes=[r_oacc[tt]])
                        if hl == 0:
                            S.op("dve", lambda e: e.tensor_scalar(out=impacc[:, tt, :], in0=acc[:, 65:97],
                                                                  scalar1=st[:, 1:2], scalar2=None, op0=ALU.mult),
                                 reads=[r_acc, r_st], writes=[r_imp[tt]])
                        else:
                            S.op("dve", lambda e: e.scalar_tensor_tensor(
                                out=impacc[:, tt, :], in0=acc[:, 65:97], scalar=st[:, 1:2], in1=impacc[:, tt, :],
                                op0=ALU.mult, op1=ALU.add), reads=[r_acc, r_st, r_imp[tt]], writes=[r_imp[tt]])
                    return ev

                rounds = []
                for hl in range(4):
                    h = 4 * g + hl
                    for qb in range(4):
                        rounds.append(dict(q=q_aug[hl], r_q=[r_q[hl][qb], r_qst], krows=99, kT=kc_aug, r_k=r_kc, qb=qb,
                                           tiles=[(0, 0, 512, None, None)], kpart=127, mask=cmask,
                                           V=lambda kt: vcA[0:127, 0:97], r_v=r_vc, nv=97,
                                           bias=lambda kt, h=h, qb=qb: cbias[0:127, 4 * h + qb:4 * h + qb + 1],
                                           evac=mk_cmp_evac(hl, qb)))
                run_rounds(rounds)
                if dbg and s == 0 and l == 0 and g == 0:
                    dump("oacc_cmp", oacc, r_oacc)
                    dump("kc_aug", kc_aug, [r_kc])
                    dump("vcA", vcA, [r_vc])
                    dump("impacc", impacc, r_imp)

                for tt in range(16):
                    sc, r_sc = tmpf_ring.next()
                    st, r_st = sm_ring.next()
                    tr, r_tr = trin_ring.next()
                    S.op("dve", lambda e, sc=sc, tt=tt: e.tensor_tensor(out=sc[:, 0:32], in0=impacc[:, tt, :],
                                                                        in1=nsa_mult[:, tt, :], op=ALU.mult),
                         reads=[r_imp[tt], r_c], writes=[r_sc])
                    S.op("dve", lambda e, sc=sc, tt=tt: e.tensor_tensor(out=sc[:, 0:32], in0=sc[:, 0:32],
                                                                        in1=nsa_add[:, tt, :], op=ALU.add),
                         reads=[r_sc, r_c], writes=[r_sc])
                    S.op("dve", lambda e, sc=sc, st=st: e.max(out=st[:, 0:8], in_=sc[:, 0:32]), reads=[r_sc], writes=[r_st])
                    S.op("dve", lambda e, sc=sc, st=st, tr=tr: e.tensor_scalar(
                        out=tr[:, 64:96], in0=sc[:, 0:32], scalar1=st[:, 7:8], scalar2=-BIG, op0=ALU.is_lt, op1=ALU.mult),
                        reads=[r_sc, r_st], writes=[r_tr])
                    ps, r_ps = misc_ring.next()
                    psb = ps[:].bitcast(BF16)
                    S.op("pe", lambda e, psb=psb, tr=tr: e.transpose(out=psb[0:96, 0:128], in_=tr[:, 0:96],
                                                                     identity=ident_b[:]),
                         reads=[r_tr, r_ident], writes=[r_ps])
                    for hl in range(4):
                        S.op("dve", lambda e, psb=psb, hl=hl, tt=tt: e.tensor_copy(
                            out=q_aug[hl][64:96, tt * 128:(tt + 1) * 128], in_=psb[64:96, 0:128]),
                            reads=[r_ps], writes=[r_qs[hl][tt // 4]])

                def mk_evac(hl, qb, br):
                    h = 4 * g + hl

                    def ev(u, acc, r_acc):
                        tt = 4 * qb + u
                        st, r_st = sm_ring.next()
                        S.op("dve", lambda e: e.reciprocal(out=st[:, 1:2], in_=acc[:, 64:65]), reads=[r_acc], writes=[r_st])
                        S.op("dve", lambda e: e.tensor_tensor(out=st[:, 2:3], in0=st[:, 1:2],
                                                              in1=gsig[:, tt, 3 * h + br:3 * h + br + 1], op=ALU.mult),
                             reads=[r_st, r_gsig], writes=[r_st])
                        S.op("dve", lambda e: e.scalar_tensor_tensor(
                            out=oacc[:, tt, hl * 64:(hl + 1) * 64], in0=acc[:, 0:64], scalar=st[:, 2:3],
                            in1=oacc[:, tt, hl * 64:(hl + 1) * 64], op0=ALU.mult, op1=ALU.add),
                            reads=[r_acc, r_st, r_oacc[tt]], writes=[r_oacc[tt]])
                    return ev

                rounds = []
                for hl in range(4):
                    h = 4 * g + hl
                    for qb in range(4):
                        rounds.append(dict(q=q_aug[hl], r_q=[r_q[hl][qb], r_qst], krows=99, kT=kw_aug, r_k=r_kw, qb=qb,
                                           tiles=window_tiles(qb), V=lambda kt: vwA[:, kt, :], r_v=r_vw, nv=65,
                                           bias=lambda kt, h=h, qb=qb: abias[:, h * 19 + (kt - 4 * qb + 15):h * 19 + (kt - 4 * qb + 15) + 1],
                                           evac=mk_evac(hl, qb, 2)))
                if dbg and s == 0 and l == 0 and g == 0:
                    run_rounds(rounds)
                    rounds = []
                    dump("oacc_win", oacc, r_oacc)
                for hl in range(4):
                    h = 4 * g + hl
                    for qb in range(4):
                        rounds.append(dict(q=q_aug[hl], r_q=[r_q[hl][qb], r_qs[hl][qb], r_qst], krows=99, kT=ks_aug,
                                           r_k=r_ks, qb=qb, tiles=causal_tiles(qb), V=lambda kt: vsA[:, kt, :], r_v=r_vs,
                                           nv=65,
                                           bias=lambda kt, h=h, qb=qb: abias[:, h * 19 + (kt - 4 * qb + 15):h * 19 + (kt - 4 * qb + 15) + 1],
                                           evac=mk_evac(hl, qb, 1)))
                run_rounds(rounds)
                if dbg and s == 0 and l == 0 and g == 0:
                    dump("oacc_all", oacc, r_oacc)
                    dump("q0", q_aug[0], [r_q[0][b] for b in range(4)] + [r_qs[0][b] for b in range(4)] + [r_qst])
                    dump("ks_aug", ks_aug, [r_ks])
                for tt in range(16):
                    ob, r_ob = ob_ring.next()
                    S.op("act", lambda e, ob=ob, tt=tt: e.copy(out=ob, in_=oacc[:, tt, :]), reads=[r_oacc[tt]], writes=[r_ob])
                    transpose_tile(ob, r_ob, onT[:, 2 * g:2 * g + 2, tt * 128:(tt + 1) * 128], r_onT[tt], nk=2,
                                   evac="dve", ring=misc_ring)

            for hf in range(2):
                S.barrier()
                A.off = region0
                q_aug = [A.alloc([128, T], BF16) for _ in range(4)]
                k_aug = [A.alloc([128, T], BF16) for _ in range(4)]
                r_q = [[Res() for _ in range(4)] for _ in range(4)]
                r_qs = [[Res() for _ in range(4)] for _ in range(4)]
                r_qst = Res()
                r_k = [Res() for _ in range(4)]
                vmA = A.alloc([128, 16, 4, 65], BF16)
                r_vm = Res()
                kmean_f = A.alloc([64, 4, 8], F32)
                kmean_b = A.alloc([64, 4, 8], BF16)
                r_km = Res()
                omb = A.alloc([128, 16, 256], BF16)
                r_omb = [Res() for _ in range(16)]
                trin_ring = Ring([(A.alloc([128, 72], BF16), Res()) for _ in range(4)])
                for tr, r_tr in trin_ring.items:
                    S.op("pool", lambda e, tr=tr: e.memset(tr[:, 0:64], 0.0), writes=[r_tr])
                S.op("pool", lambda e: e.memset(vmA[:, :, :, 64:65], 1.0), writes=[r_vm])
                for hl in range(4):
                    h = 4 * hf + hl
                    S.op("pool", lambda e, hl=hl, h=h: e.dma_start(
                        out=q_aug[hl][72:75, :].rearrange("p (b i) -> p b i", b=4),
                        in_=C["qalibi"][8 + h, :, :].unsqueeze(1).to_broadcast([3, 4, 512])), writes=[r_qst], dma=True)
                    S.op("pool", lambda e, hl=hl: e.dma_start(out=q_aug[hl][64:72, :], in_=C["zeros32"][0:8, :]),
                         writes=r_qs[hl], dma=True)
                    S.op("pool", lambda e, hl=hl: e.dma_start(out=k_aug[hl][64:72, :], in_=C["e8"][:, :]),
                         writes=[r_k[hl]], dma=True)
                    S.op("pool", lambda e, hl=hl: e.dma_start(out=k_aug[hl][72:75, :], in_=C["ones3"][:, :]),
                         writes=[r_k[hl]], dma=True)
                for hl in range(4):
                    h = 4 * hf + hl
                    proj_fm_head(MOBA_COLS["q"] + 64 * h, lambda b, hl=hl: q_aug[hl][0:64, b * 512:(b + 1) * 512],
                                 lambda b, hl=hl: r_q[hl][b], gq_m[:, 0:1], True)
                    proj_fm_head(MOBA_COLS["k"] + 64 * h, lambda b, hl=hl: k_aug[hl][0:64, b * 512:(b + 1) * 512],
                                 lambda b, hl=hl: r_k[hl], gq_m[:, 1:2], True)
                    S.op("dve", lambda e, hl=hl: e.tensor_reduce(
                        out=kmean_f[:, hl, :], in_=k_aug[hl][0:64, :].rearrange("p (n k) -> p n k", k=256),
                        axis=AX.X, op=ALU.add), reads=[r_k[hl]], writes=[r_km])
                S.op("dve", lambda e: e.tensor_scalar(out=kmean_b, in0=kmean_f, scalar1=1.0 / 256, scalar2=None,
                                                      op0=ALU.mult), reads=[r_km], writes=[r_km])
                w, r_w = load_w(win_cols(MOBA_COLS["v"] + 256 * hf, 256), 256)
                for i in range(16):
                    ps, r_ps = ps_ring.next()
                    fns = [lambda e, k=k, ps=ps, i=i, w=w: e.matmul(ps[:, 0:256], lhsT=hT[:, k, i * 128:(i + 1) * 128],
                                                                    rhs=w[:, k, 0:256], start=(k == 0), stop=(k == 7))
                           for k in range(8)]
                    S.op("pe", fns, reads=[r_w, r_hT[i]], writes=[r_ps])
                    S.op("act", lambda e, ps=ps, i=i: e.copy(out=vmA[:, i, :, 0:64],
                                                             in_=ps[:, 0:256].rearrange("p (h d) -> p h d", d=64)),
                         reads=[r_ps], writes=[r_vm])
                for tt in range(16):
                    cur = tt // 2
                    if tt >= 8:
                        ps, r_ps = misc_ring.next()
                        fns = [lambda e, hl=hl, ps=ps, tt=tt: e.matmul(ps[:, hl * 8:(hl + 1) * 8],
                                                                       lhsT=q_aug[hl][0:64, tt * 128:(tt + 1) * 128],
                                                                       rhs=kmean_b[:, hl, :], start=True, stop=True)
                               for hl in range(4)]
                        S.op("pe", fns, reads=[r_km] + [r_q[hl][tt // 4] for hl in range(4)], writes=[r_ps])
                        sc, r_sc = tmpf_ring.next()
                        for hl in range(4):
                            S.op("dve", lambda e, hl=hl, ps=ps, sc=sc, tt=tt: e.tensor_tensor(
                                out=sc[:, hl * 8:(hl + 1) * 8], in0=ps[:, hl * 8:(hl + 1) * 8], in1=moba_add[:, tt, :],
                                op=ALU.add), reads=[r_ps, r_c], writes=[r_sc])
                    for hl in range(4):
                        tr, r_tr = trin_ring.next()
                        S.op("pool", lambda e, tr=tr, cur=cur: e.memset(tr[:, 64 + cur:65 + cur], 0.0), writes=[r_tr])
                        if cur < 7:
                            S.op("pool", lambda e, tr=tr, cur=cur: e.memset(tr[:, 65 + cur:72], -BIG), writes=[r_tr])
                        if tt < 8:
                            if cur > 0:
                                S.op("pool", lambda e, tr=tr, cur=cur: e.memset(tr[:, 64:64 + cur], 0.0), writes=[r_tr])
                        else:
                            st, r_st = sm_ring.next()
                            S.op("dve", lambda e, sc=sc, st=st, hl=hl: e.max(out=st[:, 0:8], in_=sc[:, hl * 8:(hl + 1) * 8]),
                                 reads=[r_sc], writes=[r_st])
                            S.op("dve", lambda e, sc=sc, st=st, tr=tr, hl=hl, cur=cur: e.tensor_scalar(
                                out=tr[:, 64:64 + cur], in0=sc[:, hl * 8:hl * 8 + cur], scalar1=st[:, 2:3], scalar2=-BIG,
                                op0=ALU.is_lt, op1=ALU.mult), reads=[r_sc, r_st], writes=[r_tr])
                        ps2, r_ps2 = ps_ring.next()
                        psb = ps2[:].bitcast(BF16)
                        S.op("pe", lambda e, psb=psb, tr=tr: e.transpose(out=psb[0:72, 0:128], in_=tr[:, 0:72],
                                                                         identity=ident_b[:]),
                             reads=[r_tr, r_ident], writes=[r_ps2])
                        S.op("dve", lambda e, psb=psb, hl=hl, tt=tt: e.tensor_copy(
                            out=q_aug[hl][64:72, tt * 128:(tt + 1) * 128], in_=psb[64:72, 0:128]),
                            reads=[r_ps2], writes=[r_qs[hl][tt // 4]])

                def mk_evac_m(hl, qb):
                    def ev(u, acc, r_acc):
                        tt = 4 * qb + u
                        st, r_st = sm_ring.next()
                        S.op("dve", lambda e: e.reciprocal(out=st[:, 1:2], in_=acc[:, 64:65]), reads=[r_acc], writes=[r_st])
                        S.op("dve", lambda e: e.tensor_scalar(out=omb[:, tt, hl * 64:(hl + 1) * 64], in0=acc[:, 0:64],
                                                              scalar1=st[:, 1:2], scalar2=None, op0=ALU.mult),
                             reads=[r_acc, r_st], writes=[r_omb[tt]])
                    return ev

                rounds = []
                for hl in range(4):
                    h = 4 * hf + hl
                    for qb in range(4):
                        rounds.append(dict(q=q_aug[hl], r_q=[r_q[hl][qb], r_qs[hl][qb], r_qst], krows=75, kT=k_aug[hl],
                                           r_k=r_k[hl], qb=qb, tiles=causal_tiles(qb),
                                           V=lambda kt, hl=hl: vmA[:, kt, hl, :], r_v=r_vm, nv=65,
                                           bias=lambda kt, h=h, qb=qb: abias[:, (8 + h) * 19 + (kt - 4 * qb + 15):(8 + h) * 19 + (kt - 4 * qb + 15) + 1],
                                           evac=mk_evac_m(hl, qb)))
                run_rounds(rounds)
                for tt in range(16):
                    transpose_tile(omb[:, tt, :], r_omb[tt], omT[:, 2 * hf:2 * hf + 2, tt * 128:(tt + 1) * 128],
                                   r_omT[tt], nk=2, evac="dve", ring=misc_ring)

            if dbg and s == 0 and l == 0:
                for nm, src, rr in (("onT", onT, r_onT), ("omT", omT, r_omT)):
                    if nm in dbg_d:
                        S.op("pool", lambda e, nm=nm, src=src: e.dma_start(
                            out=dbg_d[nm][:, :].rearrange("p (a b) -> p a b", a=4), in_=src), reads=rr,
                            writes=[Res()], dma=True)

            S.barrier()
            A.off = region0
            yT = A.alloc([128, 8, T], BF16)
            r_yT = [Res() for _ in range(8)]
            wout = A.alloc([128, 8, D], BF16)
            r_wout = Res()
            S.op("pool", lambda e: e.dma_start(out=wout, in_=wout_d[l, :, :].rearrange("(k p) c -> p k c", p=128)),
                 writes=[r_wout], dma=True)
            for oc in range(8):
                wgn, r_wgn = load_w(win_cols(GATE_N + 128 * oc, 128), 128)
                wgm, r_wgm = load_w(win_cols(GATE_M + 128 * oc, 128), 128)
                wu, r_wu = wch_ring.next()
                S.op("pool", lambda e, wu=wu, oc=oc: e.dma_start(
                    out=wu[:, 0:4, 0:128], in_=wupn_d[l, :, oc * 128:(oc + 1) * 128].rearrange("(k p) c -> p k c", p=128)),
                    writes=[r_wu], dma=True)
                S.op("pool", lambda e, wu=wu, oc=oc: e.dma_start(
                    out=wu[:, 4:8, 0:128], in_=wupm_d[l, :, oc * 128:(oc + 1) * 128].rearrange("(k p) c -> p k c", p=128)),
                    writes=[r_wu], dma=True)
                for b in range(4):
                    bs = slice(b * 512, (b + 1) * 512)
                    res = []
                    for (wg, r_wg, oT, r_oT, ko) in ((wgn, r_wgn, onT, r_onT, 0), (wgm, r_wgm, omT, r_omT, 4)):
                        pg, r_pg = ps_ring.next()
                        fns = [lambda e, k=k, pg=pg, wg=wg, bs=bs: e.matmul(pg[:], lhsT=wg[:, k, 0:128], rhs=hT[:, k, bs],
                                                                     start=(k == 0), stop=(k == 7)) for k in range(8)]
                        S.op("pe", fns, reads=[r_wg] + r_hT[4 * b:4 * b + 4], writes=[r_pg])
                        pu, r_pu = ps_ring.next()
                        fns = [lambda e, k=k, pu=pu, oT=oT, ko=ko, wu=wu, bs=bs: e.matmul(pu[:], lhsT=wu[:, ko + k, 0:128], rhs=oT[:, k, bs],
                                                                            start=(k == 0), stop=(k == 3)) for k in range(4)]
                        S.op("pe", fns, reads=[r_wu] + r_oT[4 * b:4 * b + 4], writes=[r_pu])
                        sg, r_sg = tmpf_ring.next()
                        S.op("act", lambda e, pg=pg, sg=sg: e.activation(out=sg, in_=pg[:], func=AF.Sigmoid),
                             reads=[r_pg], writes=[r_sg])
                        S.op("dve", lambda e, pu=pu, sg=sg: e.tensor_tensor(out=sg, in0=sg, in1=pu[:], op=ALU.mult),
                             reads=[r_pu, r_sg], writes=[r_sg])
                        res.append((sg, r_sg))
                    S.op("dve", lambda e, a=res[0][0], b_=res[1][0], oc=oc, bs=bs: e.tensor_tensor(
                        out=yT[:, oc, bs], in0=a, in1=b_, op=ALU.add), reads=[res[0][1], res[1][1]], writes=[r_yT[oc]])
            for i in range(16):
                tt = tt0 + i
                xa, r_xa = xt_ring.next()
                S.op("sp", lambda e, xa=xa, tt=tt: e.dma_start(out=xa, in_=src_d[tt * 128:(tt + 1) * 128, :]),
                     reads=[r_src[tt]], writes=[r_xa], dma=True)
                for h2 in range(2):
                    po, r_po = ps_ring.next()
                    fns = [lambda e, oc=oc, po=po, i=i, h2=h2: e.matmul(po[:], lhsT=yT[:, oc, i * 128:(i + 1) * 128],
                                                                        rhs=wout[:, oc, h2 * 512:(h2 + 1) * 512],
                                                                        start=(oc == 0), stop=(oc == 7)) for oc in range(8)]
                    S.op("pe", fns, reads=r_yT + [r_wout], writes=[r_po])
                    S.op("dve", lambda e, po=po, xa=xa, h2=h2: e.tensor_tensor(
                        out=xa[:, h2 * 512:(h2 + 1) * 512], in0=po[:], in1=xa[:, h2 * 512:(h2 + 1) * 512], op=ALU.add),
                        reads=[r_po, r_xa], writes=[r_xa])
                S.op("sp", lambda e, xa=xa, tt=tt: e.dma_start(out=y_d[tt * 128:(tt + 1) * 128, :], in_=xa),
                     reads=[r_xa], writes=[r_y[tt]], dma=True)

    cur_d, cur_r = x_d, r_x
    for l in range(depth):
        if f"ffn{2 * l}" in phases or "all" in phases:
            ffn_phase(l, 0, cur_d, cur_r)
            cur_d, cur_r = y_d, r_y
        if f"mix{l}" in phases or "all" in phases:
            mix_phase(l, cur_d, cur_r)
            cur_d, cur_r = y_d, r_y
        if f"ffn{2 * l + 1}" in phases or "all" in phases:
            ffn_phase(l, 1, cur_d, cur_r)
            cur_d, cur_r = y_d, r_y

    S.barrier()
    S.finish("sp", r_y)
    sems = [es.enter_context(nc.semaphore(f"s{i}")) for i in range(S.nsem)]
    S.emit(nc, sems)
    es.close()
    return nc, S


def make_in_maps(inputs, nseq, ncores):
    x = np.ascontiguousarray(inputs["x"], dtype=np.float32).reshape(-1, nseq * T, D)
    consts = make_consts()
    shared = {}
    for k in ("norm_g", "ffn_w1", "ffn_w3", "ffn_w2", "w_in", "g_qk_nsa", "cmp_w1", "cmp_w2", "w_up_nsa", "w_up_moba",
              "w_out"):
        shared[k] = np.ascontiguousarray(inputs[k], dtype=np.float32)
    shared["g_qk_nsaT"] = np.ascontiguousarray(np.transpose(np.asarray(inputs["g_qk_nsa"], np.float32), (0, 2, 1)))
    shared["g_qk_mobaT"] = np.ascontiguousarray(np.transpose(np.asarray(inputs["g_qk_moba"], np.float32), (0, 2, 1)))
    shared["cmp_posT"] = np.ascontiguousarray(np.transpose(np.asarray(inputs["cmp_pos"], np.float32), (0, 1, 3, 2)))
    shared.update(consts)
    in_maps = []
    for c in range(ncores):
        m = {"x": x[c]}
        m.update(shared)
        in_maps.append(m)
    return in_maps


_CACHE = {}


def kernel(**inputs):
    nseq = 16 // NCORES
    if "full" not in _CACHE:
        _CACHE["full"] = build_program(nseq=nseq, phases=("all",))
    nc, S = _CACHE["full"]
    in_maps = make_in_maps(inputs, nseq, NCORES)
    res = run_bass_kernel_spmd(nc, in_maps, core_ids=list(range(NCORES)))
    y = np.stack([np.asarray(r["y"]) for r in res.results], axis=0)
    return y.reshape(16, T, D).astype(np.float32)
```

```python
import numpy as np
from contextlib import ExitStack
import concourse.bass as bass
import concourse.mybir as mybir
from concourse.bass_utils import run_bass_kernel_spmd

F32 = mybir.dt.float32
BF16 = mybir.dt.bfloat16
AF = mybir.ActivationFunctionType
ALU = mybir.AluOpType
AX = mybir.AxisListType

NCORES = 8
D = 1024
T = 2048
DFF = 2816
NF = DFF // 128
DEPTH = 2
EPS = 1e-6


class Res:
    __slots__ = ("w", "r", "name")

    def __init__(self, name=""):
        self.w = None
        self.r = {}
        self.name = name


class _Eng:
    def __init__(self, name, sem):
        self.name = name
        self.sem = sem
        self.count = 0
        self.known = {}
        self.ops = []
        self.dma_sems = []
        self.dma_count = 0


class Sched:
    NDMA = 8

    def __init__(self):
        self.nsem = 0
        self.eng = {}
        for n in ("pe", "act", "dve", "pool", "sp"):
            self.eng[n] = _Eng(n, self._newsem())
        for n in ("sp", "pool", "act"):
            self.eng[n].dma_sems = [self._newsem() for _ in range(self.NDMA)]

    def _newsem(self):
        s = self.nsem
        self.nsem += 1
        return s

    def op(self, eng, fns, reads=(), writes=(), dma=False):
        E = self.eng[eng]
        if not isinstance(fns, (list, tuple)):
            fns = [fns]
        need = {}

        def req(tok):
            if tok is None:
                return
            s, v, clk = tok
            o = need.get(s)
            if o is None or o[0] < v:
                need[s] = (v, clk)

        for r in reads:
            req(r.w)
        for w in writes:
            req(w.w)
            for s, (v, clk) in w.r.items():
                req((s, v, clk))
        implied = {}
        for s, (v, clk) in need.items():
            for cs, cv in clk.items():
                if implied.get(cs, 0) < cv:
                    implied[cs] = cv
        waits = []
        known = E.known
        for s, (v, clk) in need.items():
            if known.get(s, 0) >= v or implied.get(s, 0) >= v:
                continue
            if eng == "pe" and s == E.sem:
                continue
            waits.append((s, v))
        for s, v in implied.items():
            if known.get(s, 0) < v:
                known[s] = v
        for s, (v, clk) in need.items():
            if known.get(s, 0) < v:
                known[s] = v
        if dma:
            j = E.dma_count
            E.dma_count += 1
            s = E.dma_sems[j % self.NDMA]
            prev = 16 * (j // self.NDMA)
            if prev > 0 and known.get(s, 0) < prev:
                waits.append((s, prev))
                known[s] = prev
            val = prev + 16
            inc = 16
        else:
            E.count += 1
            s = E.sem
            val = E.count
            inc = 1
        clk = dict(known)
        tok = (s, val, clk)
        E.ops.append((waits, list(fns), s, inc))
        for r in reads:
            o = r.r.get(s)
            if o is None or o[0] < val:
                r.r[s] = (val, clk)
        for w in writes:
            w.w = tok
            w.r = {}
        return tok

    def finish(self, eng, resources):
        E = self.eng[eng]
        need = {}
        for r in resources:
            toks = []
            if r.w is not None:
                toks.append(r.w)
            for s, (v, clk) in r.r.items():
                toks.append((s, v, clk))
            for s, v, clk in toks:
                if need.get(s, 0) < v:
                    need[s] = v
        waits = [(s, v) for s, v in need.items() if E.known.get(s, 0) < v]
        E.ops.append((waits, [], None, 0))

    def emit(self, nc, sems):
        def replay(E):
            def body(e):
                for waits, fns, s, inc in E.ops:
                    for ws, wv in waits:
                        e.wait_ge(sems[ws], wv)
                    if not fns:
                        continue
                    for fn in fns[:-1]:
                        fn(e)
                    fns[-1](e).then_inc(sems[s], inc)
            return body

        with nc.Block() as block:
            block.sync(replay(self.eng["sp"]))
            block.scalar(replay(self.eng["act"]))
            block.vector(replay(self.eng["dve"]))
            block.gpsimd(replay(self.eng["pool"]))
            block.tensor(replay(self.eng["pe"]))


class Ring:
    def __init__(self, items):
        self.items = items
        self.i = 0

    def next(self):
        it = self.items[self.i % len(self.items)]
        self.i += 1
        return it


def _barrier(self):
    toks = {}
    for E in self.eng.values():
        if E.count > 0:
            toks[E.sem] = E.count
        for i, s in enumerate(E.dma_sems):
            if E.dma_count > i:
                toks[s] = 16 * ((E.dma_count - i + self.NDMA - 1) // self.NDMA)
    for E in self.eng.values():
        waits = []
        for s, v in toks.items():
            if E.known.get(s, 0) >= v:
                continue
            if E.name == "pe" and s == E.sem:
                continue
            waits.append((s, v))
            E.known[s] = v
        if waits:
            E.ops.append((waits, [], None, 0))


Sched.barrier = _barrier


class Arena:
    def __init__(self, t, nel):
        self.t = t
        self.nel = nel
        self.off = 0

    def alloc(self, shape, dt):
        n = 1
        for d in shape[1:]:
            n *= d
        sz = n * (2 if dt == F32 else 1)
        self.off = (self.off + 1) // 2 * 2
        o = self.off
        self.off += sz
        assert self.off <= self.nel, ("arena overflow", self.off, self.nel)
        ap = self.t[0:shape[0], o:o + sz]
        if dt == F32:
            ap = ap.bitcast(F32)
        if len(shape) == 3:
            ap = ap.rearrange("p (a b) -> p a b", a=shape[1])
        elif len(shape) == 4:
            ap = ap.rearrange("p (a b c) -> p a b c", a=shape[1], b=shape[2])
        return ap


BIG = 30000.0
NSA_COLS = dict(q=0, kc=512, vc=640, ks=768, vs=896, kw=1024, vw=1152, g=1280)
MOBA_COLS = dict(q=1304, k=1816, v=2328)
GATE_N, GATE_M = 2840, 3864
INC = 4888


def slopes_all():
    s = (2.0 ** (-8.0 * np.arange(1, 17) / 16)).astype(np.float32)
    return s[0::2].copy(), s[1::2].copy()


def _bf16_round(a):
    a = np.asarray(a, dtype=np.float32)
    u = a.view(np.uint32).astype(np.uint64)
    r = ((u + 0x7FFF + ((u >> 16) & 1)) >> 16) << 16
    return r.astype(np.uint32).view(np.float32)


def make_consts():
    c = {}
    c["ident"] = np.eye(128, dtype=np.float32)
    j = np.arange(128)[:, None]
    i = np.arange(128)[None, :]
    c["tri_c"] = np.where(j > i, -BIG, 0.0).astype(np.float32)
    c["tri_a"] = np.where(j <= i, -BIG, 0.0).astype(np.float32)
    c["ones64"] = np.ones((64, 64), np.float32)
    sn, sm = slopes_all()
    sl = np.concatenate([sn, sm])
    ab = np.zeros((128, 16, 19), np.float32)
    for h in range(16):
        for d in range(-15, 4):
            ab[:, h, d + 15] = sl[h].astype(np.float64) * (128 * d + np.arange(128))
    c["abias"] = ab.reshape(128, 16 * 19)
    cb = np.zeros((128, 8, 4), np.float32)
    cc = np.arange(128)
    for h in range(8):
        for qb in range(4):
            cb[:, h, qb] = sn[h].astype(np.float64) * (16 * cc + 15.5 - 512 * qb)
    c["cbias"] = cb.reshape(128, 32)
    qa = np.zeros((16, 3, 512), np.float32)
    for h in range(16):
        v = (-(sl[h].astype(np.float64)) * np.arange(512)).astype(np.float32)
        v1 = _bf16_round(v)
        v2 = _bf16_round(v - v1)
        v3 = _bf16_round(v - v1 - v2)
        qa[h, 0], qa[h, 1], qa[h, 2] = v1, v2, v3
    c["qalibi"] = qa
    key = np.arange(T)
    c["e32"] = (key[None, :] // 64 == np.arange(32)[:, None]).astype(np.float32)
    c["e8"] = (key[None, :] // 256 == np.arange(8)[:, None]).astype(np.float32)
    c["ones3"] = np.ones((3, T), np.float32)
    c["zeros32"] = np.zeros((32, T), np.float32)
    cm = np.zeros((128, T), np.float32)
    cidx = np.arange(128)[:, None]
    cm[:] = np.where(16 * cidx + 31 <= key[None, :], 0.0, -BIG)
    c["cmask"] = cm
    ci = np.arange(127)[:, None] * 16
    sj = np.arange(32)[None, :] * 64
    ov = np.clip(np.minimum(ci + 32, sj + 64) - np.maximum(ci, sj), 0, None)
    M = np.zeros((128, 32), np.float32)
    M[:127] = ov / 32.0
    c["cmp2slc"] = M
    blk = np.arange(32)[None, None, :]
    t = (np.arange(16)[None, :, None] * 128 + np.arange(128)[:, None, None])
    cur = t // 64
    forced = (blk == 0) | (blk == cur) | (blk == cur - 1)
    valid = blk <= cur
    c["nsa_mult"] = np.where(forced | ~valid, 0.0, 1.0).astype(np.float32).reshape(128, 16 * 32)
    c["nsa_add"] = np.where(forced, 1e9, np.where(valid, 0.0, -1e30)).astype(np.float32).reshape(128, 16 * 32)
    n8 = np.arange(8)[None, None, :]
    curm = t // 256
    c["moba_add"] = np.broadcast_to(np.where(n8 < curm, 0.0, -1e30), (128, 16, 8)).astype(np.float32).reshape(128, 128).copy()
    return c


CONST_SHAPES = dict(ident=[128, 128], tri_c=[128, 128], tri_a=[128, 128], ones64=[64, 64], abias=[128, 304],
                    cbias=[128, 32], qalibi=[16, 3, 512], e32=[32, T], e8=[8, T], ones3=[3, T], zeros32=[32, T],
                    cmask=[128, T], cmp2slc=[128, 32], nsa_mult=[128, 512], nsa_add=[128, 512], moba_add=[128, 128])


def build_program(nseq=2, phases=("all",), depth=DEPTH, dbg=None):
    nc = bass.Bass("TRN2", target_bir_lowering=False)
    NT = nseq * T
    NTT = NT // 128
    S = Sched()
    es = ExitStack()

    def dram_in(name, shape, dt=F32):
        return nc.dram_tensor(name, list(shape), dt, kind="ExternalInput").ap()

    x_d = dram_in("x", [NT, D])
    normg_d = dram_in("norm_g", [DEPTH, 3, D])
    w1_d = dram_in("ffn_w1", [DEPTH, 2, D, DFF])
    w3_d = dram_in("ffn_w3", [DEPTH, 2, D, DFF])
    w2_d = dram_in("ffn_w2", [DEPTH, 2, DFF, D])
    win_d = dram_in("w_in", [DEPTH, D, INC])
    gqn_d = dram_in("g_qk_nsa", [DEPTH, 4, 64])
    gqnT_d = dram_in("g_qk_nsaT", [DEPTH, 64, 4])
    gqmT_d = dram_in("g_qk_mobaT", [DEPTH, 64, 2])
    posT_d = dram_in("cmp_posT", [DEPTH, 2, 64, 32])
    cw1_d = dram_in("cmp_w1", [DEPTH, 2, 2048, 256])
    cw2_d = dram_in("cmp_w2", [DEPTH, 2, 256, 64])
    wupn_d = dram_in("w_up_nsa", [DEPTH, 512, D])
    wupm_d = dram_in("w_up_moba", [DEPTH, 512, D])
    wout_d = dram_in("w_out", [DEPTH, D, D])
    C = {k: dram_in(k, v) for k, v in CONST_SHAPES.items()}
    y_d = nc.dram_tensor("y", [NT, D], F32, kind="ExternalOutput").ap()
    dbg_d = {}
    if dbg:
        for k, shp in dbg.items():
            dbg_d[k] = nc.dram_tensor("dbg_" + k, list(shp), F32, kind="ExternalOutput").ap()

    def sb(name, shape, dt):
        return es.enter_context(nc.sbuf_tensor(name, list(shape), dt))

    banks = []
    for i in range(8):
        t = es.enter_context(nc.psum_tensor(f"ps{i}", [128, 512], F32))
        banks.append((t, Res(f"ps{i}")))
    ps_ring = Ring(banks)
    acc_banks = banks[0:4]
    acc_ring = Ring(banks[0:4])
    sc_ring = Ring(banks[4:7])
    misc_ring = Ring(banks[7:8])

    ident_b = sb("ident_b", [128, 128], BF16)
    r_ident = Res("ident")
    S.op("pool", lambda e: e.dma_start(out=ident_b[:], in_=C["ident"][:, :]), writes=[r_ident], dma=True)
    stat = [(sb(f"stat{i}", [128, 4], F32), Res(f"stat{i}")) for i in range(4)]
    stat_ring = Ring(stat)
    ARENA_EL = 99000
    arena_t = sb("arena", [128, ARENA_EL], BF16)
    A = Arena(arena_t, ARENA_EL)

    r_y = [Res(f"y{i}") for i in range(NTT)]
    r_x = [Res(f"x{i}") for i in range(NTT)]

    def rmsnorm_tile(x_ap, r_x_, g_ap, r_gres, out_ap, r_out, jk, r_jk):
        st, r_st = stat_ring.next()
        S.op("act", lambda e: e.activation(out=jk, in_=x_ap, func=AF.Square, accum_out=st[:, 0:1]),
             reads=[r_x_], writes=[r_jk, r_st])
        S.op("act", lambda e: e.activation(out=st[:, 1:2], in_=st[:, 0:1], func=AF.Sqrt, scale=1.0 / D, bias=EPS),
             reads=[r_st], writes=[r_st])
        S.op("dve", lambda e: e.reciprocal(out=st[:, 2:3], in_=st[:, 1:2]), reads=[r_st], writes=[r_st])
        S.op("dve", lambda e: e.scalar_tensor_tensor(out=out_ap, in0=x_ap, scalar=st[:, 2:3], in1=g_ap,
                                                     op0=ALU.mult, op1=ALU.mult),
             reads=[r_x_, r_st, r_gres], writes=[r_out])

    def transpose_tile(in_tile, r_in, out_ap3, r_out, nk=8, evac="act", ring=None):
        ps, r_ps = (ring or ps_ring).next()
        psb = ps[:].bitcast(BF16)
        fns = []
        for k in range(nk):
            fns.append(lambda e, k=k: e.transpose(out=psb[:, k * 128:(k + 1) * 128],
                                                  in_=in_tile[:, k * 128:(k + 1) * 128], identity=ident_b[:]))
        S.op("pe", fns, reads=[r_in, r_ident], writes=[r_ps])
        src = psb[:, 0:nk * 128].rearrange("p (k t) -> p k t", k=nk)
        if evac == "act":
            S.op("act", lambda e: e.copy(out=out_ap3, in_=src), reads=[r_ps], writes=[r_out])
        else:
            S.op("dve", lambda e: e.tensor_copy(out=out_ap3, in_=src), reads=[r_ps], writes=[r_out])

    def ffn_phase(l, j, src_d, r_src):
        S.barrier()
        A.off = 0
        w1_sb = A.alloc([128, 8, DFF], BF16)
        w3_sb = A.alloc([128, 8, DFF], BF16)
        w2_sb = A.alloc([128, NF, D], BF16)
        r_w1 = [Res() for f in range(NF)]
        r_w3 = [Res() for f in range(NF)]
        r_w2 = [Res() for f in range(NF)]
        g_rep = A.alloc([128, D], F32)
        r_g = Res()
        xt = [(A.alloc([128, D], F32), Res()) for i in range(4)]
        hb_ring = Ring([(A.alloc([128, D], BF16), Res()) for i in range(2)])
        hT = A.alloc([128, 8, 512], BF16)
        r_hT = [Res() for i in range(4)]
        gT = A.alloc([128, NF, 512], BF16)
        r_gT = [Res() for f in range(NF)]
        su_ring = Ring([(A.alloc([128, 512], F32), Res()) for i in range(2)])
        junk = (A.alloc([128, D], BF16), Res())

        S.op("pool", lambda e: e.dma_start(out=g_rep, in_=normg_d[l, 2 * j, :].partition_broadcast(128)),
             writes=[r_g], dma=True)
        for f in range(NF):
            S.op("pool", lambda e, f=f: e.dma_start(
                out=w1_sb[:, :, f * 128:(f + 1) * 128],
                in_=w1_d[l, j, :, f * 128:(f + 1) * 128].rearrange("(k p) c -> p k c", p=128)),
                writes=[r_w1[f]], dma=True)
            S.op("pool", lambda e, f=f: e.dma_start(
                out=w3_sb[:, :, f * 128:(f + 1) * 128],
                in_=w3_d[l, j, :, f * 128:(f + 1) * 128].rearrange("(k p) c -> p k c", p=128)),
                writes=[r_w3[f]], dma=True)
        for f in range(NF):
            S.op("pool", lambda e, f=f: e.dma_start(out=w2_sb[:, f, :], in_=w2_d[l, j, f * 128:(f + 1) * 128, :]),
                 writes=[r_w2[f]], dma=True)

        for blk in range(NT // 512):
            for i in range(4):
                tt = blk * 4 + i
                xa, r_xa = xt[i]
                S.op("sp", lambda e, xa=xa, tt=tt: e.dma_start(out=xa, in_=src_d[tt * 128:(tt + 1) * 128, :]),
                     reads=[r_src[tt]], writes=[r_xa], dma=True)
                hbt, r_hb = hb_ring.next()
                rmsnorm_tile(xa, r_xa, g_rep, r_g, hbt, r_hb, junk[0], junk[1])
                transpose_tile(hbt, r_hb, hT[:, :, i * 128:(i + 1) * 128], r_hT[i])
            for f in range(NF):
                pu, r_pu = ps_ring.next()
                pv, r_pv = ps_ring.next()
                fns = []
                for k in range(8):
                    fns.append(lambda e, k=k, f=f, pu=pu: e.matmul(pu[:], lhsT=w1_sb[:, k, f * 128:(f + 1) * 128],
                                                                    rhs=hT[:, k, :], start=(k == 0), stop=(k == 7)))
                S.op("pe", fns, reads=[r_w1[f]] + r_hT, writes=[r_pu])
                fns = []
                for k in range(8):
                    fns.append(lambda e, k=k, f=f, pv=pv: e.matmul(pv[:], lhsT=w3_sb[:, k, f * 128:(f + 1) * 128],
                                                                    rhs=hT[:, k, :], start=(k == 0), stop=(k == 7)))
                S.op("pe", fns, reads=[r_w3[f]] + r_hT, writes=[r_pv])
                s_t, r_s = su_ring.next()
                S.op("act", lambda e, s_t=s_t, pu=pu: e.activation(out=s_t, in_=pu[:], func=AF.Silu),
                     reads=[r_pu], writes=[r_s])
                S.op("dve", lambda e, s_t=s_t, pv=pv, f=f: e.tensor_tensor(out=gT[:, f, :], in0=s_t, in1=pv[:],
                                                                             op=ALU.mult),
                     reads=[r_s, r_pv], writes=[r_gT[f]])
            for i in range(4):
                tt = blk * 4 + i
                xa, r_xa = xt[i]
                for h in range(2):
                    po, r_po = ps_ring.next()
                    fns = []
                    for f in range(NF):
                        fns.append(lambda e, f=f, po=po, i=i, h=h: e.matmul(
                            po[:], lhsT=gT[:, f, i * 128:(i + 1) * 128], rhs=w2_sb[:, f, h * 512:(h + 1) * 512],
                            start=(f == 0), stop=(f == NF - 1)))
                    S.op("pe", fns, reads=r_gT + r_w2, writes=[r_po])
                    S.op("dve", lambda e, po=po, xa=xa, h=h: e.scalar_tensor_tensor(
                        out=xa[:, h * 512:(h + 1) * 512], in0=po[:], scalar=0.5, in1=xa[:, h * 512:(h + 1) * 512],
                        op0=ALU.mult, op1=ALU.add), reads=[r_po, r_xa], writes=[r_xa])
                S.op("sp", lambda e, xa=xa, tt=tt: e.dma_start(out=y_d[tt * 128:(tt + 1) * 128, :], in_=xa),
                     reads=[r_xa], writes=[r_y[tt]], dma=True)

    def mix_phase(l, src_d, r_src):
        S.barrier()
        A.off = 0
        r_c = Res()
        tri_c = A.alloc([128, 128], BF16)
        tri_a = A.alloc([128, 128], BF16)
        ones64 = A.alloc([64, 64], BF16)
        abias = A.alloc([128, 304], F32)
        cbias = A.alloc([128, 32], F32)
        cmask = A.alloc([128, T], BF16)
        nsa_mult = A.alloc([128, 16, 32], F32)
        nsa_add = A.alloc([128, 16, 32], F32)
        moba_add = A.alloc([128, 16, 8], F32)
        gq_n = A.alloc([64, 4], F32)
        gq_m = A.alloc([64, 2], F32)
        gkc_rep = A.alloc([128, 64], F32)
        g_rep = A.alloc([128, D], F32)
        for dst, src in ((tri_c, C["tri_c"][:, :]), (tri_a, C["tri_a"][:, :]), (ones64, C["ones64"][:, :]),
                         (abias, C["abias"][:, :]), (cbias, C["cbias"][:, :]), (cmask, C["cmask"][:, :]),
                         (nsa_mult, C["nsa_mult"][:, :].rearrange("p (a b) -> p a b", a=16)),
                         (nsa_add, C["nsa_add"][:, :].rearrange("p (a b) -> p a b", a=16)),
                         (moba_add, C["moba_add"][:, :].rearrange("p (a b) -> p a b", a=16)),
                         (gq_n, gqnT_d[l, :, :]), (gq_m, gqmT_d[l, :, :]),
                         (gkc_rep, gqn_d[l, 1, :].partition_broadcast(128)),
                         (g_rep, normg_d[l, 1, :].partition_broadcast(128))):
            S.op("pool", lambda e, dst=dst, src=src: e.dma_start(out=dst, in_=src), writes=[r_c], dma=True)
        S.op("dve", lambda e: e.tensor_scalar(out=gq_n[:, 0:1], in0=gq_n[:, 0:1], scalar1=0.125, scalar2=None,
                                              op0=ALU.mult), reads=[r_c], writes=[r_c])
        S.op("dve", lambda e: e.tensor_scalar(out=gq_m[:, 0:1], in0=gq_m[:, 0:1], scalar1=0.125, scalar2=None,
                                              op0=ALU.mult), reads=[r_c], writes=[r_c])

        hT = A.alloc([128, 8, T], BF16)
        r_hT = [Res() for _ in range(16)]
        onT = A.alloc([128, 4, T], BF16)
        omT = A.alloc([128, 4, T], BF16)
        r_onT = [Res() for _ in range(16)]
        r_omT = [Res() for _ in range(16)]
        gsig = A.alloc([128, 16, 24], F32)
        r_gsig = Res()
        wch_ring = Ring([(A.alloc([128, 8, 256], BF16), Res()) for _ in range(3)])
        pT_ring = Ring([(A.alloc([128, 512], BF16), Res()) for _ in range(3)])
        tmpf_ring = Ring([(A.alloc([128, 512], F32), Res()) for _ in range(3)])
        sqb_ring = Ring([(A.alloc([128, 512], BF16), Res()) for _ in range(2)])
        xt_ring = Ring([(A.alloc([128, D], F32), Res()) for _ in range(2)])
        hb_ring = Ring([(A.alloc([128, D], BF16), Res()) for _ in range(2)])
        junk = (A.alloc([128, D], BF16), Res())
        sm_ring = Ring([(A.alloc([128, 16], F32), Res()) for _ in range(8)])
        region0 = A.off

        def load_w(src_ap3, ncols):
            w, r_w = wch_ring.next()
            S.op("pool", lambda e: e.dma_start(out=w[:, :, 0:ncols], in_=src_ap3), writes=[r_w], dma=True)
            return w, r_w

        def win_cols(c0, n):
            return win_d[l, :, c0:c0 + n].rearrange("(k p) c -> p k c", p=128)

        def proj_fm_head(c0, dest_fn, r_dest_fn, gcol, want_norm):
            w, r_w = load_w(win_cols(c0, 64), 64)
            for b in range(4):
                ps, r_ps = ps_ring.next()
                fns = [lambda e, k=k, ps=ps, b=b: e.matmul(ps[0:64, :], lhsT=w[:, k, 0:64],
                                                           rhs=hT[:, k, b * 512:(b + 1) * 512],
                                                           start=(k == 0), stop=(k == 7)) for k in range(8)]
                S.op("pe", fns, reads=[r_w] + r_hT[4 * b:4 * b + 4], writes=[r_ps])
                dst, r_dst = dest_fn(b), r_dest_fn(b)
                if not want_norm:
                    S.op("act", lambda e, ps=ps, dst=dst: e.copy(out=dst, in_=ps[0:64, :]),
                         reads=[r_ps], writes=[r_dst])
                    continue
                sq, r_sq = sqb_ring.next()
                S.op("act", lambda e, ps=ps, sq=sq: e.activation(out=sq[0:64, :], in_=ps[0:64, :], func=AF.Square),
                     reads=[r_ps], writes=[r_sq])
                p2, r_p2 = ps_ring.next()
                S.op("pe", lambda e, p2=p2, sq=sq: e.matmul(p2[0:64, :], lhsT=ones64[:, :], rhs=sq[0:64, :],
                                                            start=True, stop=True),
                     reads=[r_sq, r_c], writes=[r_p2])
                tf, r_tf = tmpf_ring.next()
                st, r_st = sm_ring.next()
                S.op("act", lambda e, p2=p2, tf=tf: e.activation(out=tf[0:64, :], in_=p2[0:64, :], func=AF.Ln,
                                                                 scale=1.0 / 64, bias=EPS),
                     reads=[r_p2], writes=[r_tf])
                S.op("act", lambda e, tf=tf: e.activation(out=tf[0:64, :], in_=tf[0:64, :], func=AF.Exp, scale=-0.5),
                     reads=[r_tf], writes=[r_tf])
                S.op("dve", lambda e, ps=ps, tf=tf, dst=dst: e.scalar_tensor_tensor(
                    out=dst, in0=ps[0:64, :], scalar=gcol, in1=tf[0:64, :], op0=ALU.mult, op1=ALU.mult),
                    reads=[r_ps, r_tf, r_c], writes=[r_dst])

        def causal_tiles(qb):
            tl = []
            for kt in range(4 * qb + 4):
                c = kt - 4 * qb
                if c < 0:
                    tl.append((kt, 0, 512, None, None))
                else:
                    tl.append((kt, 128 * c, 512, "c", c))
            return tl

        def window_tiles(qb):
            tl = []
            for c in (0, 1, 2, 3, -1, -2, -3, -4):
                kt = 4 * qb + c
                if kt < 0:
                    continue
                if c >= 0:
                    tl.append((kt, 128 * c, 512, "c", c))
                else:
                    m = 4 + c
                    tl.append((kt, 0, 128 * (m + 1), "a", m))
            return tl

        def run_rounds(rounds):
            items = []
            for R in rounds:
                for ti, tile in enumerate(R["tiles"]):
                    items.append((R, ti, tile))

            def emit_qk(it):
                R, ti, (kt, c0, c1, tri, tu) = it
                ps, r_ps = sc_ring.next()
                kp = R.get("kpart", 128)
                K = R["krows"]
                qb = R["qb"]
                fns = [lambda e: e.matmul(ps[0:kp, c0:c1], lhsT=R["kT"][0:K, kt * 128:kt * 128 + kp],
                                          rhs=R["q"][0:K, qb * 512 + c0:qb * 512 + c1], start=True,
                                          stop=(tri is None and "mask" not in R))]
                reads = [R["r_k"]] + R["r_q"] + [r_c]
                if "mask" in R:
                    fns.append(lambda e: e.matmul(ps[0:kp, c0:c1], lhsT=ident_b[0:kp, 0:kp],
                                                  rhs=R["mask"][0:kp, qb * 512 + c0:qb * 512 + c1],
                                                  start=False, stop=True))
                if tri is not None:
                    tm = tri_c if tri == "c" else tri_a
                    fns.append(lambda e: e.matmul(ps[:, 128 * tu:128 * tu + 128], lhsT=ident_b[:, :], rhs=tm[:, :],
                                                  start=False, stop=True))
                S.op("pe", fns, reads=reads + [r_ident], writes=[r_ps])
                return ps, r_ps

            pend = emit_qk(items[0]) if items else None
            for idx, it in enumerate(items):
                R, ti, (kt, c0, c1, tri, tu) = it
                ps, r_ps = pend
                if idx + 1 < len(items):
                    pend = emit_qk(items[idx + 1])
                kp = R.get("kpart", 128)
                pT, r_pT = pT_ring.next()
                bias_ap = R["bias"](kt)
                S.op("act", lambda e, ps=ps, pT=pT, bias_ap=bias_ap, kp=kp, c0=c0, c1=c1: e.activation(
                    out=pT[0:kp, c0:c1], in_=ps[0:kp, c0:c1], func=AF.Exp, bias=bias_ap, scale=1.0),
                    reads=[r_ps, r_c], writes=[r_pT])
                us = [u for u in range(4) if c0 <= 128 * u < c1]
                nv = R["nv"]
                if ti == 0:
                    R["acc"] = acc_ring.next()
                acc, r_acc = R["acc"]
                fns = []
                V = R["V"](kt)
                nt = len(R["tiles"])
                for u in us:
                    fns.append(lambda e, u=u, acc=acc, V=V, first=(ti == 0 and u == us[0]),
                               last=(ti == nt - 1 and u == us[-1]), pT=pT, kp=kp:
                               e.matmul(acc[:, 128 * u:128 * u + nv], lhsT=pT[0:kp, 128 * u:128 * u + 128], rhs=V,
                                        start=first, stop=last))
                S.op("pe", fns, reads=[r_pT, R["r_v"]], writes=[r_acc])
                if ti == nt - 1:
                    R["evac"](acc[:].rearrange("p (u c) -> p u c", u=4), r_acc)

        sn_, sm_ = slopes_all()

        def dump(name, ap, reads):
            if name in dbg_d:
                dst = dbg_d[name][:, :]
                if len(ap.shape) == 3:
                    dst = dst.rearrange("p (a b) -> p a b", a=ap.shape[1])
                S.op("pool", lambda e: e.dma_start(out=dst[0:ap.shape[0]], in_=ap), reads=reads, writes=[Res()], dma=True)

        for s in range(nseq):
            tt0 = s * 16
            for i in range(16):
                xa, r_xa = xt_ring.next()
                S.op("sp", lambda e, xa=xa, i=i, tt0=tt0: e.dma_start(out=xa, in_=src_d[(tt0 + i) * 128:(tt0 + i + 1) * 128, :]),
                     reads=[r_src[tt0 + i]], writes=[r_xa], dma=True)
                hbt, r_hb = hb_ring.next()
                rmsnorm_tile(xa, r_xa, g_rep, r_c, hbt, r_hb, junk[0], junk[1])
                transpose_tile(hbt, r_hb, hT[:, :, i * 128:(i + 1) * 128], r_hT[i])

            w, r_w = load_w(win_cols(NSA_COLS["g"], 64), 64)
            for i in range(16):
                ps, r_ps = ps_ring.next()
                fns = [lambda e, k=k, ps=ps, i=i, w=w: e.matmul(ps[:, 0:24], lhsT=hT[:, k, i * 128:(i + 1) * 128],
                                                                rhs=w[:, k, 0:24], start=(k == 0), stop=(k == 7))
                       for k in range(8)]
                S.op("pe", fns, reads=[r_w, r_hT[i]], writes=[r_ps])
                S.op("act", lambda e, ps=ps, i=i: e.activation(out=gsig[:, i, :], in_=ps[:, 0:24], func=AF.Sigmoid),
                     reads=[r_ps], writes=[r_gsig])

            if dbg and s == 0 and l == 0:
                dump("gsig", gsig, [r_gsig])
            for g in range(2):
                S.barrier()
                A.off = region0
                q_aug = [A.alloc([128, T], BF16) for _ in range(4)]
                r_q = [[Res() for _ in range(4)] for _ in range(4)]
                r_qs = [[Res() for _ in range(4)] for _ in range(4)]
                r_qst = Res()
                ks_aug = A.alloc([128, T], BF16)
                kw_aug = A.alloc([128, T], BF16)
                r_ks = Res()
                r_kw = Res()
                kcraw = A.alloc([64, T], BF16)
                r_kcraw = Res()
                vsA = A.alloc([128, 16, 65], BF16)
                vwA = A.alloc([128, 16, 65], BF16)
                r_vs = Res()
                r_vw = Res()
                w1c = A.alloc([64, 32, 256], BF16)
                r_w1c = Res()
                w2c = A.alloc([128, 2, 64], BF16)
                posT = A.alloc([64, 32], BF16)
                r_w2c = Res()
                kc_aug = A.alloc([128, 128], BF16)
                r_kc = Res()
                kctm = A.alloc([128, 128], BF16)
                r_kctm = Res()
                vcA = A.alloc([128, 97], BF16)
                r_vc = Res()
                hid = A.alloc([128, 2, 128], BF16)
                r_hid = Res()
                pbias = A.alloc([128, 2], F32)
                r_pb = Res()
                oacc = A.alloc([128, 16, 256], F32)
                r_oacc = [Res() for _ in range(16)]
                impacc = A.alloc([128, 16, 32], F32)
                r_imp = [Res() for _ in range(16)]
                trin_all = [(A.alloc([128, 96], BF16), Res()) for _ in range(16)]
                ob_ring = Ring([(A.alloc([128, 256], BF16), Res()) for _ in range(2)])

                for hl in range(4):
                    h = 4 * g + hl
                    S.op("pool", lambda e, hl=hl, h=h: e.dma_start(
                        out=q_aug[hl][96:99, :].rearrange("p (b i) -> p b i", b=4),
                        in_=C["qalibi"][h, :, :].unsqueeze(1).to_broadcast([3, 4, 512])), writes=[r_qst], dma=True)
                    S.op("pool", lambda e, hl=hl: e.dma_start(out=q_aug[hl][64:96, :], in_=C["zeros32"][:, :]),
                         writes=r_qs[hl], dma=True)
                for dst, r_dst, mid in ((ks_aug, r_ks, C["e32"]), (kw_aug, r_kw, C["zeros32"])):
                    S.op("pool", lambda e, dst=dst, mid=mid: e.dma_start(out=dst[64:96, :], in_=mid[:, :]),
                         writes=[r_dst], dma=True)
                    S.op("pool", lambda e, dst=dst: e.dma_start(out=dst[96:99, :], in_=C["ones3"][:, :]),
                         writes=[r_dst], dma=True)
                S.op("pool", lambda e: e.memset(kctm[:, 64:96], 0.0), writes=[r_kctm])
                S.op("pool", lambda e: e.memset(kctm[:, 96:99], 1.0), writes=[r_kctm])
                S.op("pool", lambda e: e.memset(kctm[:, 0:64], 0.0), writes=[r_kctm])
                S.op("pool", lambda e: e.memset(vsA[:, :, 64:65], 1.0), writes=[r_vs])
                S.op("pool", lambda e: e.memset(vwA[:, :, 64:65], 1.0), writes=[r_vw])
                S.op("pool", lambda e: e.memset(vcA[:, 64:65], 1.0), writes=[r_vc])
                S.op("pool", lambda e: e.dma_start(out=vcA[:, 65:97], in_=C["cmp2slc"][:, :]), writes=[r_vc], dma=True)
                for tr, r_tr in trin_all:
                    S.op("pool", lambda e, tr=tr: e.memset(tr[:, 0:64], 0.0), writes=[r_tr])

                for kv in range(2):
                    proj_fm_head(NSA_COLS["kc" if kv == 0 else "vc"] + 64 * g,
                                 lambda b: kcraw[0:64, b * 512:(b + 1) * 512], lambda b: r_kcraw, None, False)
                    S.op("pool", lambda e, kv=kv: e.dma_start(
                        out=w1c, in_=cw1_d[l, kv, :, :].rearrange("(l d) h -> d l h", d=64)), writes=[r_w1c], dma=True)
                    S.op("pool", lambda e, kv=kv: e.dma_start(
                        out=w2c, in_=cw2_d[l, kv, :, :].rearrange("(a p) d -> p a d", p=128)), writes=[r_w2c], dma=True)
                    S.op("pool", lambda e, kv=kv: e.dma_start(out=posT, in_=posT_d[l, kv, :, :]), writes=[r_w2c], dma=True)
                    for hh in range(2):
                        ps, r_ps = ps_ring.next()
                        fns = [lambda e, ll=ll, ps=ps, hh=hh: e.matmul(
                            ps[:, 0:127], lhsT=w1c[:, ll, hh * 128:(hh + 1) * 128],
                            rhs=kcraw[0:64, ll:ll + 16 * 126 + 1:16], start=(ll == 0), stop=(ll == 31)) for ll in range(32)]
                        fns += [lambda e, ll=ll, ps=ps, hh=hh: e.matmul(
                            ps[:, 128:129], lhsT=w1c[:, ll, hh * 128:(hh + 1) * 128],
                            rhs=posT[:, ll:ll + 1], start=(ll == 0), stop=(ll == 31)) for ll in range(32)]
                        S.op("pe", fns, reads=[r_w1c, r_w2c, r_kcraw], writes=[r_ps])
                        S.op("dve", lambda e, ps=ps, hh=hh: e.tensor_copy(out=pbias[:, hh:hh + 1], in_=ps[:, 128:129]),
                             reads=[r_ps], writes=[r_pb])
                        xh, r_xh = tmpf_ring.next()
                        x2, r_x2 = tmpf_ring.next()
                        S.op("dve", lambda e, ps=ps, hh=hh, xh=xh: e.tensor_scalar(
                            out=xh[:, 0:127], in0=ps[:, 0:127], scalar1=pbias[:, hh:hh + 1], scalar2=None, op0=ALU.add),
                            reads=[r_ps, r_pb], writes=[r_xh])
                        S.op("dve", lambda e, xh=xh, x2=x2: e.tensor_tensor(out=x2[:, 0:127], in0=xh[:, 0:127],
                                                                            in1=xh[:, 0:127], op=ALU.mult),
                             reads=[r_xh], writes=[r_x2])
                        S.op("dve", lambda e, x2=x2: e.tensor_scalar(out=x2[:, 0:127], in0=x2[:, 0:127], scalar1=0.044715,
                                                                     scalar2=1.0, op0=ALU.mult, op1=ALU.add),
                             reads=[r_x2], writes=[r_x2])
                        S.op("dve", lambda e, xh=xh, x2=x2: e.tensor_tensor(out=x2[:, 0:127], in0=x2[:, 0:127],
                                                                            in1=xh[:, 0:127], op=ALU.mult),
                             reads=[r_xh, r_x2], writes=[r_x2])
                        S.op("act", lambda e, x2=x2: e.activation(out=x2[:, 0:127], in_=x2[:, 0:127], func=AF.Tanh,
                                                                  scale=0.7978845608028654),
                             reads=[r_x2], writes=[r_x2])
                        S.op("dve", lambda e, xh=xh, x2=x2, hh=hh: e.scalar_tensor_tensor(
                            out=hid[:, hh, 0:127], in0=x2[:, 0:127], scalar=1.0, in1=xh[:, 0:127],
                            op0=ALU.add, op1=ALU.mult), reads=[r_xh, r_x2], writes=[r_hid])
                    ps, r_ps = ps_ring.next()
                    fns = [lambda e, hh=hh, ps=ps: e.matmul(ps[0:127, 0:64], lhsT=hid[:, hh, 0:127], rhs=w2c[:, hh, :],
                                                            start=(hh == 0), stop=(hh == 1)) for hh in range(2)]
                    S.op("pe", fns, reads=[r_hid, r_w2c], writes=[r_ps])
                    if kv == 1:
                        S.op("dve", lambda e, ps=ps: e.tensor_scalar(out=vcA[0:127, 0:64], in0=ps[0:127, 0:64],
                                                                     scalar1=0.5, scalar2=None, op0=ALU.mult),
                             reads=[r_ps], writes=[r_vc])
                    else:
                        tf, r_tf = tmpf_ring.next()
                        st, r_st = sm_ring.next()
                        S.op("dve", lambda e, ps=ps, tf=tf: e.tensor_scalar(out=tf[0:127, 0:64], in0=ps[0:127, 0:64],
                                                                            scalar1=0.5, scalar2=None, op0=ALU.mult),
                             reads=[r_ps], writes=[r_tf])
                        S.op("act", lambda e, tf=tf, st=st: e.activation(out=tf[0:127, 64:128], in_=tf[0:127, 0:64],
                                                                         func=AF.Square, accum_out=st[0:127, 0:1]),
                             reads=[r_tf], writes=[r_tf, r_st])
                        S.op("act", lambda e, st=st: e.activation(out=st[0:127, 1:2], in_=st[0:127, 0:1], func=AF.Sqrt,
                                                                  scale=1.0 / 64, bias=EPS), reads=[r_st], writes=[r_st])
                        S.op("dve", lambda e, st=st: e.reciprocal(out=st[0:127, 2:3], in_=st[0:127, 1:2]),
                             reads=[r_st], writes=[r_st])
                        S.op("dve", lambda e, tf=tf, st=st: e.scalar_tensor_tensor(
                            out=kctm[0:127, 0:64], in0=tf[0:127, 0:64], scalar=st[0:127, 2:3], in1=gkc_rep[0:127, :],
                            op0=ALU.mult, op1=ALU.mult), reads=[r_tf, r_st, r_c], writes=[r_kctm])
                        ps2, r_ps2 = ps_ring.next()
                        psb2 = ps2[:].bitcast(BF16)
                        S.op("pe", lambda e, psb2=psb2: e.transpose(out=psb2[0:99, 0:128], in_=kctm[:, 0:99],
                                                                    identity=ident_b[:]),
                             reads=[r_kctm, r_ident], writes=[r_ps2])
                        S.op("dve", lambda e, psb2=psb2: e.tensor_copy(out=kc_aug[0:99, 0:128], in_=psb2[0:99, 0:128]),
                             reads=[r_ps2], writes=[r_kc])

                for hl in range(4):
                    h = 4 * g + hl
                    proj_fm_head(NSA_COLS["q"] + 64 * h, lambda b, hl=hl: q_aug[hl][0:64, b * 512:(b + 1) * 512],
                                 lambda b, hl=hl: r_q[hl][b], gq_n[:, 0:1], True)
                proj_fm_head(NSA_COLS["ks"] + 64 * g, lambda b: ks_aug[0:64, b * 512:(b + 1) * 512],
                             lambda b: r_ks, gq_n[:, 2:3], True)
                proj_fm_head(NSA_COLS["kw"] + 64 * g, lambda b: kw_aug[0:64, b * 512:(b + 1) * 512],
                             lambda b: r_kw, gq_n[:, 3:4], True)
                for nm, dstA, r_dst in (("vs", vsA, r_vs), ("vw", vwA, r_vw)):
                    w, r_w = load_w(win_cols(NSA_COLS[nm] + 64 * g, 64), 64)
                    for i in range(16):
                        ps, r_ps = ps_ring.next()
                        fns = [lambda e, k=k, ps=ps, i=i, w=w: e.matmul(ps[:, 0:64], lhsT=hT[:, k, i * 128:(i + 1) * 128],
                                                                        rhs=w[:, k, 0:64], start=(k == 0), stop=(k == 7))
                               for k in range(8)]
                        S.op("pe", fns, reads=[r_w, r_hT[i]], writes=[r_ps])
                        S.op("act", lambda e, ps=ps, i=i, dstA=dstA: e.copy(out=dstA[:, i, 0:64], in_=ps[:, 0:64]),
                             reads=[r_ps], writes=[r_dst])
                def mk_cmp_evac(hl, qb):
                    h = 4 * g + hl

                    def ev(acc3, r_acc):
                        tts = slice(4 * qb, 4 * qb + 4)
                        r_o = r_oacc[4 * qb:4 * qb + 4]
                        r_i = r_imp[4 * qb:4 * qb + 4]
                        st, r_st = sm_ring.next()
                        rd = st[:, 4:8].unsqueeze(2)
                        cf = st[:, 8:12].unsqueeze(2)
                        S.op("dve", lambda e: e.tensor_scalar(out=st[:, 0:4].unsqueeze(2), in0=acc3[:, :, 64:65], scalar1=1e-30,
                                                              scalar2=None, op0=ALU.add), reads=[r_acc], writes=[r_st])
                        S.op("dve", lambda e: e.reciprocal(out=st[:, 4:8], in_=st[:, 0:4]), reads=[r_st], writes=[r_st])
                        S.op("dve", lambda e: e.tensor_tensor(out=cf, in0=rd, in1=gsig[:, tts, 3 * h:3 * h + 1], op=ALU.mult),
                             reads=[r_st, r_gsig], writes=[r_st])
                        S.op("dve", lambda e: e.tensor_tensor(out=oacc[:, tts, hl * 64:(hl + 1) * 64], in0=acc3[:, :, 0:64],
                                                              in1=cf.to_broadcast([128, 4, 64]), op=ALU.mult),
                             reads=[r_acc, r_st], writes=r_o)
                        if hl == 0:
                            S.op("dve", lambda e: e.tensor_tensor(out=impacc[:, tts, :], in0=acc3[:, :, 65:97],
                                                                  in1=rd.to_broadcast([128, 4, 32]), op=ALU.mult),
                                 reads=[r_acc, r_st], writes=r_i)
                        else:
                            tf, r_tf = tmpf_ring.next()
                            tf3 = tf[:, 0:128].rearrange("p (u c) -> p u c", u=4)
                            S.op("dve", lambda e: e.tensor_tensor(out=tf3, in0=acc3[:, :, 65:97],
                                                                  in1=rd.to_broadcast([128, 4, 32]), op=ALU.mult),
                                 reads=[r_acc, r_st], writes=[r_tf])
                            S.op("pool", lambda e: e.tensor_tensor(out=impacc[:, tts, :], in0=impacc[:, tts, :], in1=tf3,
                                                                   op=ALU.add), reads=[r_tf] + r_i, writes=r_i)
                    return ev

                rounds = []
                for hl in range(4):
                    h = 4 * g + hl
                    for qb in range(4):
                        rounds.append(dict(q=q_aug[hl], r_q=[r_q[hl][qb], r_qst], krows=99, kT=kc_aug, r_k=r_kc, qb=qb,
                                           tiles=[(0, 0, 512, None, None)], kpart=127, mask=cmask,
                                           V=lambda kt: vcA[0:127, 0:97], r_v=r_vc, nv=97,
                                           bias=lambda kt, h=h, qb=qb: cbias[0:127, 4 * h + qb:4 * h + qb + 1],
                                           evac=mk_cmp_evac(hl, qb)))
                run_rounds(rounds)
                if dbg and s == 0 and l == 0 and g == 0:
                    dump("oacc_cmp", oacc, r_oacc)
                    dump("kc_aug", kc_aug, [r_kc])
                    dump("vcA", vcA, [r_vc])
                    dump("impacc", impacc, r_imp)

                sel_tr = []
                for tt in range(16):
                    sc, r_sc = tmpf_ring.next()
                    st, r_st = sm_ring.next()
                    tr, r_tr = trin_all[tt]
                    S.op("dve", lambda e, sc=sc, tt=tt: e.tensor_tensor(out=sc[:, 0:32], in0=impacc[:, tt, :],
                                                                        in1=nsa_mult[:, tt, :], op=ALU.mult),
                         reads=[r_imp[tt], r_c], writes=[r_sc])
                    S.op("dve", lambda e, sc=sc, tt=tt: e.tensor_tensor(out=sc[:, 0:32], in0=sc[:, 0:32],
                                                                        in1=nsa_add[:, tt, :], op=ALU.add),
                         reads=[r_sc, r_c], writes=[r_sc])
                    S.op("dve", lambda e, sc=sc, st=st: e.max(out=st[:, 0:8], in_=sc[:, 0:32]), reads=[r_sc], writes=[r_st])
                    S.op("dve", lambda e, sc=sc, st=st, tr=tr: e.tensor_scalar(
                        out=tr[:, 64:96], in0=sc[:, 0:32], scalar1=st[:, 7:8], scalar2=-BIG, op0=ALU.is_lt, op1=ALU.mult),
                        reads=[r_sc, r_st], writes=[r_tr])

                def emit_sel_transposes():
                    for tt in range(16):
                        tr, r_tr = trin_all[tt]
                        ps, r_ps = misc_ring.next()
                        psb = ps[:].bitcast(BF16)
                        S.op("pe", lambda e, psb=psb, tr=tr: e.transpose(out=psb[0:96, 0:128], in_=tr[:, 0:96],
                                                                         identity=ident_b[:]),
                             reads=[r_tr, r_ident], writes=[r_ps])
                        for hl in range(4):
                            S.op("dve", lambda e, psb=psb, hl=hl, tt=tt: e.tensor_copy(
                                out=q_aug[hl][64:96, tt * 128:(tt + 1) * 128], in_=psb[64:96, 0:128]),
                                reads=[r_ps], writes=[r_qs[hl][tt // 4]])

                def mk_evac(hl, qb, br):
                    h = 4 * g + hl

                    def ev(acc3, r_acc):
                        tts = slice(4 * qb, 4 * qb + 4)
                        r_o = r_oacc[4 * qb:4 * qb + 4]
                        st, r_st = sm_ring.next()
                        rd = st[:, 4:8].unsqueeze(2)
                        cf = st[:, 8:12].unsqueeze(2)
                        S.op("dve", lambda e: e.reciprocal(out=rd, in_=acc3[:, :, 64:65]), reads=[r_acc], writes=[r_st])
                        S.op("dve", lambda e: e.tensor_tensor(out=cf, in0=rd, in1=gsig[:, tts, 3 * h + br:3 * h + br + 1],
                                                              op=ALU.mult), reads=[r_st, r_gsig], writes=[r_st])
                        tf, r_tf = tmpf_ring.next()
                        tf3 = tf[:, 0:256].rearrange("p (u c) -> p u c", u=4)
                        S.op("dve", lambda e: e.tensor_tensor(out=tf3, in0=acc3[:, :, 0:64], in1=cf.to_broadcast([128, 4, 64]),
                                                              op=ALU.mult), reads=[r_acc, r_st], writes=[r_tf])
                        S.op("pool", lambda e: e.tensor_tensor(out=oacc[:, tts, hl * 64:(hl + 1) * 64],
                                                               in0=oacc[:, tts, hl * 64:(hl + 1) * 64], in1=tf3, op=ALU.add),
                             reads=[r_tf] + r_o, writes=r_o)
                    return ev

                rounds = []
                for hl in range(4):
                    h = 4 * g + hl
                    for qb in range(4):
                        rounds.append(dict(q=q_aug[hl], r_q=[r_q[hl][qb], r_qst], krows=99, kT=kw_aug, r_k=r_kw, qb=qb,
                                           tiles=window_tiles(qb), V=lambda kt: vwA[:, kt, :], r_v=r_vw, nv=65,
                                           bias=lambda kt, h=h, qb=qb: abias[:, h * 19 + (kt - 4 * qb + 15):h * 19 + (kt - 4 * qb + 15) + 1],
                                           evac=mk_evac(hl, qb, 2)))
                run_rounds(rounds)
                rounds = []
                emit_sel_transposes()
                if dbg and s == 0 and l == 0 and g == 0:
                    dump("oacc_win", oacc, r_oacc)
                for hl in range(4):
                    h = 4 * g + hl
                    for qb in range(4):
                        rounds.append(dict(q=q_aug[hl], r_q=[r_q[hl][qb], r_qs[hl][qb], r_qst], krows=99, kT=ks_aug,
                                           r_k=r_ks, qb=qb, tiles=causal_tiles(qb), V=lambda kt: vsA[:, kt, :], r_v=r_vs,
                                           nv=65,
                                           bias=lambda kt, h=h, qb=qb: abias[:, h * 19 + (kt - 4 * qb + 15):h * 19 + (kt - 4 * qb + 15) + 1],
                                           evac=mk_evac(hl, qb, 1)))
                run_rounds(rounds)
                if dbg and s == 0 and l == 0 and g == 0:
                    dump("oacc_all", oacc, r_oacc)
                    dump("q0", q_aug[0], [r_q[0][b] for b in range(4)] + [r_qs[0][b] for b in range(4)] + [r_qst])
                    dump("ks_aug", ks_aug, [r_ks])
                for tt in range(16):
                    ob, r_ob = ob_ring.next()
                    S.op("act", lambda e, ob=ob, tt=tt: e.copy(out=ob, in_=oacc[:, tt, :]), reads=[r_oacc[tt]], writes=[r_ob])
                    transpose_tile(ob, r_ob, onT[:, 2 * g:2 * g + 2, tt * 128:(tt + 1) * 128], r_onT[tt], nk=2,
                                   evac="dve", ring=misc_ring)

            for hf in range(2):
                S.barrier()
                A.off = region0
                q_aug = [A.alloc([128, T], BF16) for _ in range(4)]
                k_aug = [A.alloc([128, T], BF16) for _ in range(4)]
                r_q = [[Res() for _ in range(4)] for _ in range(4)]
                r_qs = [[Res() for _ in range(4)] for _ in range(4)]
                r_qst = Res()
                r_k = [Res() for _ in range(4)]
                vmA = A.alloc([128, 16, 4, 65], BF16)
                r_vm = Res()
                kmean_f = A.alloc([64, 4, 8], F32)
                kmean_b = A.alloc([64, 4, 8], BF16)
                r_km = Res()
                omb = A.alloc([128, 16, 256], BF16)
                r_omb = [Res() for _ in range(16)]
                trin_ring = Ring([(A.alloc([128, 72], BF16), Res()) for _ in range(4)])
                for tr, r_tr in trin_ring.items:
                    S.op("pool", lambda e, tr=tr: e.memset(tr[:, 0:64], 0.0), writes=[r_tr])
                S.op("pool", lambda e: e.memset(vmA[:, :, :, 64:65], 1.0), writes=[r_vm])
                for hl in range(4):
                    h = 4 * hf + hl
                    S.op("pool", lambda e, hl=hl, h=h: e.dma_start(
                        out=q_aug[hl][72:75, :].rearrange("p (b i) -> p b i", b=4),
                        in_=C["qalibi"][8 + h, :, :].unsqueeze(1).to_broadcast([3, 4, 512])), writes=[r_qst], dma=True)
                    S.op("pool", lambda e, hl=hl: e.dma_start(out=q_aug[hl][64:72, :], in_=C["zeros32"][0:8, :]),
                         writes=r_qs[hl], dma=True)
                    S.op("pool", lambda e, hl=hl: e.dma_start(out=k_aug[hl][64:72, :], in_=C["e8"][:, :]),
                         writes=[r_k[hl]], dma=True)
                    S.op("pool", lambda e, hl=hl: e.dma_start(out=k_aug[hl][72:75, :], in_=C["ones3"][:, :]),
                         writes=[r_k[hl]], dma=True)
                for hl in range(4):
                    h = 4 * hf + hl
                    proj_fm_head(MOBA_COLS["q"] + 64 * h, lambda b, hl=hl: q_aug[hl][0:64, b * 512:(b + 1) * 512],
                                 lambda b, hl=hl: r_q[hl][b], gq_m[:, 0:1], True)
                    proj_fm_head(MOBA_COLS["k"] + 64 * h, lambda b, hl=hl: k_aug[hl][0:64, b * 512:(b + 1) * 512],
                                 lambda b, hl=hl: r_k[hl], gq_m[:, 1:2], True)
                    S.op("dve", lambda e, hl=hl: e.tensor_reduce(
                        out=kmean_f[:, hl, :], in_=k_aug[hl][0:64, :].rearrange("p (n k) -> p n k", k=256),
                        axis=AX.X, op=ALU.add), reads=[r_k[hl]], writes=[r_km])
                S.op("dve", lambda e: e.tensor_scalar(out=kmean_b, in0=kmean_f, scalar1=1.0 / 256, scalar2=None,
                                                      op0=ALU.mult), reads=[r_km], writes=[r_km])
                w, r_w = load_w(win_cols(MOBA_COLS["v"] + 256 * hf, 256), 256)
                for i in range(16):
                    ps, r_ps = ps_ring.next()
                    fns = [lambda e, k=k, ps=ps, i=i, w=w: e.matmul(ps[:, 0:256], lhsT=hT[:, k, i * 128:(i + 1) * 128],
                                                                    rhs=w[:, k, 0:256], start=(k == 0), stop=(k == 7))
                           for k in range(8)]
                    S.op("pe", fns, reads=[r_w, r_hT[i]], writes=[r_ps])
                    S.op("act", lambda e, ps=ps, i=i: e.copy(out=vmA[:, i, :, 0:64],
                                                             in_=ps[:, 0:256].rearrange("p (h d) -> p h d", d=64)),
                         reads=[r_ps], writes=[r_vm])
                for tt in range(16):
                    cur = tt // 2
                    if tt >= 8:
                        ps, r_ps = misc_ring.next()
                        fns = [lambda e, hl=hl, ps=ps, tt=tt: e.matmul(ps[:, hl * 8:(hl + 1) * 8],
                                                                       lhsT=q_aug[hl][0:64, tt * 128:(tt + 1) * 128],
                                                                       rhs=kmean_b[:, hl, :], start=True, stop=True)
                               for hl in range(4)]
                        S.op("pe", fns, reads=[r_km] + [r_q[hl][tt // 4] for hl in range(4)], writes=[r_ps])
                        sc, r_sc = tmpf_ring.next()
                        for hl in range(4):
                            S.op("dve", lambda e, hl=hl, ps=ps, sc=sc, tt=tt: e.tensor_tensor(
                                out=sc[:, hl * 8:(hl + 1) * 8], in0=ps[:, hl * 8:(hl + 1) * 8], in1=moba_add[:, tt, :],
                                op=ALU.add), reads=[r_ps, r_c], writes=[r_sc])
                    for hl in range(4):
                        tr, r_tr = trin_ring.next()
                        S.op("pool", lambda e, tr=tr, cur=cur: e.memset(tr[:, 64 + cur:65 + cur], 0.0), writes=[r_tr])
                        if cur < 7:
                            S.op("pool", lambda e, tr=tr, cur=cur: e.memset(tr[:, 65 + cur:72], -BIG), writes=[r_tr])
                        if tt < 8:
                            if cur > 0:
                                S.op("pool", lambda e, tr=tr, cur=cur: e.memset(tr[:, 64:64 + cur], 0.0), writes=[r_tr])
                        else:
                            st, r_st = sm_ring.next()
                            S.op("dve", lambda e, sc=sc, st=st, hl=hl: e.max(out=st[:, 0:8], in_=sc[:, hl * 8:(hl + 1) * 8]),
                                 reads=[r_sc], writes=[r_st])
                            S.op("dve", lambda e, sc=sc, st=st, tr=tr, hl=hl, cur=cur: e.tensor_scalar(
                                out=tr[:, 64:64 + cur], in0=sc[:, hl * 8:hl * 8 + cur], scalar1=st[:, 2:3], scalar2=-BIG,
                                op0=ALU.is_lt, op1=ALU.mult), reads=[r_sc, r_st], writes=[r_tr])
                        ps2, r_ps2 = ps_ring.next()
                        psb = ps2[:].bitcast(BF16)
                        S.op("pe", lambda e, psb=psb, tr=tr: e.transpose(out=psb[0:72, 0:128], in_=tr[:, 0:72],
                                                                         identity=ident_b[:]),
                             reads=[r_tr, r_ident], writes=[r_ps2])
                        S.op("dve", lambda e, psb=psb, hl=hl, tt=tt: e.tensor_copy(
                            out=q_aug[hl][64:72, tt * 128:(tt + 1) * 128], in_=psb[64:72, 0:128]),
                            reads=[r_ps2], writes=[r_qs[hl][tt // 4]])

                def mk_evac_m(hl, qb):
                    def ev(acc3, r_acc):
                        tts = slice(4 * qb, 4 * qb + 4)
                        st, r_st = sm_ring.next()
                        rd = st[:, 4:8].unsqueeze(2)
                        S.op("dve", lambda e: e.reciprocal(out=rd, in_=acc3[:, :, 64:65]), reads=[r_acc], writes=[r_st])
                        S.op("dve", lambda e: e.tensor_tensor(out=omb[:, tts, hl * 64:(hl + 1) * 64], in0=acc3[:, :, 0:64],
                                                              in1=rd.to_broadcast([128, 4, 64]), op=ALU.mult),
                             reads=[r_acc, r_st], writes=r_omb[4 * qb:4 * qb + 4])
                    return ev

                rounds = []
                for hl in range(4):
                    h = 4 * hf + hl
                    for qb in range(4):
                        rounds.append(dict(q=q_aug[hl], r_q=[r_q[hl][qb], r_qs[hl][qb], r_qst], krows=75, kT=k_aug[hl],
                                           r_k=r_k[hl], qb=qb, tiles=causal_tiles(qb),
                                           V=lambda kt, hl=hl: vmA[:, kt, hl, :], r_v=r_vm, nv=65,
                                           bias=lambda kt, h=h, qb=qb: abias[:, (8 + h) * 19 + (kt - 4 * qb + 15):(8 + h) * 19 + (kt - 4 * qb + 15) + 1],
                                           evac=mk_evac_m(hl, qb)))
                run_rounds(rounds)
                for tt in range(16):
                    transpose_tile(omb[:, tt, :], r_omb[tt], omT[:, 2 * hf:2 * hf + 2, tt * 128:(tt + 1) * 128],
                                   r_omT[tt], nk=2, evac="dve", ring=misc_ring)

            if dbg and s == 0 and l == 0:
                for nm, src, rr in (("onT", onT, r_onT), ("omT", omT, r_omT)):
                    if nm in dbg_d:
                        S.op("pool", lambda e, nm=nm, src=src: e.dma_start(
                            out=dbg_d[nm][:, :].rearrange("p (a b) -> p a b", a=4), in_=src), reads=rr,
                            writes=[Res()], dma=True)

            S.barrier()
            A.off = region0
            yT = A.alloc([128, 8, T], BF16)
            r_yT = [Res() for _ in range(8)]
            wout = A.alloc([128, 8, D], BF16)
            r_wout = Res()
            S.op("pool", lambda e: e.dma_start(out=wout, in_=wout_d[l, :, :].rearrange("(k p) c -> p k c", p=128)),
                 writes=[r_wout], dma=True)
            for oc in range(8):
                wgn, r_wgn = load_w(win_cols(GATE_N + 128 * oc, 128), 128)
                wgm, r_wgm = load_w(win_cols(GATE_M + 128 * oc, 128), 128)
                wu, r_wu = wch_ring.next()
                S.op("pool", lambda e, wu=wu, oc=oc: e.dma_start(
                    out=wu[:, 0:4, 0:128], in_=wupn_d[l, :, oc * 128:(oc + 1) * 128].rearrange("(k p) c -> p k c", p=128)),
                    writes=[r_wu], dma=True)
                S.op("pool", lambda e, wu=wu, oc=oc: e.dma_start(
                    out=wu[:, 4:8, 0:128], in_=wupm_d[l, :, oc * 128:(oc + 1) * 128].rearrange("(k p) c -> p k c", p=128)),
                    writes=[r_wu], dma=True)
                for b in range(4):
                    bs = slice(b * 512, (b + 1) * 512)
                    res = []
                    for (wg, r_wg, oT, r_oT, ko) in ((wgn, r_wgn, onT, r_onT, 0), (wgm, r_wgm, omT, r_omT, 4)):
                        pg, r_pg = ps_ring.next()
                        fns = [lambda e, k=k, pg=pg, wg=wg, bs=bs: e.matmul(pg[:], lhsT=wg[:, k, 0:128], rhs=hT[:, k, bs],
                                                                     start=(k == 0), stop=(k == 7)) for k in range(8)]
                        S.op("pe", fns, reads=[r_wg] + r_hT[4 * b:4 * b + 4], writes=[r_pg])
                        pu, r_pu = ps_ring.next()
                        fns = [lambda e, k=k, pu=pu, oT=oT, ko=ko, wu=wu, bs=bs: e.matmul(pu[:], lhsT=wu[:, ko + k, 0:128], rhs=oT[:, k, bs],
                                                                            start=(k == 0), stop=(k == 3)) for k in range(4)]
                        S.op("pe", fns, reads=[r_wu] + r_oT[4 * b:4 * b + 4], writes=[r_pu])
                        sg, r_sg = tmpf_ring.next()
                        S.op("act", lambda e, pg=pg, sg=sg: e.activation(out=sg, in_=pg[:], func=AF.Sigmoid),
                             reads=[r_pg], writes=[r_sg])
                        S.op("dve", lambda e, pu=pu, sg=sg: e.tensor_tensor(out=sg, in0=sg, in1=pu[:], op=ALU.mult),
                             reads=[r_pu, r_sg], writes=[r_sg])
                        res.append((sg, r_sg))
                    S.op("dve", lambda e, a=res[0][0], b_=res[1][0], oc=oc, bs=bs: e.tensor_tensor(
                        out=yT[:, oc, bs], in0=a, in1=b_, op=ALU.add), reads=[res[0][1], res[1][1]], writes=[r_yT[oc]])
            for i in range(16):
                tt = tt0 + i
                xa, r_xa = xt_ring.next()
                S.op("sp", lambda e, xa=xa, tt=tt: e.dma_start(out=xa, in_=src_d[tt * 128:(tt + 1) * 128, :]),
                     reads=[r_src[tt]], writes=[r_xa], dma=True)
                for h2 in range(2):
                    po, r_po = ps_ring.next()
                    fns = [lambda e, oc=oc, po=po, i=i, h2=h2: e.matmul(po[:], lhsT=yT[:, oc, i * 128:(i + 1) * 128],
                                                                        rhs=wout[:, oc, h2 * 512:(h2 + 1) * 512],
                                                                        start=(oc == 0), stop=(oc == 7)) for oc in range(8)]
                    S.op("pe", fns, reads=r_yT + [r_wout], writes=[r_po])
                    S.op("dve", lambda e, po=po, xa=xa, h2=h2: e.tensor_tensor(
                        out=xa[:, h2 * 512:(h2 + 1) * 512], in0=po[:], in1=xa[:, h2 * 512:(h2 + 1) * 512], op=ALU.add),
                        reads=[r_po, r_xa], writes=[r_xa])
                S.op("sp", lambda e, xa=xa, tt=tt: e.dma_start(out=y_d[tt * 128:(tt + 1) * 128, :], in_=xa),
                     reads=[r_xa], writes=[r_y[tt]], dma=True)

    cur_d, cur_r = x_d, r_x
    for l in range(depth):
        if f"ffn{2 * l}" in phases or "all" in phases:
            ffn_phase(l, 0, cur_d, cur_r)
            cur_d, cur_r = y_d, r_y
        if f"mix{l}" in phases or "all" in phases:
            mix_phase(l, cur_d, cur_r)
            cur_d, cur_r = y_d, r_y
        if f"ffn{2 * l + 1}" in phases or "all" in phases:
            ffn_phase(l, 1, cur_d, cur_r)
            cur_d, cur_r = y_d, r_y

    S.barrier()
    S.finish("sp", r_y)
    sems = [es.enter_context(nc.semaphore(f"s{i}")) for i in range(S.nsem)]
    S.emit(nc, sems)
    es.close()
    return nc, S


def make_in_maps(inputs, nseq, ncores):
    x = np.ascontiguousarray(inputs["x"], dtype=np.float32).reshape(-1, nseq * T, D)
    consts = make_consts()
    shared = {}
    for k in ("norm_g", "ffn_w1", "ffn_w3", "ffn_w2", "w_in", "g_qk_nsa", "cmp_w1", "cmp_w2", "w_up_nsa", "w_up_moba",
              "w_out"):
        shared[k] = np.ascontiguousarray(inputs[k], dtype=np.float32)
    shared["g_qk_nsaT"] = np.ascontiguousarray(np.transpose(np.asarray(inputs["g_qk_nsa"], np.float32), (0, 2, 1)))
    shared["g_qk_mobaT"] = np.ascontiguousarray(np.transpose(np.asarray(inputs["g_qk_moba"], np.float32), (0, 2, 1)))
    shared["cmp_posT"] = np.ascontiguousarray(np.transpose(np.asarray(inputs["cmp_pos"], np.float32), (0, 1, 3, 2)))
    shared.update(consts)
    in_maps = []
    for c in range(ncores):
        m = {"x": x[c]}
        m.update(shared)
        in_maps.append(m)
    return in_maps


_CACHE = {}


def kernel(**inputs):
    nseq = 16 // NCORES
    if "full" not in _CACHE:
        _CACHE["full"] = build_program(nseq=nseq, phases=("all",))
    nc, S = _CACHE["full"]
    in_maps = make_in_maps(inputs, nseq, NCORES)
    res = run_bass_kernel_spmd(nc, in_maps, core_ids=list(range(NCORES)))
    y = np.stack([np.asarray(r["y"]) for r in res.results], axis=0)
    return y.reshape(16, T, D).astype(np.float32)
```

```python
import numpy as np
from contextlib import ExitStack
import concourse.bass as bass
import concourse.mybir as mybir
from concourse.bass_utils import run_bass_kernel_spmd

F32 = mybir.dt.float32
BF16 = mybir.dt.bfloat16
AF = mybir.ActivationFunctionType
ALU = mybir.AluOpType
AX = mybir.AxisListType

NCORES = 8
D = 1024
T = 2048
DFF = 2816
NF = DFF // 128
DEPTH = 2
EPS = 1e-6


class Res:
    __slots__ = ("w", "r", "name")

    def __init__(self, name=""):
        self.w = None
        self.r = {}
        self.name = name


class _Eng:
    def __init__(self, name, sem):
        self.name = name
        self.sem = sem
        self.count = 0
        self.known = {}
        self.ops = []
        self.dma_sems = []
        self.dma_count = 0


class Sched:
    NDMA = 8

    def __init__(self):
        self.nsem = 0
        self.eng = {}
        for n in ("pe", "act", "dve", "pool", "sp"):
            self.eng[n] = _Eng(n, self._newsem())
        for n in ("sp", "pool", "act"):
            self.eng[n].dma_sems = [self._newsem() for _ in range(self.NDMA)]

    def _newsem(self):
        s = self.nsem
        self.nsem += 1
        return s

    def op(self, eng, fns, reads=(), writes=(), dma=False):
        E = self.eng[eng]
        if not isinstance(fns, (list, tuple)):
            fns = [fns]
        need = {}

        def req(tok):
            if tok is None:
                return
            s, v, clk = tok
            o = need.get(s)
            if o is None or o[0] < v:
                need[s] = (v, clk)

        for r in reads:
            req(r.w)
        for w in writes:
            req(w.w)
            for s, (v, clk) in w.r.items():
                req((s, v, clk))
        implied = {}
        for s, (v, clk) in need.items():
            for cs, cv in clk.items():
                if implied.get(cs, 0) < cv:
                    implied[cs] = cv
        waits = []
        known = E.known
        for s, (v, clk) in need.items():
            if known.get(s, 0) >= v or implied.get(s, 0) >= v:
                continue
            if eng == "pe" and s == E.sem:
                continue
            waits.append((s, v))
        for s, v in implied.items():
            if known.get(s, 0) < v:
                known[s] = v
        for s, (v, clk) in need.items():
            if known.get(s, 0) < v:
                known[s] = v
        if dma:
            j = E.dma_count
            E.dma_count += 1
            s = E.dma_sems[j % self.NDMA]
            prev = 16 * (j // self.NDMA)
            if prev > 0 and known.get(s, 0) < prev:
                waits.append((s, prev))
                known[s] = prev
            val = prev + 16
            inc = 16
        else:
            E.count += 1
            s = E.sem
            val = E.count
            inc = 1
        clk = dict(known)
        tok = (s, val, clk)
        E.ops.append((waits, list(fns), s, inc))
        for r in reads:
            o = r.r.get(s)
            if o is None or o[0] < val:
                r.r[s] = (val, clk)
        for w in writes:
            w.w = tok
            w.r = {}
        return tok

    def finish(self, eng, resources):
        E = self.eng[eng]
        need = {}
        for r in resources:
            toks = []
            if r.w is not None:
                toks.append(r.w)
            for s, (v, clk) in r.r.items():
                toks.append((s, v, clk))
            for s, v, clk in toks:
                if need.get(s, 0) < v:
                    need[s] = v
        waits = [(s, v) for s, v in need.items() if E.known.get(s, 0) < v]
        E.ops.append((waits, [], None, 0))

    def emit(self, nc, sems):
        def replay(E):
            def body(e):
                for waits, fns, s, inc in E.ops:
                    for ws, wv in waits:
                        e.wait_ge(sems[ws], wv)
                    if not fns:
                        continue
                    for fn in fns[:-1]:
                        fn(e)
                    fns[-1](e).then_inc(sems[s], inc)
            return body

        with nc.Block() as block:
            block.sync(replay(self.eng["sp"]))
            block.scalar(replay(self.eng["act"]))
            block.vector(replay(self.eng["dve"]))
            block.gpsimd(replay(self.eng["pool"]))
            block.tensor(replay(self.eng["pe"]))


class Ring:
    def __init__(self, items):
        self.items = items
        self.i = 0

    def next(self):
        it = self.items[self.i % len(self.items)]
        self.i += 1
        return it


def _barrier(self):
    toks = {}
    for E in self.eng.values():
        if E.count > 0:
            toks[E.sem] = E.count
        for i, s in enumerate(E.dma_sems):
            if E.dma_count > i:
                toks[s] = 16 * ((E.dma_count - i + self.NDMA - 1) // self.NDMA)
    for E in self.eng.values():
        waits = []
        for s, v in toks.items():
            if E.known.get(s, 0) >= v:
                continue
            if E.name == "pe" and s == E.sem:
                continue
            waits.append((s, v))
            E.known[s] = v
        if waits:
            E.ops.append((waits, [], None, 0))


Sched.barrier = _barrier


class Arena:
    def __init__(self, t, nel):
        self.t = t
        self.nel = nel
        self.off = 0

    def alloc(self, shape, dt):
        n = 1
        for d in shape[1:]:
            n *= d
        sz = n * (2 if dt == F32 else 1)
        self.off = (self.off + 1) // 2 * 2
        o = self.off
        self.off += sz
        assert self.off <= self.nel, ("arena overflow", self.off, self.nel)
        ap = self.t[0:shape[0], o:o + sz]
        if dt == F32:
            ap = ap.bitcast(F32)
        if len(shape) == 3:
            ap = ap.rearrange("p (a b) -> p a b", a=shape[1])
        elif len(shape) == 4:
            ap = ap.rearrange("p (a b c) -> p a b c", a=shape[1], b=shape[2])
        return ap


BIG = 30000.0
NSA_COLS = dict(q=0, kc=512, vc=640, ks=768, vs=896, kw=1024, vw=1152, g=1280)
MOBA_COLS = dict(q=1304, k=1816, v=2328)
GATE_N, GATE_M = 2840, 3864
INC = 4888


def slopes_all():
    s = (2.0 ** (-8.0 * np.arange(1, 17) / 16)).astype(np.float32)
    return s[0::2].copy(), s[1::2].copy()


def _bf16_round(a):
    a = np.asarray(a, dtype=np.float32)
    u = a.view(np.uint32).astype(np.uint64)
    r = ((u + 0x7FFF + ((u >> 16) & 1)) >> 16) << 16
    return r.astype(np.uint32).view(np.float32)


def make_consts():
    c = {}
    c["ident"] = np.eye(128, dtype=np.float32)
    j = np.arange(128)[:, None]
    i = np.arange(128)[None, :]
    c["tri_c"] = np.where(j > i, -BIG, 0.0).astype(np.float32)
    c["tri_a"] = np.where(j <= i, -BIG, 0.0).astype(np.float32)
    c["ones64"] = np.ones((64, 64), np.float32)
    bd = np.zeros((128, 128), np.float32)
    bd[:64, :64] = 1.0
    bd[64:, 64:] = 1.0
    c["bd_ones"] = bd
    sn, sm = slopes_all()
    sl = np.concatenate([sn, sm])
    ab = np.zeros((128, 16, 19), np.float32)
    for h in range(16):
        for d in range(-15, 4):
            ab[:, h, d + 15] = sl[h].astype(np.float64) * (128 * d + np.arange(128))
    c["abias"] = ab.reshape(128, 16 * 19)
    cb = np.zeros((128, 8, 4), np.float32)
    cc = np.arange(128)
    for h in range(8):
        for qb in range(4):
            cb[:, h, qb] = sn[h].astype(np.float64) * (16 * cc + 15.5 - 512 * qb)
    c["cbias"] = cb.reshape(128, 32)
    qa = np.zeros((16, 3, 512), np.float32)
    for h in range(16):
        v = (-(sl[h].astype(np.float64)) * np.arange(512)).astype(np.float32)
        v1 = _bf16_round(v)
        v2 = _bf16_round(v - v1)
        v3 = _bf16_round(v - v1 - v2)
        qa[h, 0], qa[h, 1], qa[h, 2] = v1, v2, v3
    c["qalibi"] = qa
    key = np.arange(T)
    c["e32"] = (key[None, :] // 64 == np.arange(32)[:, None]).astype(np.float32)
    c["e8"] = (key[None, :] // 256 == np.arange(8)[:, None]).astype(np.float32)
    c["ones3"] = np.ones((3, T), np.float32)
    c["zeros32"] = np.zeros((32, T), np.float32)
    cm = np.zeros((128, T), np.float32)
    cidx = np.arange(128)[:, None]
    cm[:] = np.where(16 * cidx + 31 <= key[None, :], 0.0, -BIG)
    c["cmask"] = cm
    ci = np.arange(127)[:, None] * 16
    sj = np.arange(32)[None, :] * 64
    ov = np.clip(np.minimum(ci + 32, sj + 64) - np.maximum(ci, sj), 0, None)
    M = np.zeros((128, 32), np.float32)
    M[:127] = ov / 32.0
    c["cmp2slc"] = M
    blk = np.arange(32)[None, None, :]
    t = (np.arange(16)[None, :, None] * 128 + np.arange(128)[:, None, None])
    cur = t // 64
    forced = (blk == 0) | (blk == cur) | (blk == cur - 1)
    valid = blk <= cur
    c["nsa_mult"] = np.where(forced | ~valid, 0.0, 1.0).astype(np.float32).reshape(128, 16 * 32)
    c["nsa_add"] = np.where(forced, 1e9, np.where(valid, 0.0, -1e30)).astype(np.float32).reshape(128, 16 * 32)
    n8 = np.arange(8)[None, None, :]
    curm = t // 256
    c["moba_add"] = np.broadcast_to(np.where(n8 < curm, 0.0, -1e30), (128, 16, 8)).astype(np.float32).reshape(128, 128).copy()
    return c


CONST_SHAPES = dict(ident=[128, 128], tri_c=[128, 128], tri_a=[128, 128], ones64=[64, 64], bd_ones=[128, 128], abias=[128, 304],
                    cbias=[128, 32], qalibi=[16, 3, 512], e32=[32, T], e8=[8, T], ones3=[3, T], zeros32=[32, T],
                    cmask=[128, T], cmp2slc=[128, 32], nsa_mult=[128, 512], nsa_add=[128, 512], moba_add=[128, 128])


def build_program(nseq=2, phases=("all",), depth=DEPTH, dbg=None):
    nc = bass.Bass("TRN2", target_bir_lowering=False)
    NT = nseq * T
    NTT = NT // 128
    S = Sched()
    es = ExitStack()

    def dram_in(name, shape, dt=F32):
        return nc.dram_tensor(name, list(shape), dt, kind="ExternalInput").ap()

    x_d = dram_in("x", [NT, D])
    normg_d = dram_in("norm_g", [DEPTH, 3, D])
    w1_d = dram_in("ffn_w1", [DEPTH, 2, D, DFF])
    w3_d = dram_in("ffn_w3", [DEPTH, 2, D, DFF])
    w2_d = dram_in("ffn_w2", [DEPTH, 2, DFF, D])
    win_d = dram_in("w_in", [DEPTH, D, INC])
    gqn_d = dram_in("g_qk_nsa", [DEPTH, 4, 64])
    gqnT_d = dram_in("g_qk_nsaT", [DEPTH, 64, 4])
    gqm_d = dram_in("g_qk_moba", [DEPTH, 2, 64])
    gqmT_d = dram_in("g_qk_mobaT", [DEPTH, 64, 2])
    posT_d = dram_in("cmp_posT", [DEPTH, 2, 64, 32])
    cw1_d = dram_in("cmp_w1", [DEPTH, 2, 2048, 256])
    cw2_d = dram_in("cmp_w2", [DEPTH, 2, 256, 64])
    wupn_d = dram_in("w_up_nsa", [DEPTH, 512, D])
    wupm_d = dram_in("w_up_moba", [DEPTH, 512, D])
    wout_d = dram_in("w_out", [DEPTH, D, D])
    C = {k: dram_in(k, v) for k, v in CONST_SHAPES.items()}
    y_d = nc.dram_tensor("y", [NT, D], F32, kind="ExternalOutput").ap()
    dbg_d = {}
    if dbg:
        for k, shp in dbg.items():
            dbg_d[k] = nc.dram_tensor("dbg_" + k, list(shp), F32, kind="ExternalOutput").ap()

    def sb(name, shape, dt):
        return es.enter_context(nc.sbuf_tensor(name, list(shape), dt))

    banks = []
    for i in range(8):
        t = es.enter_context(nc.psum_tensor(f"ps{i}", [128, 512], F32))
        banks.append((t, Res(f"ps{i}")))
    ps_ring = Ring(banks)
    acc_banks = banks[0:4]
    acc_ring = Ring(banks[0:4])
    sc_ring = Ring(banks[4:7])
    misc_ring = Ring(banks[7:8])

    ident_b = sb("ident_b", [128, 128], BF16)
    r_ident = Res("ident")
    S.op("pool", lambda e: e.dma_start(out=ident_b[:], in_=C["ident"][:, :]), writes=[r_ident], dma=True)
    stat = [(sb(f"stat{i}", [128, 4], F32), Res(f"stat{i}")) for i in range(4)]
    stat_ring = Ring(stat)
    ARENA_EL = 105000
    arena_t = sb("arena", [128, ARENA_EL], BF16)
    A = Arena(arena_t, ARENA_EL)

    r_y = [Res(f"y{i}") for i in range(NTT)]
    r_x = [Res(f"x{i}") for i in range(NTT)]

    def rmsnorm_tile(x_ap, r_x_, g_ap, r_gres, out_ap, r_out, jk, r_jk):
        st, r_st = stat_ring.next()
        S.op("act", lambda e: e.activation(out=jk, in_=x_ap, func=AF.Square, accum_out=st[:, 0:1]),
             reads=[r_x_], writes=[r_jk, r_st])
        S.op("act", lambda e: e.activation(out=st[:, 1:2], in_=st[:, 0:1], func=AF.Sqrt, scale=1.0 / D, bias=EPS),
             reads=[r_st], writes=[r_st])
        S.op("dve", lambda e: e.reciprocal(out=st[:, 2:3], in_=st[:, 1:2]), reads=[r_st], writes=[r_st])
        S.op("dve", lambda e: e.scalar_tensor_tensor(out=out_ap, in0=x_ap, scalar=st[:, 2:3], in1=g_ap,
                                                     op0=ALU.mult, op1=ALU.mult),
             reads=[r_x_, r_st, r_gres], writes=[r_out])

    def transpose_tile(in_tile, r_in, out_ap3, r_out, nk=8, evac="act", ring=None):
        ps, r_ps = (ring or ps_ring).next()
        psb = ps[:].bitcast(BF16)
        fns = []
        for k in range(nk):
            fns.append(lambda e, k=k: e.transpose(out=psb[:, k * 128:(k + 1) * 128],
                                                  in_=in_tile[:, k * 128:(k + 1) * 128], identity=ident_b[:]))
        S.op("pe", fns, reads=[r_in, r_ident], writes=[r_ps])
        src = psb[:, 0:nk * 128].rearrange("p (k t) -> p k t", k=nk)
        if evac == "act":
            S.op("act", lambda e: e.copy(out=out_ap3, in_=src), reads=[r_ps], writes=[r_out])
        else:
            S.op("dve", lambda e: e.tensor_copy(out=out_ap3, in_=src), reads=[r_ps], writes=[r_out])

    def ffn_phase(l, j, src_d, r_src):
        S.barrier()
        A.off = 0
        w1_sb = A.alloc([128, 8, DFF], BF16)
        w3_sb = A.alloc([128, 8, DFF], BF16)
        w2_sb = A.alloc([128, NF, D], BF16)
        r_w1 = [Res() for f in range(NF)]
        r_w3 = [Res() for f in range(NF)]
        r_w2 = [Res() for f in range(NF)]
        g_rep = A.alloc([128, D], F32)
        r_g = Res()
        xn_ring = Ring([(A.alloc([128, D], F32), Res()) for i in range(2)])
        xr_ring = Ring([(A.alloc([128, D], F32), Res()) for i in range(2)])
        hb = [(A.alloc([128, D], BF16), Res()) for i in range(4)]
        hT2 = [A.alloc([128, 8, 512], BF16) for i in range(2)]
        r_hT2 = [[Res() for i in range(4)] for _ in range(2)]
        gT = A.alloc([128, NF, 512], BF16)
        r_gT = [Res() for f in range(NF)]
        su_ring = Ring([(A.alloc([128, 512], F32), Res()) for i in range(2)])
        junk = (A.alloc([128, D], BF16), Res())
        NB = NT // 512

        S.op("pool", lambda e: e.dma_start(out=g_rep, in_=normg_d[l, 2 * j, :].partition_broadcast(128)),
             writes=[r_g], dma=True)
        for f in range(NF):
            S.op("pool", lambda e, f=f: e.dma_start(
                out=w1_sb[:, :, f * 128:(f + 1) * 128],
                in_=w1_d[l, j, :, f * 128:(f + 1) * 128].rearrange("(k p) c -> p k c", p=128)),
                writes=[r_w1[f]], dma=True)
            S.op("pool", lambda e, f=f: e.dma_start(
                out=w3_sb[:, :, f * 128:(f + 1) * 128],
                in_=w3_d[l, j, :, f * 128:(f + 1) * 128].rearrange("(k p) c -> p k c", p=128)),
                writes=[r_w3[f]], dma=True)
        for f in range(NF):
            S.op("pool", lambda e, f=f: e.dma_start(out=w2_sb[:, f, :], in_=w2_d[l, j, f * 128:(f + 1) * 128, :]),
                 writes=[r_w2[f]], dma=True)

        def norm_part(blk):
            for i in range(4):
                tt = blk * 4 + i
                xa, r_xa = xn_ring.next()
                S.op("sp", lambda e, xa=xa, tt=tt: e.dma_start(out=xa, in_=src_d[tt * 128:(tt + 1) * 128, :]),
                     reads=[r_src[tt]], writes=[r_xa], dma=True)
                hbt, r_hb = hb[i]
                rmsnorm_tile(xa, r_xa, g_rep, r_g, hbt, r_hb, junk[0], junk[1])

        def transpose_part(blk):
            hT = hT2[blk % 2]
            for i in range(4):
                hbt, r_hb = hb[i]
                transpose_tile(hbt, r_hb, hT[:, :, i * 128:(i + 1) * 128], r_hT2[blk % 2][i])

        def up_part(blk):
            hT = hT2[blk % 2]
            r_hT = r_hT2[blk % 2]
            for f in range(NF):
                pu, r_pu = ps_ring.next()
                pv, r_pv = ps_ring.next()
                fns = [lambda e, k=k, f=f, pu=pu: e.matmul(pu[:], lhsT=w1_sb[:, k, f * 128:(f + 1) * 128],
                                                           rhs=hT[:, k, :], start=(k == 0), stop=(k == 7)) for k in range(8)]
                S.op("pe", fns, reads=[r_w1[f]] + r_hT, writes=[r_pu])
                fns = [lambda e, k=k, f=f, pv=pv: e.matmul(pv[:], lhsT=w3_sb[:, k, f * 128:(f + 1) * 128],
                                                           rhs=hT[:, k, :], start=(k == 0), stop=(k == 7)) for k in range(8)]
                S.op("pe", fns, reads=[r_w3[f]] + r_hT, writes=[r_pv])
                s_t, r_s = su_ring.next()
                S.op("act", lambda e, s_t=s_t, pu=pu: e.activation(out=s_t, in_=pu[:], func=AF.Silu),
                     reads=[r_pu], writes=[r_s])
                S.op("dve", lambda e, s_t=s_t, pv=pv, f=f: e.tensor_tensor(out=gT[:, f, :], in0=s_t, in1=pv[:],
                                                                             op=ALU.mult),
                     reads=[r_s, r_pv], writes=[r_gT[f]])

        def down_part(blk):
            for i in range(4):
                tt = blk * 4 + i
                xa, r_xa = xr_ring.next()
                S.op("sp", lambda e, xa=xa, tt=tt: e.dma_start(out=xa, in_=src_d[tt * 128:(tt + 1) * 128, :]),
                     reads=[r_src[tt]], writes=[r_xa], dma=True)
                for h in range(2):
                    po, r_po = ps_ring.next()
                    fns = [lambda e, f=f, po=po, i=i, h=h: e.matmul(
                        po[:], lhsT=gT[:, f, i * 128:(i + 1) * 128], rhs=w2_sb[:, f, h * 512:(h + 1) * 512],
                        start=(f == 0), stop=(f == NF - 1)) for f in range(NF)]
                    S.op("pe", fns, reads=r_gT + r_w2, writes=[r_po])
                    S.op("dve", lambda e, po=po, xa=xa, h=h: e.scalar_tensor_tensor(
                        out=xa[:, h * 512:(h + 1) * 512], in0=po[:], scalar=0.5, in1=xa[:, h * 512:(h + 1) * 512],
                        op0=ALU.mult, op1=ALU.add), reads=[r_po, r_xa], writes=[r_xa])
                S.op("sp", lambda e, xa=xa, tt=tt: e.dma_start(out=y_d[tt * 128:(tt + 1) * 128, :], in_=xa),
                     reads=[r_xa], writes=[r_y[tt]], dma=True)

        norm_part(0)
        transpose_part(0)
        for blk in range(NB):
            if blk + 1 < NB:
                norm_part(blk + 1)
            up_part(blk)
            if blk + 1 < NB:
                transpose_part(blk + 1)
            down_part(blk)

    def mix_phase(l, src_d, r_src):
        S.barrier()
        A.off = 0
        r_c = Res()
        tri_c = A.alloc([128, 128], BF16)
        tri_a = A.alloc([128, 128], BF16)
        ones64 = A.alloc([64, 64], BF16)
        bd_ones = A.alloc([128, 128], BF16)
        gtab = A.alloc([128, 4], F32)
        abias = A.alloc([128, 304], F32)
        cbias = A.alloc([128, 32], F32)
        cmask = A.alloc([128, T], BF16)
        nsa_mult = A.alloc([128, 16, 32], F32)
        nsa_add = A.alloc([128, 16, 32], F32)
        moba_add = A.alloc([128, 16, 8], F32)
        gq_n = A.alloc([64, 4], F32)
        gq_m = A.alloc([64, 2], F32)
        gkc_rep = A.alloc([128, 64], F32)
        g_rep = A.alloc([128, D], F32)
        for dst, src in ((tri_c, C["tri_c"][:, :]), (tri_a, C["tri_a"][:, :]), (ones64, C["ones64"][:, :]), (bd_ones, C["bd_ones"][:, :]),
                         (gtab[0:64, 0:1], gqn_d[l, 0, :].unsqueeze(1)), (gtab[64:128, 0:1], gqn_d[l, 0, :].unsqueeze(1)),
                         (gtab[0:64, 1:2], gqn_d[l, 2, :].unsqueeze(1)), (gtab[64:128, 1:2], gqn_d[l, 3, :].unsqueeze(1)),
                         (gtab[0:64, 2:3], gqm_d[l, 0, :].unsqueeze(1)), (gtab[64:128, 2:3], gqm_d[l, 0, :].unsqueeze(1)),
                         (gtab[0:64, 3:4], gqm_d[l, 1, :].unsqueeze(1)), (gtab[64:128, 3:4], gqm_d[l, 1, :].unsqueeze(1)),
                         (abias, C["abias"][:, :]), (cbias, C["cbias"][:, :]), (cmask, C["cmask"][:, :]),
                         (nsa_mult, C["nsa_mult"][:, :].rearrange("p (a b) -> p a b", a=16)),
                         (nsa_add, C["nsa_add"][:, :].rearrange("p (a b) -> p a b", a=16)),
                         (moba_add, C["moba_add"][:, :].rearrange("p (a b) -> p a b", a=16)),
                         (gq_n, gqnT_d[l, :, :]), (gq_m, gqmT_d[l, :, :]),
                         (gkc_rep, gqn_d[l, 1, :].partition_broadcast(128)),
                         (g_rep, normg_d[l, 1, :].partition_broadcast(128))):
            S.op("pool", lambda e, dst=dst, src=src: e.dma_start(out=dst, in_=src), writes=[r_c], dma=True)
        S.op("dve", lambda e: e.tensor_scalar(out=gq_n[:, 0:1], in0=gq_n[:, 0:1], scalar1=0.125, scalar2=None,
                                              op0=ALU.mult), reads=[r_c], writes=[r_c])
        S.op("dve", lambda e: e.tensor_scalar(out=gq_m[:, 0:1], in0=gq_m[:, 0:1], scalar1=0.125, scalar2=None,
                                              op0=ALU.mult), reads=[r_c], writes=[r_c])
        S.op("dve", lambda e: e.tensor_scalar(out=gtab[:, 0:1], in0=gtab[:, 0:1], scalar1=0.125, scalar2=None,
                                              op0=ALU.mult), reads=[r_c], writes=[r_c])
        S.op("dve", lambda e: e.tensor_scalar(out=gtab[:, 2:3], in0=gtab[:, 2:3], scalar1=0.125, scalar2=None,
                                              op0=ALU.mult), reads=[r_c], writes=[r_c])

        hT = A.alloc([128, 8, T], BF16)
        r_hT = [Res() for _ in range(16)]
        onT = A.alloc([128, 4, T], BF16)
        omT = A.alloc([128, 4, T], BF16)
        r_onT = [Res() for _ in range(16)]
        r_omT = [Res() for _ in range(16)]
        gsig = A.alloc([128, 16, 24], F32)
        r_gsig = Res()
        wch_ring = Ring([(A.alloc([128, 8, 256], BF16), Res()) for _ in range(3)])
        pT_ring = Ring([(A.alloc([128, 512], BF16), Res()) for _ in range(3)])
        tmpf_ring = Ring([(A.alloc([128, 512], F32), Res()) for _ in range(3)])
        sqb_ring = Ring([(A.alloc([128, 512], BF16), Res()) for _ in range(3)])
        xt_ring = Ring([(A.alloc([128, D], F32), Res()) for _ in range(3)])
        hb_ring = Ring([(A.alloc([128, D], BF16), Res()) for _ in range(3)])
        junk = (A.alloc([128, D], BF16), Res())
        sm_ring = Ring([(A.alloc([128, 16], F32), Res()) for _ in range(8)])
        region0 = A.off

        def load_w(src_ap3, ncols):
            w, r_w = wch_ring.next()
            S.op("pool", lambda e: e.dma_start(out=w[:, :, 0:ncols], in_=src_ap3), writes=[r_w], dma=True)
            return w, r_w

        def win_cols(c0, n):
            return win_d[l, :, c0:c0 + n].rearrange("(k p) c -> p k c", p=128)

        def proj_fm_pairs(pairs):
            items = [(pi, b) for pi in range(len(pairs)) for b in range(4)]
            wts = {}
            stt = {}

            def ensure_w(pi):
                if pi < len(pairs) and pi not in wts:
                    c0A, c0B = pairs[pi][0], pairs[pi][1]
                    w, r_w = wch_ring.next()
                    if c0B == c0A + 64:
                        S.op("pool", lambda e: e.dma_start(out=w[:, :, 0:128], in_=win_cols(c0A, 128)), writes=[r_w], dma=True)
                    else:
                        S.op("pool", lambda e: e.dma_start(out=w[:, :, 0:64], in_=win_cols(c0A, 64)), writes=[r_w], dma=True)
                        S.op("pool", lambda e: e.dma_start(out=w[:, :, 64:128], in_=win_cols(c0B, 64)), writes=[r_w], dma=True)
                    wts[pi] = (w, r_w)

            def mm(i):
                pi, b = items[i]
                ensure_w(pi)
                if b == 0:
                    ensure_w(pi + 1)
                w, r_w = wts[pi]
                ps, r_ps = ps_ring.next()
                fns = [lambda e, k=k: e.matmul(ps[:, :], lhsT=w[:, k, 0:128], rhs=hT[:, k, b * 512:(b + 1) * 512],
                                               start=(k == 0), stop=(k == 7)) for k in range(8)]
                S.op("pe", fns, reads=[r_w] + r_hT[4 * b:4 * b + 4], writes=[r_ps])
                c0A, c0B, dA, rA, dB, rB, gcol = pairs[pi]
                if gcol is None:
                    S.op("act", lambda e: e.copy(out=dA(b), in_=ps[0:64, :]), reads=[r_ps], writes=[rA(b)])
                    S.op("act", lambda e: e.copy(out=dB(b), in_=ps[64:128, :]), reads=[r_ps], writes=[rB(b)])
                    stt[i] = None
                    return
                sq, r_sq = sqb_ring.next()
                S.op("act", lambda e: e.activation(out=sq[:, :], in_=ps[:, :], func=AF.Square), reads=[r_ps], writes=[r_sq])
                stt[i] = (ps, r_ps, sq, r_sq, b)

            def rest(i):
                if stt[i] is None:
                    return
                ps, r_ps, sq, r_sq, b = stt[i]
                c0A, c0B, dA, rA, dB, rB, gcol = pairs[items[i][0]]
                p2, r_p2 = ps_ring.next()
                S.op("pe", lambda e: e.matmul(p2[:, :], lhsT=bd_ones[:, :], rhs=sq[:, :], start=True, stop=True),
                     reads=[r_sq, r_c], writes=[r_p2])
                tf, r_tf = tmpf_ring.next()
                S.op("act", lambda e: e.activation(out=tf[:, :], in_=p2[:, :], func=AF.Ln, scale=1.0 / 64, bias=EPS),
                     reads=[r_p2], writes=[r_tf])
                S.op("act", lambda e: e.activation(out=tf[:, :], in_=tf[:, :], func=AF.Exp, scale=-0.5),
                     reads=[r_tf], writes=[r_tf])
                S.op("dve", lambda e: e.scalar_tensor_tensor(out=dA(b), in0=ps[0:64, :], scalar=gcol[0:64, :], in1=tf[0:64, :],
                                                             op0=ALU.mult, op1=ALU.mult),
                     reads=[r_ps, r_tf, r_c], writes=[rA(b)])
                S.op("dve", lambda e: e.scalar_tensor_tensor(out=dB(b), in0=ps[64:128, :], scalar=gcol[64:128, :],
                                                             in1=tf[64:128, :], op0=ALU.mult, op1=ALU.mult),
                     reads=[r_ps, r_tf, r_c], writes=[rB(b)])

            mm(0)
            for i in range(len(items)):
                if i + 1 < len(items):
                    mm(i + 1)
                rest(i)

        def causal_tiles(qb):
            tl = []
            for kt in range(4 * qb + 4):
                c = kt - 4 * qb
                if c < 0:
                    tl.append((kt, 0, 512, None, None))
                else:
                    tl.append((kt, 128 * c, 512, "c", c))
            return tl

        def window_tiles(qb):
            tl = []
            for c in (0, 1, 2, 3, -1, -2, -3, -4):
                kt = 4 * qb + c
                if kt < 0:
                    continue
                if c >= 0:
                    tl.append((kt, 128 * c, 512, "c", c))
                else:
                    m = 4 + c
                    tl.append((kt, 0, 128 * (m + 1), "a", m))
            return tl

        def run_rounds(rounds):
            items = []
            for R in rounds:
                for ti, tile in enumerate(R["tiles"]):
                    items.append((R, ti, tile))

            def emit_qk(it):
                R, ti, (kt, c0, c1, tri, tu) = it
                ps, r_ps = sc_ring.next()
                kp = R.get("kpart", 128)
                K = R["krows"]
                qb = R["qb"]
                fns = [lambda e: e.matmul(ps[0:kp, c0:c1], lhsT=R["kT"][0:K, kt * 128:kt * 128 + kp],
                                          rhs=R["q"][0:K, qb * 512 + c0:qb * 512 + c1], start=True,
                                          stop=(tri is None and "mask" not in R))]
                reads = [R["r_k"]] + R["r_q"] + [r_c]
                if "mask" in R:
                    fns.append(lambda e: e.matmul(ps[0:kp, c0:c1], lhsT=ident_b[0:kp, 0:kp],
                                                  rhs=R["mask"][0:kp, qb * 512 + c0:qb * 512 + c1],
                                                  start=False, stop=True))
                if tri is not None:
                    tm = tri_c if tri == "c" else tri_a
                    fns.append(lambda e: e.matmul(ps[:, 128 * tu:128 * tu + 128], lhsT=ident_b[:, :], rhs=tm[:, :],
                                                  start=False, stop=True))
                S.op("pe", fns, reads=reads + [r_ident], writes=[r_ps])
                return ps, r_ps

            pend = emit_qk(items[0]) if items else None
            for idx, it in enumerate(items):
                R, ti, (kt, c0, c1, tri, tu) = it
                ps, r_ps = pend
                if idx + 1 < len(items):
                    pend = emit_qk(items[idx + 1])
                kp = R.get("kpart", 128)
                pT, r_pT = pT_ring.next()
                bias_ap = R["bias"](kt)
                S.op("act", lambda e, ps=ps, pT=pT, bias_ap=bias_ap, kp=kp, c0=c0, c1=c1: e.activation(
                    out=pT[0:kp, c0:c1], in_=ps[0:kp, c0:c1], func=AF.Exp, bias=bias_ap, scale=1.0),
                    reads=[r_ps, r_c], writes=[r_pT])
                us = [u for u in range(4) if c0 <= 128 * u < c1]
                nv = R["nv"]
                if ti == 0:
                    R["acc"] = acc_ring.next()
                acc, r_acc = R["acc"]
                fns = []
                V = R["V"](kt)
                nt = len(R["tiles"])
                for u in us:
                    fns.append(lambda e, u=u, acc=acc, V=V, first=(ti == 0 and u == us[0]),
                               last=(ti == nt - 1 and u == us[-1]), pT=pT, kp=kp:
                               e.matmul(acc[:, 128 * u:128 * u + nv], lhsT=pT[0:kp, 128 * u:128 * u + 128], rhs=V,
                                        start=first, stop=last))
                S.op("pe", fns, reads=[r_pT, R["r_v"]], writes=[r_acc])
                if ti == nt - 1:
                    R["evac"](acc[:].rearrange("p (u c) -> p u c", u=4), r_acc)

        sn_, sm_ = slopes_all()

        def dump(name, ap, reads):
            if name in dbg_d:
                dst = dbg_d[name][:, :]
                if len(ap.shape) == 3:
                    dst = dst.rearrange("p (a b) -> p a b", a=ap.shape[1])
                S.op("pool", lambda e: e.dma_start(out=dst[0:ap.shape[0]], in_=ap), reads=reads, writes=[Res()], dma=True)

        for s in range(nseq):
            tt0 = s * 16
            prev = None
            for i in range(16):
                xa, r_xa = xt_ring.next()
                S.op("sp", lambda e, xa=xa, i=i, tt0=tt0: e.dma_start(out=xa, in_=src_d[(tt0 + i) * 128:(tt0 + i + 1) * 128, :]),
                     reads=[r_src[tt0 + i]], writes=[r_xa], dma=True)
                hbt, r_hb = hb_ring.next()
                st, r_st = stat_ring.next()
                jk, r_jk = junk
                S.op("act", lambda e, xa=xa, st=st: e.activation(out=jk, in_=xa, func=AF.Square, accum_out=st[:, 0:1]),
                     reads=[r_xa], writes=[r_jk, r_st])
                S.op("act", lambda e, st=st: e.activation(out=st[:, 1:2], in_=st[:, 0:1], func=AF.Sqrt, scale=1.0 / D, bias=EPS),
                     reads=[r_st], writes=[r_st])
                if prev is not None:
                    prev()
                S.op("dve", lambda e, st=st: e.reciprocal(out=st[:, 2:3], in_=st[:, 1:2]), reads=[r_st], writes=[r_st])
                S.op("dve", lambda e, xa=xa, st=st, hbt=hbt: e.scalar_tensor_tensor(
                    out=hbt, in0=xa, scalar=st[:, 2:3], in1=g_rep, op0=ALU.mult, op1=ALU.mult),
                    reads=[r_xa, r_st, r_c], writes=[r_hb])
                ps, r_ps = ps_ring.next()
                psb = ps[:].bitcast(BF16)
                fns = [lambda e, k=k, psb=psb, hbt=hbt: e.transpose(out=psb[:, k * 128:(k + 1) * 128],
                                                                    in_=hbt[:, k * 128:(k + 1) * 128], identity=ident_b[:])
                       for k in range(8)]
                S.op("pe", fns, reads=[r_hb, r_ident], writes=[r_ps])

                def evac(psb=psb, r_ps=r_ps, i=i):
                    S.op("act", lambda e: e.copy(out=hT[:, :, i * 128:(i + 1) * 128],
                                                 in_=psb.rearrange("p (k t) -> p k t", k=8)), reads=[r_ps], writes=[r_hT[i]])
                prev = evac
            prev()

            w, r_w = load_w(win_cols(NSA_COLS["g"], 64), 64)
            for i in range(16):
                ps, r_ps = ps_ring.next()
                fns = [lambda e, k=k, ps=ps, i=i, w=w: e.matmul(ps[:, 0:24], lhsT=hT[:, k, i * 128:(i + 1) * 128],
                                                                rhs=w[:, k, 0:24], start=(k == 0), stop=(k == 7))
                       for k in range(8)]
                S.op("pe", fns, reads=[r_w, r_hT[i]], writes=[r_ps])
                S.op("act", lambda e, ps=ps, i=i: e.activation(out=gsig[:, i, :], in_=ps[:, 0:24], func=AF.Sigmoid),
                     reads=[r_ps], writes=[r_gsig])

            if dbg and s == 0 and l == 0:
                dump("gsig", gsig, [r_gsig])
            for g in range(2):
                S.barrier()
                A.off = region0
                q_aug = [A.alloc([128, T], BF16) for _ in range(4)]
                r_q = [[Res() for _ in range(4)] for _ in range(4)]
                r_qs = [[Res() for _ in range(4)] for _ in range(4)]
                r_qst = Res()
                ks_aug = A.alloc([128, T], BF16)
                kw_aug = A.alloc([128, T], BF16)
                r_ks = Res()
                r_kw = Res()
                kcraw = A.alloc([64, T], BF16)
                r_kcraw = Res()
                vcraw = A.alloc([64, T], BF16)
                r_vcraw = Res()
                vsA = A.alloc([128, 16, 65], BF16)
                vwA = A.alloc([128, 16, 65], BF16)
                r_vs = Res()
                r_vw = Res()
                w1c = A.alloc([64, 32, 256], BF16)
                r_w1c = Res()
                w2c = A.alloc([128, 2, 64], BF16)
                posT = A.alloc([64, 32], BF16)
                r_w2c = Res()
                kc_aug = A.alloc([128, 128], BF16)
                r_kc = Res()
                kctm = A.alloc([128, 128], BF16)
                r_kctm = Res()
                vcA = A.alloc([128, 97], BF16)
                r_vc = Res()
                hid = A.alloc([128, 2, 128], BF16)
                r_hid = Res()
                pbias = A.alloc([128, 2], F32)
                r_pb = Res()
                oacc = A.alloc([128, 16, 256], F32)
                r_oacc = [Res() for _ in range(16)]
                impacc = A.alloc([128, 16, 32], F32)
                r_imp = [Res() for _ in range(16)]
                trin_all = [(A.alloc([128, 96], BF16), Res()) for _ in range(16)]
                ob_ring = Ring([(A.alloc([128, 256], BF16), Res()) for _ in range(2)])

                for hl in range(4):
                    h = 4 * g + hl
                    S.op("pool", lambda e, hl=hl, h=h: e.dma_start(
                        out=q_aug[hl][96:99, :].rearrange("p (b i) -> p b i", b=4),
                        in_=C["qalibi"][h, :, :].unsqueeze(1).to_broadcast([3, 4, 512])), writes=[r_qst], dma=True)
                    S.op("pool", lambda e, hl=hl: e.dma_start(out=q_aug[hl][64:96, :], in_=C["zeros32"][:, :]),
                         writes=r_qs[hl], dma=True)
                for dst, r_dst, mid in ((ks_aug, r_ks, C["e32"]), (kw_aug, r_kw, C["zeros32"])):
                    S.op("pool", lambda e, dst=dst, mid=mid: e.dma_start(out=dst[64:96, :], in_=mid[:, :]),
                         writes=[r_dst], dma=True)
                    S.op("pool", lambda e, dst=dst: e.dma_start(out=dst[96:99, :], in_=C["ones3"][:, :]),
                         writes=[r_dst], dma=True)
                S.op("pool", lambda e: e.memset(kctm[:, 64:96], 0.0), writes=[r_kctm])
                S.op("pool", lambda e: e.memset(kctm[:, 96:99], 1.0), writes=[r_kctm])
                S.op("pool", lambda e: e.memset(kctm[:, 0:64], 0.0), writes=[r_kctm])
                S.op("pool", lambda e: e.memset(vsA[:, :, 64:65], 1.0), writes=[r_vs])
                S.op("pool", lambda e: e.memset(vwA[:, :, 64:65], 1.0), writes=[r_vw])
                S.op("pool", lambda e: e.memset(vcA[:, 64:65], 1.0), writes=[r_vc])
                S.op("pool", lambda e: e.dma_start(out=vcA[:, 65:97], in_=C["cmp2slc"][:, :]), writes=[r_vc], dma=True)
                for tr, r_tr in trin_all:
                    S.op("pool", lambda e, tr=tr: e.memset(tr[:, 0:64], 0.0), writes=[r_tr])

                proj_fm_pairs([(NSA_COLS["kc"] + 64 * g, NSA_COLS["vc"] + 64 * g,
                                lambda b: kcraw[0:64, b * 512:(b + 1) * 512], lambda b: r_kcraw,
                                lambda b: vcraw[0:64, b * 512:(b + 1) * 512], lambda b: r_vcraw, None)])
                for kv in range(2):
                    craw, r_craw = (kcraw, r_kcraw) if kv == 0 else (vcraw, r_vcraw)
                    S.op("pool", lambda e, kv=kv: e.dma_start(
                        out=w1c, in_=cw1_d[l, kv, :, :].rearrange("(l d) h -> d l h", d=64)), writes=[r_w1c], dma=True)
                    S.op("pool", lambda e, kv=kv: e.dma_start(
                        out=w2c, in_=cw2_d[l, kv, :, :].rearrange("(a p) d -> p a d", p=128)), writes=[r_w2c], dma=True)
                    S.op("pool", lambda e, kv=kv: e.dma_start(out=posT, in_=posT_d[l, kv, :, :]), writes=[r_w2c], dma=True)
                    for hh in range(2):
                        ps, r_ps = ps_ring.next()
                        fns = [lambda e, ll=ll, ps=ps, hh=hh, craw=craw: e.matmul(
                            ps[:, 0:127], lhsT=w1c[:, ll, hh * 128:(hh + 1) * 128],
                            rhs=craw[0:64, ll:ll + 16 * 126 + 1:16], start=(ll == 0), stop=(ll == 31)) for ll in range(32)]
                        fns += [lambda e, ll=ll, ps=ps, hh=hh: e.matmul(
                            ps[:, 128:129], lhsT=w1c[:, ll, hh * 128:(hh + 1) * 128],
                            rhs=posT[:, ll:ll + 1], start=(ll == 0), stop=(ll == 31)) for ll in range(32)]
                        S.op("pe", fns, reads=[r_w1c, r_w2c, r_craw], writes=[r_ps])
                        S.op("dve", lambda e, ps=ps, hh=hh: e.tensor_copy(out=pbias[:, hh:hh + 1], in_=ps[:, 128:129]),
                             reads=[r_ps], writes=[r_pb])
                        xh, r_xh = tmpf_ring.next()
                        x2, r_x2 = tmpf_ring.next()
                        S.op("dve", lambda e, ps=ps, hh=hh, xh=xh: e.tensor_scalar(
                            out=xh[:, 0:127], in0=ps[:, 0:127], scalar1=pbias[:, hh:hh + 1], scalar2=None, op0=ALU.add),
                            reads=[r_ps, r_pb], writes=[r_xh])
                        S.op("dve", lambda e, xh=xh, x2=x2: e.tensor_tensor(out=x2[:, 0:127], in0=xh[:, 0:127],
                                                                            in1=xh[:, 0:127], op=ALU.mult),
                             reads=[r_xh], writes=[r_x2])
                        S.op("dve", lambda e, x2=x2: e.tensor_scalar(out=x2[:, 0:127], in0=x2[:, 0:127], scalar1=0.044715,
                                                                     scalar2=1.0, op0=ALU.mult, op1=ALU.add),
                             reads=[r_x2], writes=[r_x2])
                        S.op("dve", lambda e, xh=xh, x2=x2: e.tensor_tensor(out=x2[:, 0:127], in0=x2[:, 0:127],
                                                                            in1=xh[:, 0:127], op=ALU.mult),
                             reads=[r_xh, r_x2], writes=[r_x2])
                        S.op("act", lambda e, x2=x2: e.activation(out=x2[:, 0:127], in_=x2[:, 0:127], func=AF.Tanh,
                                                                  scale=0.7978845608028654),
                             reads=[r_x2], writes=[r_x2])
                        S.op("dve", lambda e, xh=xh, x2=x2, hh=hh: e.scalar_tensor_tensor(
                            out=hid[:, hh, 0:127], in0=x2[:, 0:127], scalar=1.0, in1=xh[:, 0:127],
                            op0=ALU.add, op1=ALU.mult), reads=[r_xh, r_x2], writes=[r_hid])
                    ps, r_ps = ps_ring.next()
                    fns = [lambda e, hh=hh, ps=ps: e.matmul(ps[0:127, 0:64], lhsT=hid[:, hh, 0:127], rhs=w2c[:, hh, :],
                                                            start=(hh == 0), stop=(hh == 1)) for hh in range(2)]
                    S.op("pe", fns, reads=[r_hid, r_w2c], writes=[r_ps])
                    if kv == 1:
                        S.op("dve", lambda e, ps=ps: e.tensor_scalar(out=vcA[0:127, 0:64], in0=ps[0:127, 0:64],
                                                                     scalar1=0.5, scalar2=None, op0=ALU.mult),
                             reads=[r_ps], writes=[r_vc])
                    else:
                        tf, r_tf = tmpf_ring.next()
                        st, r_st = sm_ring.next()
                        S.op("dve", lambda e, ps=ps, tf=tf: e.tensor_scalar(out=tf[0:127, 0:64], in0=ps[0:127, 0:64],
                                                                            scalar1=0.5, scalar2=None, op0=ALU.mult),
                             reads=[r_ps], writes=[r_tf])
                        S.op("act", lambda e, tf=tf, st=st: e.activation(out=tf[0:127, 64:128], in_=tf[0:127, 0:64],
                                                                         func=AF.Square, accum_out=st[0:127, 0:1]),
                             reads=[r_tf], writes=[r_tf, r_st])
                        S.op("act", lambda e, st=st: e.activation(out=st[0:127, 1:2], in_=st[0:127, 0:1], func=AF.Sqrt,
                                                                  scale=1.0 / 64, bias=EPS), reads=[r_st], writes=[r_st])
                        S.op("dve", lambda e, st=st: e.reciprocal(out=st[0:127, 2:3], in_=st[0:127, 1:2]),
                             reads=[r_st], writes=[r_st])
                        S.op("dve", lambda e, tf=tf, st=st: e.scalar_tensor_tensor(
                            out=kctm[0:127, 0:64], in0=tf[0:127, 0:64], scalar=st[0:127, 2:3], in1=gkc_rep[0:127, :],
                            op0=ALU.mult, op1=ALU.mult), reads=[r_tf, r_st, r_c], writes=[r_kctm])
                        ps2, r_ps2 = ps_ring.next()
                        psb2 = ps2[:].bitcast(BF16)
                        S.op("pe", lambda e, psb2=psb2: e.transpose(out=psb2[0:99, 0:128], in_=kctm[:, 0:99],
                                                                    identity=ident_b[:]),
                             reads=[r_kctm, r_ident], writes=[r_ps2])
                        S.op("dve", lambda e, psb2=psb2: e.tensor_copy(out=kc_aug[0:99, 0:128], in_=psb2[0:99, 0:128]),
                             reads=[r_ps2], writes=[r_kc])

                prs = []
                for hp in range(2):
                    hA, hB = 2 * hp, 2 * hp + 1
                    prs.append((NSA_COLS["q"] + 64 * (4 * g + hA), NSA_COLS["q"] + 64 * (4 * g + hB),
                                lambda b, hA=hA: q_aug[hA][0:64, b * 512:(b + 1) * 512], lambda b, hA=hA: r_q[hA][b],
                                lambda b, hB=hB: q_aug[hB][0:64, b * 512:(b + 1) * 512], lambda b, hB=hB: r_q[hB][b],
                                gtab[:, 0:1]))
                prs.append((NSA_COLS["ks"] + 64 * g, NSA_COLS["kw"] + 64 * g,
                            lambda b: ks_aug[0:64, b * 512:(b + 1) * 512], lambda b: r_ks,
                            lambda b: kw_aug[0:64, b * 512:(b + 1) * 512], lambda b: r_kw, gtab[:, 1:2]))
                proj_fm_pairs(prs)
                for nm, dstA, r_dst in (("vs", vsA, r_vs), ("vw", vwA, r_vw)):
                    w, r_w = load_w(win_cols(NSA_COLS[nm] + 64 * g, 64), 64)
                    for i in range(16):
                        ps, r_ps = ps_ring.next()
                        fns = [lambda e, k=k, ps=ps, i=i, w=w: e.matmul(ps[:, 0:64], lhsT=hT[:, k, i * 128:(i + 1) * 128],
                                                                        rhs=w[:, k, 0:64], start=(k == 0), stop=(k == 7))
                               for k in range(8)]
                        S.op("pe", fns, reads=[r_w, r_hT[i]], writes=[r_ps])
                        S.op("act", lambda e, ps=ps, i=i, dstA=dstA: e.copy(out=dstA[:, i, 0:64], in_=ps[:, 0:64]),
                             reads=[r_ps], writes=[r_dst])
                def mk_cmp_evac(hl, qb):
                    h = 4 * g + hl

                    def ev(acc3, r_acc):
                        tts = slice(4 * qb, 4 * qb + 4)
                        r_o = r_oacc[4 * qb:4 * qb + 4]
                        r_i = r_imp[4 * qb:4 * qb + 4]
                        st, r_st = sm_ring.next()
                        rd = st[:, 4:8].unsqueeze(2)
                        cf = st[:, 8:12].unsqueeze(2)
                        S.op("dve", lambda e: e.tensor_scalar(out=st[:, 0:4].unsqueeze(2), in0=acc3[:, :, 64:65], scalar1=1e-30,
                                                              scalar2=None, op0=ALU.add), reads=[r_acc], writes=[r_st])
                        S.op("dve", lambda e: e.reciprocal(out=st[:, 4:8], in_=st[:, 0:4]), reads=[r_st], writes=[r_st])
                        S.op("dve", lambda e: e.tensor_tensor(out=cf, in0=rd, in1=gsig[:, tts, 3 * h:3 * h + 1], op=ALU.mult),
                             reads=[r_st, r_gsig], writes=[r_st])
                        S.op("dve", lambda e: e.tensor_tensor(out=oacc[:, tts, hl * 64:(hl + 1) * 64], in0=acc3[:, :, 0:64],
                                                              in1=cf.to_broadcast([128, 4, 64]), op=ALU.mult),
                             reads=[r_acc, r_st], writes=r_o)
                        if hl == 0:
                            S.op("dve", lambda e: e.tensor_tensor(out=impacc[:, tts, :], in0=acc3[:, :, 65:97],
                                                                  in1=rd.to_broadcast([128, 4, 32]), op=ALU.mult),
                                 reads=[r_acc, r_st], writes=r_i)
                        else:
                            tf, r_tf = tmpf_ring.next()
                            tf3 = tf[:, 0:128].rearrange("p (u c) -> p u c", u=4)
                            S.op("dve", lambda e: e.tensor_tensor(out=tf3, in0=acc3[:, :, 65:97],
                                                                  in1=rd.to_broadcast([128, 4, 32]), op=ALU.mult),
                                 reads=[r_acc, r_st], writes=[r_tf])
                            S.op("pool", lambda e: e.tensor_tensor(out=impacc[:, tts, :], in0=impacc[:, tts, :], in1=tf3,
                                                                   op=ALU.add), reads=[r_tf] + r_i, writes=r_i)
                    return ev

                rounds = []
                for hl in range(4):
                    h = 4 * g + hl
                    for qb in range(4):
                        rounds.append(dict(q=q_aug[hl], r_q=[r_q[hl][qb], r_qst], krows=99, kT=kc_aug, r_k=r_kc, qb=qb,
                                           tiles=[(0, 0, 512, None, None)], kpart=127, mask=cmask,
                                           V=lambda kt: vcA[0:127, 0:97], r_v=r_vc, nv=97,
                                           bias=lambda kt, h=h, qb=qb: cbias[0:127, 4 * h + qb:4 * h + qb + 1],
                                           evac=mk_cmp_evac(hl, qb)))
                run_rounds(rounds)
                if dbg and s == 0 and l == 0 and g == 0:
                    dump("oacc_cmp", oacc, r_oacc)
                    dump("kc_aug", kc_aug, [r_kc])
                    dump("vcA", vcA, [r_vc])
                    dump("impacc", impacc, r_imp)

                sel_tr = []
                for tt in range(16):
                    sc, r_sc = tmpf_ring.next()
                    st, r_st = sm_ring.next()
                    tr, r_tr = trin_all[tt]
                    S.op("dve", lambda e, sc=sc, tt=tt: e.tensor_tensor(out=sc[:, 0:32], in0=impacc[:, tt, :],
                                                                        in1=nsa_mult[:, tt, :], op=ALU.mult),
                         reads=[r_imp[tt], r_c], writes=[r_sc])
                    S.op("dve", lambda e, sc=sc, tt=tt: e.tensor_tensor(out=sc[:, 0:32], in0=sc[:, 0:32],
                                                                        in1=nsa_add[:, tt, :], op=ALU.add),
                         reads=[r_sc, r_c], writes=[r_sc])
                    S.op("dve", lambda e, sc=sc, st=st: e.max(out=st[:, 0:8], in_=sc[:, 0:32]), reads=[r_sc], writes=[r_st])
                    S.op("dve", lambda e, sc=sc, st=st, tr=tr: e.tensor_scalar(
                        out=tr[:, 64:96], in0=sc[:, 0:32], scalar1=st[:, 7:8], scalar2=-BIG, op0=ALU.is_lt, op1=ALU.mult),
                        reads=[r_sc, r_st], writes=[r_tr])

                def emit_sel_transposes():
                    for tt in range(16):
                        tr, r_tr = trin_all[tt]
                        ps, r_ps = misc_ring.next()
                        psb = ps[:].bitcast(BF16)
                        S.op("pe", lambda e, psb=psb, tr=tr: e.transpose(out=psb[0:96, 0:128], in_=tr[:, 0:96],
                                                                         identity=ident_b[:]),
                             reads=[r_tr, r_ident], writes=[r_ps])
                        for hl in range(4):
                            S.op("dve", lambda e, psb=psb, hl=hl, tt=tt: e.tensor_copy(
                                out=q_aug[hl][64:96, tt * 128:(tt + 1) * 128], in_=psb[64:96, 0:128]),
                                reads=[r_ps], writes=[r_qs[hl][tt // 4]])

                def mk_evac(hl, qb, br):
                    h = 4 * g + hl

                    def ev(acc3, r_acc):
                        tts = slice(4 * qb, 4 * qb + 4)
                        r_o = r_oacc[4 * qb:4 * qb + 4]
                        st, r_st = sm_ring.next()
                        rd = st[:, 4:8].unsqueeze(2)
                        cf = st[:, 8:12].unsqueeze(2)
                        S.op("dve", lambda e: e.reciprocal(out=rd, in_=acc3[:, :, 64:65]), reads=[r_acc], writes=[r_st])
                        S.op("dve", lambda e: e.tensor_tensor(out=cf, in0=rd, in1=gsig[:, tts, 3 * h + br:3 * h + br + 1],
                                                              op=ALU.mult), reads=[r_st, r_gsig], writes=[r_st])
                        tf, r_tf = tmpf_ring.next()
                        tf3 = tf[:, 0:256].rearrange("p (u c) -> p u c", u=4)
                        S.op("dve", lambda e: e.tensor_tensor(out=tf3, in0=acc3[:, :, 0:64], in1=cf.to_broadcast([128, 4, 64]),
                                                              op=ALU.mult), reads=[r_acc, r_st], writes=[r_tf])
                        S.op("pool", lambda e: e.tensor_tensor(out=oacc[:, tts, hl * 64:(hl + 1) * 64],
                                                               in0=oacc[:, tts, hl * 64:(hl + 1) * 64], in1=tf3, op=ALU.add),
                             reads=[r_tf] + r_o, writes=r_o)
                    return ev

                rounds = []
                for hl in range(4):
                    h = 4 * g + hl
                    for qb in range(4):
                        rounds.append(dict(q=q_aug[hl], r_q=[r_q[hl][qb], r_qst], krows=99, kT=kw_aug, r_k=r_kw, qb=qb,
                                           tiles=window_tiles(qb), V=lambda kt: vwA[:, kt, :], r_v=r_vw, nv=65,
                                           bias=lambda kt, h=h, qb=qb: abias[:, h * 19 + (kt - 4 * qb + 15):h * 19 + (kt - 4 * qb + 15) + 1],
                                           evac=mk_evac(hl, qb, 2)))
                run_rounds(rounds)
                rounds = []
                emit_sel_transposes()
                if dbg and s == 0 and l == 0 and g == 0:
                    dump("oacc_win", oacc, r_oacc)
                for hl in range(4):
                    h = 4 * g + hl
                    for qb in range(4):
                        rounds.append(dict(q=q_aug[hl], r_q=[r_q[hl][qb], r_qs[hl][qb], r_qst], krows=99, kT=ks_aug,
                                           r_k=r_ks, qb=qb, tiles=causal_tiles(qb), V=lambda kt: vsA[:, kt, :], r_v=r_vs,
                                           nv=65,
                                           bias=lambda kt, h=h, qb=qb: abias[:, h * 19 + (kt - 4 * qb + 15):h * 19 + (kt - 4 * qb + 15) + 1],
                                           evac=mk_evac(hl, qb, 1)))
                run_rounds(rounds)
                if dbg and s == 0 and l == 0 and g == 0:
                    dump("oacc_all", oacc, r_oacc)
                    dump("q0", q_aug[0], [r_q[0][b] for b in range(4)] + [r_qs[0][b] for b in range(4)] + [r_qst])
                    dump("ks_aug", ks_aug, [r_ks])
                for tt in range(16):
                    ob, r_ob = ob_ring.next()
                    S.op("act", lambda e, ob=ob, tt=tt: e.copy(out=ob, in_=oacc[:, tt, :]), reads=[r_oacc[tt]], writes=[r_ob])
                    transpose_tile(ob, r_ob, onT[:, 2 * g:2 * g + 2, tt * 128:(tt + 1) * 128], r_onT[tt], nk=2,
                                   evac="dve", ring=misc_ring)

            for hf in range(2):
                S.barrier()
                A.off = region0
                q_aug = [A.alloc([128, T], BF16) for _ in range(4)]
                k_aug = [A.alloc([128, T], BF16) for _ in range(4)]
                r_q = [[Res() for _ in range(4)] for _ in range(4)]
                r_qs = [[Res() for _ in range(4)] for _ in range(4)]
                r_qst = Res()
                r_k = [Res() for _ in range(4)]
                vmA = A.alloc([128, 16, 4, 65], BF16)
                r_vm = Res()
                kmean_f = A.alloc([64, 4, 8], F32)
                kmean_b = A.alloc([64, 4, 8], BF16)
                r_km = Res()
                omb = A.alloc([128, 16, 256], BF16)
                r_omb = [Res() for _ in range(16)]
                trin_ring = Ring([(A.alloc([128, 72], BF16), Res()) for _ in range(4)])
                for tr, r_tr in trin_ring.items:
                    S.op("pool", lambda e, tr=tr: e.memset(tr[:, 0:64], 0.0), writes=[r_tr])
                S.op("pool", lambda e: e.memset(vmA[:, :, :, 64:65], 1.0), writes=[r_vm])
                for hl in range(4):
                    h = 4 * hf + hl
                    S.op("pool", lambda e, hl=hl, h=h: e.dma_start(
                        out=q_aug[hl][72:75, :].rearrange("p (b i) -> p b i", b=4),
                        in_=C["qalibi"][8 + h, :, :].unsqueeze(1).to_broadcast([3, 4, 512])), writes=[r_qst], dma=True)
                    S.op("pool", lambda e, hl=hl: e.dma_start(out=q_aug[hl][64:72, :], in_=C["zeros32"][0:8, :]),
                         writes=r_qs[hl], dma=True)
                    S.op("pool", lambda e, hl=hl: e.dma_start(out=k_aug[hl][64:72, :], in_=C["e8"][:, :]),
                         writes=[r_k[hl]], dma=True)
                    S.op("pool", lambda e, hl=hl: e.dma_start(out=k_aug[hl][72:75, :], in_=C["ones3"][:, :]),
                         writes=[r_k[hl]], dma=True)
                prs = []
                for hp in range(2):
                    hA, hB = 2 * hp, 2 * hp + 1
                    prs.append((MOBA_COLS["q"] + 64 * (4 * hf + hA), MOBA_COLS["q"] + 64 * (4 * hf + hB),
                                lambda b, hA=hA: q_aug[hA][0:64, b * 512:(b + 1) * 512], lambda b, hA=hA: r_q[hA][b],
                                lambda b, hB=hB: q_aug[hB][0:64, b * 512:(b + 1) * 512], lambda b, hB=hB: r_q[hB][b],
                                gtab[:, 2:3]))
                for hp in range(2):
                    hA, hB = 2 * hp, 2 * hp + 1
                    prs.append((MOBA_COLS["k"] + 64 * (4 * hf + hA), MOBA_COLS["k"] + 64 * (4 * hf + hB),
                                lambda b, hA=hA: k_aug[hA][0:64, b * 512:(b + 1) * 512], lambda b, hA=hA: r_k[hA],
                                lambda b, hB=hB: k_aug[hB][0:64, b * 512:(b + 1) * 512], lambda b, hB=hB: r_k[hB],
                                gtab[:, 3:4]))
                proj_fm_pairs(prs)
                for hl in range(4):
                    S.op("dve", lambda e, hl=hl: e.tensor_reduce(
                        out=kmean_f[:, hl, :], in_=k_aug[hl][0:64, :].rearrange("p (n k) -> p n k", k=256),
                        axis=AX.X, op=ALU.add), reads=[r_k[hl]], writes=[r_km])
                S.op("dve", lambda e: e.tensor_scalar(out=kmean_b, in0=kmean_f, scalar1=1.0 / 256, scalar2=None,
                                                      op0=ALU.mult), reads=[r_km], writes=[r_km])
                w, r_w = load_w(win_cols(MOBA_COLS["v"] + 256 * hf, 256), 256)
                for i in range(16):
                    ps, r_ps = ps_ring.next()
                    fns = [lambda e, k=k, ps=ps, i=i, w=w: e.matmul(ps[:, 0:256], lhsT=hT[:, k, i * 128:(i + 1) * 128],
                                                                    rhs=w[:, k, 0:256], start=(k == 0), stop=(k == 7))
                           for k in range(8)]
                    S.op("pe", fns, reads=[r_w, r_hT[i]], writes=[r_ps])
                    S.op("act", lambda e, ps=ps, i=i: e.copy(out=vmA[:, i, :, 0:64],
                                                             in_=ps[:, 0:256].rearrange("p (h d) -> p h d", d=64)),
                         reads=[r_ps], writes=[r_vm])
                for tt in range(16):
                    cur = tt // 2
                    if tt >= 8:
                        ps, r_ps = misc_ring.next()
                        fns = [lambda e, hl=hl, ps=ps, tt=tt: e.matmul(ps[:, hl * 8:(hl + 1) * 8],
                                                                       lhsT=q_aug[hl][0:64, tt * 128:(tt + 1) * 128],
                                                                       rhs=kmean_b[:, hl, :], start=True, stop=True)
                               for hl in range(4)]
                        S.op("pe", fns, reads=[r_km] + [r_q[hl][tt // 4] for hl in range(4)], writes=[r_ps])
                        sc, r_sc = tmpf_ring.next()
                        for hl in range(4):
                            S.op("dve", lambda e, hl=hl, ps=ps, sc=sc, tt=tt: e.tensor_tensor(
                                out=sc[:, hl * 8:(hl + 1) * 8], in0=ps[:, hl * 8:(hl + 1) * 8], in1=moba_add[:, tt, :],
                                op=ALU.add), reads=[r_ps, r_c], writes=[r_sc])
                    for hl in range(4):
                        tr, r_tr = trin_ring.next()
                        S.op("pool", lambda e, tr=tr, cur=cur: e.memset(tr[:, 64 + cur:65 + cur], 0.0), writes=[r_tr])
                        if cur < 7:
                            S.op("pool", lambda e, tr=tr, cur=cur: e.memset(tr[:, 65 + cur:72], -BIG), writes=[r_tr])
                        if tt < 8:
                            if cur > 0:
                                S.op("pool", lambda e, tr=tr, cur=cur: e.memset(tr[:, 64:64 + cur], 0.0), writes=[r_tr])
                        else:
                            st, r_st = sm_ring.next()
                            S.op("dve", lambda e, sc=sc, st=st, hl=hl: e.max(out=st[:, 0:8], in_=sc[:, hl * 8:(hl + 1) * 8]),
                                 reads=[r_sc], writes=[r_st])
                            S.op("dve", lambda e, sc=sc, st=st, tr=tr, hl=hl, cur=cur: e.tensor_scalar(
                                out=tr[:, 64:64 + cur], in0=sc[:, hl * 8:hl * 8 + cur], scalar1=st[:, 2:3], scalar2=-BIG,
                                op0=ALU.is_lt, op1=ALU.mult), reads=[r_sc, r_st], writes=[r_tr])
                        ps2, r_ps2 = ps_ring.next()
                        psb = ps2[:].bitcast(BF16)
                        S.op("pe", lambda e, psb=psb, tr=tr: e.transpose(out=psb[0:72, 0:128], in_=tr[:, 0:72],
                                                                         identity=ident_b[:]),
                             reads=[r_tr, r_ident], writes=[r_ps2])
                        S.op("dve", lambda e, psb=psb, hl=hl, tt=tt: e.tensor_copy(
                            out=q_aug[hl][64:72, tt * 128:(tt + 1) * 128], in_=psb[64:72, 0:128]),
                            reads=[r_ps2], writes=[r_qs[hl][tt // 4]])

                def mk_evac_m(hl, qb):
                    def ev(acc3, r_acc):
                        tts = slice(4 * qb, 4 * qb + 4)
                        st, r_st = sm_ring.next()
                        rd = st[:, 4:8].unsqueeze(2)
                        S.op("dve", lambda e: e.reciprocal(out=rd, in_=acc3[:, :, 64:65]), reads=[r_acc], writes=[r_st])
                        S.op("dve", lambda e: e.tensor_tensor(out=omb[:, tts, hl * 64:(hl + 1) * 64], in0=acc3[:, :, 0:64],
                                                              in1=rd.to_broadcast([128, 4, 64]), op=ALU.mult),
                             reads=[r_acc, r_st], writes=r_omb[4 * qb:4 * qb + 4])
                    return ev

                rounds = []
                for hl in range(4):
                    h = 4 * hf + hl
                    for qb in range(4):
                        rounds.append(dict(q=q_aug[hl], r_q=[r_q[hl][qb], r_qs[hl][qb], r_qst], krows=75, kT=k_aug[hl],
                                           r_k=r_k[hl], qb=qb, tiles=causal_tiles(qb),
                                           V=lambda kt, hl=hl: vmA[:, kt, hl, :], r_v=r_vm, nv=65,
                                           bias=lambda kt, h=h, qb=qb: abias[:, (8 + h) * 19 + (kt - 4 * qb + 15):(8 + h) * 19 + (kt - 4 * qb + 15) + 1],
                                           evac=mk_evac_m(hl, qb)))
                run_rounds(rounds)
                for tt in range(16):
                    transpose_tile(omb[:, tt, :], r_omb[tt], omT[:, 2 * hf:2 * hf + 2, tt * 128:(tt + 1) * 128],
                                   r_omT[tt], nk=2, evac="dve", ring=misc_ring)

            if dbg and s == 0 and l == 0:
                for nm, src, rr in (("onT", onT, r_onT), ("omT", omT, r_omT)):
                    if nm in dbg_d:
                        S.op("pool", lambda e, nm=nm, src=src: e.dma_start(
                            out=dbg_d[nm][:, :].rearrange("p (a b) -> p a b", a=4), in_=src), reads=rr,
                            writes=[Res()], dma=True)

            S.barrier()
            A.off = region0
            yT = A.alloc([128, 8, T], BF16)
            r_yT = [Res() for _ in range(8)]
            wout = A.alloc([128, 8, D], BF16)
            r_wout = Res()
            S.op("pool", lambda e: e.dma_start(out=wout, in_=wout_d[l, :, :].rearrange("(k p) c -> p k c", p=128)),
                 writes=[r_wout], dma=True)
            for oc in range(8):
                wgn, r_wgn = load_w(win_cols(GATE_N + 128 * oc, 128), 128)
                wgm, r_wgm = load_w(win_cols(GATE_M + 128 * oc, 128), 128)
                wu, r_wu = wch_ring.next()
                S.op("pool", lambda e, wu=wu, oc=oc: e.dma_start(
                    out=wu[:, 0:4, 0:128], in_=wupn_d[l, :, oc * 128:(oc + 1) * 128].rearrange("(k p) c -> p k c", p=128)),
                    writes=[r_wu], dma=True)
                S.op("pool", lambda e, wu=wu, oc=oc: e.dma_start(
                    out=wu[:, 4:8, 0:128], in_=wupm_d[l, :, oc * 128:(oc + 1) * 128].rearrange("(k p) c -> p k c", p=128)),
                    writes=[r_wu], dma=True)
                for b in range(4):
                    bs = slice(b * 512, (b + 1) * 512)
                    res = []
                    for (wg, r_wg, oT, r_oT, ko) in ((wgn, r_wgn, onT, r_onT, 0), (wgm, r_wgm, omT, r_omT, 4)):
                        pg, r_pg = ps_ring.next()
                        fns = [lambda e, k=k, pg=pg, wg=wg, bs=bs: e.matmul(pg[:], lhsT=wg[:, k, 0:128], rhs=hT[:, k, bs],
                                                                     start=(k == 0), stop=(k == 7)) for k in range(8)]
                        S.op("pe", fns, reads=[r_wg] + r_hT[4 * b:4 * b + 4], writes=[r_pg])
                        pu, r_pu = ps_ring.next()
                        fns = [lambda e, k=k, pu=pu, oT=oT, ko=ko, wu=wu, bs=bs: e.matmul(pu[:], lhsT=wu[:, ko + k, 0:128], rhs=oT[:, k, bs],
                                                                            start=(k == 0), stop=(k == 3)) for k in range(4)]
                        S.op("pe", fns, reads=[r_wu] + r_oT[4 * b:4 * b + 4], writes=[r_pu])
                        sg, r_sg = tmpf_ring.next()
                        S.op("act", lambda e, pg=pg, sg=sg: e.activation(out=sg, in_=pg[:], func=AF.Sigmoid),
                             reads=[r_pg], writes=[r_sg])
                        S.op("dve", lambda e, pu=pu, sg=sg: e.tensor_tensor(out=sg, in0=sg, in1=pu[:], op=ALU.mult),
                             reads=[r_pu, r_sg], writes=[r_sg])
                        res.append((sg, r_sg))
                    S.op("dve", lambda e, a=res[0][0], b_=res[1][0], oc=oc, bs=bs: e.tensor_tensor(
                        out=yT[:, oc, bs], in0=a, in1=b_, op=ALU.add), reads=[res[0][1], res[1][1]], writes=[r_yT[oc]])
            for i in range(16):
                tt = tt0 + i
                xa, r_xa = xt_ring.next()
                S.op("sp", lambda e, xa=xa, tt=tt: e.dma_start(out=xa, in_=src_d[tt * 128:(tt + 1) * 128, :]),
                     reads=[r_src[tt]], writes=[r_xa], dma=True)
                for h2 in range(2):
                    po, r_po = ps_ring.next()
                    fns = [lambda e, oc=oc, po=po, i=i, h2=h2: e.matmul(po[:], lhsT=yT[:, oc, i * 128:(i + 1) * 128],
                                                                        rhs=wout[:, oc, h2 * 512:(h2 + 1) * 512],
                                                                        start=(oc == 0), stop=(oc == 7)) for oc in range(8)]
                    S.op("pe", fns, reads=r_yT + [r_wout], writes=[r_po])
                    S.op("dve", lambda e, po=po, xa=xa, h2=h2: e.tensor_tensor(
                        out=xa[:, h2 * 512:(h2 + 1) * 512], in0=po[:], in1=xa[:, h2 * 512:(h2 + 1) * 512], op=ALU.add),
                        reads=[r_po, r_xa], writes=[r_xa])
                S.op("sp", lambda e, xa=xa, tt=tt: e.dma_start(out=y_d[tt * 128:(tt + 1) * 128, :], in_=xa),
                     reads=[r_xa], writes=[r_y[tt]], dma=True)

    cur_d, cur_r = x_d, r_x
    for l in range(depth):
        if f"ffn{2 * l}" in phases or "all" in phases:
            ffn_phase(l, 0, cur_d, cur_r)
            cur_d, cur_r = y_d, r_y
        if f"mix{l}" in phases or "all" in phases:
            mix_phase(l, cur_d, cur_r)
            cur_d, cur_r = y_d, r_y
        if f"ffn{2 * l + 1}" in phases or "all" in phases:
            ffn_phase(l, 1, cur_d, cur_r)
            cur_d, cur_r = y_d, r_y

    S.barrier()
    S.finish("sp", r_y)
    sems = [es.enter_context(nc.semaphore(f"s{i}")) for i in range(S.nsem)]
    S.emit(nc, sems)
    es.close()
    return nc, S


def make_in_maps(inputs, nseq, ncores):
    x = np.ascontiguousarray(inputs["x"], dtype=np.float32).reshape(-1, nseq * T, D)
    consts = make_consts()
    shared = {}
    for k in ("norm_g", "ffn_w1", "ffn_w3", "ffn_w2", "w_in", "g_qk_nsa", "g_qk_moba", "cmp_w1", "cmp_w2", "w_up_nsa", "w_up_moba",
              "w_out"):
        shared[k] = np.ascontiguousarray(inputs[k], dtype=np.float32)
    shared["g_qk_nsaT"] = np.ascontiguousarray(np.transpose(np.asarray(inputs["g_qk_nsa"], np.float32), (0, 2, 1)))
    shared["g_qk_mobaT"] = np.ascontiguousarray(np.transpose(np.asarray(inputs["g_qk_moba"], np.float32), (0, 2, 1)))
    shared["cmp_posT"] = np.ascontiguousarray(np.transpose(np.asarray(inputs["cmp_pos"], np.float32), (0, 1, 3, 2)))
    shared.update(consts)
    in_maps = []
    for c in range(ncores):
        m = {"x": x[c]}
        m.update(shared)
        in_maps.append(m)
    return in_maps


_CACHE = {}


def kernel(**inputs):
    nseq = 16 // NCORES
    if "full" not in _CACHE:
        _CACHE["full"] = build_program(nseq=nseq, phases=("all",))
    nc, S = _CACHE["full"]
    in_maps = make_in_maps(inputs, nseq, NCORES)
    res = run_bass_kernel_spmd(nc, in_maps, core_ids=list(range(NCORES)))
    y = np.stack([np.asarray(r["y"]) for r in res.results], axis=0)
    return y.reshape(16, T, D).astype(np.float32)
```

```python
import numpy as np
from contextlib import ExitStack
import concourse.bass as bass
import concourse.mybir as mybir
from concourse.bass_utils import run_bass_kernel_spmd

F32 = mybir.dt.float32
BF16 = mybir.dt.bfloat16
AF = mybir.ActivationFunctionType
ALU = mybir.AluOpType
AX = mybir.AxisListType

NCORES = 8
D = 1024
T = 2048
DFF = 2816
NF = DFF // 128
DEPTH = 2
EPS = 1e-6


class Res:
    __slots__ = ("w", "r", "name")

    def __init__(self, name=""):
        self.w = None
        self.r = {}
        self.name = name


class _Eng:
    def __init__(self, name, sem):
        self.name = name
        self.sem = sem
        self.count = 0
        self.known = {}
        self.ops = []
        self.dma_sems = []
        self.dma_count = 0


class Sched:
    NDMA = 8

    def __init__(self):
        self.nsem = 0
        self.eng = {}
        for n in ("pe", "act", "dve", "pool", "sp"):
            self.eng[n] = _Eng(n, self._newsem())
        for n in ("sp", "pool", "act"):
            self.eng[n].dma_sems = [self._newsem() for _ in range(self.NDMA)]

    def _newsem(self):
        s = self.nsem
        self.nsem += 1
        return s

    def op(self, eng, fns, reads=(), writes=(), dma=False):
        E = self.eng[eng]
        if not isinstance(fns, (list, tuple)):
            fns = [fns]
        need = {}

        def req(tok):
            if tok is None:
                return
            s, v, clk = tok
            o = need.get(s)
            if o is None or o[0] < v:
                need[s] = (v, clk)

        for r in reads:
            req(r.w)
        for w in writes:
            req(w.w)
            for s, (v, clk) in w.r.items():
                req((s, v, clk))
        implied = {}
        for s, (v, clk) in need.items():
            for cs, cv in clk.items():
                if implied.get(cs, 0) < cv:
                    implied[cs] = cv
        waits = []
        known = E.known
        for s, (v, clk) in need.items():
            if known.get(s, 0) >= v or implied.get(s, 0) >= v:
                continue
            if eng == "pe" and s == E.sem:
                continue
            waits.append((s, v))
        for s, v in implied.items():
            if known.get(s, 0) < v:
                known[s] = v
        for s, (v, clk) in need.items():
            if known.get(s, 0) < v:
                known[s] = v
        if dma:
            j = E.dma_count
            E.dma_count += 1
            s = E.dma_sems[j % self.NDMA]
            prev = 16 * (j // self.NDMA)
            if prev > 0 and known.get(s, 0) < prev:
                waits.append((s, prev))
                known[s] = prev
            val = prev + 16
            inc = 16
        else:
            E.count += 1
            s = E.sem
            val = E.count
            inc = 1
        clk = dict(known)
        tok = (s, val, clk)
        E.ops.append((waits, list(fns), s, inc))
        for r in reads:
            o = r.r.get(s)
            if o is None or o[0] < val:
                r.r[s] = (val, clk)
        for w in writes:
            w.w = tok
            w.r = {}
        return tok

    def finish(self, eng, resources):
        E = self.eng[eng]
        need = {}
        for r in resources:
            toks = []
            if r.w is not None:
                toks.append(r.w)
            for s, (v, clk) in r.r.items():
                toks.append((s, v, clk))
            for s, v, clk in toks:
                if need.get(s, 0) < v:
                    need[s] = v
        waits = [(s, v) for s, v in need.items() if E.known.get(s, 0) < v]
        E.ops.append((waits, [], None, 0))

    def emit(self, nc, sems):
        def replay(E):
            def body(e):
                for waits, fns, s, inc in E.ops:
                    for ws, wv in waits:
                        e.wait_ge(sems[ws], wv)
                    if not fns:
                        continue
                    for fn in fns[:-1]:
                        fn(e)
                    fns[-1](e).then_inc(sems[s], inc)
            return body

        with nc.Block() as block:
            block.sync(replay(self.eng["sp"]))
            block.scalar(replay(self.eng["act"]))
            block.vector(replay(self.eng["dve"]))
            block.gpsimd(replay(self.eng["pool"]))
            block.tensor(replay(self.eng["pe"]))


class Ring:
    def __init__(self, items):
        self.items = items
        self.i = 0

    def next(self):
        it = self.items[self.i % len(self.items)]
        self.i += 1
        return it


def _barrier(self):
    toks = {}
    for E in self.eng.values():
        if E.count > 0:
            toks[E.sem] = E.count
        for i, s in enumerate(E.dma_sems):
            if E.dma_count > i:
                toks[s] = 16 * ((E.dma_count - i + self.NDMA - 1) // self.NDMA)
    for E in self.eng.values():
        waits = []
        for s, v in toks.items():
            if E.known.get(s, 0) >= v:
                continue
            if E.name == "pe" and s == E.sem:
                continue
            waits.append((s, v))
            E.known[s] = v
        if waits:
            E.ops.append((waits, [], None, 0))


Sched.barrier = _barrier


class Arena:
    def __init__(self, t, nel):
        self.t = t
        self.nel = nel
        self.off = 0

    def alloc(self, shape, dt):
        n = 1
        for d in shape[1:]:
            n *= d
        sz = n * (2 if dt == F32 else 1)
        self.off = (self.off + 1) // 2 * 2
        o = self.off
        self.off += sz
        assert self.off <= self.nel, ("arena overflow", self.off, self.nel)
        ap = self.t[0:shape[0], o:o + sz]
        if dt == F32:
            ap = ap.bitcast(F32)
        if len(shape) == 3:
            ap = ap.rearrange("p (a b) -> p a b", a=shape[1])
        elif len(shape) == 4:
            ap = ap.rearrange("p (a b c) -> p a b c", a=shape[1], b=shape[2])
        return ap


BIG = 30000.0
NSA_COLS = dict(q=0, kc=512, vc=640, ks=768, vs=896, kw=1024, vw=1152, g=1280)
MOBA_COLS = dict(q=1304, k=1816, v=2328)
GATE_N, GATE_M = 2840, 3864
INC = 4888


def slopes_all():
    s = (2.0 ** (-8.0 * np.arange(1, 17) / 16)).astype(np.float32)
    return s[0::2].copy(), s[1::2].copy()


def _bf16_round(a):
    a = np.asarray(a, dtype=np.float32)
    u = a.view(np.uint32).astype(np.uint64)
    r = ((u + 0x7FFF + ((u >> 16) & 1)) >> 16) << 16
    return r.astype(np.uint32).view(np.float32)


def make_consts():
    c = {}
    c["ident"] = np.eye(128, dtype=np.float32)
    j = np.arange(128)[:, None]
    i = np.arange(128)[None, :]
    c["tri_c"] = np.where(j > i, -BIG, 0.0).astype(np.float32)
    c["tri_a"] = np.where(j <= i, -BIG, 0.0).astype(np.float32)
    c["ones64"] = np.ones((64, 64), np.float32)
    bd = np.zeros((128, 128), np.float32)
    bd[:64, :64] = 1.0
    bd[64:, 64:] = 1.0
    c["bd_ones"] = bd
    sn, sm = slopes_all()
    sl = np.concatenate([sn, sm])
    ab = np.zeros((128, 16, 19), np.float32)
    for h in range(16):
        for d in range(-15, 4):
            ab[:, h, d + 15] = sl[h].astype(np.float64) * (128 * d + np.arange(128))
    c["abias"] = ab.reshape(128, 16 * 19)
    cb = np.zeros((128, 8, 4), np.float32)
    cc = np.arange(128)
    for h in range(8):
        for qb in range(4):
            cb[:, h, qb] = sn[h].astype(np.float64) * (16 * cc + 15.5 - 512 * qb)
    c["cbias"] = cb.reshape(128, 32)
    qa = np.zeros((16, 3, 512), np.float32)
    for h in range(16):
        v = (-(sl[h].astype(np.float64)) * np.arange(512)).astype(np.float32)
        v1 = _bf16_round(v)
        v2 = _bf16_round(v - v1)
        v3 = _bf16_round(v - v1 - v2)
        qa[h, 0], qa[h, 1], qa[h, 2] = v1, v2, v3
    c["qalibi"] = qa
    key = np.arange(T)
    c["e32"] = (key[None, :] // 64 == np.arange(32)[:, None]).astype(np.float32)
    c["e8"] = (key[None, :] // 256 == np.arange(8)[:, None]).astype(np.float32)
    c["ones3"] = np.ones((3, T), np.float32)
    c["zeros32"] = np.zeros((32, T), np.float32)
    cm = np.zeros((128, T), np.float32)
    cidx = np.arange(128)[:, None]
    cm[:] = np.where(16 * cidx + 31 <= key[None, :], 0.0, -BIG)
    c["cmask"] = cm
    ci = np.arange(127)[:, None] * 16
    sj = np.arange(32)[None, :] * 64
    ov = np.clip(np.minimum(ci + 32, sj + 64) - np.maximum(ci, sj), 0, None)
    M = np.zeros((128, 32), np.float32)
    M[:127] = ov / 32.0
    c["cmp2slc"] = M
    blk = np.arange(32)[None, None, :]
    t = (np.arange(16)[None, :, None] * 128 + np.arange(128)[:, None, None])
    cur = t // 64
    forced = (blk == 0) | (blk == cur) | (blk == cur - 1)
    valid = blk <= cur
    c["nsa_mult"] = np.where(forced | ~valid, 0.0, 1.0).astype(np.float32).reshape(128, 16 * 32)
    c["nsa_add"] = np.where(forced, 1e9, np.where(valid, 0.0, -1e30)).astype(np.float32).reshape(128, 16 * 32)
    n8 = np.arange(8)[None, None, :]
    curm = t // 256
    c["moba_add"] = np.broadcast_to(np.where(n8 < curm, 0.0, -1e30), (128, 16, 8)).astype(np.float32).reshape(128, 128).copy()
    return c


CONST_SHAPES = dict(ident=[128, 128], tri_c=[128, 128], tri_a=[128, 128], ones64=[64, 64], bd_ones=[128, 128], abias=[128, 304],
                    cbias=[128, 32], qalibi=[16, 3, 512], e32=[32, T], e8=[8, T], ones3=[3, T], zeros32=[32, T],
                    cmask=[128, T], cmp2slc=[128, 32], nsa_mult=[128, 512], nsa_add=[128, 512], moba_add=[128, 128])


def build_program(nseq=2, phases=("all",), depth=DEPTH, dbg=None):
    nc = bass.Bass("TRN2", target_bir_lowering=False)
    NT = nseq * T
    NTT = NT // 128
    S = Sched()
    es = ExitStack()

    def dram_in(name, shape, dt=F32):
        return nc.dram_tensor(name, list(shape), dt, kind="ExternalInput").ap()

    x_d = dram_in("x", [NT, D])
    normg_d = dram_in("norm_g", [DEPTH, 3, D])
    w1_d = dram_in("ffn_w1", [DEPTH, 2, D, DFF])
    w3_d = dram_in("ffn_w3", [DEPTH, 2, D, DFF])
    w2_d = dram_in("ffn_w2", [DEPTH, 2, DFF, D])
    win_d = dram_in("w_in", [DEPTH, D, INC])
    gqn_d = dram_in("g_qk_nsa", [DEPTH, 4, 64])
    gqnT_d = dram_in("g_qk_nsaT", [DEPTH, 64, 4])
    gqm_d = dram_in("g_qk_moba", [DEPTH, 2, 64])
    gqmT_d = dram_in("g_qk_mobaT", [DEPTH, 64, 2])
    posT_d = dram_in("cmp_posT", [DEPTH, 2, 64, 32])
    cw1_d = dram_in("cmp_w1", [DEPTH, 2, 2048, 256])
    cw2_d = dram_in("cmp_w2", [DEPTH, 2, 256, 64])
    wupn_d = dram_in("w_up_nsa", [DEPTH, 512, D])
    wupm_d = dram_in("w_up_moba", [DEPTH, 512, D])
    wout_d = dram_in("w_out", [DEPTH, D, D])
    C = {k: dram_in(k, v) for k, v in CONST_SHAPES.items()}
    y_d = nc.dram_tensor("y", [NT, D], F32, kind="ExternalOutput").ap()
    dbg_d = {}
    if dbg:
        for k, shp in dbg.items():
            dbg_d[k] = nc.dram_tensor("dbg_" + k, list(shp), F32, kind="ExternalOutput").ap()

    def sb(name, shape, dt):
        return es.enter_context(nc.sbuf_tensor(name, list(shape), dt))

    banks = []
    for i in range(8):
        t = es.enter_context(nc.psum_tensor(f"ps{i}", [128, 512], F32))
        banks.append((t, Res(f"ps{i}")))
    ps_ring = Ring(banks)
    acc_banks = banks[0:4]
    acc_ring = Ring(banks[0:4])
    sc_ring = Ring(banks[4:7])
    misc_ring = Ring(banks[7:8])

    ident_b = sb("ident_b", [128, 128], BF16)
    r_ident = Res("ident")
    S.op("pool", lambda e: e.dma_start(out=ident_b[:], in_=C["ident"][:, :]), writes=[r_ident], dma=True)
    stat = [(sb(f"stat{i}", [128, 4], F32), Res(f"stat{i}")) for i in range(4)]
    stat_ring = Ring(stat)
    ARENA_EL = 105000
    arena_t = sb("arena", [128, ARENA_EL], BF16)
    A = Arena(arena_t, ARENA_EL)

    r_y = [Res(f"y{i}") for i in range(NTT)]
    r_x = [Res(f"x{i}") for i in range(NTT)]

    def rmsnorm_tile(x_ap, r_x_, g_ap, r_gres, out_ap, r_out, jk, r_jk):
        st, r_st = stat_ring.next()
        S.op("act", lambda e: e.activation(out=jk, in_=x_ap, func=AF.Square, accum_out=st[:, 0:1]),
             reads=[r_x_], writes=[r_jk, r_st])
        S.op("act", lambda e: e.activation(out=st[:, 1:2], in_=st[:, 0:1], func=AF.Sqrt, scale=1.0 / D, bias=EPS),
             reads=[r_st], writes=[r_st])
        S.op("dve", lambda e: e.reciprocal(out=st[:, 2:3], in_=st[:, 1:2]), reads=[r_st], writes=[r_st])
        S.op("dve", lambda e: e.scalar_tensor_tensor(out=out_ap, in0=x_ap, scalar=st[:, 2:3], in1=g_ap,
                                                     op0=ALU.mult, op1=ALU.mult),
             reads=[r_x_, r_st, r_gres], writes=[r_out])

    def transpose_tile(in_tile, r_in, out_ap3, r_out, nk=8, evac="act", ring=None):
        ps, r_ps = (ring or ps_ring).next()
        psb = ps[:].bitcast(BF16)
        fns = []
        for k in range(nk):
            fns.append(lambda e, k=k: e.transpose(out=psb[:, k * 128:(k + 1) * 128],
                                                  in_=in_tile[:, k * 128:(k + 1) * 128], identity=ident_b[:]))
        S.op("pe", fns, reads=[r_in, r_ident], writes=[r_ps])
        src = psb[:, 0:nk * 128].rearrange("p (k t) -> p k t", k=nk)
        if evac == "act":
            S.op("act", lambda e: e.copy(out=out_ap3, in_=src), reads=[r_ps], writes=[r_out])
        else:
            S.op("dve", lambda e: e.tensor_copy(out=out_ap3, in_=src), reads=[r_ps], writes=[r_out])

    def ffn_phase(l, j, src_d, r_src):
        S.barrier()
        A.off = 0
        w1_sb = A.alloc([128, 8, DFF], BF16)
        w3_sb = A.alloc([128, 8, DFF], BF16)
        w2_sb = A.alloc([128, NF, D], BF16)
        r_w1 = [Res() for f in range(NF)]
        r_w3 = [Res() for f in range(NF)]
        r_w2 = [Res() for f in range(NF)]
        g_rep = A.alloc([128, D], F32)
        r_g = Res()
        xn_ring = Ring([(A.alloc([128, D], F32), Res()) for i in range(2)])
        xr_ring = Ring([(A.alloc([128, D], F32), Res()) for i in range(2)])
        hb = [(A.alloc([128, D], BF16), Res()) for i in range(4)]
        hT2 = [A.alloc([128, 8, 512], BF16) for i in range(2)]
        r_hT2 = [[Res() for i in range(4)] for _ in range(2)]
        gT = A.alloc([128, NF, 512], BF16)
        r_gT = [Res() for f in range(NF)]
        su_ring = Ring([(A.alloc([128, 512], F32), Res()) for i in range(2)])
        junk = (A.alloc([128, D], BF16), Res())
        NB = NT // 512

        S.op("pool", lambda e: e.dma_start(out=g_rep, in_=normg_d[l, 2 * j, :].partition_broadcast(128)),
             writes=[r_g], dma=True)
        for f in range(NF):
            S.op("pool", lambda e, f=f: e.dma_start(
                out=w1_sb[:, :, f * 128:(f + 1) * 128],
                in_=w1_d[l, j, :, f * 128:(f + 1) * 128].rearrange("(k p) c -> p k c", p=128)),
                writes=[r_w1[f]], dma=True)
            S.op("pool", lambda e, f=f: e.dma_start(
                out=w3_sb[:, :, f * 128:(f + 1) * 128],
                in_=w3_d[l, j, :, f * 128:(f + 1) * 128].rearrange("(k p) c -> p k c", p=128)),
                writes=[r_w3[f]], dma=True)
        for f in range(NF):
            S.op("pool", lambda e, f=f: e.dma_start(out=w2_sb[:, f, :], in_=w2_d[l, j, f * 128:(f + 1) * 128, :]),
                 writes=[r_w2[f]], dma=True)

        def norm_part(blk):
            for i in range(4):
                tt = blk * 4 + i
                xa, r_xa = xn_ring.next()
                S.op("sp", lambda e, xa=xa, tt=tt: e.dma_start(out=xa, in_=src_d[tt * 128:(tt + 1) * 128, :]),
                     reads=[r_src[tt]], writes=[r_xa], dma=True)
                hbt, r_hb = hb[i]
                rmsnorm_tile(xa, r_xa, g_rep, r_g, hbt, r_hb, junk[0], junk[1])

        def transpose_part(blk):
            hT = hT2[blk % 2]
            for i in range(4):
                hbt, r_hb = hb[i]
                transpose_tile(hbt, r_hb, hT[:, :, i * 128:(i + 1) * 128], r_hT2[blk % 2][i])

        def up_part(blk):
            hT = hT2[blk % 2]
            r_hT = r_hT2[blk % 2]
            for f in range(NF):
                pu, r_pu = ps_ring.next()
                pv, r_pv = ps_ring.next()
                fns = [lambda e, k=k, f=f, pu=pu: e.matmul(pu[:], lhsT=w1_sb[:, k, f * 128:(f + 1) * 128],
                                                           rhs=hT[:, k, :], start=(k == 0), stop=(k == 7)) for k in range(8)]
                S.op("pe", fns, reads=[r_w1[f]] + r_hT, writes=[r_pu])
                fns = [lambda e, k=k, f=f, pv=pv: e.matmul(pv[:], lhsT=w3_sb[:, k, f * 128:(f + 1) * 128],
                                                           rhs=hT[:, k, :], start=(k == 0), stop=(k == 7)) for k in range(8)]
                S.op("pe", fns, reads=[r_w3[f]] + r_hT, writes=[r_pv])
                s_t, r_s = su_ring.next()
                S.op("act", lambda e, s_t=s_t, pu=pu: e.activation(out=s_t, in_=pu[:], func=AF.Silu),
                     reads=[r_pu], writes=[r_s])
                S.op("dve", lambda e, s_t=s_t, pv=pv, f=f: e.tensor_tensor(out=gT[:, f, :], in0=s_t, in1=pv[:],
                                                                             op=ALU.mult),
                     reads=[r_s, r_pv], writes=[r_gT[f]])

        def down_part(blk):
            for i in range(4):
                tt = blk * 4 + i
                xa, r_xa = xr_ring.next()
                S.op("sp", lambda e, xa=xa, tt=tt: e.dma_start(out=xa, in_=src_d[tt * 128:(tt + 1) * 128, :]),
                     reads=[r_src[tt]], writes=[r_xa], dma=True)
                for h in range(2):
                    po, r_po = ps_ring.next()
                    fns = [lambda e, f=f, po=po, i=i, h=h: e.matmul(
                        po[:], lhsT=gT[:, f, i * 128:(i + 1) * 128], rhs=w2_sb[:, f, h * 512:(h + 1) * 512],
                        start=(f == 0), stop=(f == NF - 1)) for f in range(NF)]
                    S.op("pe", fns, reads=r_gT + r_w2, writes=[r_po])
                    S.op("dve", lambda e, po=po, xa=xa, h=h: e.scalar_tensor_tensor(
                        out=xa[:, h * 512:(h + 1) * 512], in0=po[:], scalar=0.5, in1=xa[:, h * 512:(h + 1) * 512],
                        op0=ALU.mult, op1=ALU.add), reads=[r_po, r_xa], writes=[r_xa])
                S.op("sp", lambda e, xa=xa, tt=tt: e.dma_start(out=y_d[tt * 128:(tt + 1) * 128, :], in_=xa),
                     reads=[r_xa], writes=[r_y[tt]], dma=True)

        norm_part(0)
        transpose_part(0)
        for blk in range(NB):
            if blk + 1 < NB:
                norm_part(blk + 1)
            up_part(blk)
            if blk + 1 < NB:
                transpose_part(blk + 1)
            down_part(blk)

    def mix_phase(l, src_d, r_src):
        S.barrier()
        A.off = 0
        r_c = Res()
        tri_c = A.alloc([128, 128], BF16)
        tri_a = A.alloc([128, 128], BF16)
        ones64 = A.alloc([64, 64], BF16)
        bd_ones = A.alloc([128, 128], BF16)
        gtab = A.alloc([128, 4], F32)
        abias = A.alloc([128, 304], F32)
        cbias = A.alloc([128, 32], F32)
        cmask = A.alloc([128, T], BF16)
        nsa_mult = A.alloc([128, 16, 32], F32)
        nsa_add = A.alloc([128, 16, 32], F32)
        moba_add = A.alloc([128, 16, 8], F32)
        gq_n = A.alloc([64, 4], F32)
        gq_m = A.alloc([64, 2], F32)
        gkc_rep = A.alloc([128, 64], F32)
        g_rep = A.alloc([128, D], F32)
        for dst, src in ((tri_c, C["tri_c"][:, :]), (tri_a, C["tri_a"][:, :]), (ones64, C["ones64"][:, :]), (bd_ones, C["bd_ones"][:, :]),
                         (gtab[0:64, 0:1], gqn_d[l, 0, :].unsqueeze(1)), (gtab[64:128, 0:1], gqn_d[l, 0, :].unsqueeze(1)),
                         (gtab[0:64, 1:2], gqn_d[l, 2, :].unsqueeze(1)), (gtab[64:128, 1:2], gqn_d[l, 3, :].unsqueeze(1)),
                         (gtab[0:64, 2:3], gqm_d[l, 0, :].unsqueeze(1)), (gtab[64:128, 2:3], gqm_d[l, 0, :].unsqueeze(1)),
                         (gtab[0:64, 3:4], gqm_d[l, 1, :].unsqueeze(1)), (gtab[64:128, 3:4], gqm_d[l, 1, :].unsqueeze(1)),
                         (abias, C["abias"][:, :]), (cbias, C["cbias"][:, :]), (cmask, C["cmask"][:, :]),
                         (nsa_mult, C["nsa_mult"][:, :].rearrange("p (a b) -> p a b", a=16)),
                         (nsa_add, C["nsa_add"][:, :].rearrange("p (a b) -> p a b", a=16)),
                         (moba_add, C["moba_add"][:, :].rearrange("p (a b) -> p a b", a=16)),
                         (gq_n, gqnT_d[l, :, :]), (gq_m, gqmT_d[l, :, :]),
                         (gkc_rep, gqn_d[l, 1, :].partition_broadcast(128)),
                         (g_rep, normg_d[l, 1, :].partition_broadcast(128))):
            S.op("pool", lambda e, dst=dst, src=src: e.dma_start(out=dst, in_=src), writes=[r_c], dma=True)
        S.op("dve", lambda e: e.tensor_scalar(out=gq_n[:, 0:1], in0=gq_n[:, 0:1], scalar1=0.125, scalar2=None,
                                              op0=ALU.mult), reads=[r_c], writes=[r_c])
        S.op("dve", lambda e: e.tensor_scalar(out=gq_m[:, 0:1], in0=gq_m[:, 0:1], scalar1=0.125, scalar2=None,
                                              op0=ALU.mult), reads=[r_c], writes=[r_c])
        S.op("dve", lambda e: e.tensor_scalar(out=gtab[:, 0:1], in0=gtab[:, 0:1], scalar1=0.125, scalar2=None,
                                              op0=ALU.mult), reads=[r_c], writes=[r_c])
        S.op("dve", lambda e: e.tensor_scalar(out=gtab[:, 2:3], in0=gtab[:, 2:3], scalar1=0.125, scalar2=None,
                                              op0=ALU.mult), reads=[r_c], writes=[r_c])

        hT = A.alloc([128, 8, T], BF16)
        r_hT = [Res() for _ in range(16)]
        onT = A.alloc([128, 4, T], BF16)
        omT = A.alloc([128, 4, T], BF16)
        r_onT = [Res() for _ in range(16)]
        r_omT = [Res() for _ in range(16)]
        gsig = A.alloc([128, 16, 24], F32)
        r_gsig = Res()
        wch_ring = Ring([(A.alloc([128, 8, 256], BF16), Res()) for _ in range(3)])
        pT_ring = Ring([(A.alloc([128, 512], BF16), Res()) for _ in range(3)])
        tmpf_ring = Ring([(A.alloc([128, 512], F32), Res()) for _ in range(3)])
        sqb_ring = Ring([(A.alloc([128, 512], BF16), Res()) for _ in range(3)])
        xt_ring = Ring([(A.alloc([128, D], F32), Res()) for _ in range(3)])
        hb_ring = Ring([(A.alloc([128, D], BF16), Res()) for _ in range(3)])
        junk = (A.alloc([128, D], BF16), Res())
        sm_ring = Ring([(A.alloc([128, 16], F32), Res()) for _ in range(8)])
        region0 = A.off

        def load_w(src_ap3, ncols):
            w, r_w = wch_ring.next()
            S.op("pool", lambda e: e.dma_start(out=w[:, :, 0:ncols], in_=src_ap3), writes=[r_w], dma=True)
            return w, r_w

        def win_cols(c0, n):
            return win_d[l, :, c0:c0 + n].rearrange("(k p) c -> p k c", p=128)

        def proj_fm_pairs(pairs):
            items = [(pi, b) for pi in range(len(pairs)) for b in range(4)]
            wts = {}
            stt = {}

            def ensure_w(pi):
                if pi < len(pairs) and pi not in wts:
                    c0A, c0B = pairs[pi][0], pairs[pi][1]
                    w, r_w = wch_ring.next()
                    if c0B == c0A + 64:
                        S.op("pool", lambda e: e.dma_start(out=w[:, :, 0:128], in_=win_cols(c0A, 128)), writes=[r_w], dma=True)
                    else:
                        S.op("pool", lambda e: e.dma_start(out=w[:, :, 0:64], in_=win_cols(c0A, 64)), writes=[r_w], dma=True)
                        S.op("pool", lambda e: e.dma_start(out=w[:, :, 64:128], in_=win_cols(c0B, 64)), writes=[r_w], dma=True)
                    wts[pi] = (w, r_w)

            def mm(i):
                pi, b = items[i]
                ensure_w(pi)
                if b == 0:
                    ensure_w(pi + 1)
                w, r_w = wts[pi]
                ps, r_ps = ps_ring.next()
                fns = [lambda e, k=k: e.matmul(ps[:, :], lhsT=w[:, k, 0:128], rhs=hT[:, k, b * 512:(b + 1) * 512],
                                               start=(k == 0), stop=(k == 7)) for k in range(8)]
                S.op("pe", fns, reads=[r_w] + r_hT[4 * b:4 * b + 4], writes=[r_ps])
                c0A, c0B, dA, rA, dB, rB, gcol = pairs[pi]
                if gcol is None:
                    S.op("act", lambda e: e.copy(out=dA(b), in_=ps[0:64, :]), reads=[r_ps], writes=[rA(b)])
                    S.op("act", lambda e: e.copy(out=dB(b), in_=ps[64:128, :]), reads=[r_ps], writes=[rB(b)])
                    stt[i] = None
                    return
                sq, r_sq = sqb_ring.next()
                S.op("act", lambda e: e.activation(out=sq[:, :], in_=ps[:, :], func=AF.Square), reads=[r_ps], writes=[r_sq])
                stt[i] = (ps, r_ps, sq, r_sq, b)

            def rest(i):
                if stt[i] is None:
                    return
                ps, r_ps, sq, r_sq, b = stt[i]
                c0A, c0B, dA, rA, dB, rB, gcol = pairs[items[i][0]]
                p2, r_p2 = ps_ring.next()
                S.op("pe", lambda e: e.matmul(p2[:, :], lhsT=bd_ones[:, :], rhs=sq[:, :], start=True, stop=True),
                     reads=[r_sq, r_c], writes=[r_p2])
                tf, r_tf = tmpf_ring.next()
                S.op("act", lambda e: e.activation(out=tf[:, :], in_=p2[:, :], func=AF.Ln, scale=1.0 / 64, bias=EPS),
                     reads=[r_p2], writes=[r_tf])
                S.op("act", lambda e: e.activation(out=tf[:, :], in_=tf[:, :], func=AF.Exp, scale=-0.5),
                     reads=[r_tf], writes=[r_tf])
                S.op("dve", lambda e: e.scalar_tensor_tensor(out=dA(b), in0=ps[0:64, :], scalar=gcol[0:64, :], in1=tf[0:64, :],
                                                             op0=ALU.mult, op1=ALU.mult),
                     reads=[r_ps, r_tf, r_c], writes=[rA(b)])
                S.op("dve", lambda e: e.scalar_tensor_tensor(out=dB(b), in0=ps[64:128, :], scalar=gcol[64:128, :],
                                                             in1=tf[64:128, :], op0=ALU.mult, op1=ALU.mult),
                     reads=[r_ps, r_tf, r_c], writes=[rB(b)])

            mm(0)
            for i in range(len(items)):
                if i + 1 < len(items):
                    mm(i + 1)
                rest(i)

        def causal_tiles(qb):
            tl = []
            for kt in range(4 * qb + 4):
                c = kt - 4 * qb
                if c < 0:
                    tl.append((kt, 0, 512, None, None))
                else:
                    tl.append((kt, 128 * c, 512, "c", c))
            return tl

        def window_tiles(qb):
            tl = []
            for c in (0, 1, 2, 3, -1, -2, -3, -4):
                kt = 4 * qb + c
                if kt < 0:
                    continue
                if c >= 0:
                    tl.append((kt, 128 * c, 512, "c", c))
                else:
                    m = 4 + c
                    tl.append((kt, 0, 128 * (m + 1), "a", m))
            return tl

        def run_rounds(rounds):
            items = []
            for R in rounds:
                for ti, tile in enumerate(R["tiles"]):
                    items.append((R, ti, tile))

            def emit_qk(it):
                R, ti, (kt, c0, c1, tri, tu) = it
                ps, r_ps = sc_ring.next()
                kp = R.get("kpart", 128)
                K = R["krows"]
                qb = R["qb"]
                fns = [lambda e: e.matmul(ps[0:kp, c0:c1], lhsT=R["kT"][0:K, kt * 128:kt * 128 + kp],
                                          rhs=R["q"][0:K, qb * 512 + c0:qb * 512 + c1], start=True,
                                          stop=(tri is None and "mask" not in R))]
                reads = [R["r_k"]] + R["r_q"] + [r_c]
                if "mask" in R:
                    fns.append(lambda e: e.matmul(ps[0:kp, c0:c1], lhsT=ident_b[0:kp, 0:kp],
                                                  rhs=R["mask"][0:kp, qb * 512 + c0:qb * 512 + c1],
                                                  start=False, stop=True))
                if tri is not None:
                    tm = tri_c if tri == "c" else tri_a
                    fns.append(lambda e: e.matmul(ps[:, 128 * tu:128 * tu + 128], lhsT=ident_b[:, :], rhs=tm[:, :],
                                                  start=False, stop=True))
                S.op("pe", fns, reads=reads + [r_ident], writes=[r_ps])
                return ps, r_ps

            pend = emit_qk(items[0]) if items else None
            for idx, it in enumerate(items):
                R, ti, (kt, c0, c1, tri, tu) = it
                ps, r_ps = pend
                if idx + 1 < len(items):
                    pend = emit_qk(items[idx + 1])
                kp = R.get("kpart", 128)
                pT, r_pT = pT_ring.next()
                bias_ap = R["bias"](kt)
                S.op("act", lambda e, ps=ps, pT=pT, bias_ap=bias_ap, kp=kp, c0=c0, c1=c1: e.activation(
                    out=pT[0:kp, c0:c1], in_=ps[0:kp, c0:c1], func=AF.Exp, bias=bias_ap, scale=1.0),
                    reads=[r_ps, r_c], writes=[r_pT])
                us = [u for u in range(4) if c0 <= 128 * u < c1]
                nv = R["nv"]
                if ti == 0:
                    R["acc"] = acc_ring.next()
                acc, r_acc = R["acc"]
                fns = []
                V = R["V"](kt)
                nt = len(R["tiles"])
                for u in us:
                    fns.append(lambda e, u=u, acc=acc, V=V, first=(ti == 0 and u == us[0]),
                               last=(ti == nt - 1 and u == us[-1]), pT=pT, kp=kp:
                               e.matmul(acc[:, 128 * u:128 * u + nv], lhsT=pT[0:kp, 128 * u:128 * u + 128], rhs=V,
                                        start=first, stop=last))
                S.op("pe", fns, reads=[r_pT, R["r_v"]], writes=[r_acc])
                if ti == nt - 1:
                    R["evac"](acc[:].rearrange("p (u c) -> p u c", u=4), r_acc)

        sn_, sm_ = slopes_all()

        def dump(name, ap, reads):
            if name in dbg_d:
                dst = dbg_d[name][:, :]
                if len(ap.shape) == 3:
                    dst = dst.rearrange("p (a b) -> p a b", a=ap.shape[1])
                S.op("pool", lambda e: e.dma_start(out=dst[0:ap.shape[0]], in_=ap), reads=reads, writes=[Res()], dma=True)

        for s in range(nseq):
            tt0 = s * 16
            prev = None
            for i in range(16):
                xa, r_xa = xt_ring.next()
                S.op("sp", lambda e, xa=xa, i=i, tt0=tt0: e.dma_start(out=xa, in_=src_d[(tt0 + i) * 128:(tt0 + i + 1) * 128, :]),
                     reads=[r_src[tt0 + i]], writes=[r_xa], dma=True)
                hbt, r_hb = hb_ring.next()
                st, r_st = stat_ring.next()
                jk, r_jk = junk
                S.op("act", lambda e, xa=xa, st=st: e.activation(out=jk, in_=xa, func=AF.Square, accum_out=st[:, 0:1]),
                     reads=[r_xa], writes=[r_jk, r_st])
                S.op("act", lambda e, st=st: e.activation(out=st[:, 1:2], in_=st[:, 0:1], func=AF.Sqrt, scale=1.0 / D, bias=EPS),
                     reads=[r_st], writes=[r_st])
                if prev is not None:
                    prev()
                S.op("dve", lambda e, st=st: e.reciprocal(out=st[:, 2:3], in_=st[:, 1:2]), reads=[r_st], writes=[r_st])
                S.op("dve", lambda e, xa=xa, st=st, hbt=hbt: e.scalar_tensor_tensor(
                    out=hbt, in0=xa, scalar=st[:, 2:3], in1=g_rep, op0=ALU.mult, op1=ALU.mult),
                    reads=[r_xa, r_st, r_c], writes=[r_hb])
                ps, r_ps = ps_ring.next()
                psb = ps[:].bitcast(BF16)
                fns = [lambda e, k=k, psb=psb, hbt=hbt: e.transpose(out=psb[:, k * 128:(k + 1) * 128],
                                                                    in_=hbt[:, k * 128:(k + 1) * 128], identity=ident_b[:])
                       for k in range(8)]
                S.op("pe", fns, reads=[r_hb, r_ident], writes=[r_ps])

                def evac(psb=psb, r_ps=r_ps, i=i):
                    S.op("act", lambda e: e.copy(out=hT[:, :, i * 128:(i + 1) * 128],
                                                 in_=psb.rearrange("p (k t) -> p k t", k=8)), reads=[r_ps], writes=[r_hT[i]])
                prev = evac
            prev()

            w, r_w = load_w(win_cols(NSA_COLS["g"], 64), 64)
            for i in range(16):
                ps, r_ps = ps_ring.next()
                fns = [lambda e, k=k, ps=ps, i=i, w=w: e.matmul(ps[:, 0:24], lhsT=hT[:, k, i * 128:(i + 1) * 128],
                                                                rhs=w[:, k, 0:24], start=(k == 0), stop=(k == 7))
                       for k in range(8)]
                S.op("pe", fns, reads=[r_w, r_hT[i]], writes=[r_ps])
                S.op("act", lambda e, ps=ps, i=i: e.activation(out=gsig[:, i, :], in_=ps[:, 0:24], func=AF.Sigmoid),
                     reads=[r_ps], writes=[r_gsig])

            if dbg and s == 0 and l == 0:
                dump("gsig", gsig, [r_gsig])
            S.barrier()
            A.off = region0
            q_aug = [A.alloc([128, T], BF16) for _ in range(4)]
            r_q = [[Res() for _ in range(4)] for _ in range(4)]
            r_qs = [[Res() for _ in range(4)] for _ in range(4)]
            r_qst = Res()
            ks_aug = A.alloc([128, T], BF16)
            kw_aug = A.alloc([128, T], BF16)
            r_ks = Res()
            r_kw = Res()
            kcraw = A.alloc([64, T], BF16)
            r_kcraw = Res()
            vcraw = A.alloc([64, T], BF16)
            r_vcraw = Res()
            vsA = A.alloc([128, 16, 65], BF16)
            vwA = A.alloc([128, 16, 65], BF16)
            r_vs = Res()
            r_vw = Res()
            w1c = A.alloc([64, 32, 256], BF16)
            r_w1c = Res()
            w2c = A.alloc([128, 2, 64], BF16)
            posT = A.alloc([64, 32], BF16)
            r_w2c = Res()
            kc_aug = A.alloc([128, 128], BF16)
            r_kc = Res()
            kctm = A.alloc([128, 128], BF16)
            r_kctm = Res()
            vcA = A.alloc([128, 97], BF16)
            r_vc = Res()
            hid = A.alloc([128, 2, 128], BF16)
            r_hid = Res()
            pbias = A.alloc([128, 2], F32)
            r_pb = Res()
            oacc = A.alloc([128, 16, 256], F32)
            r_oacc = [Res() for _ in range(16)]
            impacc = A.alloc([128, 16, 32], F32)
            r_imp = [Res() for _ in range(16)]
            trin_all = [(A.alloc([128, 96], BF16), Res()) for _ in range(16)]
            ob_ring = Ring([(A.alloc([128, 256], BF16), Res()) for _ in range(2)])

            for g in range(2):
                for hl in range(4):
                    h = 4 * g + hl
                    S.op("pool", lambda e, hl=hl, h=h: e.dma_start(
                        out=q_aug[hl][96:99, :].rearrange("p (b i) -> p b i", b=4),
                        in_=C["qalibi"][h, :, :].unsqueeze(1).to_broadcast([3, 4, 512])), writes=[r_qst], dma=True)
                    S.op("pool", lambda e, hl=hl: e.dma_start(out=q_aug[hl][64:96, :], in_=C["zeros32"][:, :]),
                         writes=r_qs[hl], dma=True)
                for dst, r_dst, mid in ((ks_aug, r_ks, C["e32"]), (kw_aug, r_kw, C["zeros32"])):
                    S.op("pool", lambda e, dst=dst, mid=mid: e.dma_start(out=dst[64:96, :], in_=mid[:, :]),
                         writes=[r_dst], dma=True)
                    S.op("pool", lambda e, dst=dst: e.dma_start(out=dst[96:99, :], in_=C["ones3"][:, :]),
                         writes=[r_dst], dma=True)
                S.op("pool", lambda e: e.memset(kctm[:, 64:96], 0.0), writes=[r_kctm])
                S.op("pool", lambda e: e.memset(kctm[:, 96:99], 1.0), writes=[r_kctm])
                S.op("pool", lambda e: e.memset(kctm[:, 0:64], 0.0), writes=[r_kctm])
                S.op("pool", lambda e: e.memset(vsA[:, :, 64:65], 1.0), writes=[r_vs])
                S.op("pool", lambda e: e.memset(vwA[:, :, 64:65], 1.0), writes=[r_vw])
                S.op("pool", lambda e: e.memset(vcA[:, 64:65], 1.0), writes=[r_vc])
                S.op("pool", lambda e: e.dma_start(out=vcA[:, 65:97], in_=C["cmp2slc"][:, :]), writes=[r_vc], dma=True)
                for tr, r_tr in trin_all:
                    S.op("pool", lambda e, tr=tr: e.memset(tr[:, 0:64], 0.0), writes=[r_tr])

                proj_fm_pairs([(NSA_COLS["kc"] + 64 * g, NSA_COLS["vc"] + 64 * g,
                                lambda b: kcraw[0:64, b * 512:(b + 1) * 512], lambda b: r_kcraw,
                                lambda b: vcraw[0:64, b * 512:(b + 1) * 512], lambda b: r_vcraw, None)])
                for kv in range(2):
                    craw, r_craw = (kcraw, r_kcraw) if kv == 0 else (vcraw, r_vcraw)
                    S.op("pool", lambda e, kv=kv: e.dma_start(
                        out=w1c, in_=cw1_d[l, kv, :, :].rearrange("(l d) h -> d l h", d=64)), writes=[r_w1c], dma=True)
                    S.op("pool", lambda e, kv=kv: e.dma_start(
                        out=w2c, in_=cw2_d[l, kv, :, :].rearrange("(a p) d -> p a d", p=128)), writes=[r_w2c], dma=True)
                    S.op("pool", lambda e, kv=kv: e.dma_start(out=posT, in_=posT_d[l, kv, :, :]), writes=[r_w2c], dma=True)
                    for hh in range(2):
                        ps, r_ps = ps_ring.next()
                        fns = [lambda e, ll=ll, ps=ps, hh=hh, craw=craw: e.matmul(
                            ps[:, 0:127], lhsT=w1c[:, ll, hh * 128:(hh + 1) * 128],
                            rhs=craw[0:64, ll:ll + 16 * 126 + 1:16], start=(ll == 0), stop=(ll == 31)) for ll in range(32)]
                        fns += [lambda e, ll=ll, ps=ps, hh=hh: e.matmul(
                            ps[:, 128:129], lhsT=w1c[:, ll, hh * 128:(hh + 1) * 128],
                            rhs=posT[:, ll:ll + 1], start=(ll == 0), stop=(ll == 31)) for ll in range(32)]
                        S.op("pe", fns, reads=[r_w1c, r_w2c, r_craw], writes=[r_ps])
                        S.op("dve", lambda e, ps=ps, hh=hh: e.tensor_copy(out=pbias[:, hh:hh + 1], in_=ps[:, 128:129]),
                             reads=[r_ps], writes=[r_pb])
                        xh, r_xh = tmpf_ring.next()
                        x2, r_x2 = tmpf_ring.next()
                        S.op("dve", lambda e, ps=ps, hh=hh, xh=xh: e.tensor_scalar(
                            out=xh[:, 0:127], in0=ps[:, 0:127], scalar1=pbias[:, hh:hh + 1], scalar2=None, op0=ALU.add),
                            reads=[r_ps, r_pb], writes=[r_xh])
                        S.op("dve", lambda e, xh=xh, x2=x2: e.tensor_tensor(out=x2[:, 0:127], in0=xh[:, 0:127],
                                                                            in1=xh[:, 0:127], op=ALU.mult),
                             reads=[r_xh], writes=[r_x2])
                        S.op("dve", lambda e, x2=x2: e.tensor_scalar(out=x2[:, 0:127], in0=x2[:, 0:127], scalar1=0.044715,
                                                                     scalar2=1.0, op0=ALU.mult, op1=ALU.add),
                             reads=[r_x2], writes=[r_x2])
                        S.op("dve", lambda e, xh=xh, x2=x2: e.tensor_tensor(out=x2[:, 0:127], in0=x2[:, 0:127],
                                                                            in1=xh[:, 0:127], op=ALU.mult),
                             reads=[r_xh, r_x2], writes=[r_x2])
                        S.op("act", lambda e, x2=x2: e.activation(out=x2[:, 0:127], in_=x2[:, 0:127], func=AF.Tanh,
                                                                  scale=0.7978845608028654),
                             reads=[r_x2], writes=[r_x2])
                        S.op("dve", lambda e, xh=xh, x2=x2, hh=hh: e.scalar_tensor_tensor(
                            out=hid[:, hh, 0:127], in0=x2[:, 0:127], scalar=1.0, in1=xh[:, 0:127],
                            op0=ALU.add, op1=ALU.mult), reads=[r_xh, r_x2], writes=[r_hid])
                    ps, r_ps = ps_ring.next()
                    fns = [lambda e, hh=hh, ps=ps: e.matmul(ps[0:127, 0:64], lhsT=hid[:, hh, 0:127], rhs=w2c[:, hh, :],
                                                            start=(hh == 0), stop=(hh == 1)) for hh in range(2)]
                    S.op("pe", fns, reads=[r_hid, r_w2c], writes=[r_ps])
                    if kv == 1:
                        S.op("dve", lambda e, ps=ps: e.tensor_scalar(out=vcA[0:127, 0:64], in0=ps[0:127, 0:64],
                                                                     scalar1=0.5, scalar2=None, op0=ALU.mult),
                             reads=[r_ps], writes=[r_vc])
                    else:
                        tf, r_tf = tmpf_ring.next()
                        st, r_st = sm_ring.next()
                        S.op("dve", lambda e, ps=ps, tf=tf: e.tensor_scalar(out=tf[0:127, 0:64], in0=ps[0:127, 0:64],
                                                                            scalar1=0.5, scalar2=None, op0=ALU.mult),
                             reads=[r_ps], writes=[r_tf])
                        S.op("act", lambda e, tf=tf, st=st: e.activation(out=tf[0:127, 64:128], in_=tf[0:127, 0:64],
                                                                         func=AF.Square, accum_out=st[0:127, 0:1]),
                             reads=[r_tf], writes=[r_tf, r_st])
                        S.op("act", lambda e, st=st: e.activation(out=st[0:127, 1:2], in_=st[0:127, 0:1], func=AF.Sqrt,
                                                                  scale=1.0 / 64, bias=EPS), reads=[r_st], writes=[r_st])
                        S.op("dve", lambda e, st=st: e.reciprocal(out=st[0:127, 2:3], in_=st[0:127, 1:2]),
                             reads=[r_st], writes=[r_st])
                        S.op("dve", lambda e, tf=tf, st=st: e.scalar_tensor_tensor(
                            out=kctm[0:127, 0:64], in0=tf[0:127, 0:64], scalar=st[0:127, 2:3], in1=gkc_rep[0:127, :],
                            op0=ALU.mult, op1=ALU.mult), reads=[r_tf, r_st, r_c], writes=[r_kctm])
                        ps2, r_ps2 = ps_ring.next()
                        psb2 = ps2[:].bitcast(BF16)
                        S.op("pe", lambda e, psb2=psb2: e.transpose(out=psb2[0:99, 0:128], in_=kctm[:, 0:99],
                                                                    identity=ident_b[:]),
                             reads=[r_kctm, r_ident], writes=[r_ps2])
                        S.op("dve", lambda e, psb2=psb2: e.tensor_copy(out=kc_aug[0:99, 0:128], in_=psb2[0:99, 0:128]),
                             reads=[r_ps2], writes=[r_kc])

                prs = []
                for hp in range(2):
                    hA, hB = 2 * hp, 2 * hp + 1
                    prs.append((NSA_COLS["q"] + 64 * (4 * g + hA), NSA_COLS["q"] + 64 * (4 * g + hB),
                                lambda b, hA=hA: q_aug[hA][0:64, b * 512:(b + 1) * 512], lambda b, hA=hA: r_q[hA][b],
                                lambda b, hB=hB: q_aug[hB][0:64, b * 512:(b + 1) * 512], lambda b, hB=hB: r_q[hB][b],
                                gtab[:, 0:1]))
                prs.append((NSA_COLS["ks"] + 64 * g, NSA_COLS["kw"] + 64 * g,
                            lambda b: ks_aug[0:64, b * 512:(b + 1) * 512], lambda b: r_ks,
                            lambda b: kw_aug[0:64, b * 512:(b + 1) * 512], lambda b: r_kw, gtab[:, 1:2]))
                proj_fm_pairs(prs)
                for nm, dstA, r_dst in (("vs", vsA, r_vs), ("vw", vwA, r_vw)):
                    w, r_w = load_w(win_cols(NSA_COLS[nm] + 64 * g, 64), 64)
                    for i in range(16):
                        ps, r_ps = ps_ring.next()
                        fns = [lambda e, k=k, ps=ps, i=i, w=w: e.matmul(ps[:, 0:64], lhsT=hT[:, k, i * 128:(i + 1) * 128],
                                                                        rhs=w[:, k, 0:64], start=(k == 0), stop=(k == 7))
                               for k in range(8)]
                        S.op("pe", fns, reads=[r_w, r_hT[i]], writes=[r_ps])
                        S.op("act", lambda e, ps=ps, i=i, dstA=dstA: e.copy(out=dstA[:, i, 0:64], in_=ps[:, 0:64]),
                             reads=[r_ps], writes=[r_dst])
                def mk_cmp_evac(hl, qb):
                    h = 4 * g + hl

                    def ev(acc3, r_acc):
                        tts = slice(4 * qb, 4 * qb + 4)
                        r_o = r_oacc[4 * qb:4 * qb + 4]
                        r_i = r_imp[4 * qb:4 * qb + 4]
                        st, r_st = sm_ring.next()
                        rd = st[:, 4:8].unsqueeze(2)
                        cf = st[:, 8:12].unsqueeze(2)
                        S.op("dve", lambda e: e.tensor_scalar(out=st[:, 0:4].unsqueeze(2), in0=acc3[:, :, 64:65], scalar1=1e-30,
                                                              scalar2=None, op0=ALU.add), reads=[r_acc], writes=[r_st])
                        S.op("dve", lambda e: e.reciprocal(out=st[:, 4:8], in_=st[:, 0:4]), reads=[r_st], writes=[r_st])
                        S.op("dve", lambda e: e.tensor_tensor(out=cf, in0=rd, in1=gsig[:, tts, 3 * h:3 * h + 1], op=ALU.mult),
                             reads=[r_st, r_gsig], writes=[r_st])
                        S.op("dve", lambda e: e.tensor_tensor(out=oacc[:, tts, hl * 64:(hl + 1) * 64], in0=acc3[:, :, 0:64],
                                                              in1=cf.to_broadcast([128, 4, 64]), op=ALU.mult),
                             reads=[r_acc, r_st], writes=r_o)
                        if hl == 0:
                            S.op("dve", lambda e: e.tensor_tensor(out=impacc[:, tts, :], in0=acc3[:, :, 65:97],
                                                                  in1=rd.to_broadcast([128, 4, 32]), op=ALU.mult),
                                 reads=[r_acc, r_st], writes=r_i)
                        else:
                            tf, r_tf = tmpf_ring.next()
                            tf3 = tf[:, 0:128].rearrange("p (u c) -> p u c", u=4)
                            S.op("dve", lambda e: e.tensor_tensor(out=tf3, in0=acc3[:, :, 65:97],
                                                                  in1=rd.to_broadcast([128, 4, 32]), op=ALU.mult),
                                 reads=[r_acc, r_st], writes=[r_tf])
                            S.op("pool", lambda e: e.tensor_tensor(out=impacc[:, tts, :], in0=impacc[:, tts, :], in1=tf3,
                                                                   op=ALU.add), reads=[r_tf] + r_i, writes=r_i)
                    return ev

                rounds = []
                for hl in range(4):
                    h = 4 * g + hl
                    for qb in range(4):
                        rounds.append(dict(q=q_aug[hl], r_q=[r_q[hl][qb], r_qst], krows=99, kT=kc_aug, r_k=r_kc, qb=qb,
                                           tiles=[(0, 0, 512, None, None)], kpart=127, mask=cmask,
                                           V=lambda kt: vcA[0:127, 0:97], r_v=r_vc, nv=97,
                                           bias=lambda kt, h=h, qb=qb: cbias[0:127, 4 * h + qb:4 * h + qb + 1],
                                           evac=mk_cmp_evac(hl, qb)))
                run_rounds(rounds)
                if dbg and s == 0 and l == 0 and g == 0:
                    dump("oacc_cmp", oacc, r_oacc)
                    dump("kc_aug", kc_aug, [r_kc])
                    dump("vcA", vcA, [r_vc])
                    dump("impacc", impacc, r_imp)

                sel_tr = []
                for tt in range(16):
                    sc, r_sc = tmpf_ring.next()
                    st, r_st = sm_ring.next()
                    tr, r_tr = trin_all[tt]
                    S.op("dve", lambda e, sc=sc, tt=tt: e.tensor_tensor(out=sc[:, 0:32], in0=impacc[:, tt, :],
                                                                        in1=nsa_mult[:, tt, :], op=ALU.mult),
                         reads=[r_imp[tt], r_c], writes=[r_sc])
                    S.op("dve", lambda e, sc=sc, tt=tt: e.tensor_tensor(out=sc[:, 0:32], in0=sc[:, 0:32],
                                                                        in1=nsa_add[:, tt, :], op=ALU.add),
                         reads=[r_sc, r_c], writes=[r_sc])
                    S.op("dve", lambda e, sc=sc, st=st: e.max(out=st[:, 0:8], in_=sc[:, 0:32]), reads=[r_sc], writes=[r_st])
                    S.op("dve", lambda e, sc=sc, st=st, tr=tr: e.tensor_scalar(
                        out=tr[:, 64:96], in0=sc[:, 0:32], scalar1=st[:, 7:8], scalar2=-BIG, op0=ALU.is_lt, op1=ALU.mult),
                        reads=[r_sc, r_st], writes=[r_tr])

                def emit_sel_transposes():
                    for tt in range(16):
                        tr, r_tr = trin_all[tt]
                        ps, r_ps = misc_ring.next()
                        psb = ps[:].bitcast(BF16)
                        S.op("pe", lambda e, psb=psb, tr=tr: e.transpose(out=psb[0:96, 0:128], in_=tr[:, 0:96],
                                                                         identity=ident_b[:]),
                             reads=[r_tr, r_ident], writes=[r_ps])
                        for hl in range(4):
                            S.op("dve", lambda e, psb=psb, hl=hl, tt=tt: e.tensor_copy(
                                out=q_aug[hl][64:96, tt * 128:(tt + 1) * 128], in_=psb[64:96, 0:128]),
                                reads=[r_ps], writes=[r_qs[hl][tt // 4]])

                def mk_evac(hl, qb, br):
                    h = 4 * g + hl

                    def ev(acc3, r_acc):
                        tts = slice(4 * qb, 4 * qb + 4)
                        r_o = r_oacc[4 * qb:4 * qb + 4]
                        st, r_st = sm_ring.next()
                        rd = st[:, 4:8].unsqueeze(2)
                        cf = st[:, 8:12].unsqueeze(2)
                        S.op("dve", lambda e: e.reciprocal(out=rd, in_=acc3[:, :, 64:65]), reads=[r_acc], writes=[r_st])
                        S.op("dve", lambda e: e.tensor_tensor(out=cf, in0=rd, in1=gsig[:, tts, 3 * h + br:3 * h + br + 1],
                                                              op=ALU.mult), reads=[r_st, r_gsig], writes=[r_st])
                        tf, r_tf = tmpf_ring.next()
                        tf3 = tf[:, 0:256].rearrange("p (u c) -> p u c", u=4)
                        S.op("dve", lambda e: e.tensor_tensor(out=tf3, in0=acc3[:, :, 0:64], in1=cf.to_broadcast([128, 4, 64]),
                                                              op=ALU.mult), reads=[r_acc, r_st], writes=[r_tf])
                        S.op("pool", lambda e: e.tensor_tensor(out=oacc[:, tts, hl * 64:(hl + 1) * 64],
                                                               in0=oacc[:, tts, hl * 64:(hl + 1) * 64], in1=tf3, op=ALU.add),
                             reads=[r_tf] + r_o, writes=r_o)
                    return ev

                rounds = []
                for hl in range(4):
                    h = 4 * g + hl
                    for qb in range(4):
                        rounds.append(dict(q=q_aug[hl], r_q=[r_q[hl][qb], r_qst], krows=99, kT=kw_aug, r_k=r_kw, qb=qb,
                                           tiles=window_tiles(qb), V=lambda kt: vwA[:, kt, :], r_v=r_vw, nv=65,
                                           bias=lambda kt, h=h, qb=qb: abias[:, h * 19 + (kt - 4 * qb + 15):h * 19 + (kt - 4 * qb + 15) + 1],
                                           evac=mk_evac(hl, qb, 2)))
                run_rounds(rounds)
                rounds = []
                emit_sel_transposes()
                if dbg and s == 0 and l == 0 and g == 0:
                    dump("oacc_win", oacc, r_oacc)
                for hl in range(4):
                    h = 4 * g + hl
                    for qb in range(4):
                        rounds.append(dict(q=q_aug[hl], r_q=[r_q[hl][qb], r_qs[hl][qb], r_qst], krows=99, kT=ks_aug,
                                           r_k=r_ks, qb=qb, tiles=causal_tiles(qb), V=lambda kt: vsA[:, kt, :], r_v=r_vs,
                                           nv=65,
                                           bias=lambda kt, h=h, qb=qb: abias[:, h * 19 + (kt - 4 * qb + 15):h * 19 + (kt - 4 * qb + 15) + 1],
                                           evac=mk_evac(hl, qb, 1)))
                run_rounds(rounds)
                if dbg and s == 0 and l == 0 and g == 0:
                    dump("oacc_all", oacc, r_oacc)
                    dump("q0", q_aug[0], [r_q[0][b] for b in range(4)] + [r_qs[0][b] for b in range(4)] + [r_qst])
                    dump("ks_aug", ks_aug, [r_ks])
                for tt in range(16):
                    ob, r_ob = ob_ring.next()
                    S.op("act", lambda e, ob=ob, tt=tt: e.copy(out=ob, in_=oacc[:, tt, :]), reads=[r_oacc[tt]], writes=[r_ob])
                    transpose_tile(ob, r_ob, onT[:, 2 * g:2 * g + 2, tt * 128:(tt + 1) * 128], r_onT[tt], nk=2,
                                   evac="dve", ring=misc_ring)

            S.barrier()
            A.off = region0
            q_aug = [A.alloc([128, T], BF16) for _ in range(4)]
            k_aug = [A.alloc([128, T], BF16) for _ in range(4)]
            r_q = [[Res() for _ in range(4)] for _ in range(4)]
            r_qs = [[Res() for _ in range(4)] for _ in range(4)]
            r_qst = Res()
            r_k = [Res() for _ in range(4)]
            vmA = A.alloc([128, 16, 4, 65], BF16)
            r_vm = Res()
            kmean_f = A.alloc([64, 4, 8], F32)
            kmean_b = A.alloc([64, 4, 8], BF16)
            r_km = Res()
            omb = A.alloc([128, 16, 256], BF16)
            r_omb = [Res() for _ in range(16)]
            trin_ring = Ring([(A.alloc([128, 72], BF16), Res()) for _ in range(4)])
            for hf in range(2):
                for tr, r_tr in trin_ring.items:
                    S.op("pool", lambda e, tr=tr: e.memset(tr[:, 0:64], 0.0), writes=[r_tr])
                S.op("pool", lambda e: e.memset(vmA[:, :, :, 64:65], 1.0), writes=[r_vm])
                for hl in range(4):
                    h = 4 * hf + hl
                    S.op("pool", lambda e, hl=hl, h=h: e.dma_start(
                        out=q_aug[hl][72:75, :].rearrange("p (b i) -> p b i", b=4),
                        in_=C["qalibi"][8 + h, :, :].unsqueeze(1).to_broadcast([3, 4, 512])), writes=[r_qst], dma=True)
                    S.op("pool", lambda e, hl=hl: e.dma_start(out=q_aug[hl][64:72, :], in_=C["zeros32"][0:8, :]),
                         writes=r_qs[hl], dma=True)
                    S.op("pool", lambda e, hl=hl: e.dma_start(out=k_aug[hl][64:72, :], in_=C["e8"][:, :]),
                         writes=[r_k[hl]], dma=True)
                    S.op("pool", lambda e, hl=hl: e.dma_start(out=k_aug[hl][72:75, :], in_=C["ones3"][:, :]),
                         writes=[r_k[hl]], dma=True)
                prs = []
                for hp in range(2):
                    hA, hB = 2 * hp, 2 * hp + 1
                    prs.append((MOBA_COLS["q"] + 64 * (4 * hf + hA), MOBA_COLS["q"] + 64 * (4 * hf + hB),
                                lambda b, hA=hA: q_aug[hA][0:64, b * 512:(b + 1) * 512], lambda b, hA=hA: r_q[hA][b],
                                lambda b, hB=hB: q_aug[hB][0:64, b * 512:(b + 1) * 512], lambda b, hB=hB: r_q[hB][b],
                                gtab[:, 2:3]))
                for hp in range(2):
                    hA, hB = 2 * hp, 2 * hp + 1
                    prs.append((MOBA_COLS["k"] + 64 * (4 * hf + hA), MOBA_COLS["k"] + 64 * (4 * hf + hB),
                                lambda b, hA=hA: k_aug[hA][0:64, b * 512:(b + 1) * 512], lambda b, hA=hA: r_k[hA],
                                lambda b, hB=hB: k_aug[hB][0:64, b * 512:(b + 1) * 512], lambda b, hB=hB: r_k[hB],
                                gtab[:, 3:4]))
                proj_fm_pairs(prs)
                for hl in range(4):
                    S.op("dve", lambda e, hl=hl: e.tensor_reduce(
                        out=kmean_f[:, hl, :], in_=k_aug[hl][0:64, :].rearrange("p (n k) -> p n k", k=256),
                        axis=AX.X, op=ALU.add), reads=[r_k[hl]], writes=[r_km])
                S.op("dve", lambda e: e.tensor_scalar(out=kmean_b, in0=kmean_f, scalar1=1.0 / 256, scalar2=None,
                                                      op0=ALU.mult), reads=[r_km], writes=[r_km])
                w, r_w = load_w(win_cols(MOBA_COLS["v"] + 256 * hf, 256), 256)
                for i in range(16):
                    ps, r_ps = ps_ring.next()
                    fns = [lambda e, k=k, ps=ps, i=i, w=w: e.matmul(ps[:, 0:256], lhsT=hT[:, k, i * 128:(i + 1) * 128],
                                                                    rhs=w[:, k, 0:256], start=(k == 0), stop=(k == 7))
                           for k in range(8)]
                    S.op("pe", fns, reads=[r_w, r_hT[i]], writes=[r_ps])
                    S.op("act", lambda e, ps=ps, i=i: e.copy(out=vmA[:, i, :, 0:64],
                                                             in_=ps[:, 0:256].rearrange("p (h d) -> p h d", d=64)),
                         reads=[r_ps], writes=[r_vm])
                for tt in range(16):
                    cur = tt // 2
                    if tt >= 8:
                        ps, r_ps = misc_ring.next()
                        fns = [lambda e, hl=hl, ps=ps, tt=tt: e.matmul(ps[:, hl * 8:(hl + 1) * 8],
                                                                       lhsT=q_aug[hl][0:64, tt * 128:(tt + 1) * 128],
                                                                       rhs=kmean_b[:, hl, :], start=True, stop=True)
                               for hl in range(4)]
                        S.op("pe", fns, reads=[r_km] + [r_q[hl][tt // 4] for hl in range(4)], writes=[r_ps])
                        sc, r_sc = tmpf_ring.next()
                        for hl in range(4):
                            S.op("dve", lambda e, hl=hl, ps=ps, sc=sc, tt=tt: e.tensor_tensor(
                                out=sc[:, hl * 8:(hl + 1) * 8], in0=ps[:, hl * 8:(hl + 1) * 8], in1=moba_add[:, tt, :],
                                op=ALU.add), reads=[r_ps, r_c], writes=[r_sc])
                    for hl in range(4):
                        tr, r_tr = trin_ring.next()
                        S.op("pool", lambda e, tr=tr, cur=cur: e.memset(tr[:, 64 + cur:65 + cur], 0.0), writes=[r_tr])
                        if cur < 7:
                            S.op("pool", lambda e, tr=tr, cur=cur: e.memset(tr[:, 65 + cur:72], -BIG), writes=[r_tr])
                        if tt < 8:
                            if cur > 0:
                                S.op("pool", lambda e, tr=tr, cur=cur: e.memset(tr[:, 64:64 + cur], 0.0), writes=[r_tr])
                        else:
                            st, r_st = sm_ring.next()
                            S.op("dve", lambda e, sc=sc, st=st, hl=hl: e.max(out=st[:, 0:8], in_=sc[:, hl * 8:(hl + 1) * 8]),
                                 reads=[r_sc], writes=[r_st])
                            S.op("dve", lambda e, sc=sc, st=st, tr=tr, hl=hl, cur=cur: e.tensor_scalar(
                                out=tr[:, 64:64 + cur], in0=sc[:, hl * 8:hl * 8 + cur], scalar1=st[:, 2:3], scalar2=-BIG,
                                op0=ALU.is_lt, op1=ALU.mult), reads=[r_sc, r_st], writes=[r_tr])
                        ps2, r_ps2 = ps_ring.next()
                        psb = ps2[:].bitcast(BF16)
                        S.op("pe", lambda e, psb=psb, tr=tr: e.transpose(out=psb[0:72, 0:128], in_=tr[:, 0:72],
                                                                         identity=ident_b[:]),
                             reads=[r_tr, r_ident], writes=[r_ps2])
                        S.op("dve", lambda e, psb=psb, hl=hl, tt=tt: e.tensor_copy(
                            out=q_aug[hl][64:72, tt * 128:(tt + 1) * 128], in_=psb[64:72, 0:128]),
                            reads=[r_ps2], writes=[r_qs[hl][tt // 4]])

                def mk_evac_m(hl, qb):
                    def ev(acc3, r_acc):
                        tts = slice(4 * qb, 4 * qb + 4)
                        st, r_st = sm_ring.next()
                        rd = st[:, 4:8].unsqueeze(2)
                        S.op("dve", lambda e: e.reciprocal(out=rd, in_=acc3[:, :, 64:65]), reads=[r_acc], writes=[r_st])
                        S.op("dve", lambda e: e.tensor_tensor(out=omb[:, tts, hl * 64:(hl + 1) * 64], in0=acc3[:, :, 0:64],
                                                              in1=rd.to_broadcast([128, 4, 64]), op=ALU.mult),
                             reads=[r_acc, r_st], writes=r_omb[4 * qb:4 * qb + 4])
                    return ev

                rounds = []
                for hl in range(4):
                    h = 4 * hf + hl
                    for qb in range(4):
                        rounds.append(dict(q=q_aug[hl], r_q=[r_q[hl][qb], r_qs[hl][qb], r_qst], krows=75, kT=k_aug[hl],
                                           r_k=r_k[hl], qb=qb, tiles=causal_tiles(qb),
                                           V=lambda kt, hl=hl: vmA[:, kt, hl, :], r_v=r_vm, nv=65,
                                           bias=lambda kt, h=h, qb=qb: abias[:, (8 + h) * 19 + (kt - 4 * qb + 15):(8 + h) * 19 + (kt - 4 * qb + 15) + 1],
                                           evac=mk_evac_m(hl, qb)))
                run_rounds(rounds)
                for tt in range(16):
                    transpose_tile(omb[:, tt, :], r_omb[tt], omT[:, 2 * hf:2 * hf + 2, tt * 128:(tt + 1) * 128],
                                   r_omT[tt], nk=2, evac="dve", ring=misc_ring)

            if dbg and s == 0 and l == 0:
                for nm, src, rr in (("onT", onT, r_onT), ("omT", omT, r_omT)):
                    if nm in dbg_d:
                        S.op("pool", lambda e, nm=nm, src=src: e.dma_start(
                            out=dbg_d[nm][:, :].rearrange("p (a b) -> p a b", a=4), in_=src), reads=rr,
                            writes=[Res()], dma=True)

            S.barrier()
            A.off = region0
            yT = A.alloc([128, 8, T], BF16)
            r_yT = [Res() for _ in range(8)]
            wout = A.alloc([128, 8, D], BF16)
            r_wout = Res()
            S.op("pool", lambda e: e.dma_start(out=wout, in_=wout_d[l, :, :].rearrange("(k p) c -> p k c", p=128)),
                 writes=[r_wout], dma=True)
            for oc in range(8):
                wgn, r_wgn = load_w(win_cols(GATE_N + 128 * oc, 128), 128)
                wgm, r_wgm = load_w(win_cols(GATE_M + 128 * oc, 128), 128)
                wu, r_wu = wch_ring.next()
                S.op("pool", lambda e, wu=wu, oc=oc: e.dma_start(
                    out=wu[:, 0:4, 0:128], in_=wupn_d[l, :, oc * 128:(oc + 1) * 128].rearrange("(k p) c -> p k c", p=128)),
                    writes=[r_wu], dma=True)
                S.op("pool", lambda e, wu=wu, oc=oc: e.dma_start(
                    out=wu[:, 4:8, 0:128], in_=wupm_d[l, :, oc * 128:(oc + 1) * 128].rearrange("(k p) c -> p k c", p=128)),
                    writes=[r_wu], dma=True)
                for b in range(4):
                    bs = slice(b * 512, (b + 1) * 512)
                    res = []
                    for (wg, r_wg, oT, r_oT, ko) in ((wgn, r_wgn, onT, r_onT, 0), (wgm, r_wgm, omT, r_omT, 4)):
                        pg, r_pg = ps_ring.next()
                        fns = [lambda e, k=k, pg=pg, wg=wg, bs=bs: e.matmul(pg[:], lhsT=wg[:, k, 0:128], rhs=hT[:, k, bs],
                                                                     start=(k == 0), stop=(k == 7)) for k in range(8)]
                        S.op("pe", fns, reads=[r_wg] + r_hT[4 * b:4 * b + 4], writes=[r_pg])
                        pu, r_pu = ps_ring.next()
                        fns = [lambda e, k=k, pu=pu, oT=oT, ko=ko, wu=wu, bs=bs: e.matmul(pu[:], lhsT=wu[:, ko + k, 0:128], rhs=oT[:, k, bs],
                                                                            start=(k == 0), stop=(k == 3)) for k in range(4)]
                        S.op("pe", fns, reads=[r_wu] + r_oT[4 * b:4 * b + 4], writes=[r_pu])
                        sg, r_sg = tmpf_ring.next()
                        S.op("act", lambda e, pg=pg, sg=sg: e.activation(out=sg, in_=pg[:], func=AF.Sigmoid),
                             reads=[r_pg], writes=[r_sg])
                        S.op("dve", lambda e, pu=pu, sg=sg: e.tensor_tensor(out=sg, in0=sg, in1=pu[:], op=ALU.mult),
                             reads=[r_pu, r_sg], writes=[r_sg])
                        res.append((sg, r_sg))
                    S.op("dve", lambda e, a=res[0][0], b_=res[1][0], oc=oc, bs=bs: e.tensor_tensor(
                        out=yT[:, oc, bs], in0=a, in1=b_, op=ALU.add), reads=[res[0][1], res[1][1]], writes=[r_yT[oc]])
            for i in range(16):
                tt = tt0 + i
                xa, r_xa = xt_ring.next()
                S.op("sp", lambda e, xa=xa, tt=tt: e.dma_start(out=xa, in_=src_d[tt * 128:(tt + 1) * 128, :]),
                     reads=[r_src[tt]], writes=[r_xa], dma=True)
                for h2 in range(2):
                    po, r_po = ps_ring.next()
                    fns = [lambda e, oc=oc, po=po, i=i, h2=h2: e.matmul(po[:], lhsT=yT[:, oc, i * 128:(i + 1) * 128],
                                                                        rhs=wout[:, oc, h2 * 512:(h2 + 1) * 512],
                                                                        start=(oc == 0), stop=(oc == 7)) for oc in range(8)]
                    S.op("pe", fns, reads=r_yT + [r_wout], writes=[r_po])
                    S.op("dve", lambda e, po=po, xa=xa, h2=h2: e.tensor_tensor(
                        out=xa[:, h2 * 512:(h2 + 1) * 512], in0=po[:], in1=xa[:, h2 * 512:(h2 + 1) * 512], op=ALU.add),
                        reads=[r_po, r_xa], writes=[r_xa])
                S.op("sp", lambda e, xa=xa, tt=tt: e.dma_start(out=y_d[tt * 128:(tt + 1) * 128, :], in_=xa),
                     reads=[r_xa], writes=[r_y[tt]], dma=True)

    cur_d, cur_r = x_d, r_x
    for l in range(depth):
        if f"ffn{2 * l}" in phases or "all" in phases:
            ffn_phase(l, 0, cur_d, cur_r)
            cur_d, cur_r = y_d, r_y
        if f"mix{l}" in phases or "all" in phases:
            mix_phase(l, cur_d, cur_r)
            cur_d, cur_r = y_d, r_y
        if f"ffn{2 * l + 1}" in phases or "all" in phases:
            ffn_phase(l, 1, cur_d, cur_r)
            cur_d, cur_r = y_d, r_y

    S.barrier()
    S.finish("sp", r_y)
    sems = [es.enter_context(nc.semaphore(f"s{i}")) for i in range(S.nsem)]
    S.emit(nc, sems)
    es.close()
    return nc, S


def make_in_maps(inputs, nseq, ncores):
    x = np.ascontiguousarray(inputs["x"], dtype=np.float32).reshape(-1, nseq * T, D)
    consts = make_consts()
    shared = {}
    for k in ("norm_g", "ffn_w1", "ffn_w3", "ffn_w2", "w_in", "g_qk_nsa", "g_qk_moba", "cmp_w1", "cmp_w2", "w_up_nsa", "w_up_moba",
              "w_out"):
        shared[k] = np.ascontiguousarray(inputs[k], dtype=np.float32)
    shared["g_qk_nsaT"] = np.ascontiguousarray(np.transpose(np.asarray(inputs["g_qk_nsa"], np.float32), (0, 2, 1)))
    shared["g_qk_mobaT"] = np.ascontiguousarray(np.transpose(np.asarray(inputs["g_qk_moba"], np.float32), (0, 2, 1)))
    shared["cmp_posT"] = np.ascontiguousarray(np.transpose(np.asarray(inputs["cmp_pos"], np.float32), (0, 1, 3, 2)))
    shared.update(consts)
    in_maps = []
    for c in range(ncores):
        m = {"x": x[c]}
        m.update(shared)
        in_maps.append(m)
    return in_maps


_CACHE = {}


def kernel(**inputs):
    nseq = 16 // NCORES
    if "full" not in _CACHE:
        _CACHE["full"] = build_program(nseq=nseq, phases=("all",))
    nc, S = _CACHE["full"]
    in_maps = make_in_maps(inputs, nseq, NCORES)
    res = run_bass_kernel_spmd(nc, in_maps, core_ids=list(range(NCORES)))
    y = np.stack([np.asarray(r["y"]) for r in res.results], axis=0)
    return y.reshape(16, T, D).astype(np.float32)
```

```python
import numpy as np
from contextlib import ExitStack
import concourse.bass as bass
import concourse.mybir as mybir
from concourse.bass_utils import run_bass_kernel_spmd

F32 = mybir.dt.float32
BF16 = mybir.dt.bfloat16
AF = mybir.ActivationFunctionType
ALU = mybir.AluOpType
AX = mybir.AxisListType

NCORES = 8
D = 1024
T = 2048
DFF = 2816
NF = DFF // 128
DEPTH = 2
EPS = 1e-6


class Res:
    __slots__ = ("w", "r", "name")

    def __init__(self, name=""):
        self.w = None
        self.r = {}
        self.name = name


class _Eng:
    def __init__(self, name, sem):
        self.name = name
        self.sem = sem
        self.count = 0
        self.known = {}
        self.ops = []
        self.dma_sems = []
        self.dma_count = 0


class Sched:
    NDMA = 8

    def __init__(self):
        self.nsem = 0
        self.eng = {}
        for n in ("pe", "act", "dve", "pool", "sp"):
            self.eng[n] = _Eng(n, self._newsem())
        for n in ("sp", "pool", "act"):
            self.eng[n].dma_sems = [self._newsem() for _ in range(self.NDMA)]

    def _newsem(self):
        s = self.nsem
        self.nsem += 1
        return s

    def op(self, eng, fns, reads=(), writes=(), dma=False):
        E = self.eng[eng]
        if not isinstance(fns, (list, tuple)):
            fns = [fns]
        need = {}

        def req(tok):
            if tok is None:
                return
            s, v, clk = tok
            o = need.get(s)
            if o is None or o[0] < v:
                need[s] = (v, clk)

        for r in reads:
            req(r.w)
        for w in writes:
            req(w.w)
            for s, (v, clk) in w.r.items():
                req((s, v, clk))
        implied = {}
        for s, (v, clk) in need.items():
            for cs, cv in clk.items():
                if implied.get(cs, 0) < cv:
                    implied[cs] = cv
        waits = []
        known = E.known
        for s, (v, clk) in need.items():
            if known.get(s, 0) >= v or implied.get(s, 0) >= v:
                continue
            if eng == "pe" and s == E.sem:
                continue
            waits.append((s, v))
        for s, v in implied.items():
            if known.get(s, 0) < v:
                known[s] = v
        for s, (v, clk) in need.items():
            if known.get(s, 0) < v:
                known[s] = v
        if dma:
            j = E.dma_count
            E.dma_count += 1
            s = E.dma_sems[j % self.NDMA]
            prev = 16 * (j // self.NDMA)
            if prev > 0 and known.get(s, 0) < prev:
                waits.append((s, prev))
                known[s] = prev
            val = prev + 16
            inc = 16
        else:
            E.count += 1
            s = E.sem
            val = E.count
            inc = 1
        clk = dict(known)
        tok = (s, val, clk)
        E.ops.append((waits, list(fns), s, inc))
        for r in reads:
            o = r.r.get(s)
            if o is None or o[0] < val:
                r.r[s] = (val, clk)
        for w in writes:
            w.w = tok
            w.r = {}
        return tok

    def finish(self, eng, resources):
        E = self.eng[eng]
        need = {}
        for r in resources:
            toks = []
            if r.w is not None:
                toks.append(r.w)
            for s, (v, clk) in r.r.items():
                toks.append((s, v, clk))
            for s, v, clk in toks:
                if need.get(s, 0) < v:
                    need[s] = v
        waits = [(s, v) for s, v in need.items() if E.known.get(s, 0) < v]
        E.ops.append((waits, [], None, 0))

    def emit(self, nc, sems):
        def replay(E):
            def body(e):
                for waits, fns, s, inc in E.ops:
                    for ws, wv in waits:
                        e.wait_ge(sems[ws], wv)
                    if not fns:
                        continue
                    for fn in fns[:-1]:
                        fn(e)
                    fns[-1](e).then_inc(sems[s], inc)
            return body

        with nc.Block() as block:
            block.sync(replay(self.eng["sp"]))
            block.scalar(replay(self.eng["act"]))
            block.vector(replay(self.eng["dve"]))
            block.gpsimd(replay(self.eng["pool"]))
            block.tensor(replay(self.eng["pe"]))


class Ring:
    def __init__(self, items):
        self.items = items
        self.i = 0

    def next(self):
        it = self.items[self.i % len(self.items)]
        self.i += 1
        return it


def _barrier(self):
    toks = {}
    for E in self.eng.values():
        if E.count > 0:
            toks[E.sem] = E.count
        for i, s in enumerate(E.dma_sems):
            if E.dma_count > i:
                toks[s] = 16 * ((E.dma_count - i + self.NDMA - 1) // self.NDMA)
    for E in self.eng.values():
        waits = []
        for s, v in toks.items():
            if E.known.get(s, 0) >= v:
                continue
            if E.name == "pe" and s == E.sem:
                continue
            waits.append((s, v))
            E.known[s] = v
        if waits:
            E.ops.append((waits, [], None, 0))


Sched.barrier = _barrier


class Arena:
    def __init__(self, t, nel):
        self.t = t
        self.nel = nel
        self.off = 0

    def alloc(self, shape, dt):
        n = 1
        for d in shape[1:]:
            n *= d
        sz = n * (2 if dt == F32 else 1)
        self.off = (self.off + 1) // 2 * 2
        o = self.off
        self.off += sz
        assert self.off <= self.nel, ("arena overflow", self.off, self.nel)
        ap = self.t[0:shape[0], o:o + sz]
        if dt == F32:
            ap = ap.bitcast(F32)
        if len(shape) == 3:
            ap = ap.rearrange("p (a b) -> p a b", a=shape[1])
        elif len(shape) == 4:
            ap = ap.rearrange("p (a b c) -> p a b c", a=shape[1], b=shape[2])
        return ap


BIG = 30000.0
NSA_COLS = dict(q=0, kc=512, vc=640, ks=768, vs=896, kw=1024, vw=1152, g=1280)
MOBA_COLS = dict(q=1304, k=1816, v=2328)
GATE_N, GATE_M = 2840, 3864
INC = 4888


def slopes_all():
    s = (2.0 ** (-8.0 * np.arange(1, 17) / 16)).astype(np.float32)
    return s[0::2].copy(), s[1::2].copy()


def _bf16_round(a):
    a = np.asarray(a, dtype=np.float32)
    u = a.view(np.uint32).astype(np.uint64)
    r = ((u + 0x7FFF + ((u >> 16) & 1)) >> 16) << 16
    return r.astype(np.uint32).view(np.float32)


def make_consts():
    c = {}
    c["ident"] = np.eye(128, dtype=np.float32)
    j = np.arange(128)[:, None]
    i = np.arange(128)[None, :]
    c["tri_c"] = np.where(j > i, -BIG, 0.0).astype(np.float32)
    c["tri_a"] = np.where(j <= i, -BIG, 0.0).astype(np.float32)
    c["ones64"] = np.ones((64, 64), np.float32)
    bd = np.zeros((128, 128), np.float32)
    bd[:64, :64] = 1.0
    bd[64:, 64:] = 1.0
    c["bd_ones"] = bd
    sn, sm = slopes_all()
    sl = np.concatenate([sn, sm])
    ab = np.zeros((128, 16, 19), np.float32)
    for h in range(16):
        for d in range(-15, 4):
            ab[:, h, d + 15] = sl[h].astype(np.float64) * (128 * d + np.arange(128))
    c["abias"] = ab.reshape(128, 16 * 19)
    cb = np.zeros((128, 8, 4), np.float32)
    cc = np.arange(128)
    for h in range(8):
        for qb in range(4):
            cb[:, h, qb] = sn[h].astype(np.float64) * (16 * cc + 15.5 - 512 * qb)
    c["cbias"] = cb.reshape(128, 32)
    qa = np.zeros((16, 3, 512), np.float32)
    for h in range(16):
        v = (-(sl[h].astype(np.float64)) * np.arange(512)).astype(np.float32)
        v1 = _bf16_round(v)
        v2 = _bf16_round(v - v1)
        v3 = _bf16_round(v - v1 - v2)
        qa[h, 0], qa[h, 1], qa[h, 2] = v1, v2, v3
    c["qalibi"] = qa
    key = np.arange(T)
    c["e32"] = (key[None, :] // 64 == np.arange(32)[:, None]).astype(np.float32)
    c["e8"] = (key[None, :] // 256 == np.arange(8)[:, None]).astype(np.float32)
    c["ones3"] = np.ones((3, T), np.float32)
    c["zeros32"] = np.zeros((32, T), np.float32)
    cm = np.zeros((128, T), np.float32)
    cidx = np.arange(128)[:, None]
    cm[:] = np.where(16 * cidx + 31 <= key[None, :], 0.0, -BIG)
    c["cmask"] = cm
    ci = np.arange(127)[:, None] * 16
    sj = np.arange(32)[None, :] * 64
    ov = np.clip(np.minimum(ci + 32, sj + 64) - np.maximum(ci, sj), 0, None)
    M = np.zeros((128, 32), np.float32)
    M[:127] = ov / 32.0
    c["cmp2slc"] = M
    blk = np.arange(32)[None, None, :]
    t = (np.arange(16)[None, :, None] * 128 + np.arange(128)[:, None, None])
    cur = t // 64
    forced = (blk == 0) | (blk == cur) | (blk == cur - 1)
    valid = blk <= cur
    c["nsa_mult"] = np.where(forced | ~valid, 0.0, 1.0).astype(np.float32).reshape(128, 16 * 32)
    c["nsa_add"] = np.where(forced, 1e9, np.where(valid, 0.0, -1e30)).astype(np.float32).reshape(128, 16 * 32)
    n8 = np.arange(8)[None, None, :]
    curm = t // 256
    c["moba_add"] = np.broadcast_to(np.where(n8 < curm, 0.0, -1e30), (128, 16, 8)).astype(np.float32).reshape(128, 128).copy()
    return c


CONST_SHAPES = dict(ident=[128, 128], tri_c=[128, 128], tri_a=[128, 128], ones64=[64, 64], bd_ones=[128, 128], abias=[128, 304],
                    cbias=[128, 32], qalibi=[16, 3, 512], e32=[32, T], e8=[8, T], ones3=[3, T], zeros32=[32, T],
                    cmask=[128, T], cmp2slc=[128, 32], nsa_mult=[128, 512], nsa_add=[128, 512], moba_add=[128, 128])


def build_program(nseq=2, phases=("all",), depth=DEPTH, dbg=None):
    nc = bass.Bass("TRN2", target_bir_lowering=False)
    NT = nseq * T
    NTT = NT // 128
    S = Sched()
    es = ExitStack()

    def dram_in(name, shape, dt=F32):
        return nc.dram_tensor(name, list(shape), dt, kind="ExternalInput").ap()

    x_d = dram_in("x", [NT, D])
    normg_d = dram_in("norm_g", [DEPTH, 3, D])
    w1_d = dram_in("ffn_w1", [DEPTH, 2, D, DFF])
    w3_d = dram_in("ffn_w3", [DEPTH, 2, D, DFF])
    w2_d = dram_in("ffn_w2", [DEPTH, 2, DFF, D])
    win_d = dram_in("w_in", [DEPTH, D, INC])
    gqn_d = dram_in("g_qk_nsa", [DEPTH, 4, 64])
    gqnT_d = dram_in("g_qk_nsaT", [DEPTH, 64, 4])
    gqm_d = dram_in("g_qk_moba", [DEPTH, 2, 64])
    gqmT_d = dram_in("g_qk_mobaT", [DEPTH, 64, 2])
    posT_d = dram_in("cmp_posT", [DEPTH, 2, 64, 32])
    cw1_d = dram_in("cmp_w1", [DEPTH, 2, 2048, 256])
    cw2_d = dram_in("cmp_w2", [DEPTH, 2, 256, 64])
    wupn_d = dram_in("w_up_nsa", [DEPTH, 512, D])
    wupm_d = dram_in("w_up_moba", [DEPTH, 512, D])
    wout_d = dram_in("w_out", [DEPTH, D, D])
    C = {k: dram_in(k, v) for k, v in CONST_SHAPES.items()}
    y_d = nc.dram_tensor("y", [NT, D], F32, kind="ExternalOutput").ap()
    dbg_d = {}
    if dbg:
        for k, shp in dbg.items():
            dbg_d[k] = nc.dram_tensor("dbg_" + k, list(shp), F32, kind="ExternalOutput").ap()

    def sb(name, shape, dt):
        return es.enter_context(nc.sbuf_tensor(name, list(shape), dt))

    banks = []
    for i in range(8):
        t = es.enter_context(nc.psum_tensor(f"ps{i}", [128, 512], F32))
        banks.append((t, Res(f"ps{i}")))
    ps_ring = Ring(banks)
    acc_banks = banks[0:4]
    acc_ring = Ring(banks[0:4])
    sc_ring = Ring(banks[4:7])
    misc_ring = Ring(banks[7:8])

    ident_b = sb("ident_b", [128, 128], BF16)
    r_ident = Res("ident")
    S.op("pool", lambda e: e.dma_start(out=ident_b[:], in_=C["ident"][:, :]), writes=[r_ident], dma=True)
    stat = [(sb(f"stat{i}", [128, 4], F32), Res(f"stat{i}")) for i in range(4)]
    stat_ring = Ring(stat)
    ARENA_EL = 105000
    arena_t = sb("arena", [128, ARENA_EL], BF16)
    A = Arena(arena_t, ARENA_EL)

    r_y = [Res(f"y{i}") for i in range(NTT)]
    r_x = [Res(f"x{i}") for i in range(NTT)]

    def rmsnorm_tile(x_ap, r_x_, g_ap, r_gres, out_ap, r_out, jk, r_jk):
        st, r_st = stat_ring.next()
        S.op("act", lambda e: e.activation(out=jk, in_=x_ap, func=AF.Square, accum_out=st[:, 0:1]),
             reads=[r_x_], writes=[r_jk, r_st])
        S.op("act", lambda e: e.activation(out=st[:, 1:2], in_=st[:, 0:1], func=AF.Sqrt, scale=1.0 / D, bias=EPS),
             reads=[r_st], writes=[r_st])
        S.op("dve", lambda e: e.reciprocal(out=st[:, 2:3], in_=st[:, 1:2]), reads=[r_st], writes=[r_st])
        S.op("dve", lambda e: e.scalar_tensor_tensor(out=out_ap, in0=x_ap, scalar=st[:, 2:3], in1=g_ap,
                                                     op0=ALU.mult, op1=ALU.mult),
             reads=[r_x_, r_st, r_gres], writes=[r_out])

    def transpose_tile(in_tile, r_in, out_ap3, r_out, nk=8, evac="act", ring=None):
        ps, r_ps = (ring or ps_ring).next()
        psb = ps[:].bitcast(BF16)
        fns = []
        for k in range(nk):
            fns.append(lambda e, k=k: e.transpose(out=psb[:, k * 128:(k + 1) * 128],
                                                  in_=in_tile[:, k * 128:(k + 1) * 128], identity=ident_b[:]))
        S.op("pe", fns, reads=[r_in, r_ident], writes=[r_ps])
        src = psb[:, 0:nk * 128].rearrange("p (k t) -> p k t", k=nk)
        if evac == "act":
            S.op("act", lambda e: e.copy(out=out_ap3, in_=src), reads=[r_ps], writes=[r_out])
        else:
            S.op("dve", lambda e: e.tensor_copy(out=out_ap3, in_=src), reads=[r_ps], writes=[r_out])

    def ffn_phase(l, j, src_d, r_src):
        S.barrier()
        A.off = 0
        w1_sb = A.alloc([128, 8, DFF], BF16)
        w3_sb = A.alloc([128, 8, DFF], BF16)
        w2_sb = A.alloc([128, NF, D], BF16)
        r_w1 = [Res() for f in range(NF)]
        r_w3 = [Res() for f in range(NF)]
        r_w2 = [Res() for f in range(NF)]
        g_rep = A.alloc([128, D], F32)
        r_g = Res()
        xn_ring = Ring([(A.alloc([128, D], F32), Res()) for i in range(2)])
        xr_ring = Ring([(A.alloc([128, D], F32), Res()) for i in range(2)])
        hb = [(A.alloc([128, D], BF16), Res()) for i in range(4)]
        hT2 = [A.alloc([128, 8, 512], BF16) for i in range(2)]
        r_hT2 = [[Res() for i in range(4)] for _ in range(2)]
        gT = A.alloc([128, NF, 512], BF16)
        r_gT = [Res() for f in range(NF)]
        su_ring = Ring([(A.alloc([128, 512], F32), Res()) for i in range(2)])
        junk = (A.alloc([128, D], BF16), Res())
        NB = NT // 512

        S.op("pool", lambda e: e.dma_start(out=g_rep, in_=normg_d[l, 2 * j, :].partition_broadcast(128)),
             writes=[r_g], dma=True)
        for f in range(NF):
            S.op("pool", lambda e, f=f: e.dma_start(
                out=w1_sb[:, :, f * 128:(f + 1) * 128],
                in_=w1_d[l, j, :, f * 128:(f + 1) * 128].rearrange("(k p) c -> p k c", p=128)),
                writes=[r_w1[f]], dma=True)
            S.op("pool", lambda e, f=f: e.dma_start(
                out=w3_sb[:, :, f * 128:(f + 1) * 128],
                in_=w3_d[l, j, :, f * 128:(f + 1) * 128].rearrange("(k p) c -> p k c", p=128)),
                writes=[r_w3[f]], dma=True)
        for f in range(NF):
            S.op("pool", lambda e, f=f: e.dma_start(out=w2_sb[:, f, :], in_=w2_d[l, j, f * 128:(f + 1) * 128, :]),
                 writes=[r_w2[f]], dma=True)

        def norm_part(blk):
            for i in range(4):
                tt = blk * 4 + i
                xa, r_xa = xn_ring.next()
                S.op("sp", lambda e, xa=xa, tt=tt: e.dma_start(out=xa, in_=src_d[tt * 128:(tt + 1) * 128, :]),
                     reads=[r_src[tt]], writes=[r_xa], dma=True)
                hbt, r_hb = hb[i]
                rmsnorm_tile(xa, r_xa, g_rep, r_g, hbt, r_hb, junk[0], junk[1])

        def transpose_part(blk):
            hT = hT2[blk % 2]
            for i in range(4):
                hbt, r_hb = hb[i]
                transpose_tile(hbt, r_hb, hT[:, :, i * 128:(i + 1) * 128], r_hT2[blk % 2][i])

        def up_part(blk):
            hT = hT2[blk % 2]
            r_hT = r_hT2[blk % 2]
            for f in range(NF):
                pu, r_pu = ps_ring.next()
                pv, r_pv = ps_ring.next()
                fns = [lambda e, k=k, f=f, pu=pu: e.matmul(pu[:], lhsT=w1_sb[:, k, f * 128:(f + 1) * 128],
                                                           rhs=hT[:, k, :], start=(k == 0), stop=(k == 7)) for k in range(8)]
                S.op("pe", fns, reads=[r_w1[f]] + r_hT, writes=[r_pu])
                fns = [lambda e, k=k, f=f, pv=pv: e.matmul(pv[:], lhsT=w3_sb[:, k, f * 128:(f + 1) * 128],
                                                           rhs=hT[:, k, :], start=(k == 0), stop=(k == 7)) for k in range(8)]
                S.op("pe", fns, reads=[r_w3[f]] + r_hT, writes=[r_pv])
                s_t, r_s = su_ring.next()
                S.op("act", lambda e, s_t=s_t, pu=pu: e.activation(out=s_t, in_=pu[:], func=AF.Silu),
                     reads=[r_pu], writes=[r_s])
                S.op("dve", lambda e, s_t=s_t, pv=pv, f=f: e.tensor_tensor(out=gT[:, f, :], in0=s_t, in1=pv[:],
                                                                             op=ALU.mult),
                     reads=[r_s, r_pv], writes=[r_gT[f]])

        def down_part(blk):
            for i in range(4):
                tt = blk * 4 + i
                xa, r_xa = xr_ring.next()
                S.op("sp", lambda e, xa=xa, tt=tt: e.dma_start(out=xa, in_=src_d[tt * 128:(tt + 1) * 128, :]),
                     reads=[r_src[tt]], writes=[r_xa], dma=True)
                for h in range(2):
                    po, r_po = ps_ring.next()
                    fns = [lambda e, f=f, po=po, i=i, h=h: e.matmul(
                        po[:], lhsT=gT[:, f, i * 128:(i + 1) * 128], rhs=w2_sb[:, f, h * 512:(h + 1) * 512],
                        start=(f == 0), stop=(f == NF - 1)) for f in range(NF)]
                    S.op("pe", fns, reads=r_gT + r_w2, writes=[r_po])
                    S.op("dve", lambda e, po=po, xa=xa, h=h: e.scalar_tensor_tensor(
                        out=xa[:, h * 512:(h + 1) * 512], in0=po[:], scalar=0.5, in1=xa[:, h * 512:(h + 1) * 512],
                        op0=ALU.mult, op1=ALU.add), reads=[r_po, r_xa], writes=[r_xa])
                S.op("sp", lambda e, xa=xa, tt=tt: e.dma_start(out=y_d[tt * 128:(tt + 1) * 128, :], in_=xa),
                     reads=[r_xa], writes=[r_y[tt]], dma=True)

        norm_part(0)
        transpose_part(0)
        for blk in range(NB):
            if blk + 1 < NB:
                norm_part(blk + 1)
            up_part(blk)
            if blk + 1 < NB:
                transpose_part(blk + 1)
            down_part(blk)

    def mix_phase(l, src_d, r_src):
        S.barrier()
        A.off = 0
        r_c = Res()
        tri_c = A.alloc([128, 128], BF16)
        tri_a = A.alloc([128, 128], BF16)
        ones64 = A.alloc([64, 64], BF16)
        bd_ones = A.alloc([128, 128], BF16)
        gtab = A.alloc([128, 4], F32)
        abias = A.alloc([128, 304], F32)
        cbias = A.alloc([128, 32], F32)
        cmask = A.alloc([128, T], BF16)
        nsa_mult = A.alloc([128, 16, 32], F32)
        nsa_add = A.alloc([128, 16, 32], F32)
        moba_add = A.alloc([128, 16, 8], F32)
        gq_n = A.alloc([64, 4], F32)
        gq_m = A.alloc([64, 2], F32)
        gkc_rep = A.alloc([128, 64], F32)
        g_rep = A.alloc([128, D], F32)
        for dst, src in ((tri_c, C["tri_c"][:, :]), (tri_a, C["tri_a"][:, :]), (ones64, C["ones64"][:, :]), (bd_ones, C["bd_ones"][:, :]),
                         (gtab[0:64, 0:1], gqn_d[l, 0, :].unsqueeze(1)), (gtab[64:128, 0:1], gqn_d[l, 0, :].unsqueeze(1)),
                         (gtab[0:64, 1:2], gqn_d[l, 2, :].unsqueeze(1)), (gtab[64:128, 1:2], gqn_d[l, 3, :].unsqueeze(1)),
                         (gtab[0:64, 2:3], gqm_d[l, 0, :].unsqueeze(1)), (gtab[64:128, 2:3], gqm_d[l, 0, :].unsqueeze(1)),
                         (gtab[0:64, 3:4], gqm_d[l, 1, :].unsqueeze(1)), (gtab[64:128, 3:4], gqm_d[l, 1, :].unsqueeze(1)),
                         (abias, C["abias"][:, :]), (cbias, C["cbias"][:, :]), (cmask, C["cmask"][:, :]),
                         (nsa_mult, C["nsa_mult"][:, :].rearrange("p (a b) -> p a b", a=16)),
                         (nsa_add, C["nsa_add"][:, :].rearrange("p (a b) -> p a b", a=16)),
                         (moba_add, C["moba_add"][:, :].rearrange("p (a b) -> p a b", a=16)),
                         (gq_n, gqnT_d[l, :, :]), (gq_m, gqmT_d[l, :, :]),
                         (gkc_rep, gqn_d[l, 1, :].partition_broadcast(128)),
                         (g_rep, normg_d[l, 1, :].partition_broadcast(128))):
            S.op("pool", lambda e, dst=dst, src=src: e.dma_start(out=dst, in_=src), writes=[r_c], dma=True)
        S.op("dve", lambda e: e.tensor_scalar(out=gq_n[:, 0:1], in0=gq_n[:, 0:1], scalar1=0.125, scalar2=None,
                                              op0=ALU.mult), reads=[r_c], writes=[r_c])
        S.op("dve", lambda e: e.tensor_scalar(out=gq_m[:, 0:1], in0=gq_m[:, 0:1], scalar1=0.125, scalar2=None,
                                              op0=ALU.mult), reads=[r_c], writes=[r_c])
        S.op("dve", lambda e: e.tensor_scalar(out=gtab[:, 0:1], in0=gtab[:, 0:1], scalar1=0.125, scalar2=None,
                                              op0=ALU.mult), reads=[r_c], writes=[r_c])
        S.op("dve", lambda e: e.tensor_scalar(out=gtab[:, 2:3], in0=gtab[:, 2:3], scalar1=0.125, scalar2=None,
                                              op0=ALU.mult), reads=[r_c], writes=[r_c])

        hT = A.alloc([128, 8, T], BF16)
        r_hT = [Res() for _ in range(16)]
        onT = A.alloc([128, 4, T], BF16)
        omT = A.alloc([128, 4, T], BF16)
        r_onT = [Res() for _ in range(16)]
        r_omT = [Res() for _ in range(16)]
        gsig = A.alloc([128, 16, 24], F32)
        r_gsig = Res()
        wch_ring = Ring([(A.alloc([128, 8, 256], BF16), Res()) for _ in range(3)])
        pT_ring = Ring([(A.alloc([128, 512], BF16), Res()) for _ in range(3)])
        tmpf_ring = Ring([(A.alloc([128, 512], F32), Res()) for _ in range(3)])
        sqb_ring = Ring([(A.alloc([128, 512], BF16), Res()) for _ in range(3)])
        xt_ring = Ring([(A.alloc([128, D], F32), Res()) for _ in range(3)])
        hb_ring = Ring([(A.alloc([128, D], BF16), Res()) for _ in range(3)])
        junk = (A.alloc([128, D], BF16), Res())
        sm_ring = Ring([(A.alloc([128, 16], F32), Res()) for _ in range(8)])
        region0 = A.off

        def load_w(src_ap3, ncols):
            w, r_w = wch_ring.next()
            S.op("pool", lambda e: e.dma_start(out=w[:, :, 0:ncols], in_=src_ap3), writes=[r_w], dma=True)
            return w, r_w

        def win_cols(c0, n):
            return win_d[l, :, c0:c0 + n].rearrange("(k p) c -> p k c", p=128)

        def proj_fm_pairs(pairs):
            items = [(pi, b) for pi in range(len(pairs)) for b in range(4)]
            wts = {}
            stt = {}

            def ensure_w(pi):
                if pi < len(pairs) and pi not in wts:
                    c0A, c0B = pairs[pi][0], pairs[pi][1]
                    w, r_w = wch_ring.next()
                    if c0B == c0A + 64:
                        S.op("pool", lambda e: e.dma_start(out=w[:, :, 0:128], in_=win_cols(c0A, 128)), writes=[r_w], dma=True)
                    else:
                        S.op("pool", lambda e: e.dma_start(out=w[:, :, 0:64], in_=win_cols(c0A, 64)), writes=[r_w], dma=True)
                        S.op("pool", lambda e: e.dma_start(out=w[:, :, 64:128], in_=win_cols(c0B, 64)), writes=[r_w], dma=True)
                    wts[pi] = (w, r_w)

            def mm(i):
                pi, b = items[i]
                ensure_w(pi)
                if b == 0:
                    ensure_w(pi + 1)
                w, r_w = wts[pi]
                ps, r_ps = ps_ring.next()
                fns = [lambda e, k=k: e.matmul(ps[:, :], lhsT=w[:, k, 0:128], rhs=hT[:, k, b * 512:(b + 1) * 512],
                                               start=(k == 0), stop=(k == 7)) for k in range(8)]
                S.op("pe", fns, reads=[r_w] + r_hT[4 * b:4 * b + 4], writes=[r_ps])
                c0A, c0B, dA, rA, dB, rB, gcol = pairs[pi]
                if gcol is None:
                    S.op("act", lambda e: e.copy(out=dA(b), in_=ps[0:64, :]), reads=[r_ps], writes=[rA(b)])
                    S.op("act", lambda e: e.copy(out=dB(b), in_=ps[64:128, :]), reads=[r_ps], writes=[rB(b)])
                    stt[i] = None
                    return
                sq, r_sq = sqb_ring.next()
                S.op("act", lambda e: e.activation(out=sq[:, :], in_=ps[:, :], func=AF.Square), reads=[r_ps], writes=[r_sq])
                stt[i] = (ps, r_ps, sq, r_sq, b)

            def rest(i):
                if stt[i] is None:
                    return
                ps, r_ps, sq, r_sq, b = stt[i]
                c0A, c0B, dA, rA, dB, rB, gcol = pairs[items[i][0]]
                p2, r_p2 = ps_ring.next()
                S.op("pe", lambda e: e.matmul(p2[:, :], lhsT=bd_ones[:, :], rhs=sq[:, :], start=True, stop=True),
                     reads=[r_sq, r_c], writes=[r_p2])
                tf, r_tf = tmpf_ring.next()
                S.op("act", lambda e: e.activation(out=tf[:, :], in_=p2[:, :], func=AF.Ln, scale=1.0 / 64, bias=EPS),
                     reads=[r_p2], writes=[r_tf])
                S.op("act", lambda e: e.activation(out=tf[:, :], in_=tf[:, :], func=AF.Exp, scale=-0.5),
                     reads=[r_tf], writes=[r_tf])
                S.op("dve", lambda e: e.scalar_tensor_tensor(out=dA(b), in0=ps[0:64, :], scalar=gcol[0:64, :], in1=tf[0:64, :],
                                                             op0=ALU.mult, op1=ALU.mult),
                     reads=[r_ps, r_tf, r_c], writes=[rA(b)])
                S.op("dve", lambda e: e.scalar_tensor_tensor(out=dB(b), in0=ps[64:128, :], scalar=gcol[64:128, :],
                                                             in1=tf[64:128, :], op0=ALU.mult, op1=ALU.mult),
                     reads=[r_ps, r_tf, r_c], writes=[rB(b)])

            mm(0)
            for i in range(len(items)):
                if i + 1 < len(items):
                    mm(i + 1)
                rest(i)

        def causal_tiles(qb):
            tl = []
            for kt in range(4 * qb + 4):
                c = kt - 4 * qb
                if c < 0:
                    tl.append((kt, 0, 512, None, None))
                else:
                    tl.append((kt, 128 * c, 512, "c", c))
            return tl

        def window_tiles(qb):
            tl = []
            for c in (0, 1, 2, 3, -1, -2, -3, -4):
                kt = 4 * qb + c
                if kt < 0:
                    continue
                if c >= 0:
                    tl.append((kt, 128 * c, 512, "c", c))
                else:
                    m = 4 + c
                    tl.append((kt, 0, 128 * (m + 1), "a", m))
            return tl

        def run_rounds(rounds):
            items = []
            for R in rounds:
                for ti, tile in enumerate(R["tiles"]):
                    items.append((R, ti, tile))

            def emit_qk(it):
                R, ti, (kt, c0, c1, tri, tu) = it
                ps, r_ps = sc_ring.next()
                kp = R.get("kpart", 128)
                K = R["krows"]
                qb = R["qb"]
                fns = [lambda e: e.matmul(ps[0:kp, c0:c1], lhsT=R["kT"][0:K, kt * 128:kt * 128 + kp],
                                          rhs=R["q"][0:K, qb * 512 + c0:qb * 512 + c1], start=True,
                                          stop=(tri is None and "mask" not in R))]
                reads = [R["r_k"]] + R["r_q"] + [r_c]
                if "mask" in R:
                    fns.append(lambda e: e.matmul(ps[0:kp, c0:c1], lhsT=ident_b[0:kp, 0:kp],
                                                  rhs=R["mask"][0:kp, qb * 512 + c0:qb * 512 + c1],
                                                  start=False, stop=True))
                if tri is not None:
                    tm = tri_c if tri == "c" else tri_a
                    fns.append(lambda e: e.matmul(ps[:, 128 * tu:128 * tu + 128], lhsT=ident_b[:, :], rhs=tm[:, :],
                                                  start=False, stop=True))
                S.op("pe", fns, reads=reads + [r_ident], writes=[r_ps])
                return ps, r_ps

            pend = emit_qk(items[0]) if items else None
            for idx, it in enumerate(items):
                R, ti, (kt, c0, c1, tri, tu) = it
                ps, r_ps = pend
                if idx + 1 < len(items):
                    pend = emit_qk(items[idx + 1])
                kp = R.get("kpart", 128)
                pT, r_pT = pT_ring.next()
                bias_ap = R["bias"](kt)
                S.op("act", lambda e, ps=ps, pT=pT, bias_ap=bias_ap, kp=kp, c0=c0, c1=c1: e.activation(
                    out=pT[0:kp, c0:c1], in_=ps[0:kp, c0:c1], func=AF.Exp, bias=bias_ap, scale=1.0),
                    reads=[r_ps, r_c], writes=[r_pT])
                us = [u for u in range(4) if c0 <= 128 * u < c1]
                nv = R["nv"]
                if ti == 0:
                    R["acc"] = acc_ring.next()
                acc, r_acc = R["acc"]
                fns = []
                V = R["V"](kt)
                nt = len(R["tiles"])
                for u in us:
                    fns.append(lambda e, u=u, acc=acc, V=V, first=(ti == 0 and u == us[0]),
                               last=(ti == nt - 1 and u == us[-1]), pT=pT, kp=kp:
                               e.matmul(acc[:, 128 * u:128 * u + nv], lhsT=pT[0:kp, 128 * u:128 * u + 128], rhs=V,
                                        start=first, stop=last))
                S.op("pe", fns, reads=[r_pT, R["r_v"]], writes=[r_acc])
                if ti == nt - 1:
                    R["evac"](acc[:].rearrange("p (u c) -> p u c", u=4), r_acc)

        sn_, sm_ = slopes_all()

        def dump(name, ap, reads):
            if name in dbg_d:
                dst = dbg_d[name][:, :]
                if len(ap.shape) == 3:
                    dst = dst.rearrange("p (a b) -> p a b", a=ap.shape[1])
                S.op("pool", lambda e: e.dma_start(out=dst[0:ap.shape[0]], in_=ap), reads=reads, writes=[Res()], dma=True)

        for s in range(nseq):
            tt0 = s * 16
            prev = None
            for i in range(16):
                xa, r_xa = xt_ring.next()
                S.op("sp", lambda e, xa=xa, i=i, tt0=tt0: e.dma_start(out=xa, in_=src_d[(tt0 + i) * 128:(tt0 + i + 1) * 128, :]),
                     reads=[r_src[tt0 + i]], writes=[r_xa], dma=True)
                hbt, r_hb = hb_ring.next()
                st, r_st = stat_ring.next()
                jk, r_jk = junk
                S.op("act", lambda e, xa=xa, st=st: e.activation(out=jk, in_=xa, func=AF.Square, accum_out=st[:, 0:1]),
                     reads=[r_xa], writes=[r_jk, r_st])
                S.op("act", lambda e, st=st: e.activation(out=st[:, 1:2], in_=st[:, 0:1], func=AF.Sqrt, scale=1.0 / D, bias=EPS),
                     reads=[r_st], writes=[r_st])
                if prev is not None:
                    prev()
                S.op("dve", lambda e, st=st: e.reciprocal(out=st[:, 2:3], in_=st[:, 1:2]), reads=[r_st], writes=[r_st])
                S.op("dve", lambda e, xa=xa, st=st, hbt=hbt: e.scalar_tensor_tensor(
                    out=hbt, in0=xa, scalar=st[:, 2:3], in1=g_rep, op0=ALU.mult, op1=ALU.mult),
                    reads=[r_xa, r_st, r_c], writes=[r_hb])
                ps, r_ps = ps_ring.next()
                psb = ps[:].bitcast(BF16)
                fns = [lambda e, k=k, psb=psb, hbt=hbt: e.transpose(out=psb[:, k * 128:(k + 1) * 128],
                                                                    in_=hbt[:, k * 128:(k + 1) * 128], identity=ident_b[:])
                       for k in range(8)]
                S.op("pe", fns, reads=[r_hb, r_ident], writes=[r_ps])

                def evac(psb=psb, r_ps=r_ps, i=i):
                    S.op("act", lambda e: e.copy(out=hT[:, :, i * 128:(i + 1) * 128],
                                                 in_=psb.rearrange("p (k t) -> p k t", k=8)), reads=[r_ps], writes=[r_hT[i]])
                prev = evac
            prev()

            w, r_w = load_w(win_cols(NSA_COLS["g"], 64), 64)
            for i in range(16):
                ps, r_ps = ps_ring.next()
                fns = [lambda e, k=k, ps=ps, i=i, w=w: e.matmul(ps[:, 0:24], lhsT=hT[:, k, i * 128:(i + 1) * 128],
                                                                rhs=w[:, k, 0:24], start=(k == 0), stop=(k == 7))
                       for k in range(8)]
                S.op("pe", fns, reads=[r_w, r_hT[i]], writes=[r_ps])
                S.op("act", lambda e, ps=ps, i=i: e.activation(out=gsig[:, i, :], in_=ps[:, 0:24], func=AF.Sigmoid),
                     reads=[r_ps], writes=[r_gsig])

            if dbg and s == 0 and l == 0:
                dump("gsig", gsig, [r_gsig])
            S.barrier()
            A.off = region0
            q_aug = [A.alloc([128, T], BF16) for _ in range(4)]
            r_q = [[Res() for _ in range(4)] for _ in range(4)]
            r_qs = [[Res() for _ in range(4)] for _ in range(4)]
            r_qst = Res()
            ks_aug = A.alloc([128, T], BF16)
            kw_aug = A.alloc([128, T], BF16)
            r_ks = Res()
            r_kw = Res()
            kcraw = A.alloc([64, T], BF16)
            r_kcraw = Res()
            vcraw = A.alloc([64, T], BF16)
            r_vcraw = Res()
            vsA = A.alloc([128, 16, 65], BF16)
            vwA = A.alloc([128, 16, 65], BF16)
            r_vs = Res()
            r_vw = Res()
            w1c = A.alloc([64, 32, 256], BF16)
            r_w1c = Res()
            w2c = A.alloc([128, 2, 64], BF16)
            posT = A.alloc([64, 32], BF16)
            r_w2c = Res()
            kc_aug = A.alloc([128, 128], BF16)
            r_kc = Res()
            kctm = A.alloc([128, 128], BF16)
            r_kctm = Res()
            vcA = A.alloc([128, 97], BF16)
            r_vc = Res()
            hid = A.alloc([128, 2, 128], BF16)
            r_hid = Res()
            pbias = A.alloc([128, 2], F32)
            r_pb = Res()
            oacc = A.alloc([128, 16, 256], F32)
            r_oacc = [Res() for _ in range(16)]
            impacc = A.alloc([128, 16, 32], F32)
            r_imp = [Res() for _ in range(16)]
            trin_all = [(A.alloc([128, 96], BF16), Res()) for _ in range(16)]
            ob_ring = Ring([(A.alloc([128, 256], BF16), Res()) for _ in range(2)])

            for g in range(2):
                for hl in range(4):
                    h = 4 * g + hl
                    S.op("pool", lambda e, hl=hl, h=h: e.dma_start(
                        out=q_aug[hl][96:99, :].rearrange("p (b i) -> p b i", b=4),
                        in_=C["qalibi"][h, :, :].unsqueeze(1).to_broadcast([3, 4, 512])), writes=[r_qst], dma=True)
                    if g == 0:
                        S.op("pool", lambda e, hl=hl: e.dma_start(out=q_aug[hl][64:96, :], in_=C["zeros32"][:, :]),
                             writes=r_qs[hl], dma=True)
                for dst, r_dst, mid in (((ks_aug, r_ks, C["e32"]), (kw_aug, r_kw, C["zeros32"])) if g == 0 else ()):
                    S.op("pool", lambda e, dst=dst, mid=mid: e.dma_start(out=dst[64:96, :], in_=mid[:, :]),
                         writes=[r_dst], dma=True)
                    S.op("pool", lambda e, dst=dst: e.dma_start(out=dst[96:99, :], in_=C["ones3"][:, :]),
                         writes=[r_dst], dma=True)
                if g == 0:
                    S.op("pool", lambda e: e.memset(kctm[:, 64:96], 0.0), writes=[r_kctm])
                    S.op("pool", lambda e: e.memset(kctm[:, 96:99], 1.0), writes=[r_kctm])
                    S.op("pool", lambda e: e.memset(kctm[:, 0:64], 0.0), writes=[r_kctm])
                    S.op("pool", lambda e: e.memset(vsA[:, :, 64:65], 1.0), writes=[r_vs])
                    S.op("pool", lambda e: e.memset(vwA[:, :, 64:65], 1.0), writes=[r_vw])
                    S.op("pool", lambda e: e.memset(vcA[:, 64:65], 1.0), writes=[r_vc])
                    S.op("pool", lambda e: e.dma_start(out=vcA[:, 65:97], in_=C["cmp2slc"][:, :]), writes=[r_vc], dma=True)
                    for tr, r_tr in trin_all:
                        S.op("pool", lambda e, tr=tr: e.memset(tr[:, 0:64], 0.0), writes=[r_tr])

                def load_cmp_w(kv):
                    S.op("pool", lambda e: e.dma_start(
                        out=w1c, in_=cw1_d[l, kv, :, :].rearrange("(l d) h -> d l h", d=64)), writes=[r_w1c], dma=True)
                    S.op("pool", lambda e: e.dma_start(
                        out=w2c, in_=cw2_d[l, kv, :, :].rearrange("(a p) d -> p a d", p=128)), writes=[r_w2c], dma=True)
                    S.op("pool", lambda e: e.dma_start(out=posT, in_=posT_d[l, kv, :, :]), writes=[r_w2c], dma=True)

                load_cmp_w(0)
                proj_fm_pairs([(NSA_COLS["kc"] + 64 * g, NSA_COLS["vc"] + 64 * g,
                                lambda b: kcraw[0:64, b * 512:(b + 1) * 512], lambda b: r_kcraw,
                                lambda b: vcraw[0:64, b * 512:(b + 1) * 512], lambda b: r_vcraw, None)])
                for kv in range(2):
                    craw, r_craw = (kcraw, r_kcraw) if kv == 0 else (vcraw, r_vcraw)
                    if kv == 1:
                        load_cmp_w(1)
                    for hh in range(2):
                        ps, r_ps = ps_ring.next()
                        fns = [lambda e, ll=ll, ps=ps, hh=hh, craw=craw: e.matmul(
                            ps[:, 0:127], lhsT=w1c[:, ll, hh * 128:(hh + 1) * 128],
                            rhs=craw[0:64, ll:ll + 16 * 126 + 1:16], start=(ll == 0), stop=(ll == 31)) for ll in range(32)]
                        fns += [lambda e, ll=ll, ps=ps, hh=hh: e.matmul(
                            ps[:, 128:129], lhsT=w1c[:, ll, hh * 128:(hh + 1) * 128],
                            rhs=posT[:, ll:ll + 1], start=(ll == 0), stop=(ll == 31)) for ll in range(32)]
                        S.op("pe", fns, reads=[r_w1c, r_w2c, r_craw], writes=[r_ps])
                        S.op("dve", lambda e, ps=ps, hh=hh: e.tensor_copy(out=pbias[:, hh:hh + 1], in_=ps[:, 128:129]),
                             reads=[r_ps], writes=[r_pb])
                        xh, r_xh = tmpf_ring.next()
                        x2, r_x2 = tmpf_ring.next()
                        S.op("dve", lambda e, ps=ps, hh=hh, xh=xh: e.tensor_scalar(
                            out=xh[:, 0:127], in0=ps[:, 0:127], scalar1=pbias[:, hh:hh + 1], scalar2=None, op0=ALU.add),
                            reads=[r_ps, r_pb], writes=[r_xh])
                        S.op("dve", lambda e, xh=xh, x2=x2: e.tensor_tensor(out=x2[:, 0:127], in0=xh[:, 0:127],
                                                                            in1=xh[:, 0:127], op=ALU.mult),
                             reads=[r_xh], writes=[r_x2])
                        S.op("dve", lambda e, x2=x2: e.tensor_scalar(out=x2[:, 0:127], in0=x2[:, 0:127], scalar1=0.044715,
                                                                     scalar2=1.0, op0=ALU.mult, op1=ALU.add),
                             reads=[r_x2], writes=[r_x2])
                        S.op("dve", lambda e, xh=xh, x2=x2: e.tensor_tensor(out=x2[:, 0:127], in0=x2[:, 0:127],
                                                                            in1=xh[:, 0:127], op=ALU.mult),
                             reads=[r_xh, r_x2], writes=[r_x2])
                        S.op("act", lambda e, x2=x2: e.activation(out=x2[:, 0:127], in_=x2[:, 0:127], func=AF.Tanh,
                                                                  scale=0.7978845608028654),
                             reads=[r_x2], writes=[r_x2])
                        S.op("dve", lambda e, xh=xh, x2=x2, hh=hh: e.scalar_tensor_tensor(
                            out=hid[:, hh, 0:127], in0=x2[:, 0:127], scalar=1.0, in1=xh[:, 0:127],
                            op0=ALU.add, op1=ALU.mult), reads=[r_xh, r_x2], writes=[r_hid])
                    ps, r_ps = ps_ring.next()
                    fns = [lambda e, hh=hh, ps=ps: e.matmul(ps[0:127, 0:64], lhsT=hid[:, hh, 0:127], rhs=w2c[:, hh, :],
                                                            start=(hh == 0), stop=(hh == 1)) for hh in range(2)]
                    S.op("pe", fns, reads=[r_hid, r_w2c], writes=[r_ps])
                    if kv == 1:
                        S.op("dve", lambda e, ps=ps: e.tensor_scalar(out=vcA[0:127, 0:64], in0=ps[0:127, 0:64],
                                                                     scalar1=0.5, scalar2=None, op0=ALU.mult),
                             reads=[r_ps], writes=[r_vc])
                    else:
                        tf, r_tf = tmpf_ring.next()
                        st, r_st = sm_ring.next()
                        S.op("dve", lambda e, ps=ps, tf=tf: e.tensor_scalar(out=tf[0:127, 0:64], in0=ps[0:127, 0:64],
                                                                            scalar1=0.5, scalar2=None, op0=ALU.mult),
                             reads=[r_ps], writes=[r_tf])
                        S.op("act", lambda e, tf=tf, st=st: e.activation(out=tf[0:127, 64:128], in_=tf[0:127, 0:64],
                                                                         func=AF.Square, accum_out=st[0:127, 0:1]),
                             reads=[r_tf], writes=[r_tf, r_st])
                        S.op("act", lambda e, st=st: e.activation(out=st[0:127, 1:2], in_=st[0:127, 0:1], func=AF.Sqrt,
                                                                  scale=1.0 / 64, bias=EPS), reads=[r_st], writes=[r_st])
                        S.op("dve", lambda e, st=st: e.reciprocal(out=st[0:127, 2:3], in_=st[0:127, 1:2]),
                             reads=[r_st], writes=[r_st])
                        S.op("dve", lambda e, tf=tf, st=st: e.scalar_tensor_tensor(
                            out=kctm[0:127, 0:64], in0=tf[0:127, 0:64], scalar=st[0:127, 2:3], in1=gkc_rep[0:127, :],
                            op0=ALU.mult, op1=ALU.mult), reads=[r_tf, r_st, r_c], writes=[r_kctm])
                        ps2, r_ps2 = ps_ring.next()
                        psb2 = ps2[:].bitcast(BF16)
                        S.op("pe", lambda e, psb2=psb2: e.transpose(out=psb2[0:99, 0:128], in_=kctm[:, 0:99],
                                                                    identity=ident_b[:]),
                             reads=[r_kctm, r_ident], writes=[r_ps2])
                        S.op("dve", lambda e, psb2=psb2: e.tensor_copy(out=kc_aug[0:99, 0:128], in_=psb2[0:99, 0:128]),
                             reads=[r_ps2], writes=[r_kc])

                prs = []
                for hp in range(2):
                    hA, hB = 2 * hp, 2 * hp + 1
                    prs.append((NSA_COLS["q"] + 64 * (4 * g + hA), NSA_COLS["q"] + 64 * (4 * g + hB),
                                lambda b, hA=hA: q_aug[hA][0:64, b * 512:(b + 1) * 512], lambda b, hA=hA: r_q[hA][b],
                                lambda b, hB=hB: q_aug[hB][0:64, b * 512:(b + 1) * 512], lambda b, hB=hB: r_q[hB][b],
                                gtab[:, 0:1]))
                prs.append((NSA_COLS["ks"] + 64 * g, NSA_COLS["kw"] + 64 * g,
                            lambda b: ks_aug[0:64, b * 512:(b + 1) * 512], lambda b: r_ks,
                            lambda b: kw_aug[0:64, b * 512:(b + 1) * 512], lambda b: r_kw, gtab[:, 1:2]))
                proj_fm_pairs(prs)
                for nm, dstA, r_dst in (("vs", vsA, r_vs), ("vw", vwA, r_vw)):
                    w, r_w = load_w(win_cols(NSA_COLS[nm] + 64 * g, 64), 64)
                    for i in range(16):
                        ps, r_ps = ps_ring.next()
                        fns = [lambda e, k=k, ps=ps, i=i, w=w: e.matmul(ps[:, 0:64], lhsT=hT[:, k, i * 128:(i + 1) * 128],
                                                                        rhs=w[:, k, 0:64], start=(k == 0), stop=(k == 7))
                               for k in range(8)]
                        S.op("pe", fns, reads=[r_w, r_hT[i]], writes=[r_ps])
                        S.op("act", lambda e, ps=ps, i=i, dstA=dstA: e.copy(out=dstA[:, i, 0:64], in_=ps[:, 0:64]),
                             reads=[r_ps], writes=[r_dst])
                def mk_cmp_evac(hl, qb):
                    h = 4 * g + hl

                    def ev(acc3, r_acc):
                        tts = slice(4 * qb, 4 * qb + 4)
                        r_o = r_oacc[4 * qb:4 * qb + 4]
                        r_i = r_imp[4 * qb:4 * qb + 4]
                        st, r_st = sm_ring.next()
                        rd = st[:, 4:8].unsqueeze(2)
                        cf = st[:, 8:12].unsqueeze(2)
                        S.op("dve", lambda e: e.tensor_scalar(out=st[:, 0:4].unsqueeze(2), in0=acc3[:, :, 64:65], scalar1=1e-30,
                                                              scalar2=None, op0=ALU.add), reads=[r_acc], writes=[r_st])
                        S.op("dve", lambda e: e.reciprocal(out=st[:, 4:8], in_=st[:, 0:4]), reads=[r_st], writes=[r_st])
                        S.op("dve", lambda e: e.tensor_tensor(out=cf, in0=rd, in1=gsig[:, tts, 3 * h:3 * h + 1], op=ALU.mult),
                             reads=[r_st, r_gsig], writes=[r_st])
                        S.op("dve", lambda e: e.tensor_tensor(out=oacc[:, tts, hl * 64:(hl + 1) * 64], in0=acc3[:, :, 0:64],
                                                              in1=cf.to_broadcast([128, 4, 64]), op=ALU.mult),
                             reads=[r_acc, r_st], writes=r_o)
                        if hl == 0:
                            S.op("dve", lambda e: e.tensor_tensor(out=impacc[:, tts, :], in0=acc3[:, :, 65:97],
                                                                  in1=rd.to_broadcast([128, 4, 32]), op=ALU.mult),
                                 reads=[r_acc, r_st], writes=r_i)
                        else:
                            tf, r_tf = tmpf_ring.next()
                            tf3 = tf[:, 0:128].rearrange("p (u c) -> p u c", u=4)
                            S.op("dve", lambda e: e.tensor_tensor(out=tf3, in0=acc3[:, :, 65:97],
                                                                  in1=rd.to_broadcast([128, 4, 32]), op=ALU.mult),
                                 reads=[r_acc, r_st], writes=[r_tf])
                            S.op("pool", lambda e: e.tensor_tensor(out=impacc[:, tts, :], in0=impacc[:, tts, :], in1=tf3,
                                                                   op=ALU.add), reads=[r_tf] + r_i, writes=r_i)
                    return ev

                rounds = []
                for hl in range(4):
                    h = 4 * g + hl
                    for qb in range(4):
                        rounds.append(dict(q=q_aug[hl], r_q=[r_q[hl][qb], r_qst], krows=99, kT=kc_aug, r_k=r_kc, qb=qb,
                                           tiles=[(0, 0, 512, None, None)], kpart=127, mask=cmask,
                                           V=lambda kt: vcA[0:127, 0:97], r_v=r_vc, nv=97,
                                           bias=lambda kt, h=h, qb=qb: cbias[0:127, 4 * h + qb:4 * h + qb + 1],
                                           evac=mk_cmp_evac(hl, qb)))
                run_rounds(rounds)
                if dbg and s == 0 and l == 0 and g == 0:
                    dump("oacc_cmp", oacc, r_oacc)
                    dump("kc_aug", kc_aug, [r_kc])
                    dump("vcA", vcA, [r_vc])
                    dump("impacc", impacc, r_imp)

                sel_tr = []
                for tt in range(16):
                    sc, r_sc = tmpf_ring.next()
                    st, r_st = sm_ring.next()
                    tr, r_tr = trin_all[tt]
                    S.op("dve", lambda e, sc=sc, tt=tt: e.tensor_tensor(out=sc[:, 0:32], in0=impacc[:, tt, :],
                                                                        in1=nsa_mult[:, tt, :], op=ALU.mult),
                         reads=[r_imp[tt], r_c], writes=[r_sc])
                    S.op("dve", lambda e, sc=sc, tt=tt: e.tensor_tensor(out=sc[:, 0:32], in0=sc[:, 0:32],
                                                                        in1=nsa_add[:, tt, :], op=ALU.add),
                         reads=[r_sc, r_c], writes=[r_sc])
                    S.op("dve", lambda e, sc=sc, st=st: e.max(out=st[:, 0:8], in_=sc[:, 0:32]), reads=[r_sc], writes=[r_st])
                    S.op("dve", lambda e, sc=sc, st=st, tr=tr: e.tensor_scalar(
                        out=tr[:, 64:96], in0=sc[:, 0:32], scalar1=st[:, 7:8], scalar2=-BIG, op0=ALU.is_lt, op1=ALU.mult),
                        reads=[r_sc, r_st], writes=[r_tr])

                def emit_sel_transposes():
                    for tt in range(16):
                        tr, r_tr = trin_all[tt]
                        ps, r_ps = misc_ring.next()
                        psb = ps[:].bitcast(BF16)
                        S.op("pe", lambda e, psb=psb, tr=tr: e.transpose(out=psb[0:96, 0:128], in_=tr[:, 0:96],
                                                                         identity=ident_b[:]),
                             reads=[r_tr, r_ident], writes=[r_ps])
                        for hl in range(4):
                            S.op("dve", lambda e, psb=psb, hl=hl, tt=tt: e.tensor_copy(
                                out=q_aug[hl][64:96, tt * 128:(tt + 1) * 128], in_=psb[64:96, 0:128]),
                                reads=[r_ps], writes=[r_qs[hl][tt // 4]])

                def mk_evac(hl, qb, br):
                    h = 4 * g + hl

                    def ev(acc3, r_acc):
                        tts = slice(4 * qb, 4 * qb + 4)
                        r_o = r_oacc[4 * qb:4 * qb + 4]
                        st, r_st = sm_ring.next()
                        rd = st[:, 4:8].unsqueeze(2)
                        cf = st[:, 8:12].unsqueeze(2)
                        S.op("dve", lambda e: e.reciprocal(out=rd, in_=acc3[:, :, 64:65]), reads=[r_acc], writes=[r_st])
                        S.op("dve", lambda e: e.tensor_tensor(out=cf, in0=rd, in1=gsig[:, tts, 3 * h + br:3 * h + br + 1],
                                                              op=ALU.mult), reads=[r_st, r_gsig], writes=[r_st])
                        tf, r_tf = tmpf_ring.next()
                        tf3 = tf[:, 0:256].rearrange("p (u c) -> p u c", u=4)
                        S.op("dve", lambda e: e.tensor_tensor(out=tf3, in0=acc3[:, :, 0:64], in1=cf.to_broadcast([128, 4, 64]),
                                                              op=ALU.mult), reads=[r_acc, r_st], writes=[r_tf])
                        S.op("pool", lambda e: e.tensor_tensor(out=oacc[:, tts, hl * 64:(hl + 1) * 64],
                                                               in0=oacc[:, tts, hl * 64:(hl + 1) * 64], in1=tf3, op=ALU.add),
                             reads=[r_tf] + r_o, writes=r_o)
                    return ev

                rounds = []
                for hl in range(4):
                    h = 4 * g + hl
                    for qb in range(4):
                        rounds.append(dict(q=q_aug[hl], r_q=[r_q[hl][qb], r_qst], krows=99, kT=kw_aug, r_k=r_kw, qb=qb,
                                           tiles=window_tiles(qb), V=lambda kt: vwA[:, kt, :], r_v=r_vw, nv=65,
                                           bias=lambda kt, h=h, qb=qb: abias[:, h * 19 + (kt - 4 * qb + 15):h * 19 + (kt - 4 * qb + 15) + 1],
                                           evac=mk_evac(hl, qb, 2)))
                run_rounds(rounds)
                rounds = []
                emit_sel_transposes()
                if dbg and s == 0 and l == 0 and g == 0:
                    dump("oacc_win", oacc, r_oacc)
                for hl in range(4):
                    h = 4 * g + hl
                    for qb in range(4):
                        rounds.append(dict(q=q_aug[hl], r_q=[r_q[hl][qb], r_qs[hl][qb], r_qst], krows=99, kT=ks_aug,
                                           r_k=r_ks, qb=qb, tiles=causal_tiles(qb), V=lambda kt: vsA[:, kt, :], r_v=r_vs,
                                           nv=65,
                                           bias=lambda kt, h=h, qb=qb: abias[:, h * 19 + (kt - 4 * qb + 15):h * 19 + (kt - 4 * qb + 15) + 1],
                                           evac=mk_evac(hl, qb, 1)))
                run_rounds(rounds)
                if dbg and s == 0 and l == 0 and g == 0:
                    dump("oacc_all", oacc, r_oacc)
                    dump("q0", q_aug[0], [r_q[0][b] for b in range(4)] + [r_qs[0][b] for b in range(4)] + [r_qst])
                    dump("ks_aug", ks_aug, [r_ks])
                for tt in range(16):
                    ob, r_ob = ob_ring.next()
                    S.op("act", lambda e, ob=ob, tt=tt: e.copy(out=ob, in_=oacc[:, tt, :]), reads=[r_oacc[tt]], writes=[r_ob])
                    transpose_tile(ob, r_ob, onT[:, 2 * g:2 * g + 2, tt * 128:(tt + 1) * 128], r_onT[tt], nk=2,
                                   evac="dve", ring=misc_ring)

            S.barrier()
            A.off = region0
            q_aug = [A.alloc([128, T], BF16) for _ in range(4)]
            k_aug = [A.alloc([128, T], BF16) for _ in range(4)]
            r_q = [[Res() for _ in range(4)] for _ in range(4)]
            r_qs = [[Res() for _ in range(4)] for _ in range(4)]
            r_qst = Res()
            r_k = [Res() for _ in range(4)]
            vmA = A.alloc([128, 16, 4, 65], BF16)
            r_vm = Res()
            kmean_f = A.alloc([64, 4, 8], F32)
            kmean_b = A.alloc([64, 4, 8], BF16)
            r_km = Res()
            omb = A.alloc([128, 16, 256], BF16)
            r_omb = [Res() for _ in range(16)]
            trin_ring = Ring([(A.alloc([128, 72], BF16), Res()) for _ in range(4)])
            for hf in range(2):
                if hf == 0:
                    for tr, r_tr in trin_ring.items:
                        S.op("pool", lambda e, tr=tr: e.memset(tr[:, 0:64], 0.0), writes=[r_tr])
                    S.op("pool", lambda e: e.memset(vmA[:, :, :, 64:65], 1.0), writes=[r_vm])
                for hl in range(4):
                    h = 4 * hf + hl
                    S.op("pool", lambda e, hl=hl, h=h: e.dma_start(
                        out=q_aug[hl][72:75, :].rearrange("p (b i) -> p b i", b=4),
                        in_=C["qalibi"][8 + h, :, :].unsqueeze(1).to_broadcast([3, 4, 512])), writes=[r_qst], dma=True)
                    if hf == 0:
                        S.op("pool", lambda e, hl=hl: e.dma_start(out=q_aug[hl][64:72, :], in_=C["zeros32"][0:8, :]),
                             writes=r_qs[hl], dma=True)
                        S.op("pool", lambda e, hl=hl: e.dma_start(out=k_aug[hl][64:72, :], in_=C["e8"][:, :]),
                             writes=[r_k[hl]], dma=True)
                        S.op("pool", lambda e, hl=hl: e.dma_start(out=k_aug[hl][72:75, :], in_=C["ones3"][:, :]),
                             writes=[r_k[hl]], dma=True)
                prs = []
                for hp in range(2):
                    hA, hB = 2 * hp, 2 * hp + 1
                    prs.append((MOBA_COLS["q"] + 64 * (4 * hf + hA), MOBA_COLS["q"] + 64 * (4 * hf + hB),
                                lambda b, hA=hA: q_aug[hA][0:64, b * 512:(b + 1) * 512], lambda b, hA=hA: r_q[hA][b],
                                lambda b, hB=hB: q_aug[hB][0:64, b * 512:(b + 1) * 512], lambda b, hB=hB: r_q[hB][b],
                                gtab[:, 2:3]))
                for hp in range(2):
                    hA, hB = 2 * hp, 2 * hp + 1
                    prs.append((MOBA_COLS["k"] + 64 * (4 * hf + hA), MOBA_COLS["k"] + 64 * (4 * hf + hB),
                                lambda b, hA=hA: k_aug[hA][0:64, b * 512:(b + 1) * 512], lambda b, hA=hA: r_k[hA],
                                lambda b, hB=hB: k_aug[hB][0:64, b * 512:(b + 1) * 512], lambda b, hB=hB: r_k[hB],
                                gtab[:, 3:4]))
                proj_fm_pairs(prs)
                for hl in range(4):
                    S.op("dve", lambda e, hl=hl: e.tensor_reduce(
                        out=kmean_f[:, hl, :], in_=k_aug[hl][0:64, :].rearrange("p (n k) -> p n k", k=256),
                        axis=AX.X, op=ALU.add), reads=[r_k[hl]], writes=[r_km])
                S.op("dve", lambda e: e.tensor_scalar(out=kmean_b, in0=kmean_f, scalar1=1.0 / 256, scalar2=None,
                                                      op0=ALU.mult), reads=[r_km], writes=[r_km])
                w, r_w = load_w(win_cols(MOBA_COLS["v"] + 256 * hf, 256), 256)
                for i in range(16):
                    ps, r_ps = ps_ring.next()
                    fns = [lambda e, k=k, ps=ps, i=i, w=w: e.matmul(ps[:, 0:256], lhsT=hT[:, k, i * 128:(i + 1) * 128],
                                                                    rhs=w[:, k, 0:256], start=(k == 0), stop=(k == 7))
                           for k in range(8)]
                    S.op("pe", fns, reads=[r_w, r_hT[i]], writes=[r_ps])
                    S.op("act", lambda e, ps=ps, i=i: e.copy(out=vmA[:, i, :, 0:64],
                                                             in_=ps[:, 0:256].rearrange("p (h d) -> p h d", d=64)),
                         reads=[r_ps], writes=[r_vm])
                for tt in range(16):
                    cur = tt // 2
                    if tt >= 8:
                        ps, r_ps = misc_ring.next()
                        fns = [lambda e, hl=hl, ps=ps, tt=tt: e.matmul(ps[:, hl * 8:(hl + 1) * 8],
                                                                       lhsT=q_aug[hl][0:64, tt * 128:(tt + 1) * 128],
                                                                       rhs=kmean_b[:, hl, :], start=True, stop=True)
                               for hl in range(4)]
                        S.op("pe", fns, reads=[r_km] + [r_q[hl][tt // 4] for hl in range(4)], writes=[r_ps])
                        sc, r_sc = tmpf_ring.next()
                        for hl in range(4):
                            S.op("dve", lambda e, hl=hl, ps=ps, sc=sc, tt=tt: e.tensor_tensor(
                                out=sc[:, hl * 8:(hl + 1) * 8], in0=ps[:, hl * 8:(hl + 1) * 8], in1=moba_add[:, tt, :],
                                op=ALU.add), reads=[r_ps, r_c], writes=[r_sc])
                    for hl in range(4):
                        tr, r_tr = trin_ring.next()
                        S.op("pool", lambda e, tr=tr, cur=cur: e.memset(tr[:, 64 + cur:65 + cur], 0.0), writes=[r_tr])
                        if cur < 7:
                            S.op("pool", lambda e, tr=tr, cur=cur: e.memset(tr[:, 65 + cur:72], -BIG), writes=[r_tr])
                        if tt < 8:
                            if cur > 0:
                                S.op("pool", lambda e, tr=tr, cur=cur: e.memset(tr[:, 64:64 + cur], 0.0), writes=[r_tr])
                        else:
                            st, r_st = sm_ring.next()
                            S.op("dve", lambda e, sc=sc, st=st, hl=hl: e.max(out=st[:, 0:8], in_=sc[:, hl * 8:(hl + 1) * 8]),
                                 reads=[r_sc], writes=[r_st])
                            S.op("dve", lambda e, sc=sc, st=st, tr=tr, hl=hl, cur=cur: e.tensor_scalar(
                                out=tr[:, 64:64 + cur], in0=sc[:, hl * 8:hl * 8 + cur], scalar1=st[:, 2:3], scalar2=-BIG,
                                op0=ALU.is_lt, op1=ALU.mult), reads=[r_sc, r_st], writes=[r_tr])
                        ps2, r_ps2 = ps_ring.next()
                        psb = ps2[:].bitcast(BF16)
                        S.op("pe", lambda e, psb=psb, tr=tr: e.transpose(out=psb[0:72, 0:128], in_=tr[:, 0:72],
                                                                         identity=ident_b[:]),
                             reads=[r_tr, r_ident], writes=[r_ps2])
                        S.op("dve", lambda e, psb=psb, hl=hl, tt=tt: e.tensor_copy(
                            out=q_aug[hl][64:72, tt * 128:(tt + 1) * 128], in_=psb[64:72, 0:128]),
                            reads=[r_ps2], writes=[r_qs[hl][tt // 4]])

                def mk_evac_m(hl, qb):
                    def ev(acc3, r_acc):
                        tts = slice(4 * qb, 4 * qb + 4)
                        st, r_st = sm_ring.next()
                        rd = st[:, 4:8].unsqueeze(2)
                        S.op("dve", lambda e: e.reciprocal(out=rd, in_=acc3[:, :, 64:65]), reads=[r_acc], writes=[r_st])
                        S.op("dve", lambda e: e.tensor_tensor(out=omb[:, tts, hl * 64:(hl + 1) * 64], in0=acc3[:, :, 0:64],
                                                              in1=rd.to_broadcast([128, 4, 64]), op=ALU.mult),
                             reads=[r_acc, r_st], writes=r_omb[4 * qb:4 * qb + 4])
                    return ev

                rounds = []
                for hl in range(4):
                    h = 4 * hf + hl
                    for qb in range(4):
                        rounds.append(dict(q=q_aug[hl], r_q=[r_q[hl][qb], r_qs[hl][qb], r_qst], krows=75, kT=k_aug[hl],
                                           r_k=r_k[hl], qb=qb, tiles=causal_tiles(qb),
                                           V=lambda kt, hl=hl: vmA[:, kt, hl, :], r_v=r_vm, nv=65,
                                           bias=lambda kt, h=h, qb=qb: abias[:, (8 + h) * 19 + (kt - 4 * qb + 15):(8 + h) * 19 + (kt - 4 * qb + 15) + 1],
                                           evac=mk_evac_m(hl, qb)))
                run_rounds(rounds)
                for tt in range(16):
                    transpose_tile(omb[:, tt, :], r_omb[tt], omT[:, 2 * hf:2 * hf + 2, tt * 128:(tt + 1) * 128],
                                   r_omT[tt], nk=2, evac="dve", ring=misc_ring)

            if dbg and s == 0 and l == 0:
                for nm, src, rr in (("onT", onT, r_onT), ("omT", omT, r_omT)):
                    if nm in dbg_d:
                        S.op("pool", lambda e, nm=nm, src=src: e.dma_start(
                            out=dbg_d[nm][:, :].rearrange("p (a b) -> p a b", a=4), in_=src), reads=rr,
                            writes=[Res()], dma=True)

            S.barrier()
            A.off = region0
            yT = A.alloc([128, 8, T], BF16)
            r_yT = [Res() for _ in range(8)]
            wout = A.alloc([128, 8, D], BF16)
            r_wout = Res()
            S.op("pool", lambda e: e.dma_start(out=wout, in_=wout_d[l, :, :].rearrange("(k p) c -> p k c", p=128)),
                 writes=[r_wout], dma=True)
            for oc in range(8):
                wgn, r_wgn = load_w(win_cols(GATE_N + 128 * oc, 128), 128)
                wgm, r_wgm = load_w(win_cols(GATE_M + 128 * oc, 128), 128)
                wu, r_wu = wch_ring.next()
                S.op("pool", lambda e, wu=wu, oc=oc: e.dma_start(
                    out=wu[:, 0:4, 0:128], in_=wupn_d[l, :, oc * 128:(oc + 1) * 128].rearrange("(k p) c -> p k c", p=128)),
                    writes=[r_wu], dma=True)
                S.op("pool", lambda e, wu=wu, oc=oc: e.dma_start(
                    out=wu[:, 4:8, 0:128], in_=wupm_d[l, :, oc * 128:(oc + 1) * 128].rearrange("(k p) c -> p k c", p=128)),
                    writes=[r_wu], dma=True)
                for b in range(4):
                    bs = slice(b * 512, (b + 1) * 512)
                    res = []
                    for (wg, r_wg, oT, r_oT, ko) in ((wgn, r_wgn, onT, r_onT, 0), (wgm, r_wgm, omT, r_omT, 4)):
                        pg, r_pg = ps_ring.next()
                        fns = [lambda e, k=k, pg=pg, wg=wg, bs=bs: e.matmul(pg[:], lhsT=wg[:, k, 0:128], rhs=hT[:, k, bs],
                                                                     start=(k == 0), stop=(k == 7)) for k in range(8)]
                        S.op("pe", fns, reads=[r_wg] + r_hT[4 * b:4 * b + 4], writes=[r_pg])
                        pu, r_pu = ps_ring.next()
                        fns = [lambda e, k=k, pu=pu, oT=oT, ko=ko, wu=wu, bs=bs: e.matmul(pu[:], lhsT=wu[:, ko + k, 0:128], rhs=oT[:, k, bs],
                                                                            start=(k == 0), stop=(k == 3)) for k in range(4)]
                        S.op("pe", fns, reads=[r_wu] + r_oT[4 * b:4 * b + 4], writes=[r_pu])
                        sg, r_sg = tmpf_ring.next()
                        S.op("act", lambda e, pg=pg, sg=sg: e.activation(out=sg, in_=pg[:], func=AF.Sigmoid),
                             reads=[r_pg], writes=[r_sg])
                        S.op("dve", lambda e, pu=pu, sg=sg: e.tensor_tensor(out=sg, in0=sg, in1=pu[:], op=ALU.mult),
                             reads=[r_pu, r_sg], writes=[r_sg])
                        res.append((sg, r_sg))
                    S.op("dve", lambda e, a=res[0][0], b_=res[1][0], oc=oc, bs=bs: e.tensor_tensor(
                        out=yT[:, oc, bs], in0=a, in1=b_, op=ALU.add), reads=[res[0][1], res[1][1]], writes=[r_yT[oc]])
            for i in range(16):
                tt = tt0 + i
                xa, r_xa = xt_ring.next()
                S.op("sp", lambda e, xa=xa, tt=tt: e.dma_start(out=xa, in_=src_d[tt * 128:(tt + 1) * 128, :]),
                     reads=[r_src[tt]], writes=[r_xa], dma=True)
                for h2 in range(2):
                    po, r_po = ps_ring.next()
                    fns = [lambda e, oc=oc, po=po, i=i, h2=h2: e.matmul(po[:], lhsT=yT[:, oc, i * 128:(i + 1) * 128],
                                                                        rhs=wout[:, oc, h2 * 512:(h2 + 1) * 512],
                                                                        start=(oc == 0), stop=(oc == 7)) for oc in range(8)]
                    S.op("pe", fns, reads=r_yT + [r_wout], writes=[r_po])
                    S.op("dve", lambda e, po=po, xa=xa, h2=h2: e.tensor_tensor(
                        out=xa[:, h2 * 512:(h2 + 1) * 512], in0=po[:], in1=xa[:, h2 * 512:(h2 + 1) * 512], op=ALU.add),
                        reads=[r_po, r_xa], writes=[r_xa])
                S.op("sp", lambda e, xa=xa, tt=tt: e.dma_start(out=y_d[tt * 128:(tt + 1) * 128, :], in_=xa),
                     reads=[r_xa], writes=[r_y[tt]], dma=True)

    cur_d, cur_r = x_d, r_x
    for l in range(depth):
        if f"ffn{2 * l}" in phases or "all" in phases:
            ffn_phase(l, 0, cur_d, cur_r)
            cur_d, cur_r = y_d, r_y
        if f"mix{l}" in phases or "all" in phases:
            mix_phase(l, cur_d, cur_r)
            cur_d, cur_r = y_d, r_y
        if f"ffn{2 * l + 1}" in phases or "all" in phases:
            ffn_phase(l, 1, cur_d, cur_r)
            cur_d, cur_r = y_d, r_y

    S.barrier()
    S.finish("sp", r_y)
    sems = [es.enter_context(nc.semaphore(f"s{i}")) for i in range(S.nsem)]
    S.emit(nc, sems)
    es.close()
    return nc, S


def make_in_maps(inputs, nseq, ncores):
    x = np.ascontiguousarray(inputs["x"], dtype=np.float32).reshape(-1, nseq * T, D)
    consts = make_consts()
    shared = {}
    for k in ("norm_g", "ffn_w1", "ffn_w3", "ffn_w2", "w_in", "g_qk_nsa", "g_qk_moba", "cmp_w1", "cmp_w2", "w_up_nsa", "w_up_moba",
              "w_out"):
        shared[k] = np.ascontiguousarray(inputs[k], dtype=np.float32)
    shared["g_qk_nsaT"] = np.ascontiguousarray(np.transpose(np.asarray(inputs["g_qk_nsa"], np.float32), (0, 2, 1)))
    shared["g_qk_mobaT"] = np.ascontiguousarray(np.transpose(np.asarray(inputs["g_qk_moba"], np.float32), (0, 2, 1)))
    shared["cmp_posT"] = np.ascontiguousarray(np.transpose(np.asarray(inputs["cmp_pos"], np.float32), (0, 1, 3, 2)))
    shared.update(consts)
    in_maps = []
    for c in range(ncores):
        m = {"x": x[c]}
        m.update(shared)
        in_maps.append(m)
    return in_maps


_CACHE = {}


def kernel(**inputs):
    nseq = 16 // NCORES
    if "full" not in _CACHE:
        _CACHE["full"] = build_program(nseq=nseq, phases=("all",))
    nc, S = _CACHE["full"]
    in_maps = make_in_maps(inputs, nseq, NCORES)
    res = run_bass_kernel_spmd(nc, in_maps, core_ids=list(range(NCORES)))
    y = np.stack([np.asarray(r["y"]) for r in res.results], axis=0)
    return y.reshape(16, T, D).astype(np.float32)
```

```python
import numpy as np
from contextlib import ExitStack
import concourse.bass as bass
import concourse.mybir as mybir
from concourse.bass_utils import run_bass_kernel_spmd

F32 = mybir.dt.float32
BF16 = mybir.dt.bfloat16
AF = mybir.ActivationFunctionType
ALU = mybir.AluOpType
AX = mybir.AxisListType

NCORES = 8
D = 1024
T = 2048
DFF = 2816
NF = DFF // 128
DEPTH = 2
EPS = 1e-6


class Res:
    __slots__ = ("w", "r", "name")

    def __init__(self, name=""):
        self.w = None
        self.r = {}
        self.name = name


class _Eng:
    def __init__(self, name, sem):
        self.name = name
        self.sem = sem
        self.count = 0
        self.known = {}
        self.ops = []
        self.dma_sems = []
        self.dma_count = 0


class Sched:
    NDMA = 8

    def __init__(self):
        self.nsem = 0
        self.eng = {}
        for n in ("pe", "act", "dve", "pool", "sp"):
            self.eng[n] = _Eng(n, self._newsem())
        for n in ("sp", "pool", "act"):
            self.eng[n].dma_sems = [self._newsem() for _ in range(self.NDMA)]

    def _newsem(self):
        s = self.nsem
        self.nsem += 1
        return s

    def op(self, eng, fns, reads=(), writes=(), dma=False):
        E = self.eng[eng]
        if not isinstance(fns, (list, tuple)):
            fns = [fns]
        need = {}

        def req(tok):
            if tok is None:
                return
            s, v, clk = tok
            o = need.get(s)
            if o is None or o[0] < v:
                need[s] = (v, clk)

        for r in reads:
            req(r.w)
        for w in writes:
            req(w.w)
            for s, (v, clk) in w.r.items():
                req((s, v, clk))
        implied = {}
        for s, (v, clk) in need.items():
            for cs, cv in clk.items():
                if implied.get(cs, 0) < cv:
                    implied[cs] = cv
        waits = []
        known = E.known
        for s, (v, clk) in need.items():
            if known.get(s, 0) >= v or implied.get(s, 0) >= v:
                continue
            if eng == "pe" and s == E.sem:
                continue
            waits.append((s, v))
        for s, v in implied.items():
            if known.get(s, 0) < v:
                known[s] = v
        for s, (v, clk) in need.items():
            if known.get(s, 0) < v:
                known[s] = v
        if dma:
            j = E.dma_count
            E.dma_count += 1
            s = E.dma_sems[j % self.NDMA]
            prev = 16 * (j // self.NDMA)
            if prev > 0 and known.get(s, 0) < prev:
                waits.append((s, prev))
                known[s] = prev
            val = prev + 16
            inc = 16
        else:
            E.count += 1
            s = E.sem
            val = E.count
            inc = 1
        clk = dict(known)
        tok = (s, val, clk)
        E.ops.append((waits, list(fns), s, inc))
        for r in reads:
            o = r.r.get(s)
            if o is None or o[0] < val:
                r.r[s] = (val, clk)
        for w in writes:
            w.w = tok
            w.r = {}
        return tok

    def finish(self, eng, resources):
        E = self.eng[eng]
        need = {}
        for r in resources:
            toks = []
            if r.w is not None:
                toks.append(r.w)
            for s, (v, clk) in r.r.items():
                toks.append((s, v, clk))
            for s, v, clk in toks:
                if need.get(s, 0) < v:
                    need[s] = v
        waits = [(s, v) for s, v in need.items() if E.known.get(s, 0) < v]
        E.ops.append((waits, [], None, 0))

    def emit(self, nc, sems):
        def replay(E):
            def body(e):
                for waits, fns, s, inc in E.ops:
                    for ws, wv in waits:
                        e.wait_ge(sems[ws], wv)
                    if not fns:
                        continue
                    for fn in fns[:-1]:
                        fn(e)
                    fns[-1](e).then_inc(sems[s], inc)
            return body

        with nc.Block() as block:
            block.sync(replay(self.eng["sp"]))
            block.scalar(replay(self.eng["act"]))
            block.vector(replay(self.eng["dve"]))
            block.gpsimd(replay(self.eng["pool"]))
            block.tensor(replay(self.eng["pe"]))


class Ring:
    def __init__(self, items):
        self.items = items
        self.i = 0

    def next(self):
        it = self.items[self.i % len(self.items)]
        self.i += 1
        return it


def _barrier(self):
    toks = {}
    for E in self.eng.values():
        if E.count > 0:
            toks[E.sem] = E.count
        for i, s in enumerate(E.dma_sems):
            if E.dma_count > i:
                toks[s] = 16 * ((E.dma_count - i + self.NDMA - 1) // self.NDMA)
    for E in self.eng.values():
        waits = []
        for s, v in toks.items():
            if E.known.get(s, 0) >= v:
                continue
            if E.name == "pe" and s == E.sem:
                continue
            waits.append((s, v))
            E.known[s] = v
        if waits:
            E.ops.append((waits, [], None, 0))


Sched.barrier = _barrier


class Arena:
    def __init__(self, t, nel):
        self.t = t
        self.nel = nel
        self.off = 0

    def alloc(self, shape, dt):
        n = 1
        for d in shape[1:]:
            n *= d
        sz = n * (2 if dt == F32 else 1)
        self.off = (self.off + 1) // 2 * 2
        o = self.off
        self.off += sz
        assert self.off <= self.nel, ("arena overflow", self.off, self.nel)
        ap = self.t[0:shape[0], o:o + sz]
        if dt == F32:
            ap = ap.bitcast(F32)
        if len(shape) == 3:
            ap = ap.rearrange("p (a b) -> p a b", a=shape[1])
        elif len(shape) == 4:
            ap = ap.rearrange("p (a b c) -> p a b c", a=shape[1], b=shape[2])
        return ap


BIG = 30000.0
NSA_COLS = dict(q=0, kc=512, vc=640, ks=768, vs=896, kw=1024, vw=1152, g=1280)
MOBA_COLS = dict(q=1304, k=1816, v=2328)
GATE_N, GATE_M = 2840, 3864
INC = 4888


def slopes_all():
    s = (2.0 ** (-8.0 * np.arange(1, 17) / 16)).astype(np.float32)
    return s[0::2].copy(), s[1::2].copy()


def _bf16_round(a):
    a = np.asarray(a, dtype=np.float32)
    u = a.view(np.uint32).astype(np.uint64)
    r = ((u + 0x7FFF + ((u >> 16) & 1)) >> 16) << 16
    return r.astype(np.uint32).view(np.float32)


def make_consts():
    c = {}
    c["ident"] = np.eye(128, dtype=np.float32)
    j = np.arange(128)[:, None]
    i = np.arange(128)[None, :]
    c["tri_c"] = np.where(j > i, -BIG, 0.0).astype(np.float32)
    c["tri_a"] = np.where(j <= i, -BIG, 0.0).astype(np.float32)
    c["ones64"] = np.ones((64, 64), np.float32)
    bd = np.zeros((128, 128), np.float32)
    bd[:64, :64] = 1.0
    bd[64:, 64:] = 1.0
    c["bd_ones"] = bd
    sn, sm = slopes_all()
    sl = np.concatenate([sn, sm])
    ab = np.zeros((128, 16, 19), np.float32)
    for h in range(16):
        for d in range(-15, 4):
            ab[:, h, d + 15] = sl[h].astype(np.float64) * (128 * d + np.arange(128))
    c["abias"] = ab.reshape(128, 16 * 19)
    cb = np.zeros((128, 8, 4), np.float32)
    cc = np.arange(128)
    for h in range(8):
        for qb in range(4):
            cb[:, h, qb] = sn[h].astype(np.float64) * (16 * cc + 15.5 - 512 * qb)
    c["cbias"] = cb.reshape(128, 32)
    qa = np.zeros((16, 7, 512), np.float32)
    for h in range(16):
        v = (-(sl[h].astype(np.float64)) * np.arange(512)).astype(np.float32)
        v1 = _bf16_round(v)
        v2 = _bf16_round(v - v1)
        v3 = _bf16_round(v - v1 - v2)
        qa[h, 0], qa[h, 1], qa[h, 2] = v1, v2, v3
        s1 = _bf16_round(np.float32(sl[h]))
        s2 = _bf16_round(np.float32(sl[h]) - s1)
        qa[h, 3], qa[h, 4], qa[h, 5], qa[h, 6] = 128.0 * s1, 128.0 * s2, s1, s2
    c["qalibi"] = np.ascontiguousarray(np.tile(qa, (1, 1, 4)))
    key = np.arange(T)
    c["e32"] = (key[None, :] // 64 == np.arange(32)[:, None]).astype(np.float32)
    c["e8"] = (key[None, :] // 256 == np.arange(8)[:, None]).astype(np.float32)
    kp7 = np.ones((7, T), np.float32)
    kp7[3] = kp7[4] = key // 128
    kp7[5] = kp7[6] = key % 128
    c["ones3"] = kp7
    c["zeros32"] = np.zeros((32, T), np.float32)
    cm = np.zeros((128, T), np.float32)
    cidx = np.arange(128)[:, None]
    cm[:] = np.where(16 * cidx + 31 <= key[None, :], 0.0, -BIG)
    c["cmask"] = cm
    ci = np.arange(127)[:, None] * 16
    sj = np.arange(32)[None, :] * 64
    ov = np.clip(np.minimum(ci + 32, sj + 64) - np.maximum(ci, sj), 0, None)
    M = np.zeros((128, 32), np.float32)
    M[:127] = ov / 32.0
    c["cmp2slc"] = M
    blk = np.arange(32)[None, None, :]
    t = (np.arange(16)[None, :, None] * 128 + np.arange(128)[:, None, None])
    cur = t // 64
    forced = (blk == 0) | (blk == cur) | (blk == cur - 1)
    valid = blk <= cur
    c["nsa_mult"] = np.where(forced | ~valid, 0.0, 1.0).astype(np.float32).reshape(128, 16 * 32)
    c["nsa_add"] = np.where(forced, 1e9, np.where(valid, 0.0, -1e30)).astype(np.float32).reshape(128, 16 * 32)
    n8 = np.arange(8)[None, None, :]
    curm = t // 256
    c["moba_add"] = np.broadcast_to(np.where(n8 < curm, 0.0, -1e30), (128, 16, 8)).astype(np.float32).reshape(128, 128).copy()
    return c


CONST_SHAPES = dict(ident=[128, 128], tri_c=[128, 128], tri_a=[128, 128], ones64=[64, 64], bd_ones=[128, 128], abias=[128, 304],
                    cbias=[128, 32], qalibi=[16, 7, T], e32=[32, T], e8=[8, T], ones3=[7, T], zeros32=[32, T],
                    cmask=[128, T], cmp2slc=[128, 32], nsa_mult=[128, 512], nsa_add=[128, 512], moba_add=[128, 128])


def build_program(nseq=2, phases=("all",), depth=DEPTH, dbg=None):
    nc = bass.Bass("TRN2", target_bir_lowering=False)
    NT = nseq * T
    NTT = NT // 128
    S = Sched()
    es = ExitStack()

    def dram_in(name, shape, dt=F32):
        return nc.dram_tensor(name, list(shape), dt, kind="ExternalInput").ap()

    x_d = dram_in("x", [NT, D])
    normg_d = dram_in("norm_g", [DEPTH, 3, D])
    w1_d = dram_in("ffn_w1", [DEPTH, 2, D, DFF])
    w3_d = dram_in("ffn_w3", [DEPTH, 2, D, DFF])
    w2_d = dram_in("ffn_w2", [DEPTH, 2, DFF, D])
    win_d = dram_in("w_in", [DEPTH, D, INC])
    gqn_d = dram_in("g_qk_nsa", [DEPTH, 4, 64])
    gqnT_d = dram_in("g_qk_nsaT", [DEPTH, 64, 4])
    gqm_d = dram_in("g_qk_moba", [DEPTH, 2, 64])
    gqmT_d = dram_in("g_qk_mobaT", [DEPTH, 64, 2])
    posT_d = dram_in("cmp_posT", [DEPTH, 2, 64, 32])
    cw1_d = dram_in("cmp_w1", [DEPTH, 2, 2048, 256])
    cw2_d = dram_in("cmp_w2", [DEPTH, 2, 256, 64])
    wupn_d = dram_in("w_up_nsa", [DEPTH, 512, D])
    wupm_d = dram_in("w_up_moba", [DEPTH, 512, D])
    wout_d = dram_in("w_out", [DEPTH, D, D])
    C = {k: dram_in(k, v) for k, v in CONST_SHAPES.items()}
    y_d = nc.dram_tensor("y", [NT, D], F32, kind="ExternalOutput").ap()
    dbg_d = {}
    if dbg:
        for k, shp in dbg.items():
            dbg_d[k] = nc.dram_tensor("dbg_" + k, list(shp), F32, kind="ExternalOutput").ap()

    def sb(name, shape, dt):
        return es.enter_context(nc.sbuf_tensor(name, list(shape), dt))

    banks = []
    for i in (0, 1, 2):
        t = es.enter_context(nc.psum_tensor(f"ps{i}", [128, 512], F32))
        banks.append((t[:], Res(f"ps{i}")))
    pairs_ps = []
    for i in (0, 1):
        t = es.enter_context(nc.psum_tensor(f"pp{i}", [128, 1024], F32))
        ra, rb = Res(f"pp{i}a"), Res(f"pp{i}b")
        banks.append((t[:, 0:512], ra))
        banks.append((t[:, 512:1024], rb))
        pairs_ps.append((t[:], [ra, rb]))
    t = es.enter_context(nc.psum_tensor("ps7", [128, 512], F32))
    banks.append((t[:], Res("ps7")))
    ps_ring = Ring(banks)
    acc_banks = banks[0:3]
    acc_ring = Ring(banks[0:3])
    pair_ring = Ring(pairs_ps)
    sc_ring = Ring(banks[3:7])
    misc_ring = Ring(banks[7:8])

    ident_b = sb("ident_b", [128, 128], BF16)
    r_ident = Res("ident")
    S.op("pool", lambda e: e.dma_start(out=ident_b[:], in_=C["ident"][:, :]), writes=[r_ident], dma=True)
    stat = [(sb(f"stat{i}", [128, 4], F32), Res(f"stat{i}")) for i in range(4)]
    stat_ring = Ring(stat)
    ARENA_EL = 105000
    arena_t = sb("arena", [128, ARENA_EL], BF16)
    A = Arena(arena_t, ARENA_EL)

    r_y = [Res(f"y{i}") for i in range(NTT)]
    r_x = [Res(f"x{i}") for i in range(NTT)]

    def rmsnorm_tile(x_ap, r_x_, g_ap, r_gres, out_ap, r_out, jk, r_jk):
        st, r_st = stat_ring.next()
        S.op("act", lambda e: e.activation(out=jk, in_=x_ap, func=AF.Square, accum_out=st[:, 0:1]),
             reads=[r_x_], writes=[r_jk, r_st])
        S.op("act", lambda e: e.activation(out=st[:, 1:2], in_=st[:, 0:1], func=AF.Sqrt, scale=1.0 / D, bias=EPS),
             reads=[r_st], writes=[r_st])
        S.op("dve", lambda e: e.reciprocal(out=st[:, 2:3], in_=st[:, 1:2]), reads=[r_st], writes=[r_st])
        S.op("dve", lambda e: e.scalar_tensor_tensor(out=out_ap, in0=x_ap, scalar=st[:, 2:3], in1=g_ap,
                                                     op0=ALU.mult, op1=ALU.mult),
             reads=[r_x_, r_st, r_gres], writes=[r_out])

    def transpose_tile(in_tile, r_in, out_ap3, r_out, nk=8, evac="act", ring=None):
        ps, r_ps = (ring or ps_ring).next()
        psb = ps[:].bitcast(BF16)
        fns = []
        for k in range(nk):
            fns.append(lambda e, k=k: e.transpose(out=psb[:, k * 128:(k + 1) * 128],
                                                  in_=in_tile[:, k * 128:(k + 1) * 128], identity=ident_b[:]))
        S.op("pe", fns, reads=[r_in, r_ident], writes=[r_ps])
        src = psb[:, 0:nk * 128].rearrange("p (k t) -> p k t", k=nk)
        if evac == "act":
            S.op("act", lambda e: e.copy(out=out_ap3, in_=src), reads=[r_ps], writes=[r_out])
        else:
            S.op("dve", lambda e: e.tensor_copy(out=out_ap3, in_=src), reads=[r_ps], writes=[r_out])

    def ffn_phase(l, j, src_d, r_src):
        S.barrier()
        A.off = 0
        w1_sb = A.alloc([128, 8, DFF], BF16)
        w3_sb = A.alloc([128, 8, DFF], BF16)
        w2_sb = A.alloc([128, NF, D], BF16)
        r_w1 = [Res() for f in range(NF)]
        r_w3 = [Res() for f in range(NF)]
        r_w2 = [Res() for f in range(NF)]
        g_rep = A.alloc([128, D], F32)
        r_g = Res()
        xn_ring = Ring([(A.alloc([128, D], F32), Res()) for i in range(2)])
        xr_ring = Ring([(A.alloc([128, D], F32), Res()) for i in range(2)])
        hb = [(A.alloc([128, D], BF16), Res()) for i in range(4)]
        hT2 = [A.alloc([128, 8, 512], BF16) for i in range(2)]
        r_hT2 = [[Res() for i in range(4)] for _ in range(2)]
        gT = A.alloc([128, NF, 512], BF16)
        r_gT = [Res() for f in range(NF)]
        su_ring = Ring([(A.alloc([128, 512], F32), Res()) for i in range(2)])
        junk = (A.alloc([128, D], BF16), Res())
        NB = NT // 512

        S.op("pool", lambda e: e.dma_start(out=g_rep, in_=normg_d[l, 2 * j, :].partition_broadcast(128)),
             writes=[r_g], dma=True)
        for f in range(NF):
            S.op("pool", lambda e, f=f: e.dma_start(
                out=w1_sb[:, :, f * 128:(f + 1) * 128],
                in_=w1_d[l, j, :, f * 128:(f + 1) * 128].rearrange("(k p) c -> p k c", p=128)),
                writes=[r_w1[f]], dma=True)
            S.op("pool", lambda e, f=f: e.dma_start(
                out=w3_sb[:, :, f * 128:(f + 1) * 128],
                in_=w3_d[l, j, :, f * 128:(f + 1) * 128].rearrange("(k p) c -> p k c", p=128)),
                writes=[r_w3[f]], dma=True)
        for f in range(NF):
            S.op("pool", lambda e, f=f: e.dma_start(out=w2_sb[:, f, :], in_=w2_d[l, j, f * 128:(f + 1) * 128, :]),
                 writes=[r_w2[f]], dma=True)

        def norm_part(blk):
            for i in range(4):
                tt = blk * 4 + i
                xa, r_xa = xn_ring.next()
                S.op("sp", lambda e, xa=xa, tt=tt: e.dma_start(out=xa, in_=src_d[tt * 128:(tt + 1) * 128, :]),
                     reads=[r_src[tt]], writes=[r_xa], dma=True)
                hbt, r_hb = hb[i]
                rmsnorm_tile(xa, r_xa, g_rep, r_g, hbt, r_hb, junk[0], junk[1])

        def transpose_part(blk):
            hT = hT2[blk % 2]
            for i in range(4):
                hbt, r_hb = hb[i]
                transpose_tile(hbt, r_hb, hT[:, :, i * 128:(i + 1) * 128], r_hT2[blk % 2][i])

        def up_part(blk):
            hT = hT2[blk % 2]
            r_hT = r_hT2[blk % 2]
            for f in range(NF):
                pu, r_pu = ps_ring.next()
                pv, r_pv = ps_ring.next()
                fns = [lambda e, k=k, f=f, pu=pu: e.matmul(pu[:], lhsT=w1_sb[:, k, f * 128:(f + 1) * 128],
                                                           rhs=hT[:, k, :], start=(k == 0), stop=(k == 7)) for k in range(8)]
                S.op("pe", fns, reads=[r_w1[f]] + r_hT, writes=[r_pu])
                fns = [lambda e, k=k, f=f, pv=pv: e.matmul(pv[:], lhsT=w3_sb[:, k, f * 128:(f + 1) * 128],
                                                           rhs=hT[:, k, :], start=(k == 0), stop=(k == 7)) for k in range(8)]
                S.op("pe", fns, reads=[r_w3[f]] + r_hT, writes=[r_pv])
                s_t, r_s = su_ring.next()
                S.op("act", lambda e, s_t=s_t, pu=pu: e.activation(out=s_t, in_=pu[:], func=AF.Silu),
                     reads=[r_pu], writes=[r_s])
                S.op("dve", lambda e, s_t=s_t, pv=pv, f=f: e.tensor_tensor(out=gT[:, f, :], in0=s_t, in1=pv[:],
                                                                             op=ALU.mult),
                     reads=[r_s, r_pv], writes=[r_gT[f]])

        def down_part(blk):
            for i in range(4):
                tt = blk * 4 + i
                xa, r_xa = xr_ring.next()
                S.op("sp", lambda e, xa=xa, tt=tt: e.dma_start(out=xa, in_=src_d[tt * 128:(tt + 1) * 128, :]),
                     reads=[r_src[tt]], writes=[r_xa], dma=True)
                for h in range(2):
                    po, r_po = ps_ring.next()
                    fns = [lambda e, f=f, po=po, i=i, h=h: e.matmul(
                        po[:], lhsT=gT[:, f, i * 128:(i + 1) * 128], rhs=w2_sb[:, f, h * 512:(h + 1) * 512],
                        start=(f == 0), stop=(f == NF - 1)) for f in range(NF)]
                    S.op("pe", fns, reads=r_gT + r_w2, writes=[r_po])
                    S.op("dve", lambda e, po=po, xa=xa, h=h: e.scalar_tensor_tensor(
                        out=xa[:, h * 512:(h + 1) * 512], in0=po[:], scalar=0.5, in1=xa[:, h * 512:(h + 1) * 512],
                        op0=ALU.mult, op1=ALU.add), reads=[r_po, r_xa], writes=[r_xa])
                S.op("sp", lambda e, xa=xa, tt=tt: e.dma_start(out=y_d[tt * 128:(tt + 1) * 128, :], in_=xa),
                     reads=[r_xa], writes=[r_y[tt]], dma=True)

        norm_part(0)
        transpose_part(0)
        for blk in range(NB):
            if blk + 1 < NB:
                norm_part(blk + 1)
            up_part(blk)
            if blk + 1 < NB:
                transpose_part(blk + 1)
            down_part(blk)

    def mix_phase(l, src_d, r_src):
        S.barrier()
        A.off = 0
        r_c = Res()
        tri_c = A.alloc([128, 128], BF16)
        tri_a = A.alloc([128, 128], BF16)
        ones64 = A.alloc([64, 64], BF16)
        bd_ones = A.alloc([128, 128], BF16)
        gtab = A.alloc([128, 4], F32)
        abias = A.alloc([128, 304], F32)
        cbias = A.alloc([128, 32], F32)
        cmask = A.alloc([128, T], BF16)
        nsa_mult = A.alloc([128, 16, 32], F32)
        nsa_add = A.alloc([128, 16, 32], F32)
        moba_add = A.alloc([128, 16, 8], F32)
        gq_n = A.alloc([64, 4], F32)
        gq_m = A.alloc([64, 2], F32)
        gkc_rep = A.alloc([128, 64], F32)
        g_rep = A.alloc([128, D], F32)
        for dst, src in ((tri_c, C["tri_c"][:, :]), (tri_a, C["tri_a"][:, :]), (ones64, C["ones64"][:, :]), (bd_ones, C["bd_ones"][:, :]),
                         (gtab[0:64, 0:1], gqn_d[l, 0, :].unsqueeze(1)), (gtab[64:128, 0:1], gqn_d[l, 0, :].unsqueeze(1)),
                         (gtab[0:64, 1:2], gqn_d[l, 2, :].unsqueeze(1)), (gtab[64:128, 1:2], gqn_d[l, 3, :].unsqueeze(1)),
                         (gtab[0:64, 2:3], gqm_d[l, 0, :].unsqueeze(1)), (gtab[64:128, 2:3], gqm_d[l, 0, :].unsqueeze(1)),
                         (gtab[0:64, 3:4], gqm_d[l, 1, :].unsqueeze(1)), (gtab[64:128, 3:4], gqm_d[l, 1, :].unsqueeze(1)),
                         (abias, C["abias"][:, :]), (cbias, C["cbias"][:, :]), (cmask, C["cmask"][:, :]),
                         (nsa_mult, C["nsa_mult"][:, :].rearrange("p (a b) -> p a b", a=16)),
                         (nsa_add, C["nsa_add"][:, :].rearrange("p (a b) -> p a b", a=16)),
                         (moba_add, C["moba_add"][:, :].rearrange("p (a b) -> p a b", a=16)),
                         (gq_n, gqnT_d[l, :, :]), (gq_m, gqmT_d[l, :, :]),
                         (gkc_rep, gqn_d[l, 1, :].partition_broadcast(128)),
                         (g_rep, normg_d[l, 1, :].partition_broadcast(128))):
            S.op("pool", lambda e, dst=dst, src=src: e.dma_start(out=dst, in_=src), writes=[r_c], dma=True)
        S.op("dve", lambda e: e.tensor_scalar(out=gq_n[:, 0:1], in0=gq_n[:, 0:1], scalar1=0.125, scalar2=None,
                                              op0=ALU.mult), reads=[r_c], writes=[r_c])
        S.op("dve", lambda e: e.tensor_scalar(out=gq_m[:, 0:1], in0=gq_m[:, 0:1], scalar1=0.125, scalar2=None,
                                              op0=ALU.mult), reads=[r_c], writes=[r_c])
        S.op("dve", lambda e: e.tensor_scalar(out=gtab[:, 0:1], in0=gtab[:, 0:1], scalar1=0.125, scalar2=None,
                                              op0=ALU.mult), reads=[r_c], writes=[r_c])
        S.op("dve", lambda e: e.tensor_scalar(out=gtab[:, 2:3], in0=gtab[:, 2:3], scalar1=0.125, scalar2=None,
                                              op0=ALU.mult), reads=[r_c], writes=[r_c])

        hT = A.alloc([128, 8, T], BF16)
        r_hT = [Res() for _ in range(16)]
        onT = A.alloc([128, 4, T], BF16)
        omT = A.alloc([128, 4, T], BF16)
        r_onT = [Res() for _ in range(16)]
        r_omT = [Res() for _ in range(16)]
        gsig = A.alloc([128, 16, 24], F32)
        r_gsig = Res()
        wch_ring = Ring([(A.alloc([128, 8, 256], BF16), Res()) for _ in range(3)])
        pT_ring = Ring([(A.alloc([128, 1024], BF16), Res()) for _ in range(2)])
        tmpf_ring = Ring([(A.alloc([128, 512], F32), Res()) for _ in range(3)])
        sqb_ring = Ring([(A.alloc([128, 512], BF16), Res()) for _ in range(3)])
        xt_ring = Ring([(A.alloc([128, D], F32), Res()) for _ in range(3)])
        hb_ring = Ring([(A.alloc([128, D], BF16), Res()) for _ in range(3)])
        junk = (A.alloc([128, D], BF16), Res())
        sm_ring = Ring([(A.alloc([128, 16], F32), Res()) for _ in range(8)])
        region0 = A.off

        def load_w(src_ap3, ncols):
            w, r_w = wch_ring.next()
            S.op("pool", lambda e: e.dma_start(out=w[:, :, 0:ncols], in_=src_ap3), writes=[r_w], dma=True)
            return w, r_w

        def win_cols(c0, n):
            return win_d[l, :, c0:c0 + n].rearrange("(k p) c -> p k c", p=128)

        def proj_fm_pairs(pairs):
            items = [(pi, b) for pi in range(len(pairs)) for b in range(4)]
            wts = {}
            stt = {}

            def ensure_w(pi):
                if pi < len(pairs) and pi not in wts:
                    c0A, c0B = pairs[pi][0], pairs[pi][1]
                    w, r_w = wch_ring.next()
                    if c0B == c0A + 64:
                        S.op("pool", lambda e: e.dma_start(out=w[:, :, 0:128], in_=win_cols(c0A, 128)), writes=[r_w], dma=True)
                    else:
                        S.op("pool", lambda e: e.dma_start(out=w[:, :, 0:64], in_=win_cols(c0A, 64)), writes=[r_w], dma=True)
                        S.op("pool", lambda e: e.dma_start(out=w[:, :, 64:128], in_=win_cols(c0B, 64)), writes=[r_w], dma=True)
                    wts[pi] = (w, r_w)

            def mm(i):
                pi, b = items[i]
                ensure_w(pi)
                if b == 0:
                    ensure_w(pi + 1)
                w, r_w = wts[pi]
                ps, r_ps = ps_ring.next()
                fns = [lambda e, k=k: e.matmul(ps[:, :], lhsT=w[:, k, 0:128], rhs=hT[:, k, b * 512:(b + 1) * 512],
                                               start=(k == 0), stop=(k == 7)) for k in range(8)]
                S.op("pe", fns, reads=[r_w] + r_hT[4 * b:4 * b + 4], writes=[r_ps])
                c0A, c0B, dA, rA, dB, rB, gcol = pairs[pi]
                if gcol is None:
                    S.op("act", lambda e: e.copy(out=dA(b), in_=ps[0:64, :]), reads=[r_ps], writes=[rA(b)])
                    S.op("act", lambda e: e.copy(out=dB(b), in_=ps[64:128, :]), reads=[r_ps], writes=[rB(b)])
                    stt[i] = None
                    return
                sq, r_sq = sqb_ring.next()
                S.op("act", lambda e: e.activation(out=sq[:, :], in_=ps[:, :], func=AF.Square), reads=[r_ps], writes=[r_sq])
                stt[i] = (ps, r_ps, sq, r_sq, b)

            def rest(i):
                if stt[i] is None:
                    return
                ps, r_ps, sq, r_sq, b = stt[i]
                c0A, c0B, dA, rA, dB, rB, gcol = pairs[items[i][0]]
                p2, r_p2 = ps_ring.next()
                S.op("pe", lambda e: e.matmul(p2[:, :], lhsT=bd_ones[:, :], rhs=sq[:, :], start=True, stop=True),
                     reads=[r_sq, r_c], writes=[r_p2])
                tf, r_tf = tmpf_ring.next()
                S.op("act", lambda e: e.activation(out=tf[:, :], in_=p2[:, :], func=AF.Ln, scale=1.0 / 64, bias=EPS),
                     reads=[r_p2], writes=[r_tf])
                S.op("act", lambda e: e.activation(out=tf[:, :], in_=tf[:, :], func=AF.Exp, scale=-0.5),
                     reads=[r_tf], writes=[r_tf])
                S.op("dve", lambda e: e.scalar_tensor_tensor(out=dA(b), in0=ps[0:64, :], scalar=gcol[0:64, :], in1=tf[0:64, :],
                                                             op0=ALU.mult, op1=ALU.mult),
                     reads=[r_ps, r_tf, r_c], writes=[rA(b)])
                S.op("dve", lambda e: e.scalar_tensor_tensor(out=dB(b), in0=ps[64:128, :], scalar=gcol[64:128, :],
                                                             in1=tf[64:128, :], op0=ALU.mult, op1=ALU.mult),
                     reads=[r_ps, r_tf, r_c], writes=[rB(b)])

            mm(0)
            for i in range(len(items)):
                if i + 1 < len(items):
                    mm(i + 1)
                rest(i)

        def causal_tiles(qb):
            past = [(kt, 0, 512, None, None) for kt in range(4 * qb)]
            dg = [(4 * qb + c, 128 * c, 512, "c", c) for c in range(4)]
            if qb == 0:
                return [dg[1], dg[0], dg[3], dg[2]]
            tl = [dg[1], past[0], dg[2], past[1], dg[3], past[2], dg[0], past[3]]
            return tl + past[4:]

        def window_tiles(qb):
            dg = [(4 * qb + c, 128 * c, 512, "c", c) for c in range(4)]
            if qb == 0:
                return [dg[1], dg[0], dg[3], dg[2]]
            ng = {}
            for c in (-1, -2, -3, -4):
                m = 4 + c
                ng[c] = (4 * qb + c, 0, 128 * (m + 1), "a", m)
            return [dg[0], ng[-1], dg[1], ng[-2], dg[2], ng[-3], dg[3], ng[-4]]

        def run_rounds(rounds):
            units = []
            for R in rounds:
                tl = R["tiles"]
                if "mask" in R:
                    grp = [[t] for t in tl]
                else:
                    grp = [tl[i:i + 2] for i in range(0, len(tl), 2)]
                for gi, gtiles in enumerate(grp):
                    units.append((R, gi, len(grp), gtiles))

            def emit_qk(un):
                R, gi, ng, gtiles = un
                pp, rps = pair_ring.next()
                kp = R.get("kpart", 128)
                K = R["krows"]
                qb = R["qb"]
                fns = []
                for j, (kt, c0, c1, tri, tu) in enumerate(gtiles):
                    half = pp[:, j * 512:(j + 1) * 512]
                    fns.append(lambda e, half=half, kt=kt, c0=c0, c1=c1, tri=tri: e.matmul(
                        half[0:kp, c0:c1], lhsT=R["kT"][0:K, kt * 128:kt * 128 + kp],
                        rhs=R["q"][0:K, qb * 512 + c0:qb * 512 + c1], start=True,
                        stop=(tri is None and "mask" not in R)))
                    if "mask" in R:
                        fns.append(lambda e, half=half, c0=c0, c1=c1: e.matmul(
                            half[0:kp, c0:c1], lhsT=ident_b[0:kp, 0:kp],
                            rhs=R["mask"][0:kp, qb * 512 + c0:qb * 512 + c1], start=False, stop=True))
                    if tri is not None:
                        tm = tri_c if tri == "c" else tri_a
                        fns.append(lambda e, half=half, tu=tu, tm=tm: e.matmul(
                            half[:, 128 * tu:128 * tu + 128], lhsT=ident_b[:, :], rhs=tm[:, :], start=False, stop=True))
                S.op("pe", fns, reads=[R["r_k"]] + R["r_q"] + [r_c, r_ident], writes=rps[0:len(gtiles)])
                return pp, rps

            pend = emit_qk(units[0]) if units else None
            for idx, un in enumerate(units):
                R, gi, ng, gtiles = un
                pp, rps = pend
                if idx + 1 < len(units):
                    pend = emit_qk(units[idx + 1])
                kp = R.get("kpart", 128)
                pT, r_pT = pT_ring.next()
                lo = gtiles[0][1]
                hi = 512 * (len(gtiles) - 1) + gtiles[-1][2]
                if "mask" in R:
                    bias_v = R["bias"](gtiles[0][0])
                else:
                    bias_v = R["fbias"]
                S.op("act", lambda e, pp=pp, pT=pT, bias_v=bias_v, kp=kp, lo=lo, hi=hi: e.activation(
                    out=pT[0:kp, lo:hi], in_=pp[0:kp, lo:hi], func=AF.Exp, bias=bias_v, scale=1.0),
                    reads=rps[0:len(gtiles)] + [r_c], writes=[r_pT])
                nv = R["nv"]
                if gi == 0:
                    R["acc"] = acc_ring.next()
                acc, r_acc = R["acc"]
                fns = []
                flat = [(j, u) for j, (kt, c0, c1, tri, tu) in enumerate(gtiles) for u in range(4) if c0 <= 128 * u < c1]
                for n_, (j, u) in enumerate(flat):
                    V = R["V"](gtiles[j][0])
                    fns.append(lambda e, u=u, j=j, acc=acc, V=V, first=(gi == 0 and n_ == 0),
                               last=(gi == ng - 1 and n_ == len(flat) - 1), pT=pT, kp=kp:
                               e.matmul(acc[:, 128 * u:128 * u + nv], lhsT=pT[0:kp, j * 512 + 128 * u:j * 512 + 128 * u + 128],
                                        rhs=V, start=first, stop=last))
                S.op("pe", fns, reads=[r_pT, R["r_v"]], writes=[r_acc])
                if gi == ng - 1:
                    R["evac"](acc[:, :].rearrange("p (u c) -> p u c", u=4), r_acc)

        sn_, sm_ = slopes_all()

        def dump(name, ap, reads):
            if name in dbg_d:
                dst = dbg_d[name][:, :]
                if len(ap.shape) == 3:
                    dst = dst.rearrange("p (a b) -> p a b", a=ap.shape[1])
                S.op("pool", lambda e: e.dma_start(out=dst[0:ap.shape[0]], in_=ap), reads=reads, writes=[Res()], dma=True)

        for s in range(nseq):
            tt0 = s * 16
            prev = None
            for i in range(16):
                xa, r_xa = xt_ring.next()
                S.op("sp", lambda e, xa=xa, i=i, tt0=tt0: e.dma_start(out=xa, in_=src_d[(tt0 + i) * 128:(tt0 + i + 1) * 128, :]),
                     reads=[r_src[tt0 + i]], writes=[r_xa], dma=True)
                hbt, r_hb = hb_ring.next()
                st, r_st = stat_ring.next()
                jk, r_jk = junk
                S.op("act", lambda e, xa=xa, st=st: e.activation(out=jk, in_=xa, func=AF.Square, accum_out=st[:, 0:1]),
                     reads=[r_xa], writes=[r_jk, r_st])
                S.op("act", lambda e, st=st: e.activation(out=st[:, 1:2], in_=st[:, 0:1], func=AF.Sqrt, scale=1.0 / D, bias=EPS),
                     reads=[r_st], writes=[r_st])
                if prev is not None:
                    prev()
                S.op("dve", lambda e, st=st: e.reciprocal(out=st[:, 2:3], in_=st[:, 1:2]), reads=[r_st], writes=[r_st])
                S.op("dve", lambda e, xa=xa, st=st, hbt=hbt: e.scalar_tensor_tensor(
                    out=hbt, in0=xa, scalar=st[:, 2:3], in1=g_rep, op0=ALU.mult, op1=ALU.mult),
                    reads=[r_xa, r_st, r_c], writes=[r_hb])
                ps, r_ps = ps_ring.next()
                psb = ps[:].bitcast(BF16)
                fns = [lambda e, k=k, psb=psb, hbt=hbt: e.transpose(out=psb[:, k * 128:(k + 1) * 128],
                                                                    in_=hbt[:, k * 128:(k + 1) * 128], identity=ident_b[:])
                       for k in range(8)]
                S.op("pe", fns, reads=[r_hb, r_ident], writes=[r_ps])

                def evac(psb=psb, r_ps=r_ps, i=i):
                    S.op("act", lambda e: e.copy(out=hT[:, :, i * 128:(i + 1) * 128],
                                                 in_=psb.rearrange("p (k t) -> p k t", k=8)), reads=[r_ps], writes=[r_hT[i]])
                prev = evac
            prev()

            w, r_w = load_w(win_cols(NSA_COLS["g"], 64), 64)
            for i in range(16):
                ps, r_ps = ps_ring.next()
                fns = [lambda e, k=k, ps=ps, i=i, w=w: e.matmul(ps[:, 0:24], lhsT=hT[:, k, i * 128:(i + 1) * 128],
                                                                rhs=w[:, k, 0:24], start=(k == 0), stop=(k == 7))
                       for k in range(8)]
                S.op("pe", fns, reads=[r_w, r_hT[i]], writes=[r_ps])
                S.op("act", lambda e, ps=ps, i=i: e.activation(out=gsig[:, i, :], in_=ps[:, 0:24], func=AF.Sigmoid),
                     reads=[r_ps], writes=[r_gsig])

            if dbg and s == 0 and l == 0:
                dump("gsig", gsig, [r_gsig])
            S.barrier()
            A.off = region0
            q_aug = [A.alloc([128, T], BF16) for _ in range(4)]
            r_q = [[Res() for _ in range(4)] for _ in range(4)]
            r_qs = [[Res() for _ in range(4)] for _ in range(4)]
            r_qst = Res()
            ks_aug = A.alloc([128, T], BF16)
            kw_aug = A.alloc([128, T], BF16)
            r_ks = Res()
            r_kw = Res()
            kcraw = A.alloc([64, T], BF16)
            r_kcraw = Res()
            vcraw = A.alloc([64, T], BF16)
            r_vcraw = Res()
            vsA = A.alloc([128, 16, 65], BF16)
            vwA = A.alloc([128, 16, 65], BF16)
            r_vs = Res()
            r_vw = Res()
            w1c = A.alloc([64, 32, 256], BF16)
            r_w1c = Res()
            w2c = A.alloc([128, 2, 64], BF16)
            posT = A.alloc([64, 32], BF16)
            r_w2c = Res()
            kc_aug = A.alloc([128, 128], BF16)
            r_kc = Res()
            kctm = A.alloc([128, 128], BF16)
            r_kctm = Res()
            vcA = A.alloc([128, 97], BF16)
            r_vc = Res()
            hid = A.alloc([128, 2, 128], BF16)
            r_hid = Res()
            pbias = A.alloc([128, 2], F32)
            r_pb = Res()
            oacc = A.alloc([128, 16, 256], F32)
            r_oacc = [Res() for _ in range(16)]
            impacc = A.alloc([128, 16, 32], F32)
            r_imp = [Res() for _ in range(16)]
            trin_all = [(A.alloc([128, 96], BF16), Res()) for _ in range(16)]
            ob_ring = Ring([(A.alloc([128, 256], BF16), Res()) for _ in range(2)])

            for g in range(2):
                for hl in range(4):
                    h = 4 * g + hl
                    S.op("pool", lambda e, hl=hl, h=h: e.dma_start(
                        out=q_aug[hl][96:103, :], in_=C["qalibi"][h, :, :]), writes=[r_qst], dma=True)
                    if g == 0:
                        S.op("pool", lambda e, hl=hl: e.dma_start(out=q_aug[hl][64:96, :], in_=C["zeros32"][:, :]),
                             writes=r_qs[hl], dma=True)
                for dst, r_dst, mid in (((ks_aug, r_ks, C["e32"]), (kw_aug, r_kw, C["zeros32"])) if g == 0 else ()):
                    S.op("pool", lambda e, dst=dst, mid=mid: e.dma_start(out=dst[64:96, :], in_=mid[:, :]),
                         writes=[r_dst], dma=True)
                    S.op("pool", lambda e, dst=dst: e.dma_start(out=dst[96:103, :], in_=C["ones3"][:, :]),
                         writes=[r_dst], dma=True)
                if g == 0:
                    S.op("pool", lambda e: e.memset(kctm[:, 64:96], 0.0), writes=[r_kctm])
                    S.op("pool", lambda e: e.memset(kctm[:, 96:99], 1.0), writes=[r_kctm])
                    S.op("pool", lambda e: e.memset(kctm[:, 99:103], 0.0), writes=[r_kctm])
                    S.op("pool", lambda e: e.memset(kctm[:, 0:64], 0.0), writes=[r_kctm])
                    S.op("pool", lambda e: e.memset(vsA[:, :, 64:65], 1.0), writes=[r_vs])
                    S.op("pool", lambda e: e.memset(vwA[:, :, 64:65], 1.0), writes=[r_vw])
                    S.op("pool", lambda e: e.memset(vcA[:, 64:65], 1.0), writes=[r_vc])
                    S.op("pool", lambda e: e.dma_start(out=vcA[:, 65:97], in_=C["cmp2slc"][:, :]), writes=[r_vc], dma=True)
                    for tr, r_tr in trin_all:
                        S.op("pool", lambda e, tr=tr: e.memset(tr[:, 0:64], 0.0), writes=[r_tr])

                def load_cmp_w(kv):
                    S.op("pool", lambda e: e.dma_start(
                        out=w1c, in_=cw1_d[l, kv, :, :].rearrange("(l d) h -> d l h", d=64)), writes=[r_w1c], dma=True)
                    S.op("pool", lambda e: e.dma_start(
                        out=w2c, in_=cw2_d[l, kv, :, :].rearrange("(a p) d -> p a d", p=128)), writes=[r_w2c], dma=True)
                    S.op("pool", lambda e: e.dma_start(out=posT, in_=posT_d[l, kv, :, :]), writes=[r_w2c], dma=True)

                proj_fm_pairs([(NSA_COLS["kc"] + 64 * g, NSA_COLS["vc"] + 64 * g,
                                lambda b: kcraw[0:64, b * 512:(b + 1) * 512], lambda b: r_kcraw,
                                lambda b: vcraw[0:64, b * 512:(b + 1) * 512], lambda b: r_vcraw, None)])
                for kv in range(2):
                    craw, r_craw = (kcraw, r_kcraw) if kv == 0 else (vcraw, r_vcraw)
                    load_cmp_w(kv)
                    for hh in range(2):
                        ps, r_ps = ps_ring.next()
                        fns = [lambda e, ll=ll, ps=ps, hh=hh, craw=craw: e.matmul(
                            ps[:, 0:127], lhsT=w1c[:, ll, hh * 128:(hh + 1) * 128],
                            rhs=craw[0:64, ll:ll + 16 * 126 + 1:16], start=(ll == 0), stop=(ll == 31)) for ll in range(32)]
                        fns += [lambda e, ll=ll, ps=ps, hh=hh: e.matmul(
                            ps[:, 128:129], lhsT=w1c[:, ll, hh * 128:(hh + 1) * 128],
                            rhs=posT[:, ll:ll + 1], start=(ll == 0), stop=(ll == 31)) for ll in range(32)]
                        S.op("pe", fns, reads=[r_w1c, r_w2c, r_craw], writes=[r_ps])
                        S.op("dve", lambda e, ps=ps, hh=hh: e.tensor_copy(out=pbias[:, hh:hh + 1], in_=ps[:, 128:129]),
                             reads=[r_ps], writes=[r_pb])
                        xh, r_xh = tmpf_ring.next()
                        x2, r_x2 = tmpf_ring.next()
                        S.op("dve", lambda e, ps=ps, hh=hh, xh=xh: e.tensor_scalar(
                            out=xh[:, 0:127], in0=ps[:, 0:127], scalar1=pbias[:, hh:hh + 1], scalar2=None, op0=ALU.add),
                            reads=[r_ps, r_pb], writes=[r_xh])
                        S.op("dve", lambda e, xh=xh, x2=x2: e.tensor_tensor(out=x2[:, 0:127], in0=xh[:, 0:127],
                                                                            in1=xh[:, 0:127], op=ALU.mult),
                             reads=[r_xh], writes=[r_x2])
                        S.op("dve", lambda e, x2=x2: e.tensor_scalar(out=x2[:, 0:127], in0=x2[:, 0:127], scalar1=0.044715,
                                                                     scalar2=1.0, op0=ALU.mult, op1=ALU.add),
                             reads=[r_x2], writes=[r_x2])
                        S.op("dve", lambda e, xh=xh, x2=x2: e.tensor_tensor(out=x2[:, 0:127], in0=x2[:, 0:127],
                                                                            in1=xh[:, 0:127], op=ALU.mult),
                             reads=[r_xh, r_x2], writes=[r_x2])
                        S.op("act", lambda e, x2=x2: e.activation(out=x2[:, 0:127], in_=x2[:, 0:127], func=AF.Tanh,
                                                                  scale=0.7978845608028654),
                             reads=[r_x2], writes=[r_x2])
                        S.op("dve", lambda e, xh=xh, x2=x2, hh=hh: e.scalar_tensor_tensor(
                            out=hid[:, hh, 0:127], in0=x2[:, 0:127], scalar=1.0, in1=xh[:, 0:127],
                            op0=ALU.add, op1=ALU.mult), reads=[r_xh, r_x2], writes=[r_hid])
                    ps, r_ps = ps_ring.next()
                    fns = [lambda e, hh=hh, ps=ps: e.matmul(ps[0:127, 0:64], lhsT=hid[:, hh, 0:127], rhs=w2c[:, hh, :],
                                                            start=(hh == 0), stop=(hh == 1)) for hh in range(2)]
                    S.op("pe", fns, reads=[r_hid, r_w2c], writes=[r_ps])
                    if kv == 1:
                        S.op("dve", lambda e, ps=ps: e.tensor_scalar(out=vcA[0:127, 0:64], in0=ps[0:127, 0:64],
                                                                     scalar1=0.5, scalar2=None, op0=ALU.mult),
                             reads=[r_ps], writes=[r_vc])
                    else:
                        tf, r_tf = tmpf_ring.next()
                        st, r_st = sm_ring.next()
                        S.op("dve", lambda e, ps=ps, tf=tf: e.tensor_scalar(out=tf[0:127, 0:64], in0=ps[0:127, 0:64],
                                                                            scalar1=0.5, scalar2=None, op0=ALU.mult),
                             reads=[r_ps], writes=[r_tf])
                        S.op("act", lambda e, tf=tf, st=st: e.activation(out=tf[0:127, 64:128], in_=tf[0:127, 0:64],
                                                                         func=AF.Square, accum_out=st[0:127, 0:1]),
                             reads=[r_tf], writes=[r_tf, r_st])
                        S.op("act", lambda e, st=st: e.activation(out=st[0:127, 1:2], in_=st[0:127, 0:1], func=AF.Sqrt,
                                                                  scale=1.0 / 64, bias=EPS), reads=[r_st], writes=[r_st])
                        S.op("dve", lambda e, st=st: e.reciprocal(out=st[0:127, 2:3], in_=st[0:127, 1:2]),
                             reads=[r_st], writes=[r_st])
                        S.op("dve", lambda e, tf=tf, st=st: e.scalar_tensor_tensor(
                            out=kctm[0:127, 0:64], in0=tf[0:127, 0:64], scalar=st[0:127, 2:3], in1=gkc_rep[0:127, :],
                            op0=ALU.mult, op1=ALU.mult), reads=[r_tf, r_st, r_c], writes=[r_kctm])
                        ps2, r_ps2 = ps_ring.next()
                        psb2 = ps2[:].bitcast(BF16)
                        S.op("pe", lambda e, psb2=psb2: e.transpose(out=psb2[0:103, 0:128], in_=kctm[:, 0:103],
                                                                    identity=ident_b[:]),
                             reads=[r_kctm, r_ident], writes=[r_ps2])
                        S.op("dve", lambda e, psb2=psb2: e.tensor_copy(out=kc_aug[0:103, 0:128], in_=psb2[0:103, 0:128]),
                             reads=[r_ps2], writes=[r_kc])

                prs = []
                for hp in range(2):
                    hA, hB = 2 * hp, 2 * hp + 1
                    prs.append((NSA_COLS["q"] + 64 * (4 * g + hA), NSA_COLS["q"] + 64 * (4 * g + hB),
                                lambda b, hA=hA: q_aug[hA][0:64, b * 512:(b + 1) * 512], lambda b, hA=hA: r_q[hA][b],
                                lambda b, hB=hB: q_aug[hB][0:64, b * 512:(b + 1) * 512], lambda b, hB=hB: r_q[hB][b],
                                gtab[:, 0:1]))
                prs.append((NSA_COLS["ks"] + 64 * g, NSA_COLS["kw"] + 64 * g,
                            lambda b: ks_aug[0:64, b * 512:(b + 1) * 512], lambda b: r_ks,
                            lambda b: kw_aug[0:64, b * 512:(b + 1) * 512], lambda b: r_kw, gtab[:, 1:2]))
                proj_fm_pairs(prs)
                for nm, dstA, r_dst in (("vs", vsA, r_vs), ("vw", vwA, r_vw)):
                    w, r_w = load_w(win_cols(NSA_COLS[nm] + 64 * g, 64), 64)
                    for i in range(16):
                        ps, r_ps = ps_ring.next()
                        fns = [lambda e, k=k, ps=ps, i=i, w=w: e.matmul(ps[:, 0:64], lhsT=hT[:, k, i * 128:(i + 1) * 128],
                                                                        rhs=w[:, k, 0:64], start=(k == 0), stop=(k == 7))
                               for k in range(8)]
                        S.op("pe", fns, reads=[r_w, r_hT[i]], writes=[r_ps])
                        S.op("act", lambda e, ps=ps, i=i, dstA=dstA: e.copy(out=dstA[:, i, 0:64], in_=ps[:, 0:64]),
                             reads=[r_ps], writes=[r_dst])
                def mk_cmp_evac(hl, qb):
                    h = 4 * g + hl

                    def ev(acc3, r_acc):
                        tts = slice(4 * qb, 4 * qb + 4)
                        r_o = r_oacc[4 * qb:4 * qb + 4]
                        r_i = r_imp[4 * qb:4 * qb + 4]
                        st, r_st = sm_ring.next()
                        rd = st[:, 4:8].unsqueeze(2)
                        cf = st[:, 8:12].unsqueeze(2)
                        S.op("dve", lambda e: e.tensor_scalar(out=st[:, 0:4].unsqueeze(2), in0=acc3[:, :, 64:65], scalar1=1e-30,
                                                              scalar2=None, op0=ALU.add), reads=[r_acc], writes=[r_st])
                        S.op("dve", lambda e: e.reciprocal(out=st[:, 4:8], in_=st[:, 0:4]), reads=[r_st], writes=[r_st])
                        S.op("dve", lambda e: e.tensor_tensor(out=cf, in0=rd, in1=gsig[:, tts, 3 * h:3 * h + 1], op=ALU.mult),
                             reads=[r_st, r_gsig], writes=[r_st])
                        S.op("dve", lambda e: e.tensor_tensor(out=oacc[:, tts, hl * 64:(hl + 1) * 64], in0=acc3[:, :, 0:64],
                                                              in1=cf.to_broadcast([128, 4, 64]), op=ALU.mult),
                             reads=[r_acc, r_st], writes=r_o)
                        if hl == 0:
                            S.op("dve", lambda e: e.tensor_tensor(out=impacc[:, tts, :], in0=acc3[:, :, 65:97],
                                                                  in1=rd.to_broadcast([128, 4, 32]), op=ALU.mult),
                                 reads=[r_acc, r_st], writes=r_i)
                        else:
                            tf, r_tf = tmpf_ring.next()
                            tf3 = tf[:, 0:128].rearrange("p (u c) -> p u c", u=4)
                            S.op("dve", lambda e: e.tensor_tensor(out=tf3, in0=acc3[:, :, 65:97],
                                                                  in1=rd.to_broadcast([128, 4, 32]), op=ALU.mult),
                                 reads=[r_acc, r_st], writes=[r_tf])
                            S.op("pool", lambda e: e.tensor_tensor(out=impacc[:, tts, :], in0=impacc[:, tts, :], in1=tf3,
                                                                   op=ALU.add), reads=[r_tf] + r_i, writes=r_i)
                    return ev

                rounds = []
                for hl in range(4):
                    h = 4 * g + hl
                    for qb in range(4):
                        rounds.append(dict(q=q_aug[hl], r_q=[r_q[hl][qb], r_qst], krows=103, kT=kc_aug, r_k=r_kc, qb=qb,
                                           tiles=[(0, 0, 512, None, None)], kpart=127, mask=cmask,
                                           V=lambda kt: vcA[0:127, 0:97], r_v=r_vc, nv=97,
                                           bias=lambda kt, h=h, qb=qb: cbias[0:127, 4 * h + qb:4 * h + qb + 1],
                                           evac=mk_cmp_evac(hl, qb)))
                run_rounds(rounds)
                if dbg and s == 0 and l == 0 and g == 0:
                    dump("oacc_cmp", oacc, r_oacc)
                    dump("kc_aug", kc_aug, [r_kc])
                    dump("vcA", vcA, [r_vc])
                    dump("impacc", impacc, r_imp)

                sel_tr = []
                for tt in range(16):
                    sc, r_sc = tmpf_ring.next()
                    st, r_st = sm_ring.next()
                    tr, r_tr = trin_all[tt]
                    S.op("dve", lambda e, sc=sc, tt=tt: e.tensor_tensor(out=sc[:, 0:32], in0=impacc[:, tt, :],
                                                                        in1=nsa_mult[:, tt, :], op=ALU.mult),
                         reads=[r_imp[tt], r_c], writes=[r_sc])
                    S.op("dve", lambda e, sc=sc, tt=tt: e.tensor_tensor(out=sc[:, 0:32], in0=sc[:, 0:32],
                                                                        in1=nsa_add[:, tt, :], op=ALU.add),
                         reads=[r_sc, r_c], writes=[r_sc])
                    S.op("dve", lambda e, sc=sc, st=st: e.max(out=st[:, 0:8], in_=sc[:, 0:32]), reads=[r_sc], writes=[r_st])
                    S.op("dve", lambda e, sc=sc, st=st, tr=tr: e.tensor_scalar(
                        out=tr[:, 64:96], in0=sc[:, 0:32], scalar1=st[:, 7:8], scalar2=-BIG, op0=ALU.is_lt, op1=ALU.mult),
                        reads=[r_sc, r_st], writes=[r_tr])

                def emit_sel_transposes():
                    for tt in range(16):
                        tr, r_tr = trin_all[tt]
                        ps, r_ps = misc_ring.next()
                        psb = ps[:].bitcast(BF16)
                        S.op("pe", lambda e, psb=psb, tr=tr: e.transpose(out=psb[0:96, 0:128], in_=tr[:, 0:96],
                                                                         identity=ident_b[:]),
                             reads=[r_tr, r_ident], writes=[r_ps])
                        for hl in range(4):
                            S.op("dve", lambda e, psb=psb, hl=hl, tt=tt: e.tensor_copy(
                                out=q_aug[hl][64:96, tt * 128:(tt + 1) * 128], in_=psb[64:96, 0:128]),
                                reads=[r_ps], writes=[r_qs[hl][tt // 4]])

                def mk_evac(hl, qb, br):
                    h = 4 * g + hl

                    def ev(acc3, r_acc):
                        tts = slice(4 * qb, 4 * qb + 4)
                        r_o = r_oacc[4 * qb:4 * qb + 4]
                        st, r_st = sm_ring.next()
                        rd = st[:, 4:8].unsqueeze(2)
                        cf = st[:, 8:12].unsqueeze(2)
                        S.op("dve", lambda e: e.reciprocal(out=rd, in_=acc3[:, :, 64:65]), reads=[r_acc], writes=[r_st])
                        S.op("dve", lambda e: e.tensor_tensor(out=cf, in0=rd, in1=gsig[:, tts, 3 * h + br:3 * h + br + 1],
                                                              op=ALU.mult), reads=[r_st, r_gsig], writes=[r_st])
                        tf, r_tf = tmpf_ring.next()
                        tf3 = tf[:, 0:256].rearrange("p (u c) -> p u c", u=4)
                        S.op("dve", lambda e: e.tensor_tensor(out=tf3, in0=acc3[:, :, 0:64], in1=cf.to_broadcast([128, 4, 64]),
                                                              op=ALU.mult), reads=[r_acc, r_st], writes=[r_tf])
                        S.op("pool", lambda e: e.tensor_tensor(out=oacc[:, tts, hl * 64:(hl + 1) * 64],
                                                               in0=oacc[:, tts, hl * 64:(hl + 1) * 64], in1=tf3, op=ALU.add),
                             reads=[r_tf] + r_o, writes=r_o)
                    return ev

                rounds = []
                for hl in range(4):
                    h = 4 * g + hl
                    for qb in range(4):
                        rounds.append(dict(q=q_aug[hl], r_q=[r_q[hl][qb], r_qst], krows=103, kT=kw_aug, r_k=r_kw, qb=qb,
                                           tiles=window_tiles(qb), V=lambda kt: vwA[:, kt, :], r_v=r_vw, nv=65,
                                           fbias=-float(sn_[h]) * 512.0 * qb,
                                           evac=mk_evac(hl, qb, 2)))
                run_rounds(rounds)
                rounds = []
                emit_sel_transposes()
                if dbg and s == 0 and l == 0 and g == 0:
                    dump("oacc_win", oacc, r_oacc)
                for hl in range(4):
                    h = 4 * g + hl
                    for qb in range(4):
                        rounds.append(dict(q=q_aug[hl], r_q=[r_q[hl][qb], r_qs[hl][qb], r_qst], krows=103, kT=ks_aug,
                                           r_k=r_ks, qb=qb, tiles=causal_tiles(qb), V=lambda kt: vsA[:, kt, :], r_v=r_vs,
                                           nv=65,
                                           fbias=-float(sn_[h]) * 512.0 * qb,
                                           evac=mk_evac(hl, qb, 1)))
                run_rounds(rounds)
                if dbg and s == 0 and l == 0 and g == 0:
                    dump("oacc_all", oacc, r_oacc)
                    dump("q0", q_aug[0], [r_q[0][b] for b in range(4)] + [r_qs[0][b] for b in range(4)] + [r_qst])
                    dump("ks_aug", ks_aug, [r_ks])
                for tt in range(16):
                    ob, r_ob = ob_ring.next()
                    S.op("act", lambda e, ob=ob, tt=tt: e.copy(out=ob, in_=oacc[:, tt, :]), reads=[r_oacc[tt]], writes=[r_ob])
                    transpose_tile(ob, r_ob, onT[:, 2 * g:2 * g + 2, tt * 128:(tt + 1) * 128], r_onT[tt], nk=2,
                                   evac="dve", ring=misc_ring)

            S.barrier()
            A.off = region0
            q_aug = [A.alloc([128, T], BF16) for _ in range(4)]
            k_aug = [A.alloc([128, T], BF16) for _ in range(4)]
            r_q = [[Res() for _ in range(4)] for _ in range(4)]
            r_qs = [[Res() for _ in range(4)] for _ in range(4)]
            r_qst = Res()
            r_k = [Res() for _ in range(4)]
            vmA = A.alloc([128, 16, 4, 65], BF16)
            r_vm = Res()
            kmean_f = A.alloc([64, 4, 8], F32)
            kmean_b = A.alloc([64, 4, 8], BF16)
            r_km = Res()
            omb = A.alloc([128, 16, 256], BF16)
            r_omb = [Res() for _ in range(16)]
            trin_ring = Ring([(A.alloc([128, 72], BF16), Res()) for _ in range(4)])
            for hf in range(2):
                if hf == 0:
                    for tr, r_tr in trin_ring.items:
                        S.op("pool", lambda e, tr=tr: e.memset(tr[:, 0:64], 0.0), writes=[r_tr])
                    S.op("pool", lambda e: e.memset(vmA[:, :, :, 64:65], 1.0), writes=[r_vm])
                for hl in range(4):
                    h = 4 * hf + hl
                    S.op("pool", lambda e, hl=hl, h=h: e.dma_start(
                        out=q_aug[hl][72:79, :], in_=C["qalibi"][8 + h, :, :]), writes=[r_qst], dma=True)
                    if hf == 0:
                        S.op("pool", lambda e, hl=hl: e.dma_start(out=q_aug[hl][64:72, :], in_=C["zeros32"][0:8, :]),
                             writes=r_qs[hl], dma=True)
                        S.op("pool", lambda e, hl=hl: e.dma_start(out=k_aug[hl][64:72, :], in_=C["e8"][:, :]),
                             writes=[r_k[hl]], dma=True)
                        S.op("pool", lambda e, hl=hl: e.dma_start(out=k_aug[hl][72:79, :], in_=C["ones3"][:, :]),
                             writes=[r_k[hl]], dma=True)
                prs = []
                for hp in range(2):
                    hA, hB = 2 * hp, 2 * hp + 1
                    prs.append((MOBA_COLS["q"] + 64 * (4 * hf + hA), MOBA_COLS["q"] + 64 * (4 * hf + hB),
                                lambda b, hA=hA: q_aug[hA][0:64, b * 512:(b + 1) * 512], lambda b, hA=hA: r_q[hA][b],
                                lambda b, hB=hB: q_aug[hB][0:64, b * 512:(b + 1) * 512], lambda b, hB=hB: r_q[hB][b],
                                gtab[:, 2:3]))
                for hp in range(2):
                    hA, hB = 2 * hp, 2 * hp + 1
                    prs.append((MOBA_COLS["k"] + 64 * (4 * hf + hA), MOBA_COLS["k"] + 64 * (4 * hf + hB),
                                lambda b, hA=hA: k_aug[hA][0:64, b * 512:(b + 1) * 512], lambda b, hA=hA: r_k[hA],
                                lambda b, hB=hB: k_aug[hB][0:64, b * 512:(b + 1) * 512], lambda b, hB=hB: r_k[hB],
                                gtab[:, 3:4]))
                proj_fm_pairs(prs)
                for hl in range(4):
                    S.op("dve", lambda e, hl=hl: e.tensor_reduce(
                        out=kmean_f[:, hl, :], in_=k_aug[hl][0:64, :].rearrange("p (n k) -> p n k", k=256),
                        axis=AX.X, op=ALU.add), reads=[r_k[hl]], writes=[r_km])
                S.op("dve", lambda e: e.tensor_scalar(out=kmean_b, in0=kmean_f, scalar1=1.0 / 256, scalar2=None,
                                                      op0=ALU.mult), reads=[r_km], writes=[r_km])
                w, r_w = load_w(win_cols(MOBA_COLS["v"] + 256 * hf, 256), 256)
                for i in range(16):
                    ps, r_ps = ps_ring.next()
                    fns = [lambda e, k=k, ps=ps, i=i, w=w: e.matmul(ps[:, 0:256], lhsT=hT[:, k, i * 128:(i + 1) * 128],
                                                                    rhs=w[:, k, 0:256], start=(k == 0), stop=(k == 7))
                           for k in range(8)]
                    S.op("pe", fns, reads=[r_w, r_hT[i]], writes=[r_ps])
                    S.op("act", lambda e, ps=ps, i=i: e.copy(out=vmA[:, i, :, 0:64],
                                                             in_=ps[:, 0:256].rearrange("p (h d) -> p h d", d=64)),
                         reads=[r_ps], writes=[r_vm])
                for tt in range(16):
                    cur = tt // 2
                    if tt >= 8:
                        ps, r_ps = misc_ring.next()
                        fns = [lambda e, hl=hl, ps=ps, tt=tt: e.matmul(ps[:, hl * 8:(hl + 1) * 8],
                                                                       lhsT=q_aug[hl][0:64, tt * 128:(tt + 1) * 128],
                                                                       rhs=kmean_b[:, hl, :], start=True, stop=True)
                               for hl in range(4)]
                        S.op("pe", fns, reads=[r_km] + [r_q[hl][tt // 4] for hl in range(4)], writes=[r_ps])
                        sc, r_sc = tmpf_ring.next()
                        for hl in range(4):
                            S.op("dve", lambda e, hl=hl, ps=ps, sc=sc, tt=tt: e.tensor_tensor(
                                out=sc[:, hl * 8:(hl + 1) * 8], in0=ps[:, hl * 8:(hl + 1) * 8], in1=moba_add[:, tt, :],
                                op=ALU.add), reads=[r_ps, r_c], writes=[r_sc])
                    for hl in range(4):
                        tr, r_tr = trin_ring.next()
                        S.op("pool", lambda e, tr=tr, cur=cur: e.memset(tr[:, 64 + cur:65 + cur], 0.0), writes=[r_tr])
                        if cur < 7:
                            S.op("pool", lambda e, tr=tr, cur=cur: e.memset(tr[:, 65 + cur:72], -BIG), writes=[r_tr])
                        if tt < 8:
                            if cur > 0:
                                S.op("pool", lambda e, tr=tr, cur=cur: e.memset(tr[:, 64:64 + cur], 0.0), writes=[r_tr])
                        else:
                            st, r_st = sm_ring.next()
                            S.op("dve", lambda e, sc=sc, st=st, hl=hl: e.max(out=st[:, 0:8], in_=sc[:, hl * 8:(hl + 1) * 8]),
                                 reads=[r_sc], writes=[r_st])
                            S.op("dve", lambda e, sc=sc, st=st, tr=tr, hl=hl, cur=cur: e.tensor_scalar(
                                out=tr[:, 64:64 + cur], in0=sc[:, hl * 8:hl * 8 + cur], scalar1=st[:, 2:3], scalar2=-BIG,
                                op0=ALU.is_lt, op1=ALU.mult), reads=[r_sc, r_st], writes=[r_tr])
                        ps2, r_ps2 = ps_ring.next()
                        psb = ps2[:].bitcast(BF16)
                        S.op("pe", lambda e, psb=psb, tr=tr: e.transpose(out=psb[0:72, 0:128], in_=tr[:, 0:72],
                                                                         identity=ident_b[:]),
                             reads=[r_tr, r_ident], writes=[r_ps2])
                        S.op("dve", lambda e, psb=psb, hl=hl, tt=tt: e.tensor_copy(
                            out=q_aug[hl][64:72, tt * 128:(tt + 1) * 128], in_=psb[64:72, 0:128]),
                            reads=[r_ps2], writes=[r_qs[hl][tt // 4]])

                def mk_evac_m(hl, qb):
                    def ev(acc3, r_acc):
                        tts = slice(4 * qb, 4 * qb + 4)
                        st, r_st = sm_ring.next()
                        rd = st[:, 4:8].unsqueeze(2)
                        S.op("dve", lambda e: e.reciprocal(out=rd, in_=acc3[:, :, 64:65]), reads=[r_acc], writes=[r_st])
                        S.op("dve", lambda e: e.tensor_tensor(out=omb[:, tts, hl * 64:(hl + 1) * 64], in0=acc3[:, :, 0:64],
                                                              in1=rd.to_broadcast([128, 4, 64]), op=ALU.mult),
                             reads=[r_acc, r_st], writes=r_omb[4 * qb:4 * qb + 4])
                    return ev

                rounds = []
                for hl in range(4):
                    h = 4 * hf + hl
                    for qb in range(4):
                        rounds.append(dict(q=q_aug[hl], r_q=[r_q[hl][qb], r_qs[hl][qb], r_qst], krows=79, kT=k_aug[hl],
                                           r_k=r_k[hl], qb=qb, tiles=causal_tiles(qb),
                                           V=lambda kt, hl=hl: vmA[:, kt, hl, :], r_v=r_vm, nv=65,
                                           fbias=-float(sm_[h]) * 512.0 * qb,
                                           evac=mk_evac_m(hl, qb)))
                run_rounds(rounds)
                for tt in range(16):
                    transpose_tile(omb[:, tt, :], r_omb[tt], omT[:, 2 * hf:2 * hf + 2, tt * 128:(tt + 1) * 128],
                                   r_omT[tt], nk=2, evac="dve", ring=misc_ring)

            if dbg and s == 0 and l == 0:
                for nm, src, rr in (("onT", onT, r_onT), ("omT", omT, r_omT)):
                    if nm in dbg_d:
                        S.op("pool", lambda e, nm=nm, src=src: e.dma_start(
                            out=dbg_d[nm][:, :].rearrange("p (a b) -> p a b", a=4), in_=src), reads=rr,
                            writes=[Res()], dma=True)

            S.barrier()
            A.off = region0
            yT = A.alloc([128, 8, T], BF16)
            r_yT = [Res() for _ in range(8)]
            wout = A.alloc([128, 8, D], BF16)
            r_wout = Res()
            S.op("pool", lambda e: e.dma_start(out=wout, in_=wout_d[l, :, :].rearrange("(k p) c -> p k c", p=128)),
                 writes=[r_wout], dma=True)
            for oc in range(8):
                wgn, r_wgn = load_w(win_cols(GATE_N + 128 * oc, 128), 128)
                wgm, r_wgm = load_w(win_cols(GATE_M + 128 * oc, 128), 128)
                wu, r_wu = wch_ring.next()
                S.op("pool", lambda e, wu=wu, oc=oc: e.dma_start(
                    out=wu[:, 0:4, 0:128], in_=wupn_d[l, :, oc * 128:(oc + 1) * 128].rearrange("(k p) c -> p k c", p=128)),
                    writes=[r_wu], dma=True)
                S.op("pool", lambda e, wu=wu, oc=oc: e.dma_start(
                    out=wu[:, 4:8, 0:128], in_=wupm_d[l, :, oc * 128:(oc + 1) * 128].rearrange("(k p) c -> p k c", p=128)),
                    writes=[r_wu], dma=True)
                for b in range(4):
                    bs = slice(b * 512, (b + 1) * 512)
                    res = []
                    for (wg, r_wg, oT, r_oT, ko) in ((wgn, r_wgn, onT, r_onT, 0), (wgm, r_wgm, omT, r_omT, 4)):
                        pg, r_pg = ps_ring.next()
                        fns = [lambda e, k=k, pg=pg, wg=wg, bs=bs: e.matmul(pg[:], lhsT=wg[:, k, 0:128], rhs=hT[:, k, bs],
                                                                     start=(k == 0), stop=(k == 7)) for k in range(8)]
                        S.op("pe", fns, reads=[r_wg] + r_hT[4 * b:4 * b + 4], writes=[r_pg])
                        pu, r_pu = ps_ring.next()
                        fns = [lambda e, k=k, pu=pu, oT=oT, ko=ko, wu=wu, bs=bs: e.matmul(pu[:], lhsT=wu[:, ko + k, 0:128], rhs=oT[:, k, bs],
                                                                            start=(k == 0), stop=(k == 3)) for k in range(4)]
                        S.op("pe", fns, reads=[r_wu] + r_oT[4 * b:4 * b + 4], writes=[r_pu])
                        sg, r_sg = tmpf_ring.next()
                        S.op("act", lambda e, pg=pg, sg=sg: e.activation(out=sg, in_=pg[:], func=AF.Sigmoid),
                             reads=[r_pg], writes=[r_sg])
                        S.op("dve", lambda e, pu=pu, sg=sg: e.tensor_tensor(out=sg, in0=sg, in1=pu[:], op=ALU.mult),
                             reads=[r_pu, r_sg], writes=[r_sg])
                        res.append((sg, r_sg))
                    S.op("dve", lambda e, a=res[0][0], b_=res[1][0], oc=oc, bs=bs: e.tensor_tensor(
                        out=yT[:, oc, bs], in0=a, in1=b_, op=ALU.add), reads=[res[0][1], res[1][1]], writes=[r_yT[oc]])
            for i in range(16):
                tt = tt0 + i
                xa, r_xa = xt_ring.next()
                S.op("sp", lambda e, xa=xa, tt=tt: e.dma_start(out=xa, in_=src_d[tt * 128:(tt + 1) * 128, :]),
                     reads=[r_src[tt]], writes=[r_xa], dma=True)
                for h2 in range(2):
                    po, r_po = ps_ring.next()
                    fns = [lambda e, oc=oc, po=po, i=i, h2=h2: e.matmul(po[:], lhsT=yT[:, oc, i * 128:(i + 1) * 128],
                                                                        rhs=wout[:, oc, h2 * 512:(h2 + 1) * 512],
                                                                        start=(oc == 0), stop=(oc == 7)) for oc in range(8)]
                    S.op("pe", fns, reads=r_yT + [r_wout], writes=[r_po])
                    S.op("dve", lambda e, po=po, xa=xa, h2=h2: e.tensor_tensor(
                        out=xa[:, h2 * 512:(h2 + 1) * 512], in0=po[:], in1=xa[:, h2 * 512:(h2 + 1) * 512], op=ALU.add),
                        reads=[r_po, r_xa], writes=[r_xa])
                S.op("sp", lambda e, xa=xa, tt=tt: e.dma_start(out=y_d[tt * 128:(tt + 1) * 128, :], in_=xa),
                     reads=[r_xa], writes=[r_y[tt]], dma=True)

    cur_d, cur_r = x_d, r_x
    for l in range(depth):
        if f"ffn{2 * l}" in phases or "all" in phases:
            ffn_phase(l, 0, cur_d, cur_r)
            cur_d, cur_r = y_d, r_y
        if f"mix{l}" in phases or "all" in phases:
            mix_phase(l, cur_d, cur_r)
            cur_d, cur_r = y_d, r_y
        if f"ffn{2 * l + 1}" in phases or "all" in phases:
            ffn_phase(l, 1, cur_d, cur_r)
            cur_d, cur_r = y_d, r_y

    S.barrier()
    S.finish("sp", r_y)
    sems = [es.enter_context(nc.semaphore(f"s{i}")) for i in range(S.nsem)]
    S.emit(nc, sems)
    es.close()
    return nc, S


def make_in_maps(inputs, nseq, ncores):
    x = np.ascontiguousarray(inputs["x"], dtype=np.float32).reshape(-1, nseq * T, D)
    consts = make_consts()
    shared = {}
    for k in ("norm_g", "ffn_w1", "ffn_w3", "ffn_w2", "w_in", "g_qk_nsa", "g_qk_moba", "cmp_w1", "cmp_w2", "w_up_nsa", "w_up_moba",
              "w_out"):
        shared[k] = np.ascontiguousarray(inputs[k], dtype=np.float32)
    shared["g_qk_nsaT"] = np.ascontiguousarray(np.transpose(np.asarray(inputs["g_qk_nsa"], np.float32), (0, 2, 1)))
    shared["g_qk_mobaT"] = np.ascontiguousarray(np.transpose(np.asarray(inputs["g_qk_moba"], np.float32), (0, 2, 1)))
    shared["cmp_posT"] = np.ascontiguousarray(np.transpose(np.asarray(inputs["cmp_pos"], np.float32), (0, 1, 3, 2)))
    shared.update(consts)
    in_maps = []
    for c in range(ncores):
        m = {"x": x[c]}
        m.update(shared)
        in_maps.append(m)
    return in_maps


_CACHE = {}


def kernel(**inputs):
    nseq = 16 // NCORES
    if "full" not in _CACHE:
        _CACHE["full"] = build_program(nseq=nseq, phases=("all",))
    nc, S = _CACHE["full"]
    in_maps = make_in_maps(inputs, nseq, NCORES)
    res = run_bass_kernel_spmd(nc, in_maps, core_ids=list(range(NCORES)))
    y = np.stack([np.asarray(r["y"]) for r in res.results], axis=0)
    return y.reshape(16, T, D).astype(np.float32)
```

```python
import numpy as np
from contextlib import ExitStack
import concourse.bass as bass
import concourse.mybir as mybir
from concourse.bass_utils import run_bass_kernel_spmd

F32 = mybir.dt.float32
BF16 = mybir.dt.bfloat16
AF = mybir.ActivationFunctionType
ALU = mybir.AluOpType
AX = mybir.AxisListType

NCORES = 8
D = 1024
T = 2048
DFF = 2816
NF = DFF // 128
DEPTH = 2
EPS = 1e-6


class Res:
    __slots__ = ("w", "r", "name")

    def __init__(self, name=""):
        self.w = None
        self.r = {}
        self.name = name


class _Eng:
    def __init__(self, name, sem):
        self.name = name
        self.sem = sem
        self.count = 0
        self.known = {}
        self.ops = []
        self.dma_sems = []
        self.dma_count = 0


class Sched:
    NDMA = 8

    def __init__(self):
        self.nsem = 0
        self.eng = {}
        for n in ("pe", "act", "dve", "pool", "sp"):
            self.eng[n] = _Eng(n, self._newsem())
        for n in ("sp", "pool", "act"):
            self.eng[n].dma_sems = [self._newsem() for _ in range(self.NDMA)]

    def _newsem(self):
        s = self.nsem
        self.nsem += 1
        return s

    def op(self, eng, fns, reads=(), writes=(), dma=False):
        E = self.eng[eng]
        if not isinstance(fns, (list, tuple)):
            fns = [fns]
        need = {}

        def req(tok):
            if tok is None:
                return
            s, v, clk = tok
            o = need.get(s)
            if o is None or o[0] < v:
                need[s] = (v, clk)

        for r in reads:
            req(r.w)
        for w in writes:
            req(w.w)
            for s, (v, clk) in w.r.items():
                req((s, v, clk))
        implied = {}
        for s, (v, clk) in need.items():
            for cs, cv in clk.items():
                if implied.get(cs, 0) < cv:
                    implied[cs] = cv
        waits = []
        known = E.known
        for s, (v, clk) in need.items():
            if known.get(s, 0) >= v or implied.get(s, 0) >= v:
                continue
            if eng == "pe" and s == E.sem:
                continue
            waits.append((s, v))
        for s, v in implied.items():
            if known.get(s, 0) < v:
                known[s] = v
        for s, (v, clk) in need.items():
            if known.get(s, 0) < v:
                known[s] = v
        if dma:
            j = E.dma_count
            E.dma_count += 1
            s = E.dma_sems[j % self.NDMA]
            prev = 16 * (j // self.NDMA)
            if prev > 0 and known.get(s, 0) < prev:
                waits.append((s, prev))
                known[s] = prev
            val = prev + 16
            inc = 16
        else:
            E.count += 1
            s = E.sem
            val = E.count
            inc = 1
        clk = dict(known)
        tok = (s, val, clk)
        E.ops.append((waits, list(fns), s, inc))
        for r in reads:
            o = r.r.get(s)
            if o is None or o[0] < val:
                r.r[s] = (val, clk)
        for w in writes:
            w.w = tok
            w.r = {}
        return tok

    def finish(self, eng, resources):
        E = self.eng[eng]
        need = {}
        for r in resources:
            toks = []
            if r.w is not None:
                toks.append(r.w)
            for s, (v, clk) in r.r.items():
                toks.append((s, v, clk))
            for s, v, clk in toks:
                if need.get(s, 0) < v:
                    need[s] = v
        waits = [(s, v) for s, v in need.items() if E.known.get(s, 0) < v]
        E.ops.append((waits, [], None, 0))

    def emit(self, nc, sems):
        def replay(E):
            def body(e):
                for waits, fns, s, inc in E.ops:
                    for ws, wv in waits:
                        e.wait_ge(sems[ws], wv)
                    if not fns:
                        continue
                    for fn in fns[:-1]:
                        fn(e)
                    fns[-1](e).then_inc(sems[s], inc)
            return body

        with nc.Block() as block:
            block.sync(replay(self.eng["sp"]))
            block.scalar(replay(self.eng["act"]))
            block.vector(replay(self.eng["dve"]))
            block.gpsimd(replay(self.eng["pool"]))
            block.tensor(replay(self.eng["pe"]))


class Ring:
    def __init__(self, items):
        self.items = items
        self.i = 0

    def next(self):
        it = self.items[self.i % len(self.items)]
        self.i += 1
        return it


def _barrier(self):
    toks = {}
    for E in self.eng.values():
        if E.count > 0:
            toks[E.sem] = E.count
        for i, s in enumerate(E.dma_sems):
            if E.dma_count > i:
                toks[s] = 16 * ((E.dma_count - i + self.NDMA - 1) // self.NDMA)
    for E in self.eng.values():
        waits = []
        for s, v in toks.items():
            if E.known.get(s, 0) >= v:
                continue
            if E.name == "pe" and s == E.sem:
                continue
            waits.append((s, v))
            E.known[s] = v
        if waits:
            E.ops.append((waits, [], None, 0))


Sched.barrier = _barrier


class Arena:
    def __init__(self, t, nel):
        self.t = t
        self.nel = nel
        self.off = 0

    def alloc(self, shape, dt):
        n = 1
        for d in shape[1:]:
            n *= d
        sz = n * (2 if dt == F32 else 1)
        self.off = (self.off + 1) // 2 * 2
        o = self.off
        self.off += sz
        assert self.off <= self.nel, ("arena overflow", self.off, self.nel)
        ap = self.t[0:shape[0], o:o + sz]
        if dt == F32:
            ap = ap.bitcast(F32)
        if len(shape) == 3:
            ap = ap.rearrange("p (a b) -> p a b", a=shape[1])
        elif len(shape) == 4:
            ap = ap.rearrange("p (a b c) -> p a b c", a=shape[1], b=shape[2])
        return ap


BIG = 30000.0
NSA_COLS = dict(q=0, kc=512, vc=640, ks=768, vs=896, kw=1024, vw=1152, g=1280)
MOBA_COLS = dict(q=1304, k=1816, v=2328)
GATE_N, GATE_M = 2840, 3864
INC = 4888


def slopes_all():
    s = (2.0 ** (-8.0 * np.arange(1, 17) / 16)).astype(np.float32)
    return s[0::2].copy(), s[1::2].copy()


def _bf16_round(a):
    a = np.asarray(a, dtype=np.float32)
    u = a.view(np.uint32).astype(np.uint64)
    r = ((u + 0x7FFF + ((u >> 16) & 1)) >> 16) << 16
    return r.astype(np.uint32).view(np.float32)


def make_consts():
    c = {}
    c["ident"] = np.eye(128, dtype=np.float32)
    j = np.arange(128)[:, None]
    i = np.arange(128)[None, :]
    c["tri_c"] = np.where(j > i, -BIG, 0.0).astype(np.float32)
    c["tri_a"] = np.where(j <= i, -BIG, 0.0).astype(np.float32)
    c["ones64"] = np.ones((64, 64), np.float32)
    bd = np.zeros((128, 128), np.float32)
    bd[:64, :64] = 1.0
    bd[64:, 64:] = 1.0
    c["bd_ones"] = bd
    sn, sm = slopes_all()
    sl = np.concatenate([sn, sm])
    ab = np.zeros((128, 16, 19), np.float32)
    for h in range(16):
        for d in range(-15, 4):
            ab[:, h, d + 15] = sl[h].astype(np.float64) * (128 * d + np.arange(128))
    c["abias"] = ab.reshape(128, 16 * 19)
    cb = np.zeros((128, 8, 4), np.float32)
    cc = np.arange(128)
    for h in range(8):
        for qb in range(4):
            cb[:, h, qb] = sn[h].astype(np.float64) * (16 * cc + 15.5 - 512 * qb)
    c["cbias"] = cb.reshape(128, 32)
    qa = np.zeros((16, 7, 512), np.float32)
    for h in range(16):
        v = (-(sl[h].astype(np.float64)) * np.arange(512)).astype(np.float32)
        v1 = _bf16_round(v)
        v2 = _bf16_round(v - v1)
        v3 = _bf16_round(v - v1 - v2)
        qa[h, 0], qa[h, 1], qa[h, 2] = v1, v2, v3
        s1 = _bf16_round(np.float32(sl[h]))
        s2 = _bf16_round(np.float32(sl[h]) - s1)
        qa[h, 3], qa[h, 4], qa[h, 5], qa[h, 6] = 128.0 * s1, 128.0 * s2, s1, s2
    c["qalibi"] = np.ascontiguousarray(np.tile(qa, (1, 1, 4)))
    key = np.arange(T)
    c["e32"] = (key[None, :] // 64 == np.arange(32)[:, None]).astype(np.float32)
    c["e8"] = (key[None, :] // 256 == np.arange(8)[:, None]).astype(np.float32)
    kp7 = np.ones((7, T), np.float32)
    kp7[3] = kp7[4] = key // 128
    kp7[5] = kp7[6] = key % 128
    c["ones3"] = kp7
    c["zeros32"] = np.zeros((32, T), np.float32)
    cm = np.zeros((128, T), np.float32)
    cidx = np.arange(128)[:, None]
    cm[:] = np.where(16 * cidx + 31 <= key[None, :], 0.0, -BIG)
    c["cmask"] = cm
    ci = np.arange(127)[:, None] * 16
    sj = np.arange(32)[None, :] * 64
    ov = np.clip(np.minimum(ci + 32, sj + 64) - np.maximum(ci, sj), 0, None)
    M = np.zeros((128, 32), np.float32)
    M[:127] = ov / 32.0
    c["cmp2slc"] = M
    blk = np.arange(32)[None, None, :]
    t = (np.arange(16)[None, :, None] * 128 + np.arange(128)[:, None, None])
    cur = t // 64
    forced = (blk == 0) | (blk == cur) | (blk == cur - 1)
    valid = blk <= cur
    c["nsa_mult"] = np.where(forced | ~valid, 0.0, 1.0).astype(np.float32).reshape(128, 16 * 32)
    c["nsa_add"] = np.where(forced, 1e9, np.where(valid, 0.0, -1e30)).astype(np.float32).reshape(128, 16 * 32)
    n8 = np.arange(8)[None, None, :]
    curm = t // 256
    c["moba_tmpl"] = np.broadcast_to(np.where(n8 <= curm, 0.0, -BIG), (128, 16, 8)).astype(np.float32).reshape(128, 128).copy()
    c["moba_add"] = np.broadcast_to(np.where(n8 < curm, 0.0, -1e30), (128, 16, 8)).astype(np.float32).reshape(128, 128).copy()
    return c


CONST_SHAPES = dict(ident=[128, 128], tri_c=[128, 128], tri_a=[128, 128], ones64=[64, 64], bd_ones=[128, 128], abias=[128, 304],
                    cbias=[128, 32], qalibi=[16, 7, T], e32=[32, T], e8=[8, T], ones3=[7, T], zeros32=[32, T],
                    cmask=[128, T], cmp2slc=[128, 32], nsa_mult=[128, 512], nsa_add=[128, 512], moba_add=[128, 128], moba_tmpl=[128, 128])


def build_program(nseq=2, phases=("all",), depth=DEPTH, dbg=None):
    nc = bass.Bass("TRN2", target_bir_lowering=False)
    NT = nseq * T
    NTT = NT // 128
    S = Sched()
    es = ExitStack()

    def dram_in(name, shape, dt=F32):
        return nc.dram_tensor(name, list(shape), dt, kind="ExternalInput").ap()

    x_d = dram_in("x", [NT, D])
    normg_d = dram_in("norm_g", [DEPTH, 3, D])
    w1_d = dram_in("ffn_w1", [DEPTH, 2, D, DFF])
    w3_d = dram_in("ffn_w3", [DEPTH, 2, D, DFF])
    w2_d = dram_in("ffn_w2", [DEPTH, 2, DFF, D])
    win_d = dram_in("w_in", [DEPTH, D, INC])
    gqn_d = dram_in("g_qk_nsa", [DEPTH, 4, 64])
    gqnT_d = dram_in("g_qk_nsaT", [DEPTH, 64, 4])
    gqm_d = dram_in("g_qk_moba", [DEPTH, 2, 64])
    gqmT_d = dram_in("g_qk_mobaT", [DEPTH, 64, 2])
    posT_d = dram_in("cmp_posT", [DEPTH, 2, 64, 32])
    cw1_d = dram_in("cmp_w1", [DEPTH, 2, 2048, 256])
    cw2_d = dram_in("cmp_w2", [DEPTH, 2, 256, 64])
    wupn_d = dram_in("w_up_nsa", [DEPTH, 512, D])
    wupm_d = dram_in("w_up_moba", [DEPTH, 512, D])
    wout_d = dram_in("w_out", [DEPTH, D, D])
    C = {k: dram_in(k, v) for k, v in CONST_SHAPES.items()}
    y_d = nc.dram_tensor("y", [NT, D], F32, kind="ExternalOutput").ap()
    dbg_d = {}
    if dbg:
        for k, shp in dbg.items():
            dbg_d[k] = nc.dram_tensor("dbg_" + k, list(shp), F32, kind="ExternalOutput").ap()

    def sb(name, shape, dt):
        return es.enter_context(nc.sbuf_tensor(name, list(shape), dt))

    banks = []
    for i in (0, 1, 2):
        t = es.enter_context(nc.psum_tensor(f"ps{i}", [128, 512], F32))
        banks.append((t[:], Res(f"ps{i}")))
    pairs_ps = []
    for i in (0, 1):
        t = es.enter_context(nc.psum_tensor(f"pp{i}", [128, 1024], F32))
        ra, rb = Res(f"pp{i}a"), Res(f"pp{i}b")
        banks.append((t[:, 0:512], ra))
        banks.append((t[:, 512:1024], rb))
        pairs_ps.append((t[:], [ra, rb]))
    t = es.enter_context(nc.psum_tensor("ps7", [128, 512], F32))
    banks.append((t[:], Res("ps7")))
    ps_ring = Ring(banks)
    acc_banks = banks[0:3]
    acc_ring = Ring(banks[0:3])
    pair_ring = Ring(pairs_ps)
    sc_ring = Ring(banks[3:7])
    misc_ring = Ring(banks[7:8])

    ident_b = sb("ident_b", [128, 128], BF16)
    r_ident = Res("ident")
    S.op("pool", lambda e: e.dma_start(out=ident_b[:], in_=C["ident"][:, :]), writes=[r_ident], dma=True)
    stat = [(sb(f"stat{i}", [128, 4], F32), Res(f"stat{i}")) for i in range(4)]
    stat_ring = Ring(stat)
    ARENA_EL = 105000
    arena_t = sb("arena", [128, ARENA_EL], BF16)
    A = Arena(arena_t, ARENA_EL)

    r_y = [Res(f"y{i}") for i in range(NTT)]
    r_x = [Res(f"x{i}") for i in range(NTT)]

    def rmsnorm_tile(x_ap, r_x_, g_ap, r_gres, out_ap, r_out, jk, r_jk):
        st, r_st = stat_ring.next()
        S.op("act", lambda e: e.activation(out=jk, in_=x_ap, func=AF.Square, accum_out=st[:, 0:1]),
             reads=[r_x_], writes=[r_jk, r_st])
        S.op("act", lambda e: e.activation(out=st[:, 1:2], in_=st[:, 0:1], func=AF.Sqrt, scale=1.0 / D, bias=EPS),
             reads=[r_st], writes=[r_st])
        S.op("dve", lambda e: e.reciprocal(out=st[:, 2:3], in_=st[:, 1:2]), reads=[r_st], writes=[r_st])
        S.op("dve", lambda e: e.scalar_tensor_tensor(out=out_ap, in0=x_ap, scalar=st[:, 2:3], in1=g_ap,
                                                     op0=ALU.mult, op1=ALU.mult),
             reads=[r_x_, r_st, r_gres], writes=[r_out])

    def transpose_tile(in_tile, r_in, out_ap3, r_out, nk=8, evac="act", ring=None):
        ps, r_ps = (ring or ps_ring).next()
        psb = ps[:].bitcast(BF16)
        fns = []
        for k in range(nk):
            fns.append(lambda e, k=k: e.transpose(out=psb[:, k * 128:(k + 1) * 128],
                                                  in_=in_tile[:, k * 128:(k + 1) * 128], identity=ident_b[:]))
        S.op("pe", fns, reads=[r_in, r_ident], writes=[r_ps])
        src = psb[:, 0:nk * 128].rearrange("p (k t) -> p k t", k=nk)
        if evac == "act":
            S.op("act", lambda e: e.copy(out=out_ap3, in_=src), reads=[r_ps], writes=[r_out])
        else:
            S.op("dve", lambda e: e.tensor_copy(out=out_ap3, in_=src), reads=[r_ps], writes=[r_out])

    def ffn_phase(l, j, src_d, r_src):
        S.barrier()
        A.off = 0
        w1_sb = A.alloc([128, 8, DFF], BF16)
        w3_sb = A.alloc([128, 8, DFF], BF16)
        w2_sb = A.alloc([128, NF, D], BF16)
        r_w1 = [Res() for f in range(NF)]
        r_w3 = [Res() for f in range(NF)]
        r_w2 = [Res() for f in range(NF)]
        g_rep = A.alloc([128, D], F32)
        r_g = Res()
        xn_ring = Ring([(A.alloc([128, D], F32), Res()) for i in range(2)])
        xr_ring = Ring([(A.alloc([128, D], F32), Res()) for i in range(2)])
        hb = [(A.alloc([128, D], BF16), Res()) for i in range(4)]
        hT2 = [A.alloc([128, 8, 512], BF16) for i in range(2)]
        r_hT2 = [[Res() for i in range(4)] for _ in range(2)]
        gT = A.alloc([128, NF, 512], BF16)
        r_gT = [Res() for f in range(NF)]
        su_ring = Ring([(A.alloc([128, 512], F32), Res()) for i in range(2)])
        junk = (A.alloc([128, D], BF16), Res())
        NB = NT // 512

        S.op("pool", lambda e: e.dma_start(out=g_rep, in_=normg_d[l, 2 * j, :].partition_broadcast(128)),
             writes=[r_g], dma=True)
        for f in range(NF):
            S.op("pool", lambda e, f=f: e.dma_start(
                out=w1_sb[:, :, f * 128:(f + 1) * 128],
                in_=w1_d[l, j, :, f * 128:(f + 1) * 128].rearrange("(k p) c -> p k c", p=128)),
                writes=[r_w1[f]], dma=True)
            S.op("pool", lambda e, f=f: e.dma_start(
                out=w3_sb[:, :, f * 128:(f + 1) * 128],
                in_=w3_d[l, j, :, f * 128:(f + 1) * 128].rearrange("(k p) c -> p k c", p=128)),
                writes=[r_w3[f]], dma=True)
        for f in range(NF):
            S.op("pool", lambda e, f=f: e.dma_start(out=w2_sb[:, f, :], in_=w2_d[l, j, f * 128:(f + 1) * 128, :]),
                 writes=[r_w2[f]], dma=True)

        def norm_part(blk):
            for i in range(4):
                tt = blk * 4 + i
                xa, r_xa = xn_ring.next()
                S.op("sp", lambda e, xa=xa, tt=tt: e.dma_start(out=xa, in_=src_d[tt * 128:(tt + 1) * 128, :]),
                     reads=[r_src[tt]], writes=[r_xa], dma=True)
                hbt, r_hb = hb[i]
                rmsnorm_tile(xa, r_xa, g_rep, r_g, hbt, r_hb, junk[0], junk[1])

        def transpose_part(blk):
            hT = hT2[blk % 2]
            for i in range(4):
                hbt, r_hb = hb[i]
                transpose_tile(hbt, r_hb, hT[:, :, i * 128:(i + 1) * 128], r_hT2[blk % 2][i])

        def up_part(blk):
            hT = hT2[blk % 2]
            r_hT = r_hT2[blk % 2]
            for f in range(NF):
                pu, r_pu = ps_ring.next()
                pv, r_pv = ps_ring.next()
                fns = [lambda e, k=k, f=f, pu=pu: e.matmul(pu[:], lhsT=w1_sb[:, k, f * 128:(f + 1) * 128],
                                                           rhs=hT[:, k, :], start=(k == 0), stop=(k == 7)) for k in range(8)]
                S.op("pe", fns, reads=[r_w1[f]] + r_hT, writes=[r_pu])
                fns = [lambda e, k=k, f=f, pv=pv: e.matmul(pv[:], lhsT=w3_sb[:, k, f * 128:(f + 1) * 128],
                                                           rhs=hT[:, k, :], start=(k == 0), stop=(k == 7)) for k in range(8)]
                S.op("pe", fns, reads=[r_w3[f]] + r_hT, writes=[r_pv])
                s_t, r_s = su_ring.next()
                S.op("act", lambda e, s_t=s_t, pu=pu: e.activation(out=s_t, in_=pu[:], func=AF.Silu),
                     reads=[r_pu], writes=[r_s])
                S.op("dve", lambda e, s_t=s_t, pv=pv, f=f: e.tensor_tensor(out=gT[:, f, :], in0=s_t, in1=pv[:],
                                                                             op=ALU.mult),
                     reads=[r_s, r_pv], writes=[r_gT[f]])

        def down_part(blk):
            for i in range(4):
                tt = blk * 4 + i
                xa, r_xa = xr_ring.next()
                S.op("sp", lambda e, xa=xa, tt=tt: e.dma_start(out=xa, in_=src_d[tt * 128:(tt + 1) * 128, :]),
                     reads=[r_src[tt]], writes=[r_xa], dma=True)
                for h in range(2):
                    po, r_po = ps_ring.next()
                    fns = [lambda e, f=f, po=po, i=i, h=h: e.matmul(
                        po[:], lhsT=gT[:, f, i * 128:(i + 1) * 128], rhs=w2_sb[:, f, h * 512:(h + 1) * 512],
                        start=(f == 0), stop=(f == NF - 1)) for f in range(NF)]
                    S.op("pe", fns, reads=r_gT + r_w2, writes=[r_po])
                    S.op("dve", lambda e, po=po, xa=xa, h=h: e.scalar_tensor_tensor(
                        out=xa[:, h * 512:(h + 1) * 512], in0=po[:], scalar=0.5, in1=xa[:, h * 512:(h + 1) * 512],
                        op0=ALU.mult, op1=ALU.add), reads=[r_po, r_xa], writes=[r_xa])
                S.op("sp", lambda e, xa=xa, tt=tt: e.dma_start(out=y_d[tt * 128:(tt + 1) * 128, :], in_=xa),
                     reads=[r_xa], writes=[r_y[tt]], dma=True)

        norm_part(0)
        transpose_part(0)
        for blk in range(NB):
            if blk + 1 < NB:
                norm_part(blk + 1)
            up_part(blk)
            if blk + 1 < NB:
                transpose_part(blk + 1)
            down_part(blk)

    def mix_phase(l, src_d, r_src):
        S.barrier()
        A.off = 0
        r_c = Res()
        tri_c = A.alloc([128, 128], BF16)
        tri_a = A.alloc([128, 128], BF16)
        ones64 = A.alloc([64, 64], BF16)
        bd_ones = A.alloc([128, 128], BF16)
        gtab = A.alloc([128, 4], F32)
        abias = A.alloc([128, 304], F32)
        cbias = A.alloc([128, 32], F32)
        cmask = A.alloc([128, T], BF16)
        nsa_mult = A.alloc([128, 16, 32], F32)
        nsa_add = A.alloc([128, 16, 32], F32)
        moba_add = A.alloc([128, 16, 8], F32)
        moba_tmpl = A.alloc([128, 16, 8], BF16)
        gq_n = A.alloc([64, 4], F32)
        gq_m = A.alloc([64, 2], F32)
        gkc_rep = A.alloc([128, 64], F32)
        g_rep = A.alloc([128, D], F32)
        for dst, src in ((tri_c, C["tri_c"][:, :]), (tri_a, C["tri_a"][:, :]), (ones64, C["ones64"][:, :]), (bd_ones, C["bd_ones"][:, :]),
                         (gtab[0:64, 0:1], gqn_d[l, 0, :].unsqueeze(1)), (gtab[64:128, 0:1], gqn_d[l, 0, :].unsqueeze(1)),
                         (gtab[0:64, 1:2], gqn_d[l, 2, :].unsqueeze(1)), (gtab[64:128, 1:2], gqn_d[l, 3, :].unsqueeze(1)),
                         (gtab[0:64, 2:3], gqm_d[l, 0, :].unsqueeze(1)), (gtab[64:128, 2:3], gqm_d[l, 0, :].unsqueeze(1)),
                         (gtab[0:64, 3:4], gqm_d[l, 1, :].unsqueeze(1)), (gtab[64:128, 3:4], gqm_d[l, 1, :].unsqueeze(1)),
                         (abias, C["abias"][:, :]), (cbias, C["cbias"][:, :]), (cmask, C["cmask"][:, :]),
                         (nsa_mult, C["nsa_mult"][:, :].rearrange("p (a b) -> p a b", a=16)),
                         (nsa_add, C["nsa_add"][:, :].rearrange("p (a b) -> p a b", a=16)),
                         (moba_add, C["moba_add"][:, :].rearrange("p (a b) -> p a b", a=16)),
                         (moba_tmpl, C["moba_tmpl"][:, :].rearrange("p (a b) -> p a b", a=16)),
                         (gq_n, gqnT_d[l, :, :]), (gq_m, gqmT_d[l, :, :]),
                         (gkc_rep, gqn_d[l, 1, :].partition_broadcast(128)),
                         (g_rep, normg_d[l, 1, :].partition_broadcast(128))):
            S.op("pool", lambda e, dst=dst, src=src: e.dma_start(out=dst, in_=src), writes=[r_c], dma=True)
        S.op("dve", lambda e: e.tensor_scalar(out=gq_n[:, 0:1], in0=gq_n[:, 0:1], scalar1=0.125, scalar2=None,
                                              op0=ALU.mult), reads=[r_c], writes=[r_c])
        S.op("dve", lambda e: e.tensor_scalar(out=gq_m[:, 0:1], in0=gq_m[:, 0:1], scalar1=0.125, scalar2=None,
                                              op0=ALU.mult), reads=[r_c], writes=[r_c])
        S.op("dve", lambda e: e.tensor_scalar(out=gtab[:, 0:1], in0=gtab[:, 0:1], scalar1=0.125, scalar2=None,
                                              op0=ALU.mult), reads=[r_c], writes=[r_c])
        S.op("dve", lambda e: e.tensor_scalar(out=gtab[:, 2:3], in0=gtab[:, 2:3], scalar1=0.125, scalar2=None,
                                              op0=ALU.mult), reads=[r_c], writes=[r_c])

        hT = A.alloc([128, 8, T], BF16)
        r_hT = [Res() for _ in range(16)]
        onT = A.alloc([128, 4, T], BF16)
        omT = A.alloc([128, 4, T], BF16)
        r_onT = [Res() for _ in range(16)]
        r_omT = [Res() for _ in range(16)]
        gsig = A.alloc([128, 16, 24], F32)
        r_gsig = Res()
        wch_ring = Ring([(A.alloc([128, 8, 256], BF16), Res()) for _ in range(3)])
        pT_ring = Ring([(A.alloc([128, 1024], BF16), Res()) for _ in range(2)])
        tmpf_ring = Ring([(A.alloc([128, 512], F32), Res()) for _ in range(3)])
        sqb_ring = Ring([(A.alloc([128, 512], BF16), Res()) for _ in range(3)])
        xt_ring = Ring([(A.alloc([128, D], F32), Res()) for _ in range(3)])
        hb_ring = Ring([(A.alloc([128, D], BF16), Res()) for _ in range(3)])
        junk = (A.alloc([128, D], BF16), Res())
        sm_ring = Ring([(A.alloc([128, 16], F32), Res()) for _ in range(8)])
        region0 = A.off

        def load_w(src_ap3, ncols):
            w, r_w = wch_ring.next()
            S.op("pool", lambda e: e.dma_start(out=w[:, :, 0:ncols], in_=src_ap3), writes=[r_w], dma=True)
            return w, r_w

        def win_cols(c0, n):
            return win_d[l, :, c0:c0 + n].rearrange("(k p) c -> p k c", p=128)

        def proj_fm_pairs(pairs):
            items = [(pi, b) for pi in range(len(pairs)) for b in range(4)]
            wts = {}
            stt = {}

            def ensure_w(pi):
                if pi < len(pairs) and pi not in wts:
                    c0A, c0B = pairs[pi][0], pairs[pi][1]
                    w, r_w = wch_ring.next()
                    if c0B == c0A + 64:
                        S.op("pool", lambda e: e.dma_start(out=w[:, :, 0:128], in_=win_cols(c0A, 128)), writes=[r_w], dma=True)
                    else:
                        S.op("pool", lambda e: e.dma_start(out=w[:, :, 0:64], in_=win_cols(c0A, 64)), writes=[r_w], dma=True)
                        S.op("pool", lambda e: e.dma_start(out=w[:, :, 64:128], in_=win_cols(c0B, 64)), writes=[r_w], dma=True)
                    wts[pi] = (w, r_w)

            def mm(i):
                pi, b = items[i]
                ensure_w(pi)
                if b == 0:
                    ensure_w(pi + 1)
                w, r_w = wts[pi]
                ps, r_ps = ps_ring.next()
                fns = [lambda e, k=k: e.matmul(ps[:, :], lhsT=w[:, k, 0:128], rhs=hT[:, k, b * 512:(b + 1) * 512],
                                               start=(k == 0), stop=(k == 7)) for k in range(8)]
                S.op("pe", fns, reads=[r_w] + r_hT[4 * b:4 * b + 4], writes=[r_ps])
                c0A, c0B, dA, rA, dB, rB, gcol = pairs[pi]
                if gcol is None:
                    S.op("act", lambda e: e.copy(out=dA(b), in_=ps[0:64, :]), reads=[r_ps], writes=[rA(b)])
                    S.op("act", lambda e: e.copy(out=dB(b), in_=ps[64:128, :]), reads=[r_ps], writes=[rB(b)])
                    stt[i] = None
                    return
                sq, r_sq = sqb_ring.next()
                S.op("act", lambda e: e.activation(out=sq[:, :], in_=ps[:, :], func=AF.Square), reads=[r_ps], writes=[r_sq])
                stt[i] = (ps, r_ps, sq, r_sq, b)

            def rest(i):
                if stt[i] is None:
                    return
                ps, r_ps, sq, r_sq, b = stt[i]
                c0A, c0B, dA, rA, dB, rB, gcol = pairs[items[i][0]]
                p2, r_p2 = ps_ring.next()
                S.op("pe", lambda e: e.matmul(p2[:, :], lhsT=bd_ones[:, :], rhs=sq[:, :], start=True, stop=True),
                     reads=[r_sq, r_c], writes=[r_p2])
                tf, r_tf = tmpf_ring.next()
                S.op("act", lambda e: e.activation(out=tf[:, :], in_=p2[:, :], func=AF.Ln, scale=1.0 / 64, bias=EPS),
                     reads=[r_p2], writes=[r_tf])
                S.op("act", lambda e: e.activation(out=tf[:, :], in_=tf[:, :], func=AF.Exp, scale=-0.5),
                     reads=[r_tf], writes=[r_tf])
                S.op("dve", lambda e: e.scalar_tensor_tensor(out=dA(b), in0=ps[0:64, :], scalar=gcol[0:64, :], in1=tf[0:64, :],
                                                             op0=ALU.mult, op1=ALU.mult),
                     reads=[r_ps, r_tf, r_c], writes=[rA(b)])
                S.op("dve", lambda e: e.scalar_tensor_tensor(out=dB(b), in0=ps[64:128, :], scalar=gcol[64:128, :],
                                                             in1=tf[64:128, :], op0=ALU.mult, op1=ALU.mult),
                     reads=[r_ps, r_tf, r_c], writes=[rB(b)])

            mm(0)
            for i in range(len(items)):
                if i + 1 < len(items):
                    mm(i + 1)
                rest(i)

        def causal_tiles(qb):
            past = [(kt, 0, 512, None, None) for kt in range(4 * qb)]
            dg = [(4 * qb + c, 128 * c, 512, "c", c) for c in range(4)]
            if qb == 0:
                return [dg[1], dg[0], dg[3], dg[2]]
            tl = [dg[1], past[0], dg[2], past[1], dg[3], past[2], dg[0], past[3]]
            return tl + past[4:]

        def window_tiles(qb):
            dg = [(4 * qb + c, 128 * c, 512, "c", c) for c in range(4)]
            if qb == 0:
                return [dg[1], dg[0], dg[3], dg[2]]
            ng = {}
            for c in (-1, -2, -3, -4):
                m = 4 + c
                ng[c] = (4 * qb + c, 0, 128 * (m + 1), "a", m)
            return [dg[0], ng[-1], dg[1], ng[-2], dg[2], ng[-3], dg[3], ng[-4]]

        def run_rounds(rounds):
            units = []
            for R in rounds:
                tl = R["tiles"]
                if "mask" in R:
                    grp = [[t] for t in tl]
                else:
                    grp = [tl[i:i + 2] for i in range(0, len(tl), 2)]
                for gi, gtiles in enumerate(grp):
                    units.append((R, gi, len(grp), gtiles))

            def emit_qk(un):
                R, gi, ng, gtiles = un
                pp, rps = pair_ring.next()
                kp = R.get("kpart", 128)
                K = R["krows"]
                qb = R["qb"]
                fns = []
                for j, (kt, c0, c1, tri, tu) in enumerate(gtiles):
                    half = pp[:, j * 512:(j + 1) * 512]
                    fns.append(lambda e, half=half, kt=kt, c0=c0, c1=c1, tri=tri: e.matmul(
                        half[0:kp, c0:c1], lhsT=R["kT"][0:K, kt * 128:kt * 128 + kp],
                        rhs=R["q"][0:K, qb * 512 + c0:qb * 512 + c1], start=True,
                        stop=(tri is None and "mask" not in R)))
                    if "mask" in R:
                        fns.append(lambda e, half=half, c0=c0, c1=c1: e.matmul(
                            half[0:kp, c0:c1], lhsT=ident_b[0:kp, 0:kp],
                            rhs=R["mask"][0:kp, qb * 512 + c0:qb * 512 + c1], start=False, stop=True))
                    if tri is not None:
                        tm = tri_c if tri == "c" else tri_a
                        fns.append(lambda e, half=half, tu=tu, tm=tm: e.matmul(
                            half[:, 128 * tu:128 * tu + 128], lhsT=ident_b[:, :], rhs=tm[:, :], start=False, stop=True))
                S.op("pe", fns, reads=[R["r_k"]] + R["r_q"] + [r_c, r_ident], writes=rps[0:len(gtiles)])
                return pp, rps

            pend = emit_qk(units[0]) if units else None
            for idx, un in enumerate(units):
                R, gi, ng, gtiles = un
                pp, rps = pend
                if idx + 1 < len(units):
                    pend = emit_qk(units[idx + 1])
                kp = R.get("kpart", 128)
                pT, r_pT = pT_ring.next()
                lo = gtiles[0][1]
                hi = 512 * (len(gtiles) - 1) + gtiles[-1][2]
                if "mask" in R:
                    bias_v = R["bias"](gtiles[0][0])
                else:
                    bias_v = R["fbias"]
                S.op("act", lambda e, pp=pp, pT=pT, bias_v=bias_v, kp=kp, lo=lo, hi=hi: e.activation(
                    out=pT[0:kp, lo:hi], in_=pp[0:kp, lo:hi], func=AF.Exp, bias=bias_v, scale=1.0),
                    reads=rps[0:len(gtiles)] + [r_c], writes=[r_pT])
                nv = R["nv"]
                if gi == 0:
                    R["acc"] = acc_ring.next()
                acc, r_acc = R["acc"]
                fns = []
                flat = [(j, u) for j, (kt, c0, c1, tri, tu) in enumerate(gtiles) for u in range(4) if c0 <= 128 * u < c1]
                for n_, (j, u) in enumerate(flat):
                    V = R["V"](gtiles[j][0])
                    fns.append(lambda e, u=u, j=j, acc=acc, V=V, first=(gi == 0 and n_ == 0),
                               last=(gi == ng - 1 and n_ == len(flat) - 1), pT=pT, kp=kp:
                               e.matmul(acc[:, 128 * u:128 * u + nv], lhsT=pT[0:kp, j * 512 + 128 * u:j * 512 + 128 * u + 128],
                                        rhs=V, start=first, stop=last))
                S.op("pe", fns, reads=[r_pT, R["r_v"]], writes=[r_acc])
                if gi == ng - 1:
                    R["evac"](acc[:, :].rearrange("p (u c) -> p u c", u=4), r_acc)

        sn_, sm_ = slopes_all()

        def dump(name, ap, reads):
            if name in dbg_d:
                dst = dbg_d[name][:, :]
                if len(ap.shape) == 3:
                    dst = dst.rearrange("p (a b) -> p a b", a=ap.shape[1])
                S.op("pool", lambda e: e.dma_start(out=dst[0:ap.shape[0]], in_=ap), reads=reads, writes=[Res()], dma=True)

        for s in range(nseq):
            tt0 = s * 16
            prev = None
            for i in range(16):
                xa, r_xa = xt_ring.next()
                S.op("sp", lambda e, xa=xa, i=i, tt0=tt0: e.dma_start(out=xa, in_=src_d[(tt0 + i) * 128:(tt0 + i + 1) * 128, :]),
                     reads=[r_src[tt0 + i]], writes=[r_xa], dma=True)
                hbt, r_hb = hb_ring.next()
                st, r_st = stat_ring.next()
                jk, r_jk = junk
                S.op("act", lambda e, xa=xa, st=st: e.activation(out=jk, in_=xa, func=AF.Square, accum_out=st[:, 0:1]),
                     reads=[r_xa], writes=[r_jk, r_st])
                S.op("act", lambda e, st=st: e.activation(out=st[:, 1:2], in_=st[:, 0:1], func=AF.Sqrt, scale=1.0 / D, bias=EPS),
                     reads=[r_st], writes=[r_st])
                if prev is not None:
                    prev()
                S.op("dve", lambda e, st=st: e.reciprocal(out=st[:, 2:3], in_=st[:, 1:2]), reads=[r_st], writes=[r_st])
                S.op("dve", lambda e, xa=xa, st=st, hbt=hbt: e.scalar_tensor_tensor(
                    out=hbt, in0=xa, scalar=st[:, 2:3], in1=g_rep, op0=ALU.mult, op1=ALU.mult),
                    reads=[r_xa, r_st, r_c], writes=[r_hb])
                ps, r_ps = ps_ring.next()
                psb = ps[:].bitcast(BF16)
                fns = [lambda e, k=k, psb=psb, hbt=hbt: e.transpose(out=psb[:, k * 128:(k + 1) * 128],
                                                                    in_=hbt[:, k * 128:(k + 1) * 128], identity=ident_b[:])
                       for k in range(8)]
                S.op("pe", fns, reads=[r_hb, r_ident], writes=[r_ps])

                def evac(psb=psb, r_ps=r_ps, i=i):
                    S.op("act", lambda e: e.copy(out=hT[:, :, i * 128:(i + 1) * 128],
                                                 in_=psb.rearrange("p (k t) -> p k t", k=8)), reads=[r_ps], writes=[r_hT[i]])
                prev = evac
            prev()

            w, r_w = load_w(win_cols(NSA_COLS["g"], 64), 64)
            for i in range(16):
                ps, r_ps = ps_ring.next()
                fns = [lambda e, k=k, ps=ps, i=i, w=w: e.matmul(ps[:, 0:24], lhsT=hT[:, k, i * 128:(i + 1) * 128],
                                                                rhs=w[:, k, 0:24], start=(k == 0), stop=(k == 7))
                       for k in range(8)]
                S.op("pe", fns, reads=[r_w, r_hT[i]], writes=[r_ps])
                S.op("act", lambda e, ps=ps, i=i: e.activation(out=gsig[:, i, :], in_=ps[:, 0:24], func=AF.Sigmoid),
                     reads=[r_ps], writes=[r_gsig])

            if dbg and s == 0 and l == 0:
                dump("gsig", gsig, [r_gsig])
            S.barrier()
            A.off = region0
            q_aug = [A.alloc([128, T], BF16) for _ in range(4)]
            r_q = [[Res() for _ in range(4)] for _ in range(4)]
            r_qs = [[Res() for _ in range(4)] for _ in range(4)]
            r_qst = Res()
            ks_aug = A.alloc([128, T], BF16)
            kw_aug = A.alloc([128, T], BF16)
            r_ks = Res()
            r_kw = Res()
            kcraw = A.alloc([64, T], BF16)
            r_kcraw = Res()
            vcraw = A.alloc([64, T], BF16)
            r_vcraw = Res()
            vsA = A.alloc([128, 16, 65], BF16)
            vwA = A.alloc([128, 16, 65], BF16)
            r_vs = Res()
            r_vw = Res()
            w1c = A.alloc([64, 32, 256], BF16)
            r_w1c = Res()
            w2c = A.alloc([128, 2, 64], BF16)
            posT = A.alloc([64, 32], BF16)
            r_w2c = Res()
            kc_aug = A.alloc([128, 128], BF16)
            r_kc = Res()
            kctm = A.alloc([128, 128], BF16)
            r_kctm = Res()
            vcA = A.alloc([128, 97], BF16)
            r_vc = Res()
            hid = A.alloc([128, 2, 128], BF16)
            r_hid = Res()
            pbias = A.alloc([128, 2], F32)
            r_pb = Res()
            oacc = A.alloc([128, 16, 256], F32)
            r_oacc = [Res() for _ in range(16)]
            impacc = A.alloc([128, 16, 32], F32)
            r_imp = [Res() for _ in range(16)]
            trin_all = [(A.alloc([128, 96], BF16), Res()) for _ in range(16)]
            ob_ring = Ring([(A.alloc([128, 256], BF16), Res()) for _ in range(2)])

            for g in range(2):
                for hl in range(4):
                    h = 4 * g + hl
                    S.op("pool", lambda e, hl=hl, h=h: e.dma_start(
                        out=q_aug[hl][96:103, :], in_=C["qalibi"][h, :, :]), writes=[r_qst], dma=True)
                    if g == 0:
                        S.op("pool", lambda e, hl=hl: e.dma_start(out=q_aug[hl][64:96, :], in_=C["zeros32"][:, :]),
                             writes=r_qs[hl], dma=True)
                for dst, r_dst, mid in (((ks_aug, r_ks, C["e32"]), (kw_aug, r_kw, C["zeros32"])) if g == 0 else ()):
                    S.op("pool", lambda e, dst=dst, mid=mid: e.dma_start(out=dst[64:96, :], in_=mid[:, :]),
                         writes=[r_dst], dma=True)
                    S.op("pool", lambda e, dst=dst: e.dma_start(out=dst[96:103, :], in_=C["ones3"][:, :]),
                         writes=[r_dst], dma=True)
                if g == 0:
                    S.op("pool", lambda e: e.memset(kctm[:, 64:96], 0.0), writes=[r_kctm])
                    S.op("pool", lambda e: e.memset(kctm[:, 96:99], 1.0), writes=[r_kctm])
                    S.op("pool", lambda e: e.memset(kctm[:, 99:103], 0.0), writes=[r_kctm])
                    S.op("pool", lambda e: e.memset(kctm[:, 0:64], 0.0), writes=[r_kctm])
                    S.op("pool", lambda e: e.memset(vsA[:, :, 64:65], 1.0), writes=[r_vs])
                    S.op("pool", lambda e: e.memset(vwA[:, :, 64:65], 1.0), writes=[r_vw])
                    S.op("pool", lambda e: e.memset(vcA[:, 64:65], 1.0), writes=[r_vc])
                    S.op("pool", lambda e: e.dma_start(out=vcA[:, 65:97], in_=C["cmp2slc"][:, :]), writes=[r_vc], dma=True)
                    for tr, r_tr in trin_all:
                        S.op("pool", lambda e, tr=tr: e.memset(tr[:, 0:64], 0.0), writes=[r_tr])

                def load_cmp_w(kv):
                    S.op("pool", lambda e: e.dma_start(
                        out=w1c, in_=cw1_d[l, kv, :, :].rearrange("(l d) h -> d l h", d=64)), writes=[r_w1c], dma=True)
                    S.op("pool", lambda e: e.dma_start(
                        out=w2c, in_=cw2_d[l, kv, :, :].rearrange("(a p) d -> p a d", p=128)), writes=[r_w2c], dma=True)
                    S.op("pool", lambda e: e.dma_start(out=posT, in_=posT_d[l, kv, :, :]), writes=[r_w2c], dma=True)

                proj_fm_pairs([(NSA_COLS["kc"] + 64 * g, NSA_COLS["vc"] + 64 * g,
                                lambda b: kcraw[0:64, b * 512:(b + 1) * 512], lambda b: r_kcraw,
                                lambda b: vcraw[0:64, b * 512:(b + 1) * 512], lambda b: r_vcraw, None)])
                for kv in range(2):
                    craw, r_craw = (kcraw, r_kcraw) if kv == 0 else (vcraw, r_vcraw)
                    load_cmp_w(kv)
                    for hh in range(2):
                        ps, r_ps = ps_ring.next()
                        fns = [lambda e, ll=ll, ps=ps, hh=hh, craw=craw: e.matmul(
                            ps[:, 0:127], lhsT=w1c[:, ll, hh * 128:(hh + 1) * 128],
                            rhs=craw[0:64, ll:ll + 16 * 126 + 1:16], start=(ll == 0), stop=(ll == 31)) for ll in range(32)]
                        fns += [lambda e, ll=ll, ps=ps, hh=hh: e.matmul(
                            ps[:, 128:129], lhsT=w1c[:, ll, hh * 128:(hh + 1) * 128],
                            rhs=posT[:, ll:ll + 1], start=(ll == 0), stop=(ll == 31)) for ll in range(32)]
                        S.op("pe", fns, reads=[r_w1c, r_w2c, r_craw], writes=[r_ps])
                        S.op("dve", lambda e, ps=ps, hh=hh: e.tensor_copy(out=pbias[:, hh:hh + 1], in_=ps[:, 128:129]),
                             reads=[r_ps], writes=[r_pb])
                        xh, r_xh = tmpf_ring.next()
                        x2, r_x2 = tmpf_ring.next()
                        S.op("dve", lambda e, ps=ps, hh=hh, xh=xh: e.tensor_scalar(
                            out=xh[:, 0:127], in0=ps[:, 0:127], scalar1=pbias[:, hh:hh + 1], scalar2=None, op0=ALU.add),
                            reads=[r_ps, r_pb], writes=[r_xh])
                        S.op("dve", lambda e, xh=xh, x2=x2: e.tensor_tensor(out=x2[:, 0:127], in0=xh[:, 0:127],
                                                                            in1=xh[:, 0:127], op=ALU.mult),
                             reads=[r_xh], writes=[r_x2])
                        S.op("dve", lambda e, x2=x2: e.tensor_scalar(out=x2[:, 0:127], in0=x2[:, 0:127], scalar1=0.044715,
                                                                     scalar2=1.0, op0=ALU.mult, op1=ALU.add),
                             reads=[r_x2], writes=[r_x2])
                        S.op("dve", lambda e, xh=xh, x2=x2: e.tensor_tensor(out=x2[:, 0:127], in0=x2[:, 0:127],
                                                                            in1=xh[:, 0:127], op=ALU.mult),
                             reads=[r_xh, r_x2], writes=[r_x2])
                        S.op("act", lambda e, x2=x2: e.activation(out=x2[:, 0:127], in_=x2[:, 0:127], func=AF.Tanh,
                                                                  scale=0.7978845608028654),
                             reads=[r_x2], writes=[r_x2])
                        S.op("dve", lambda e, xh=xh, x2=x2, hh=hh: e.scalar_tensor_tensor(
                            out=hid[:, hh, 0:127], in0=x2[:, 0:127], scalar=1.0, in1=xh[:, 0:127],
                            op0=ALU.add, op1=ALU.mult), reads=[r_xh, r_x2], writes=[r_hid])
                    ps, r_ps = ps_ring.next()
                    fns = [lambda e, hh=hh, ps=ps: e.matmul(ps[0:127, 0:64], lhsT=hid[:, hh, 0:127], rhs=w2c[:, hh, :],
                                                            start=(hh == 0), stop=(hh == 1)) for hh in range(2)]
                    S.op("pe", fns, reads=[r_hid, r_w2c], writes=[r_ps])
                    if kv == 1:
                        S.op("dve", lambda e, ps=ps: e.tensor_scalar(out=vcA[0:127, 0:64], in0=ps[0:127, 0:64],
                                                                     scalar1=0.5, scalar2=None, op0=ALU.mult),
                             reads=[r_ps], writes=[r_vc])
                    else:
                        tf, r_tf = tmpf_ring.next()
                        st, r_st = sm_ring.next()
                        S.op("dve", lambda e, ps=ps, tf=tf: e.tensor_scalar(out=tf[0:127, 0:64], in0=ps[0:127, 0:64],
                                                                            scalar1=0.5, scalar2=None, op0=ALU.mult),
                             reads=[r_ps], writes=[r_tf])
                        S.op("act", lambda e, tf=tf, st=st: e.activation(out=tf[0:127, 64:128], in_=tf[0:127, 0:64],
                                                                         func=AF.Square, accum_out=st[0:127, 0:1]),
                             reads=[r_tf], writes=[r_tf, r_st])
                        S.op("act", lambda e, st=st: e.activation(out=st[0:127, 1:2], in_=st[0:127, 0:1], func=AF.Sqrt,
                                                                  scale=1.0 / 64, bias=EPS), reads=[r_st], writes=[r_st])
                        S.op("dve", lambda e, st=st: e.reciprocal(out=st[0:127, 2:3], in_=st[0:127, 1:2]),
                             reads=[r_st], writes=[r_st])
                        S.op("dve", lambda e, tf=tf, st=st: e.scalar_tensor_tensor(
                            out=kctm[0:127, 0:64], in0=tf[0:127, 0:64], scalar=st[0:127, 2:3], in1=gkc_rep[0:127, :],
                            op0=ALU.mult, op1=ALU.mult), reads=[r_tf, r_st, r_c], writes=[r_kctm])
                        ps2, r_ps2 = ps_ring.next()
                        psb2 = ps2[:].bitcast(BF16)
                        S.op("pe", lambda e, psb2=psb2: e.transpose(out=psb2[0:103, 0:128], in_=kctm[:, 0:103],
                                                                    identity=ident_b[:]),
                             reads=[r_kctm, r_ident], writes=[r_ps2])
                        S.op("dve", lambda e, psb2=psb2: e.tensor_copy(out=kc_aug[0:103, 0:128], in_=psb2[0:103, 0:128]),
                             reads=[r_ps2], writes=[r_kc])

                prs = []
                for hp in range(2):
                    hA, hB = 2 * hp, 2 * hp + 1
                    prs.append((NSA_COLS["q"] + 64 * (4 * g + hA), NSA_COLS["q"] + 64 * (4 * g + hB),
                                lambda b, hA=hA: q_aug[hA][0:64, b * 512:(b + 1) * 512], lambda b, hA=hA: r_q[hA][b],
                                lambda b, hB=hB: q_aug[hB][0:64, b * 512:(b + 1) * 512], lambda b, hB=hB: r_q[hB][b],
                                gtab[:, 0:1]))
                prs.append((NSA_COLS["ks"] + 64 * g, NSA_COLS["kw"] + 64 * g,
                            lambda b: ks_aug[0:64, b * 512:(b + 1) * 512], lambda b: r_ks,
                            lambda b: kw_aug[0:64, b * 512:(b + 1) * 512], lambda b: r_kw, gtab[:, 1:2]))
                proj_fm_pairs(prs)
                for nm, dstA, r_dst in (("vs", vsA, r_vs), ("vw", vwA, r_vw)):
                    w, r_w = load_w(win_cols(NSA_COLS[nm] + 64 * g, 64), 64)
                    for i in range(16):
                        ps, r_ps = ps_ring.next()
                        fns = [lambda e, k=k, ps=ps, i=i, w=w: e.matmul(ps[:, 0:64], lhsT=hT[:, k, i * 128:(i + 1) * 128],
                                                                        rhs=w[:, k, 0:64], start=(k == 0), stop=(k == 7))
                               for k in range(8)]
                        S.op("pe", fns, reads=[r_w, r_hT[i]], writes=[r_ps])
                        S.op("act", lambda e, ps=ps, i=i, dstA=dstA: e.copy(out=dstA[:, i, 0:64], in_=ps[:, 0:64]),
                             reads=[r_ps], writes=[r_dst])
                def mk_cmp_evac(hl, qb):
                    h = 4 * g + hl

                    def ev(acc3, r_acc):
                        tts = slice(4 * qb, 4 * qb + 4)
                        r_o = r_oacc[4 * qb:4 * qb + 4]
                        r_i = r_imp[4 * qb:4 * qb + 4]
                        st, r_st = sm_ring.next()
                        rd = st[:, 4:8].unsqueeze(2)
                        cf = st[:, 8:12].unsqueeze(2)
                        S.op("dve", lambda e: e.tensor_scalar(out=st[:, 0:4].unsqueeze(2), in0=acc3[:, :, 64:65], scalar1=1e-30,
                                                              scalar2=None, op0=ALU.add), reads=[r_acc], writes=[r_st])
                        S.op("dve", lambda e: e.reciprocal(out=st[:, 4:8], in_=st[:, 0:4]), reads=[r_st], writes=[r_st])
                        S.op("dve", lambda e: e.tensor_tensor(out=cf, in0=rd, in1=gsig[:, tts, 3 * h:3 * h + 1], op=ALU.mult),
                             reads=[r_st, r_gsig], writes=[r_st])
                        S.op("dve", lambda e: e.tensor_tensor(out=oacc[:, tts, hl * 64:(hl + 1) * 64], in0=acc3[:, :, 0:64],
                                                              in1=cf.to_broadcast([128, 4, 64]), op=ALU.mult),
                             reads=[r_acc, r_st], writes=r_o)
                        if hl == 0:
                            S.op("dve", lambda e: e.tensor_tensor(out=impacc[:, tts, :], in0=acc3[:, :, 65:97],
                                                                  in1=rd.to_broadcast([128, 4, 32]), op=ALU.mult),
                                 reads=[r_acc, r_st], writes=r_i)
                        else:
                            tf, r_tf = tmpf_ring.next()
                            tf3 = tf[:, 0:128].rearrange("p (u c) -> p u c", u=4)
                            S.op("dve", lambda e: e.tensor_tensor(out=tf3, in0=acc3[:, :, 65:97],
                                                                  in1=rd.to_broadcast([128, 4, 32]), op=ALU.mult),
                                 reads=[r_acc, r_st], writes=[r_tf])
                            S.op("pool", lambda e: e.tensor_tensor(out=impacc[:, tts, :], in0=impacc[:, tts, :], in1=tf3,
                                                                   op=ALU.add), reads=[r_tf] + r_i, writes=r_i)
                    return ev

                rounds = []
                for hl in range(4):
                    h = 4 * g + hl
                    for qb in range(4):
                        rounds.append(dict(q=q_aug[hl], r_q=[r_q[hl][qb], r_qst], krows=103, kT=kc_aug, r_k=r_kc, qb=qb,
                                           tiles=[(0, 0, 512, None, None)], kpart=127, mask=cmask,
                                           V=lambda kt: vcA[0:127, 0:97], r_v=r_vc, nv=97,
                                           bias=lambda kt, h=h, qb=qb: cbias[0:127, 4 * h + qb:4 * h + qb + 1],
                                           evac=mk_cmp_evac(hl, qb)))
                run_rounds(rounds)
                if dbg and s == 0 and l == 0 and g == 0:
                    dump("oacc_cmp", oacc, r_oacc)
                    dump("kc_aug", kc_aug, [r_kc])
                    dump("vcA", vcA, [r_vc])
                    dump("impacc", impacc, r_imp)

                sel_tr = []
                for tt in range(16):
                    sc, r_sc = tmpf_ring.next()
                    st, r_st = sm_ring.next()
                    tr, r_tr = trin_all[tt]
                    S.op("dve", lambda e, sc=sc, tt=tt: e.tensor_tensor(out=sc[:, 0:32], in0=impacc[:, tt, :],
                                                                        in1=nsa_mult[:, tt, :], op=ALU.mult),
                         reads=[r_imp[tt], r_c], writes=[r_sc])
                    S.op("dve", lambda e, sc=sc, tt=tt: e.tensor_tensor(out=sc[:, 0:32], in0=sc[:, 0:32],
                                                                        in1=nsa_add[:, tt, :], op=ALU.add),
                         reads=[r_sc, r_c], writes=[r_sc])
                    S.op("dve", lambda e, sc=sc, st=st: e.max(out=st[:, 0:8], in_=sc[:, 0:32]), reads=[r_sc], writes=[r_st])
                    S.op("dve", lambda e, sc=sc, st=st, tr=tr: e.tensor_scalar(
                        out=tr[:, 64:96], in0=sc[:, 0:32], scalar1=st[:, 7:8], scalar2=-BIG, op0=ALU.is_lt, op1=ALU.mult),
                        reads=[r_sc, r_st], writes=[r_tr])

                def emit_sel_transposes():
                    for tt in range(16):
                        tr, r_tr = trin_all[tt]
                        ps, r_ps = misc_ring.next()
                        psb = ps[:].bitcast(BF16)
                        S.op("pe", lambda e, psb=psb, tr=tr: e.transpose(out=psb[0:96, 0:128], in_=tr[:, 0:96],
                                                                         identity=ident_b[:]),
                             reads=[r_tr, r_ident], writes=[r_ps])
                        for hl in range(4):
                            S.op("dve", lambda e, psb=psb, hl=hl, tt=tt: e.tensor_copy(
                                out=q_aug[hl][64:96, tt * 128:(tt + 1) * 128], in_=psb[64:96, 0:128]),
                                reads=[r_ps], writes=[r_qs[hl][tt // 4]])

                def mk_evac(hl, qb, br):
                    h = 4 * g + hl

                    def ev(acc3, r_acc):
                        tts = slice(4 * qb, 4 * qb + 4)
                        r_o = r_oacc[4 * qb:4 * qb + 4]
                        st, r_st = sm_ring.next()
                        rd = st[:, 4:8].unsqueeze(2)
                        cf = st[:, 8:12].unsqueeze(2)
                        S.op("dve", lambda e: e.reciprocal(out=rd, in_=acc3[:, :, 64:65]), reads=[r_acc], writes=[r_st])
                        S.op("dve", lambda e: e.tensor_tensor(out=cf, in0=rd, in1=gsig[:, tts, 3 * h + br:3 * h + br + 1],
                                                              op=ALU.mult), reads=[r_st, r_gsig], writes=[r_st])
                        tf, r_tf = tmpf_ring.next()
                        tf3 = tf[:, 0:256].rearrange("p (u c) -> p u c", u=4)
                        S.op("dve", lambda e: e.tensor_tensor(out=tf3, in0=acc3[:, :, 0:64], in1=cf.to_broadcast([128, 4, 64]),
                                                              op=ALU.mult), reads=[r_acc, r_st], writes=[r_tf])
                        S.op("pool", lambda e: e.tensor_tensor(out=oacc[:, tts, hl * 64:(hl + 1) * 64],
                                                               in0=oacc[:, tts, hl * 64:(hl + 1) * 64], in1=tf3, op=ALU.add),
                             reads=[r_tf] + r_o, writes=r_o)
                    return ev

                rounds = []
                for hl in range(4):
                    h = 4 * g + hl
                    for qb in range(4):
                        rounds.append(dict(q=q_aug[hl], r_q=[r_q[hl][qb], r_qst], krows=103, kT=kw_aug, r_k=r_kw, qb=qb,
                                           tiles=window_tiles(qb), V=lambda kt: vwA[:, kt, :], r_v=r_vw, nv=65,
                                           fbias=-float(sn_[h]) * 512.0 * qb,
                                           evac=mk_evac(hl, qb, 2)))
                run_rounds(rounds)
                rounds = []
                emit_sel_transposes()
                if dbg and s == 0 and l == 0 and g == 0:
                    dump("oacc_win", oacc, r_oacc)
                for hl in range(4):
                    h = 4 * g + hl
                    for qb in range(4):
                        rounds.append(dict(q=q_aug[hl], r_q=[r_q[hl][qb], r_qs[hl][qb], r_qst], krows=103, kT=ks_aug,
                                           r_k=r_ks, qb=qb, tiles=causal_tiles(qb), V=lambda kt: vsA[:, kt, :], r_v=r_vs,
                                           nv=65,
                                           fbias=-float(sn_[h]) * 512.0 * qb,
                                           evac=mk_evac(hl, qb, 1)))
                run_rounds(rounds)
                if dbg and s == 0 and l == 0 and g == 0:
                    dump("oacc_all", oacc, r_oacc)
                    dump("q0", q_aug[0], [r_q[0][b] for b in range(4)] + [r_qs[0][b] for b in range(4)] + [r_qst])
                    dump("ks_aug", ks_aug, [r_ks])
                for tt in range(16):
                    ob, r_ob = ob_ring.next()
                    S.op("act", lambda e, ob=ob, tt=tt: e.copy(out=ob, in_=oacc[:, tt, :]), reads=[r_oacc[tt]], writes=[r_ob])
                    transpose_tile(ob, r_ob, onT[:, 2 * g:2 * g + 2, tt * 128:(tt + 1) * 128], r_onT[tt], nk=2,
                                   evac="dve", ring=misc_ring)

            S.barrier()
            A.off = region0
            q_aug = [A.alloc([128, T], BF16) for _ in range(4)]
            k_aug = [A.alloc([128, T], BF16) for _ in range(4)]
            r_q = [[Res() for _ in range(4)] for _ in range(4)]
            r_qs = [[Res() for _ in range(4)] for _ in range(4)]
            r_qst = Res()
            r_k = [Res() for _ in range(4)]
            vmA = A.alloc([128, 16, 4, 65], BF16)
            r_vm = Res()
            kmean_f = A.alloc([64, 4, 8], F32)
            kmean_b = A.alloc([64, 4, 8], BF16)
            r_km = Res()
            omb = A.alloc([128, 16, 256], BF16)
            r_omb = [Res() for _ in range(16)]
            trin_ring = Ring([(A.alloc([128, 72], BF16), Res()) for _ in range(4)])
            for hf in range(2):
                if hf == 0:
                    for tr, r_tr in trin_ring.items:
                        S.op("pool", lambda e, tr=tr: e.memset(tr[:, 0:64], 0.0), writes=[r_tr])
                    S.op("pool", lambda e: e.memset(vmA[:, :, :, 64:65], 1.0), writes=[r_vm])
                for hl in range(4):
                    h = 4 * hf + hl
                    S.op("pool", lambda e, hl=hl, h=h: e.dma_start(
                        out=q_aug[hl][72:79, :], in_=C["qalibi"][8 + h, :, :]), writes=[r_qst], dma=True)
                    if hf == 0:
                        S.op("pool", lambda e, hl=hl: e.dma_start(out=q_aug[hl][64:72, :], in_=C["zeros32"][0:8, :]),
                             writes=r_qs[hl], dma=True)
                        S.op("pool", lambda e, hl=hl: e.dma_start(out=k_aug[hl][64:72, :], in_=C["e8"][:, :]),
                             writes=[r_k[hl]], dma=True)
                        S.op("pool", lambda e, hl=hl: e.dma_start(out=k_aug[hl][72:79, :], in_=C["ones3"][:, :]),
                             writes=[r_k[hl]], dma=True)
                prs = []
                for hp in range(2):
                    hA, hB = 2 * hp, 2 * hp + 1
                    prs.append((MOBA_COLS["q"] + 64 * (4 * hf + hA), MOBA_COLS["q"] + 64 * (4 * hf + hB),
                                lambda b, hA=hA: q_aug[hA][0:64, b * 512:(b + 1) * 512], lambda b, hA=hA: r_q[hA][b],
                                lambda b, hB=hB: q_aug[hB][0:64, b * 512:(b + 1) * 512], lambda b, hB=hB: r_q[hB][b],
                                gtab[:, 2:3]))
                for hp in range(2):
                    hA, hB = 2 * hp, 2 * hp + 1
                    prs.append((MOBA_COLS["k"] + 64 * (4 * hf + hA), MOBA_COLS["k"] + 64 * (4 * hf + hB),
                                lambda b, hA=hA: k_aug[hA][0:64, b * 512:(b + 1) * 512], lambda b, hA=hA: r_k[hA],
                                lambda b, hB=hB: k_aug[hB][0:64, b * 512:(b + 1) * 512], lambda b, hB=hB: r_k[hB],
                                gtab[:, 3:4]))
                proj_fm_pairs(prs)
                for hl in range(4):
                    S.op("dve", lambda e, hl=hl: e.tensor_reduce(
                        out=kmean_f[:, hl, :], in_=k_aug[hl][0:64, :].rearrange("p (n k) -> p n k", k=256),
                        axis=AX.X, op=ALU.add), reads=[r_k[hl]], writes=[r_km])
                S.op("dve", lambda e: e.tensor_scalar(out=kmean_b, in0=kmean_f, scalar1=1.0 / 256, scalar2=None,
                                                      op0=ALU.mult), reads=[r_km], writes=[r_km])
                w, r_w = load_w(win_cols(MOBA_COLS["v"] + 256 * hf, 256), 256)
                for i in range(16):
                    ps, r_ps = ps_ring.next()
                    fns = [lambda e, k=k, ps=ps, i=i, w=w: e.matmul(ps[:, 0:256], lhsT=hT[:, k, i * 128:(i + 1) * 128],
                                                                    rhs=w[:, k, 0:256], start=(k == 0), stop=(k == 7))
                           for k in range(8)]
                    S.op("pe", fns, reads=[r_w, r_hT[i]], writes=[r_ps])
                    S.op("act", lambda e, ps=ps, i=i: e.copy(out=vmA[:, i, :, 0:64],
                                                             in_=ps[:, 0:256].rearrange("p (h d) -> p h d", d=64)),
                         reads=[r_ps], writes=[r_vm])
                for tt in range(16):
                    cur = tt // 2
                    if tt >= 8:
                        ps, r_ps = misc_ring.next()
                        fns = [lambda e, hl=hl, ps=ps, tt=tt: e.matmul(ps[:, hl * 8:(hl + 1) * 8],
                                                                       lhsT=q_aug[hl][0:64, tt * 128:(tt + 1) * 128],
                                                                       rhs=kmean_b[:, hl, :], start=True, stop=True)
                               for hl in range(4)]
                        S.op("pe", fns, reads=[r_km] + [r_q[hl][tt // 4] for hl in range(4)], writes=[r_ps])
                        sc, r_sc = tmpf_ring.next()
                        for hl in range(4):
                            S.op("dve", lambda e, hl=hl, ps=ps, sc=sc, tt=tt: e.tensor_tensor(
                                out=sc[:, hl * 8:(hl + 1) * 8], in0=ps[:, hl * 8:(hl + 1) * 8], in1=moba_add[:, tt, :],
                                op=ALU.add), reads=[r_ps, r_c], writes=[r_sc])
                    for hl in range(4):
                        tr, r_tr = trin_ring.next()
                        S.op("act", lambda e, tr=tr, tt=tt: e.copy(out=tr[:, 64:72], in_=moba_tmpl[:, tt, :]),
                             reads=[r_c], writes=[r_tr])
                        if tt < 8:
                            pass
                        else:
                            st, r_st = sm_ring.next()
                            S.op("dve", lambda e, sc=sc, st=st, hl=hl: e.max(out=st[:, 0:8], in_=sc[:, hl * 8:(hl + 1) * 8]),
                                 reads=[r_sc], writes=[r_st])
                            S.op("dve", lambda e, sc=sc, st=st, tr=tr, hl=hl, cur=cur: e.tensor_scalar(
                                out=tr[:, 64:64 + cur], in0=sc[:, hl * 8:hl * 8 + cur], scalar1=st[:, 2:3], scalar2=-BIG,
                                op0=ALU.is_lt, op1=ALU.mult), reads=[r_sc, r_st], writes=[r_tr])
                        ps2, r_ps2 = ps_ring.next()
                        psb = ps2[:].bitcast(BF16)
                        S.op("pe", lambda e, psb=psb, tr=tr: e.transpose(out=psb[0:72, 0:128], in_=tr[:, 0:72],
                                                                         identity=ident_b[:]),
                             reads=[r_tr, r_ident], writes=[r_ps2])
                        S.op("dve", lambda e, psb=psb, hl=hl, tt=tt: e.tensor_copy(
                            out=q_aug[hl][64:72, tt * 128:(tt + 1) * 128], in_=psb[64:72, 0:128]),
                            reads=[r_ps2], writes=[r_qs[hl][tt // 4]])

                def mk_evac_m(hl, qb):
                    def ev(acc3, r_acc):
                        tts = slice(4 * qb, 4 * qb + 4)
                        st, r_st = sm_ring.next()
                        rd = st[:, 4:8].unsqueeze(2)
                        S.op("dve", lambda e: e.reciprocal(out=rd, in_=acc3[:, :, 64:65]), reads=[r_acc], writes=[r_st])
                        S.op("dve", lambda e: e.tensor_tensor(out=omb[:, tts, hl * 64:(hl + 1) * 64], in0=acc3[:, :, 0:64],
                                                              in1=rd.to_broadcast([128, 4, 64]), op=ALU.mult),
                             reads=[r_acc, r_st], writes=r_omb[4 * qb:4 * qb + 4])
                    return ev

                rounds = []
                for hl in range(4):
                    h = 4 * hf + hl
                    for qb in range(4):
                        rounds.append(dict(q=q_aug[hl], r_q=[r_q[hl][qb], r_qs[hl][qb], r_qst], krows=79, kT=k_aug[hl],
                                           r_k=r_k[hl], qb=qb, tiles=causal_tiles(qb),
                                           V=lambda kt, hl=hl: vmA[:, kt, hl, :], r_v=r_vm, nv=65,
                                           fbias=-float(sm_[h]) * 512.0 * qb,
                                           evac=mk_evac_m(hl, qb)))
                run_rounds(rounds)
                for tt in range(16):
                    transpose_tile(omb[:, tt, :], r_omb[tt], omT[:, 2 * hf:2 * hf + 2, tt * 128:(tt + 1) * 128],
                                   r_omT[tt], nk=2, evac="dve", ring=misc_ring)

            if dbg and s == 0 and l == 0:
                for nm, src, rr in (("onT", onT, r_onT), ("omT", omT, r_omT)):
                    if nm in dbg_d:
                        S.op("pool", lambda e, nm=nm, src=src: e.dma_start(
                            out=dbg_d[nm][:, :].rearrange("p (a b) -> p a b", a=4), in_=src), reads=rr,
                            writes=[Res()], dma=True)

            S.barrier()
            A.off = region0
            yT = A.alloc([128, 8, T], BF16)
            r_yT = [Res() for _ in range(8)]
            wout = A.alloc([128, 8, D], BF16)
            r_wout = Res()
            S.op("pool", lambda e: e.dma_start(out=wout, in_=wout_d[l, :, :].rearrange("(k p) c -> p k c", p=128)),
                 writes=[r_wout], dma=True)
            for oc in range(8):
                wgn, r_wgn = load_w(win_cols(GATE_N + 128 * oc, 128), 128)
                wgm, r_wgm = load_w(win_cols(GATE_M + 128 * oc, 128), 128)
                wu, r_wu = wch_ring.next()
                S.op("pool", lambda e, wu=wu, oc=oc: e.dma_start(
                    out=wu[:, 0:4, 0:128], in_=wupn_d[l, :, oc * 128:(oc + 1) * 128].rearrange("(k p) c -> p k c", p=128)),
                    writes=[r_wu], dma=True)
                S.op("pool", lambda e, wu=wu, oc=oc: e.dma_start(
                    out=wu[:, 4:8, 0:128], in_=wupm_d[l, :, oc * 128:(oc + 1) * 128].rearrange("(k p) c -> p k c", p=128)),
                    writes=[r_wu], dma=True)
                for b in range(4):
                    bs = slice(b * 512, (b + 1) * 512)
                    res = []
                    for (wg, r_wg, oT, r_oT, ko) in ((wgn, r_wgn, onT, r_onT, 0), (wgm, r_wgm, omT, r_omT, 4)):
                        pg, r_pg = ps_ring.next()
                        fns = [lambda e, k=k, pg=pg, wg=wg, bs=bs: e.matmul(pg[:], lhsT=wg[:, k, 0:128], rhs=hT[:, k, bs],
                                                                     start=(k == 0), stop=(k == 7)) for k in range(8)]
                        S.op("pe", fns, reads=[r_wg] + r_hT[4 * b:4 * b + 4], writes=[r_pg])
                        pu, r_pu = ps_ring.next()
                        fns = [lambda e, k=k, pu=pu, oT=oT, ko=ko, wu=wu, bs=bs: e.matmul(pu[:], lhsT=wu[:, ko + k, 0:128], rhs=oT[:, k, bs],
                                                                            start=(k == 0), stop=(k == 3)) for k in range(4)]
                        S.op("pe", fns, reads=[r_wu] + r_oT[4 * b:4 * b + 4], writes=[r_pu])
                        sg, r_sg = tmpf_ring.next()
                        S.op("act", lambda e, pg=pg, sg=sg: e.activation(out=sg, in_=pg[:], func=AF.Sigmoid),
                             reads=[r_pg], writes=[r_sg])
                        S.op("dve", lambda e, pu=pu, sg=sg: e.tensor_tensor(out=sg, in0=sg, in1=pu[:], op=ALU.mult),
                             reads=[r_pu, r_sg], writes=[r_sg])
                        res.append((sg, r_sg))
                    S.op("dve", lambda e, a=res[0][0], b_=res[1][0], oc=oc, bs=bs: e.tensor_tensor(
                        out=yT[:, oc, bs], in0=a, in1=b_, op=ALU.add), reads=[res[0][1], res[1][1]], writes=[r_yT[oc]])
            for i in range(16):
                tt = tt0 + i
                xa, r_xa = xt_ring.next()
                S.op("sp", lambda e, xa=xa, tt=tt: e.dma_start(out=xa, in_=src_d[tt * 128:(tt + 1) * 128, :]),
                     reads=[r_src[tt]], writes=[r_xa], dma=True)
                for h2 in range(2):
                    po, r_po = ps_ring.next()
                    fns = [lambda e, oc=oc, po=po, i=i, h2=h2: e.matmul(po[:], lhsT=yT[:, oc, i * 128:(i + 1) * 128],
                                                                        rhs=wout[:, oc, h2 * 512:(h2 + 1) * 512],
                                                                        start=(oc == 0), stop=(oc == 7)) for oc in range(8)]
                    S.op("pe", fns, reads=r_yT + [r_wout], writes=[r_po])
                    S.op("dve", lambda e, po=po, xa=xa, h2=h2: e.tensor_tensor(
                        out=xa[:, h2 * 512:(h2 + 1) * 512], in0=po[:], in1=xa[:, h2 * 512:(h2 + 1) * 512], op=ALU.add),
                        reads=[r_po, r_xa], writes=[r_xa])
                S.op("sp", lambda e, xa=xa, tt=tt: e.dma_start(out=y_d[tt * 128:(tt + 1) * 128, :], in_=xa),
                     reads=[r_xa], writes=[r_y[tt]], dma=True)

    cur_d, cur_r = x_d, r_x
    for l in range(depth):
        if f"ffn{2 * l}" in phases or "all" in phases:
            ffn_phase(l, 0, cur_d, cur_r)
            cur_d, cur_r = y_d, r_y
        if f"mix{l}" in phases or "all" in phases:
            mix_phase(l, cur_d, cur_r)
            cur_d, cur_r = y_d, r_y
        if f"ffn{2 * l + 1}" in phases or "all" in phases:
            ffn_phase(l, 1, cur_d, cur_r)
            cur_d, cur_r = y_d, r_y

    S.barrier()
    S.finish("sp", r_y)
    sems = [es.enter_context(nc.semaphore(f"s{i}")) for i in range(S.nsem)]
    S.emit(nc, sems)
    es.close()
    return nc, S


def make_in_maps(inputs, nseq, ncores):
    x = np.ascontiguousarray(inputs["x"], dtype=np.float32).reshape(-1, nseq * T, D)
    consts = make_consts()
    shared = {}
    for k in ("norm_g", "ffn_w1", "ffn_w3", "ffn_w2", "w_in", "g_qk_nsa", "g_qk_moba", "cmp_w1", "cmp_w2", "w_up_nsa", "w_up_moba",
              "w_out"):
        shared[k] = np.ascontiguousarray(inputs[k], dtype=np.float32)
    shared["g_qk_nsaT"] = np.ascontiguousarray(np.transpose(np.asarray(inputs["g_qk_nsa"], np.float32), (0, 2, 1)))
    shared["g_qk_mobaT"] = np.ascontiguousarray(np.transpose(np.asarray(inputs["g_qk_moba"], np.float32), (0, 2, 1)))
    shared["cmp_posT"] = np.ascontiguousarray(np.transpose(np.asarray(inputs["cmp_pos"], np.float32), (0, 1, 3, 2)))
    shared.update(consts)
    in_maps = []
    for c in range(ncores):
        m = {"x": x[c]}
        m.update(shared)
        in_maps.append(m)
    return in_maps


_CACHE = {}


def kernel(**inputs):
    nseq = 16 // NCORES
    if "full" not in _CACHE:
        _CACHE["full"] = build_program(nseq=nseq, phases=("all",))
    nc, S = _CACHE["full"]
    in_maps = make_in_maps(inputs, nseq, NCORES)
    res = run_bass_kernel_spmd(nc, in_maps, core_ids=list(range(NCORES)))
    y = np.stack([np.asarray(r["y"]) for r in res.results], axis=0)
    return y.reshape(16, T, D).astype(np.float32)
```
